# Optimizing a Trainium2 kernel written in Bass

```python
import numpy as np
import jax
import jax.numpy as jnp
from jax import lax

D_MODEL = 1024
BATCH = 8
SEQ = 2048
DEPTH = 4

HEAD_DIM = 64
ROT_DIM = HEAD_DIM // 4
ROPE_THETA = 500000.0
NORM_EPS = 1e-6
NEG_INF = -1e30
Q_BLOCK = 128

NSA_HEADS = 8
NSA_GROUPS = 2
NSA_HPG = NSA_HEADS // NSA_GROUPS
CMP_LEN = 32
CMP_STRIDE = 16
CMP_HIDDEN = 256
SLC_LEN = 64
SLC_TOPN = 8
WINDOW = 256
SB_HEADS = 8
GLA_HEADS = 4
GLA_DK = 64
GLA_DV = 128
GLA_RANK = 16
GLA_TAU = 16.0
GLA_CHUNK = 64
FOX_HEADS = 8

BRANCH_WIDTH = 512
N_BRANCH = 4
D_FF = 4 * D_MODEL

NSA_QW = NSA_HEADS * HEAD_DIM
NSA_KVW = NSA_GROUPS * HEAD_DIM
SB_W = SB_HEADS * HEAD_DIM
FOX_W = FOX_HEADS * HEAD_DIM
SPLITS = (NSA_QW, NSA_KVW, NSA_KVW, NSA_KVW, NSA_KVW, NSA_KVW, NSA_KVW, NSA_HEADS * 3,
          SB_W, SB_W, SB_W,
          GLA_HEADS * GLA_DK, GLA_HEADS * GLA_DK, GLA_HEADS * GLA_DV, GLA_RANK, GLA_HEADS * GLA_DV,
          FOX_W, FOX_W, FOX_W, FOX_HEADS,
          N_BRANCH * D_MODEL)
D_IN = sum(SPLITS)

kernel_name = 'hybrid_nsa_sb_gla_fox_block'


def rms_norm(x, g):
    xf = x.astype(jnp.float32)
    xf = xf * lax.rsqrt(jnp.mean(xf * xf, axis=-1, keepdims=True) + NORM_EPS)
    return (xf * g.astype(jnp.float32)).astype(x.dtype)


def masked_softmax(logits, mask):
    logits = jnp.where(mask, logits.astype(jnp.float32), NEG_INF)
    return jax.nn.softmax(logits, axis=-1) * mask


def rope_partial(x, pos):
    half = ROT_DIM // 2
    inv = ROPE_THETA ** (-jnp.arange(0, ROT_DIM, 2, dtype=jnp.float32) / ROT_DIM)
    ang = pos.astype(jnp.float32)[..., None] * inv
    ang = ang.reshape(ang.shape[:2] + (1,) * (x.ndim - 3) + (half,))
    cos = jnp.cos(ang).astype(x.dtype)
    sin = jnp.sin(ang).astype(x.dtype)
    x1, x2, xp = x[..., :half], x[..., half:ROT_DIM], x[..., ROT_DIM:]
    return jnp.concatenate([x1 * cos - x2 * sin, x2 * cos + x1 * sin, xp], axis=-1)


def nsa_mixer(q, k_cmp, v_cmp, k_slc, v_slc, k_win, v_win, gate_logits, pos,
              cmp_pos_k, cmp_pos_v, cmp_wk1, cmp_wk2, cmp_wv1, cmp_wv2):
    B, S = q.shape[:2]
    G, HPG, D = NSA_GROUPS, NSA_HPG, HEAD_DIM
    scale = D ** -0.5
    q = rope_partial(q.reshape(B, S, G, HPG, D), pos)
    k_cmp, v_cmp, k_slc, v_slc, k_win, v_win = [t.reshape(B, S, G, D) for t in (k_cmp, v_cmp, k_slc, v_slc, k_win, v_win)]
    k_slc = rope_partial(k_slc, pos)
    k_win = rope_partial(k_win, pos)
    tok = np.arange(S)

    n_cmp = (S - CMP_LEN) // CMP_STRIDE + 1
    cmp_start = np.arange(n_cmp) * CMP_STRIDE
    cmp_end = cmp_start + CMP_LEN - 1
    cmp_idx = cmp_start[:, None] + np.arange(CMP_LEN)[None, :]

    def compress(t, pe, w1, w2):
        blk = t[:, cmp_idx] + pe[:, None, :]
        blk = jnp.moveaxis(blk, 3, 2).reshape(B, n_cmp, G, CMP_LEN * D)
        return jax.nn.gelu(blk @ w1) @ w2

    kc = rope_partial(compress(k_cmp, cmp_pos_k, cmp_wk1, cmp_wk2), pos[:, cmp_end])
    vc = compress(v_cmp, cmp_pos_v, cmp_wv1, cmp_wv2)
    s_cmp = jnp.einsum('bsghd,bcgd->bghsc', q, kc) * scale
    p_cmp = masked_softmax(s_cmp, cmp_end[None, :] <= tok[:, None])
    o_cmp = jnp.einsum('bghsc,bcgd->bsghd', p_cmp.astype(vc.dtype), vc)

    n_sel = S // SLC_LEN
    top_n = min(SLC_TOPN, n_sel)
    sel_start = np.arange(n_sel) * SLC_LEN
    overlap = np.clip(np.minimum(cmp_start[:, None] + CMP_LEN, sel_start[None, :] + SLC_LEN)
                      - np.maximum(cmp_start[:, None], sel_start[None, :]), 0, None) / CMP_LEN
    imp = jnp.einsum('bghsc,cj->bgsj', p_cmp, jnp.asarray(overlap, jnp.float32))
    forced = (sel_start[None, :] == 0) | (np.arange(n_sel)[None, :] == (tok // SLC_LEN)[:, None])
    imp = jnp.where(forced, jnp.inf, jnp.where(sel_start[None, :] <= tok[:, None], imp, -jnp.inf))
    _, sel = lax.top_k(imp, top_n)
    n_qb = S // Q_BLOCK

    def to_blocks(t):
        return jnp.moveaxis(t.reshape(B, n_sel, SLC_LEN, G, D), 3, 1).reshape(B, G, n_sel, SLC_LEN * D)

    ks_blk = to_blocks(k_slc)
    vs_blk = to_blocks(v_slc)
    q_blk = jnp.moveaxis(q.reshape(B, n_qb, Q_BLOCK, G, HPG, D), 1, 0)
    sel_blk = jnp.moveaxis(sel.reshape(B, G, n_qb, Q_BLOCK, top_n), 2, 0)

    def select_block(args):
        qc, ic, t_start = args
        flat = ic.reshape(B, G, Q_BLOCK * top_n, 1)
        kg = jnp.take_along_axis(ks_blk, flat, axis=2).reshape(B, G, Q_BLOCK, top_n * SLC_LEN, D)
        vg = jnp.take_along_axis(vs_blk, flat, axis=2).reshape(B, G, Q_BLOCK, top_n * SLC_LEN, D)
        key_tok = (ic[..., None] * SLC_LEN + jnp.arange(SLC_LEN)).reshape(B, G, Q_BLOCK, top_n * SLC_LEN)
        t = t_start + jnp.arange(Q_BLOCK)
        s = jnp.einsum('bqghd,bgqkd->bghqk', qc, kg) * scale
        p = masked_softmax(s, (key_tok <= t[:, None])[:, :, None])
        return jnp.einsum('bghqk,bgqkd->bqghd', p.astype(vg.dtype), vg)

    o_slc = lax.map(select_block, (q_blk, sel_blk, jnp.arange(n_qb) * Q_BLOCK))
    o_slc = jnp.moveaxis(o_slc, 0, 1).reshape(B, S, G, HPG, D)

    n_band = Q_BLOCK + WINDOW
    band_idx = np.arange(n_qb)[:, None] * Q_BLOCK + np.arange(n_band)[None, :]
    band_tok = band_idx - WINDOW
    pad = ((0, 0), (WINDOW, 0), (0, 0), (0, 0))
    kb = jnp.pad(k_win, pad)[:, band_idx]
    vb = jnp.pad(v_win, pad)[:, band_idx]
    tq = tok.reshape(n_qb, Q_BLOCK)
    win_mask = ((band_tok[:, None, :] <= tq[:, :, None]) & (band_tok[:, None, :] > tq[:, :, None] - WINDOW)
                & (band_tok[:, None, :] >= 0))
    s_win = jnp.einsum('bnqghd,bnkgd->bnghqk', q.reshape(B, n_qb, Q_BLOCK, G, HPG, D), kb) * scale
    p_win = masked_softmax(s_win, win_mask[None, :, None, None])
    o_win = jnp.einsum('bnghqk,bnkgd->bnqghd', p_win.astype(vb.dtype), vb).reshape(B, S, G, HPG, D)

    g = jax.nn.sigmoid(gate_logits.astype(jnp.float32)).reshape(B, S, G, HPG, 3).astype(q.dtype)
    o = g[..., 0:1] * o_cmp + g[..., 1:2] * o_slc + g[..., 2:3] * o_win
    return o.reshape(B, S, NSA_HEADS * D)


def stick_breaking_mixer(q, k, v):
    B, S = q.shape[:2]
    H, D = SB_HEADS, HEAD_DIM
    scale = D ** -0.5
    n_qb = S // Q_BLOCK
    kT = jnp.moveaxis(k.reshape(B, S, H, D), 1, 2)
    vT = jnp.moveaxis(v.reshape(B, S, H, D), 1, 2)
    qb = q.reshape(B, n_qb, Q_BLOCK, H, D).transpose(1, 0, 3, 2, 4)
    key_pos = jnp.arange(S)

    def block(args):
        qc, t_start = args
        z = jnp.einsum('bhqd,bhkd->bhqk', qc, kT).astype(jnp.float32) * scale
        t = t_start + jnp.arange(Q_BLOCK)
        valid = key_pos[None, :] < t[:, None]
        sp = jnp.where(valid, jax.nn.softplus(z), 0.0)
        later = lax.cumsum(sp, axis=3, reverse=True) - sp
        a = jnp.where(valid, jnp.exp(jax.nn.log_sigmoid(z) - later), 0.0)
        return jnp.einsum('bhqk,bhkd->bhqd', a.astype(vT.dtype), vT)

    o = lax.map(block, (qb, jnp.arange(n_qb) * Q_BLOCK))
    return o.transpose(1, 0, 3, 2, 4).reshape(B, S, H * D)


def gla_mixer(q, k, v, a_lr, gate, w_alpha, b_alpha, norm_g):
    B, S = q.shape[:2]
    H, DK, DV, C = GLA_HEADS, GLA_DK, GLA_DV, GLA_CHUNK
    n_ch = S // C
    log_a = jax.nn.log_sigmoid((a_lr @ w_alpha + b_alpha).astype(jnp.float32)) / GLA_TAU

    def chunks(t, f):
        return t.reshape(B, n_ch, C, H, f).transpose(1, 0, 3, 2, 4)

    qc_all = chunks(q * DK ** -0.5, DK)
    kc_all = chunks(k, DK)
    vc_all = chunks(v, DV)
    la_all = chunks(log_a, DK)
    causal = np.tril(np.ones((C, C), dtype=bool))

    def step(state, inp):
        qc, kc, vc, lac = inp
        b = jnp.cumsum(lac, axis=2)
        o_inter = jnp.einsum('bhtk,bhkv->bhtv', qc * jnp.exp(b), state)
        diff = jnp.where(causal[:, :, None], b[:, :, :, None, :] - b[:, :, None, :, :], -jnp.inf)
        att = jnp.einsum('bhtk,bhsk,bhtsk->bhts', qc, kc, jnp.exp(diff))
        o_intra = jnp.einsum('bhts,bhsv->bhtv', att, vc)
        b_last = b[:, :, -1:, :]
        state = (jnp.exp(b_last[:, :, 0])[..., None] * state
                 + jnp.einsum('bhsk,bhsv->bhkv', kc * jnp.exp(b_last - b), vc))
        return state, o_inter + o_intra

    state0 = jnp.zeros((B, H, DK, DV), jnp.float32)
    _, o = lax.scan(step, state0, (qc_all, kc_all, vc_all, la_all))
    o = o.transpose(1, 0, 3, 2, 4).reshape(B, S, H, DV)
    o = o * lax.rsqrt(jnp.mean(o * o, axis=-1, keepdims=True) + NORM_EPS) * norm_g.astype(jnp.float32)
    y = o.astype(v.dtype) * jax.nn.silu(gate).reshape(B, S, H, DV)
    return y.reshape(B, S, H * DV)


def forgetting_mixer(q, k, v, f_logit, b_f):
    B, S = q.shape[:2]
    H, D = FOX_HEADS, HEAD_DIM
    scale = D ** -0.5
    n_qb = S // Q_BLOCK
    log_f = jax.nn.log_sigmoid(f_logit.astype(jnp.float32) + b_f.astype(jnp.float32))
    cumT = jnp.moveaxis(jnp.cumsum(log_f, axis=1), 1, 2)
    kT = jnp.moveaxis(k.reshape(B, S, H, D), 1, 2)
    vT = jnp.moveaxis(v.reshape(B, S, H, D), 1, 2)
    qb = q.reshape(B, n_qb, Q_BLOCK, H, D).transpose(1, 0, 3, 2, 4)
    cb = cumT.reshape(B, H, n_qb, Q_BLOCK).transpose(2, 0, 1, 3)
    key_pos = jnp.arange(S)

    def block(args):
        qc, ct, t_start = args
        s = jnp.einsum('bhqd,bhkd->bhqk', qc, kT).astype(jnp.float32) * scale
        s = s + ct[..., None] - cumT[:, :, None, :]
        t = t_start + jnp.arange(Q_BLOCK)
        p = masked_softmax(s, key_pos[None, :] <= t[:, None])
        return jnp.einsum('bhqk,bhkd->bhqd', p.astype(vT.dtype), vT)

    o = lax.map(block, (qb, cb, jnp.arange(n_qb) * Q_BLOCK))
    return o.transpose(1, 0, 3, 2, 4).reshape(B, S, H * D)


def hybrid_layer(x, positions, norm_mix, w_in, b_gate, cmp_pos_k, cmp_pos_v, cmp_wk1, cmp_wk2,
                 cmp_wv1, cmp_wv2, gla_w_alpha, gla_b_alpha, gla_norm, fox_b_f, w_branch, w_out,
                 norm_ff, w_ff1, w_ff2):
    B, S, _ = x.shape
    h = rms_norm(x, norm_mix)
    proj = h @ w_in
    offsets = np.cumsum(SPLITS)[:-1].tolist()
    (nsa_q, nsa_kc, nsa_vc, nsa_ks, nsa_vs, nsa_kw, nsa_vw, nsa_g,
     sb_q, sb_k, sb_v,
     gla_q, gla_k, gla_v, gla_a, gla_g,
     fox_q, fox_k, fox_v, fox_f,
     gate_logits) = jnp.split(proj, offsets, axis=-1)

    y_nsa = nsa_mixer(nsa_q, nsa_kc, nsa_vc, nsa_ks, nsa_vs, nsa_kw, nsa_vw, nsa_g, positions,
                      cmp_pos_k, cmp_pos_v, cmp_wk1, cmp_wk2, cmp_wv1, cmp_wv2)
    y_sb = stick_breaking_mixer(sb_q, sb_k, sb_v)
    y_gla = gla_mixer(gla_q.reshape(B, S, GLA_HEADS, GLA_DK), gla_k.reshape(B, S, GLA_HEADS, GLA_DK),
                      gla_v.reshape(B, S, GLA_HEADS, GLA_DV), gla_a, gla_g, gla_w_alpha, gla_b_alpha, gla_norm)
    y_fox = forgetting_mixer(fox_q, fox_k, fox_v, fox_f, fox_b_f)

    ys = jnp.stack([y_nsa, y_sb, y_gla, y_fox], axis=2)
    branch = jnp.einsum('bsnc,ncd->bsnd', ys, w_branch)
    gates = jax.nn.sigmoid((gate_logits + b_gate).astype(jnp.float32)).astype(x.dtype)
    mixed = jnp.sum(gates.reshape(B, S, N_BRANCH, D_MODEL) * branch, axis=2)
    x = x + mixed @ w_out
    h2 = rms_norm(x, norm_ff)
    return x + jnp.square(jax.nn.relu(h2 @ w_ff1)) @ w_ff2


def setup_inputs(seed: int = 0) -> dict:
    key = jax.random.key(seed)
    ks = jax.random.split(key, 21)
    L = DEPTH

    def nrm(k, shape, scale):
        return jax.random.normal(k, shape, jnp.float32) * scale

    x = nrm(ks[0], (BATCH, SEQ, D_MODEL), 1.0)
    offset = jax.random.randint(ks[1], (BATCH, 1), 0, 4096, dtype=jnp.int32)
    positions = jnp.arange(SEQ, dtype=jnp.int32)[None, :] + offset
    return {
        'x': x,
        'positions': positions,
        'norm_mix': 1.0 + nrm(ks[2], (L, D_MODEL), 0.02),
        'w_in': nrm(ks[3], (L, D_MODEL, D_IN), D_MODEL ** -0.5),
        'b_gate': nrm(ks[4], (L, N_BRANCH * D_MODEL), 0.02),
        'nsa_cmp_pos_k': nrm(ks[5], (L, CMP_LEN, HEAD_DIM), 0.1),
        'nsa_cmp_pos_v': nrm(ks[6], (L, CMP_LEN, HEAD_DIM), 0.1),
        'nsa_cmp_wk1': nrm(ks[7], (L, CMP_LEN * HEAD_DIM, CMP_HIDDEN), (CMP_LEN * HEAD_DIM) ** -0.5),
        'nsa_cmp_wk2': nrm(ks[8], (L, CMP_HIDDEN, HEAD_DIM), CMP_HIDDEN ** -0.5),
        'nsa_cmp_wv1': nrm(ks[9], (L, CMP_LEN * HEAD_DIM, CMP_HIDDEN), (CMP_LEN * HEAD_DIM) ** -0.5),
        'nsa_cmp_wv2': nrm(ks[10], (L, CMP_HIDDEN, HEAD_DIM), CMP_HIDDEN ** -0.5),
        'gla_w_alpha': nrm(ks[11], (L, GLA_RANK, GLA_HEADS * GLA_DK), GLA_RANK ** -0.5),
        'gla_b_alpha': nrm(ks[12], (L, GLA_HEADS * GLA_DK), 0.1),
        'gla_norm': 1.0 + nrm(ks[13], (L, GLA_DV), 0.02),
        'fox_b_f': 2.0 + nrm(ks[14], (L, FOX_HEADS), 0.1),
        'w_branch': nrm(ks[15], (L, N_BRANCH, BRANCH_WIDTH, D_MODEL), BRANCH_WIDTH ** -0.5),
        'w_out': nrm(ks[16], (L, D_MODEL, D_MODEL), D_MODEL ** -0.5),
        'norm_ff': 1.0 + nrm(ks[17], (L, D_MODEL), 0.02),
        'w_ff1': nrm(ks[18], (L, D_MODEL, D_FF), D_MODEL ** -0.5),
        'w_ff2': nrm(ks[19], (L, D_FF, D_MODEL), D_FF ** -0.5),
        'norm_final': 1.0 + nrm(ks[20], (D_MODEL,), 0.02),
    }


def reference(x, positions, norm_mix, w_in, b_gate, nsa_cmp_pos_k, nsa_cmp_pos_v, nsa_cmp_wk1,
              nsa_cmp_wk2, nsa_cmp_wv1, nsa_cmp_wv2, gla_w_alpha, gla_b_alpha, gla_norm, fox_b_f,
              w_branch, w_out, norm_ff, w_ff1, w_ff2, norm_final):
    for l in range(DEPTH):
        x = hybrid_layer(x, positions, norm_mix[l], w_in[l], b_gate[l], nsa_cmp_pos_k[l], nsa_cmp_pos_v[l],
                         nsa_cmp_wk1[l], nsa_cmp_wk2[l], nsa_cmp_wv1[l], nsa_cmp_wv2[l], gla_w_alpha[l],
                         gla_b_alpha[l], gla_norm[l], fox_b_f[l], w_branch[l], w_out[l], norm_ff[l],
                         w_ff1[l], w_ff2[l])
    return rms_norm(x, norm_final)
```

```python
import numpy as np
from contextlib import ExitStack
import concourse.bass as bass
import concourse.mybir as mybir
from concourse.bass_utils import run_bass_kernel_spmd

F32 = mybir.dt.float32
BF16 = mybir.dt.bfloat16
I32 = mybir.dt.int32
ALU = mybir.AluOpType
AF = mybir.ActivationFunctionType
AX = mybir.AxisListType

S = 2048
D = 1024
NT = S // 128
NTC = S // 512
KC = D // 128
DEPTH = 4
DFF = 4096
D_IN = 10032
EPS = 1e-6
NEG = -30000.0

SPLITS = (512, 128, 128, 128, 128, 128, 128, 24, 512, 512, 512, 256, 256, 512, 16, 512, 512, 512, 512, 8, 4096)
OFFS = np.concatenate([[0], np.cumsum(SPLITS)]).tolist()
(O_NQ, O_NKC, O_NVC, O_NKS, O_NVS, O_NKW, O_NVW, O_NG, O_SQ, O_SK, O_SV, O_GQ, O_GK, O_GV, O_GA, O_GG,
 O_FQ, O_FK, O_FV, O_FF, O_GATE) = OFFS[:21]

EPOCH = 4000


class Tok:
    __slots__ = ("w", "r", "name")

    def __init__(self, name=""):
        self.w = None
        self.r = {}
        self.name = name


class Ctx:
    def __init__(self, nc, es):
        self.nc = nc
        self.es = es
        self.eng = dict(pe=nc.tensor, act=nc.scalar, dve=nc.vector, pool=nc.gpsimd, sp=nc.sync)
        self.cur = {}
        self.nsem = 0
        self.waited = {e: {} for e in self.eng}
        for e in ("pe", "act", "dve", "pool"):
            self.cur[e] = [self._newsem(e), 0]
        self.own = {e: set() for e in self.eng}
        for e in ("pe", "act", "dve", "pool"):
            self.own[e].add(id(self.cur[e][0]))
        self.dq = {}
        for q in ("sp", "act", "pool"):
            sems = [self._newsem("d" + q) for _ in range(8 if q == "sp" else 4)]
            self.dq[q] = dict(sems=sems, tgt=[0] * len(sems), i=0)
        self.all_dma = []

    def _newsem(self, name):
        self.nsem += 1
        return self.es.enter_context(self.nc.semaphore(f"s_{name}_{self.nsem}"))

    def _wait(self, e, deps):
        w = self.waited[e]
        for (sem, val) in deps:
            if val <= 0:
                continue
            k = id(sem)
            if e == "pe" and k in self.own["pe"]:
                continue
            if w.get(k, 0) >= val:
                continue
            self.eng[e].wait_ge(sem, val)
            w[k] = val

    @staticmethod
    def _deps(reads, writes):
        deps = []
        for t in reads:
            if t.w is not None:
                deps.append(t.w)
        for t in writes:
            if t.w is not None:
                deps.append(t.w)
            deps.extend(t.r.values())
        return deps

    @staticmethod
    def _record(stamp, reads, writes):
        sem, val = stamp
        for t in reads:
            t.r[id(sem)] = stamp
        for t in writes:
            t.w = stamp
            t.r = {}

    def op(self, e, fn, reads=(), writes=(), inc=True):
        self._wait(e, self._deps(reads, writes))
        ins = fn(self.eng[e])
        sem, cnt = self.cur[e]
        stamp = (sem, cnt + 1)
        if inc:
            ins.then_inc(sem, 1)
            self.cur[e][1] = cnt + 1
        self._record(stamp, reads, writes)
        if inc and cnt + 1 >= EPOCH:
            ns = self._newsem(e)
            self.own[e].add(id(ns))
            self.cur[e] = [ns, 0]
        return ins

    def dma(self, out, in_, reads=(), writes=(), q="sp", **kw):
        dq = self.dq[q]
        i = dq["i"] % len(dq["sems"])
        dq["i"] += 1
        sem = dq["sems"][i]
        deps = self._deps(reads, writes)
        deps.append((sem, dq["tgt"][i]))
        self._wait(q, deps)
        self.eng[q].dma_start(out=out, in_=in_, **kw).then_inc(sem, 16)
        dq["tgt"][i] += 16
        stamp = (sem, dq["tgt"][i])
        self._record(stamp, reads, writes)
        return stamp

    def barrier(self):
        stamps = []
        for e in ("pe", "act", "dve", "pool"):
            sem, cnt = self.cur[e]
            stamps.append((sem, cnt))
        for q, dq in self.dq.items():
            for s, t in zip(dq["sems"], dq["tgt"]):
                stamps.append((s, t))
        for e in ("pe", "act", "dve", "pool", "sp"):
            self._wait_all(e, stamps)

    def _wait_all(self, e, stamps):
        w = self.waited[e]
        for (sem, val) in stamps:
            if val <= 0:
                continue
            k = id(sem)
            if k in self.own.get(e, ()) and (e == "pe"):
                continue
            if w.get(k, 0) >= val:
                continue
            self.eng[e].wait_ge(sem, val)
            w[k] = val


class Builder:
    def __init__(self, layers, first, last, debug=(), stage=99):
        self.stage = stage
        self.layers = layers
        self.first = first
        self.last = last
        self.debug = debug
        self.nc = bass.Bass("TRN2", target_bir_lowering=False)
        self.dbg_out = {}

    def sb(self, st, name, shape, dt):
        self._uid = getattr(self, "_uid", 0) + 1
        return st.enter_context(self.nc.sbuf_tensor(f"{name}_{self._uid}", shape, dt))

    def dram_in(self, name, shape, dt=F32):
        return self.nc.dram_tensor(name, list(shape), dt, kind="ExternalInput").ap()

    def mm(self, out, lhsT, rhs, start, stop, reads, writes, inc=None, **kw):
        if inc is None:
            inc = True
        return self.c.op("pe", lambda e: e.matmul(out, lhsT, rhs, start=start, stop=stop, **kw),
                         reads=reads, writes=writes, inc=inc)

    def bank(self):
        i = self.bank_i % 6
        self.bank_i += 1
        return self.ps[i], self.ps_tok[i]

    def bank_acc(self):
        i = 6 + self.bank_j % 2
        self.bank_j += 1
        return self.ps[i], self.ps_tok[i]

    def wload(self, src3, kc, ncols, eng="pool"):
        i = self.w_i % 2
        self.w_i += 1
        stg, stok = self.wstg[i], self.wstg_tok[i]
        wb, wtok = self.wbf[i], self.wbf_tok[i]
        n = kc * ncols
        assert n <= self.WMAX
        sv = stg[:, 0:n].rearrange("p (c n) -> p c n", c=kc)
        wv = wb[:, 0:n].rearrange("p (c n) -> p c n", c=kc)
        self.c.dma(sv, src3, reads=(), writes=(stok,))
        self.c.op(eng, lambda e: e.tensor_copy(wb[:, 0:n], stg[:, 0:n]), reads=(stok,), writes=(wtok,))
        return wv, wtok

    def build(self):
        nc = self.nc
        L = len(self.layers)
        A = {}
        A["x"] = self.dram_in("x", [S, D])
        A["w_in"] = self.dram_in("w_in", [L, D, D_IN])
        A["vecs"] = self.dram_in("vecs", [L, 72, 128])
        A["norm_final"] = self.dram_in("norm_final", [8, 128])
        A["positions"] = self.dram_in("positions", [1, S], I32)
        A["cmp_pos_k"] = self.dram_in("cmp_pos_k", [L, 32, 64])
        A["cmp_pos_v"] = self.dram_in("cmp_pos_v", [L, 32, 64])
        A["cmp_wk1"] = self.dram_in("cmp_wk1", [L, 2048, 256])
        A["cmp_wk2"] = self.dram_in("cmp_wk2", [L, 256, 64])
        A["cmp_wv1"] = self.dram_in("cmp_wv1", [L, 2048, 256])
        A["cmp_wv2"] = self.dram_in("cmp_wv2", [L, 256, 64])
        A["fox_b_f"] = self.dram_in("fox_b_f", [L, 8])
        A["gla_w_alpha"] = self.dram_in("gla_w_alpha", [L, 16, 256])
        A["gla_b_alpha"] = self.dram_in("gla_b_alpha", [L, 256])
        A["gla_norm"] = self.dram_in("gla_norm", [L, 128])
        A["w_branch"] = self.dram_in("w_branch", [L, 4, 512, D])
        A["w_out"] = self.dram_in("w_out", [L, D, D])
        A["w_ff1"] = self.dram_in("w_ff1", [L, D, DFF])
        A["w_ff2"] = self.dram_in("w_ff2", [L, DFF, D])
        self.A = A
        out = nc.dram_tensor("out", [S, D], F32, kind="ExternalOutput").ap()
        for name, shape in self.debug:
            self.dbg_out[name] = nc.dram_tensor("dbg_" + name, list(shape), F32, kind="ExternalOutput").ap()

        with ExitStack() as es:
            self.es = es
            c = self.c = Ctx(nc, es)
            self.xT = self.sb(es, "xT", [128, KC, S], F32)
            self.hT = self.sb(es, "hT", [128, KC, S], BF16)
            self.xT_tok = [Tok(f"xT{i}") for i in range(NTC)]
            self.hT_tok = [Tok(f"hT{i}") for i in range(NTC)]
            self.ident_f = self.sb(es, "ident_f", [128, 128], F32)
            self.ident_b = self.sb(es, "ident_b", [128, 128], BF16)
            self.ones_f = self.sb(es, "ones_f", [128, 128], F32)
            self.const_tok = Tok("const")
            self.vecT = self.sb(es, "vecT", [128, L * 72 + 8], F32)
            self.vec_tok = Tok("vec")
            self.WMAX = 1024
            self.wstg = [self.sb(es, f"wstg{i}", [128, self.WMAX], F32) for i in range(2)]
            self.wbf = [self.sb(es, f"wbf{i}", [128, self.WMAX], BF16) for i in range(2)]
            self.wstg_tok = [Tok() for _ in range(2)]
            self.wbf_tok = [Tok() for _ in range(2)]
            self.w_i = 0
            self.scr = [self.sb(es, f"scr{i}", [128, 512], F32) for i in range(4)]
            self.scr_tok = [Tok() for _ in range(4)]
            self.scr_i = 0
            self.ps = [es.enter_context(nc.psum_tensor(f"ps{i}", [128, 512], F32)) for i in range(8)]
            self.ps_tok = [Tok(f"ps{i}") for i in range(8)]
            self.bank_i = 0
            self.bank_j = 0

            self.make_consts()
            if self.first:
                self.load_x()
            else:
                self.load_xT()
            for li in range(L):
                if self.stage >= 1:
                    self.layer(li)
            if self.last and self.stage >= 3:
                self.final_norm_store(out)
            else:
                self.store_xT(out)
            c.barrier()
        return nc

    def nscr(self):
        i = self.scr_i % len(self.scr)
        self.scr_i += 1
        return self.scr[i], self.scr_tok[i]

    def make_consts(self):
        c = self.c
        nc = self.nc
        ct = self.const_tok
        c.op("pool", lambda e: e.memset(self.ones_f[:], 1.0), writes=(ct,))
        c.op("pool", lambda e: e.affine_select(self.ident_f[:], self.ones_f[:], [[-1, 128]], ALU.is_equal, 0.0,
                                               base=0, channel_multiplier=1), reads=(ct,), writes=(ct,))
        c.op("pool", lambda e: e.tensor_copy(self.ident_b[:], self.ident_f[:]), reads=(ct,), writes=(ct,))
        self.tri_f = self.sb(self.es, "tri_f", [128, 128], F32)
        c.op("pool", lambda e: e.affine_select(self.tri_f[:], self.ones_f[:], [[1, 128]], ALU.is_ge, 0.0,
                                               base=0, channel_multiplier=-1), reads=(ct,), writes=(ct,))
        self.zer_f = self.sb(self.es, "zer_f", [128, 128], F32)
        self.cneg_b = self.sb(self.es, "cneg_b", [128, 128], BF16)
        c.op("pool", lambda e: e.memset(self.zer_f[:], 0.0), writes=(ct,))
        self.nones_f = self.sb(self.es, "nones_f", [128, 128], F32)
        self.ones_b = self.sb(self.es, "ones_b", [128, 128], BF16)
        self.nones_b = self.sb(self.es, "nones_b", [128, 128], BF16)
        self.cnegs_b = self.sb(self.es, "cnegs_b", [128, 128], BF16)
        self.ntri_b = self.sb(self.es, "ntri_b", [128, 128], BF16)
        c.op("pool", lambda e: e.memset(self.nones_f[:], -1.0), writes=(ct,))
        c.op("pool", lambda e: e.memset(self.ones_b[:], 1.0), writes=(ct,))
        c.op("pool", lambda e: e.memset(self.nones_b[:], -1.0), writes=(ct,))
        c.op("pool", lambda e: e.affine_select(self.cnegs_b[:], self.zer_f[:], [[1, 128]], ALU.is_gt, NEG,
                                               base=0, channel_multiplier=-1), reads=(ct,), writes=(ct,))
        c.op("pool", lambda e: e.affine_select(self.ntri_b[:], self.nones_f[:], [[-1, 128]], ALU.is_ge, 0.0,
                                               base=0, channel_multiplier=1), reads=(ct,), writes=(ct,))
        c.op("pool", lambda e: e.affine_select(self.cneg_b[:], self.zer_f[:], [[1, 128]], ALU.is_ge, NEG,
                                               base=0, channel_multiplier=-1), reads=(ct,), writes=(ct,))
        L = len(self.layers)
        nrow = L * 72 + 8
        with ExitStack() as st:
            tmp = self.sb(st, "vtmp", [128, 4, 128], F32)
            tt = Tok()
            r0 = 0
            chunks = []
            while r0 < nrow:
                n = min(128, nrow - r0)
                chunks.append((r0, n))
                r0 += n
            for ci, (r0, n) in enumerate(chunks):
                a = r0
                while a < r0 + n:
                    if a < L * 72:
                        b = min(r0 + n, L * 72)
                        src = self.A["vecs"].rearrange("l r p -> (l r) p")[a:b, :]
                    else:
                        b = r0 + n
                        src = self.A["norm_final"][a - L * 72:b - L * 72, :]
                    c.dma(tmp[a - r0:b - r0, ci, :], src, writes=(tt,))
                    a = b
                pb, pt = self.bank()
                c.op("pe", lambda e: e.transpose(pb[:, 0:n], tmp[0:n, ci, :], self.ident_f[0:n, 0:n]),
                     reads=(tt, ct), writes=(pt,))
                c.op("dve", lambda e: e.tensor_copy(self.vecT[:, r0:r0 + n], pb[:, 0:n]), reads=(pt,),
                     writes=(self.vec_tok,))
            c.barrier()

    def vcol(self, li, kind, j):
        base = li * 72 + {"norm_mix": 0, "norm_ff": 8, "b_gate": 16}[kind]
        return self.vecT[:, base + j:base + j + 1]

    def load_x(self):
        c = self.c
        x = self.A["x"]
        with ExitStack() as st:
            xs = [self.sb(st, f"xs{i}", [128, D], F32) for i in range(2)]
            xs_tok = [Tok(), Tok()]
            for t in range(NT):
                b = t % 2
                c.dma(xs[b][:], x[t * 128:(t + 1) * 128, :], writes=(xs_tok[b],))
                for half in range(2):
                    pb, pt = self.bank()
                    for j in range(4):
                        cc = half * 4 + j
                        c.op("pe", lambda e: e.transpose(pb[:, j * 128:(j + 1) * 128], xs[b][:, cc * 128:(cc + 1) * 128],
                                                         self.ident_f[:]),
                             reads=(xs_tok[b], self.const_tok), writes=(pt,), inc=(j == 3))
                    dst = self.xT[:, half * 4:half * 4 + 4, t * 128:(t + 1) * 128]
                    src = pb[:].rearrange("p (j n) -> p j n", j=4)
                    eng = "dve" if half == 0 else "act"
                    if eng == "dve":
                        c.op("dve", lambda e: e.tensor_copy(dst, src), reads=(pt,), writes=(self.xT_tok[t // 4],))
                    else:
                        c.op("act", lambda e: e.copy(dst, src), reads=(pt,), writes=(self.xT_tok[t // 4],))
            c.barrier()

    def load_xT(self):
        raise NotImplementedError

    def store_xT(self, out):
        self.store_tok_major(out, normed=False)

    def rmsnorm_to_hT(self, gcol):
        c = self.c
        for tc in range(NTC):
            ts = slice(tc * 512, (tc + 1) * 512)
            pb, pt = self.bank()
            for cc in range(KC):
                sq, sqt = self.nscr()
                c.op("act", lambda e: e.activation(sq[:], self.xT[:, cc, ts], AF.Square),
                     reads=(self.xT_tok[tc],), writes=(sqt,))
                self.mm(pb[:], self.ones_f[:], sq[:], cc == 0, cc == KC - 1, reads=(sqt, self.const_tok), writes=(pt,))
            rs, rst = self.nscr()
            c.op("dve", lambda e: e.tensor_scalar(rs[:], pb[:], 1.0 / D, EPS, ALU.mult, ALU.add), reads=(pt,),
                 writes=(rst,))
            c.op("act", lambda e: e.activation(rs[:], rs[:], AF.Sqrt), reads=(rst,), writes=(rst,))
            c.op("dve", lambda e: e.reciprocal(rs[:], rs[:]), reads=(rst,), writes=(rst,))
            for cc in range(KC):
                c.op("dve", lambda e: e.scalar_tensor_tensor(self.hT[:, cc, ts], self.xT[:, cc, ts], gcol(cc), rs[:],
                                                             ALU.mult, ALU.mult),
                     reads=(self.xT_tok[tc], rst, self.vec_tok), writes=(self.hT_tok[tc],))

    def layer(self, li):
        self.rmsnorm_to_hT(lambda cc: self.vcol(li, "norm_mix", cc))
        if "hT" in self.dbg_out and li == 0:
            self.dump_featmajor_bf16(self.hT, self.hT_tok, self.dbg_out["hT"])
        if self.stage >= 4:
            self.yT = self.sb(self.es, f"yT{li}", [128, 4, S], BF16) if not hasattr(self, "yT") else self.yT
            self.yT_tok = Tok("yT")
            if self.stage >= 8:
                self.nsa(li)
                if "ynsa" in self.dbg_out and li == 0:
                    self.dump_featmajor_bf16(self.yT, [self.yT_tok], self.dbg_out["ynsa"])
                self.combine(li, 0)
            if self.stage == 8:
                return
            if self.stage >= 7:
                self.gla(li)
                if "ygla" in self.dbg_out and li == 0:
                    self.dump_featmajor_bf16(self.yT, [self.yT_tok], self.dbg_out["ygla"])
                self.combine(li, 2)
            if self.stage >= 6 and self.stage != 7:
                self.sbmix(li)
                if "ysb" in self.dbg_out and li == 0:
                    self.dump_featmajor_bf16(self.yT, [self.yT_tok], self.dbg_out["ysb"])
                self.combine(li, 1)
            if self.stage == 7:
                return
            self.fox(li)
            if "yT" in self.dbg_out and li == 0:
                self.dump_featmajor_bf16(self.yT, [self.yT_tok], self.dbg_out["yT"])
            if self.stage >= 5:
                self.combine(li, 3)
        if self.stage >= 2:
            self.rmsnorm_to_hT(lambda cc: self.vcol(li, "norm_ff", cc))
            self.ffn(li)

    def ffn(self, li):
        c = self.c
        w1 = self.A["w_ff1"][li].rearrange("(c p) n -> p c n", p=128)
        w2 = self.A["w_ff2"][li].rearrange("(f p) n -> p f n", p=128)
        G = 4
        with ExitStack() as st:
            aT = [self.sb(st, f"aT{i}", [128, G, S], BF16) for i in range(2)]
            aT_tok = [Tok(), Tok()]
            import os
            for g in range(int(os.environ.get('FFN_G', DFF // (128 * G)))):
                ab, abt = aT[g % 2], aT_tok[g % 2]
                for half in range(G):
                    f0 = g * G + half
                    wv, wt = self.wload(w1[:, :, f0 * 128:(f0 + 1) * 128], KC, 128)
                    for j in range(1):
                        for tc in range(NTC):
                            ts = slice(tc * 512, (tc + 1) * 512)
                            pb, pt = self.bank()
                            for cc in range(KC):
                                self.mm(pb[:], wv[:, cc, j * 128:(j + 1) * 128], self.hT[:, cc, ts], cc == 0, cc == KC - 1,
                                        reads=(wt, self.hT_tok[tc]), writes=(pt,))
                            r, rt = self.nscr()
                            c.op("act", lambda e: e.activation(r[:], pb[:], AF.Relu), reads=(pt,), writes=(rt,))
                            c.op("dve", lambda e: e.tensor_tensor(ab[:, half, ts], r[:], r[:], ALU.mult),
                                 reads=(rt,), writes=(abt,))
                for dh in range(4):
                    wv, wt = self.wload(w2[:, g * G:(g + 1) * G, dh * 256:(dh + 1) * 256], G, 256)
                    for j in range(2):
                        dt_ = dh * 2 + j
                        for tc in range(NTC):
                            ts = slice(tc * 512, (tc + 1) * 512)
                            pb, pt = self.bank()
                            for f in range(G):
                                self.mm(pb[:], wv[:, f, j * 128:(j + 1) * 128], ab[:, f, ts], f == 0, f == G - 1,
                                        reads=(wt, abt), writes=(pt,))
                            c.op("dve", lambda e: e.tensor_tensor(self.xT[:, dt_, ts], self.xT[:, dt_, ts], pb[:], ALU.add),
                                 reads=(pt,), writes=(self.xT_tok[tc],))
            c.barrier()


    def proj_feat(self, li, col0, ncols, evac):
        w = self.A["w_in"][li].rearrange("(c p) n -> p c n", p=128)
        n0 = 0
        while n0 < ncols:
            nn = min(128, ncols - n0)
            wv, wt = self.wload(w[:, :, col0 + n0:col0 + n0 + nn], KC, nn)
            for j in range((nn + 127) // 128):
                m = min(128, nn - j * 128)
                for tc in range(NTC):
                    ts = slice(tc * 512, (tc + 1) * 512)
                    pb, pt = self.bank()
                    for cc in range(KC):
                        self.mm(pb[0:m, :], wv[:, cc, j * 128:j * 128 + m], self.hT[:, cc, ts], cc == 0, cc == KC - 1,
                                reads=(wt, self.hT_tok[tc]), writes=(pt,))
                    evac((n0 + j * 128) // 128, tc, pb, pt)
            n0 += nn

    def proj_tok(self, li, col0, ncols, evac):
        w = self.A["w_in"][li].rearrange("(c p) n -> p c n", p=128)
        wv, wt = self.wload(w[:, :, col0:col0 + ncols], KC, ncols)
        for t in range(NT):
            pb, pt = self.bank()
            for cc in range(KC):
                self.mm(pb[:, 0:ncols], self.hT[:, cc, t * 128:(t + 1) * 128], wv[:, cc, :], cc == 0, cc == KC - 1,
                        reads=(wt, self.hT_tok[t // 4]), writes=(pt,))
            evac(t, pb, pt)

    def evac_featT(self, dst, dtok, scale=1.0):
        c = self.c
        cnt = [0]

        def f(mt, tc, pb, pt):
            ts = slice(tc * 512, (tc + 1) * 512)
            cnt[0] += 1
            if cnt[0] % 2 == 0:
                c.op("dve", lambda e: e.tensor_scalar(dst[:, mt, ts], pb[:], scale, None, ALU.mult), reads=(pt,),
                     writes=(dtok,))
            else:
                c.op("act", lambda e: e.activation(dst[:, mt, ts], pb[:], AF.Copy, scale=scale), reads=(pt,),
                     writes=(dtok,))
        return f

    def attention(self, st, name, nheads, qT, qtok, kT, ktok, V, vtok, ytok_t, ytok_tok, bias_fn=None, ycol0=0, post_qc=None):
        c = self.c
        pT = [self.sb(st, f"{name}_pT{i}", [128, 512], BF16) for i in range(3)]
        pT_tok = [Tok() for _ in range(3)]
        rc = self.sb(st, f"{name}_rc", [128, 8], F32)
        rc_tok = Tok()
        pi = 0
        for qc in range(NTC):
            for h in range(nheads):
                hp = slice((h % 2) * 64, (h % 2) * 64 + 64)
                hc = h // 2
                ob, ot = self.bank_acc()
                O = ob[:, 0:260].rearrange("p (j d) -> p j d", j=4)
                nkt = 4 * qc + 4
                for kt in range(nkt):
                    j0 = max(0, kt - 4 * qc)
                    q0 = qc * 512 + j0 * 128
                    ncol = 512 - j0 * 128
                    sb_, stk = self.bank()
                    diag = kt >= 4 * qc
                    self.mm(sb_[:, 0:ncol], kT[hp, hc, kt * 128:(kt + 1) * 128], qT[hp, hc, q0:q0 + ncol], True, not diag,
                            reads=(ktok, qtok), writes=(stk,))
                    if diag:
                        self.mm(sb_[:, 0:128], self.ident_b[:], self.cneg_b[:], False, True,
                                reads=(self.const_tok,), writes=(stk,), skip_group_check=True)
                    p, ptk = pT[pi % 3], pT_tok[pi % 3]
                    pi += 1
                    for j in range(j0, 4):
                        qt = qc * 4 + j
                        cs = slice((j - j0) * 128, (j - j0 + 1) * 128)
                        b = bias_fn(h, kt, qt) if bias_fn is not None else 0.0
                        c.op("act", lambda e: e.activation(p[:, cs], sb_[:, cs], AF.Exp, bias=b), reads=(stk, self.aux_tok),
                             writes=(ptk,))
                    for j in range(j0, 4):
                        qt = qc * 4 + j
                        cs = slice((j - j0) * 128, (j - j0 + 1) * 128)
                        self.mm(O[:, j, :], p[:, cs], V[:, kt, h, :], kt == 0 and j == 0, kt == qt, reads=(ptk, vtok), writes=(ot,),
                                skip_group_check=True)
                for j in range(4):
                    qt = qc * 4 + j
                    c.op("dve", lambda e: e.reciprocal(rc[:, j:j + 1], O[:, j, 64:65]), reads=(ot,), writes=(rc_tok,))
                    c.op("dve", lambda e: e.tensor_scalar(ytok_t[:, j, ycol0 + h * 64:ycol0 + (h + 1) * 64], O[:, j, 0:64],
                                                          rc[:, j:j + 1], None, ALU.mult),
                         reads=(ot, rc_tok), writes=(ytok_tok,))
            if post_qc is not None:
                post_qc(qc)

    def ytok_to_yT(self, ytok_t, ytok_tok, qc):
        c = self.c
        for tl in range(4):
            t = qc * 4 + tl
            pb, pt = self.bank()
            pbb = pb[:].bitcast(BF16)
            for j in range(4):
                c.op("pe", lambda e: e.transpose(pbb[:, j * 128:(j + 1) * 128], ytok_t[:, tl, j * 128:(j + 1) * 128],
                                                 self.ident_b[:]),
                     reads=(ytok_tok, self.const_tok), writes=(pt,))
            c.op("dve", lambda e: e.tensor_copy(self.yT[:, :, t * 128:(t + 1) * 128],
                                                pbb[:, 0:512].rearrange("p (j n) -> p j n", j=4)),
                 reads=(pt,), writes=(self.yT_tok,))


    def combine(self, li, bi):
        c = self.c
        wg = self.A["w_in"][li].rearrange("(c p) n -> p c n", p=128)
        wb = self.A["w_branch"][li, bi].rearrange("(c p) n -> p c n", p=128)
        wo = self.A["w_out"][li].rearrange("(c p) n -> p c n", p=128)
        with ExitStack() as st:
            mT = self.sb(st, "mT", [128, KC, S], BF16)
            mtok = Tok()
            for dt_ in range(KC):
                g0 = O_GATE + bi * D + dt_ * 128
                wgv, wgt = self.wload(wg[:, :, g0:g0 + 128], KC, 128)
                wbv, wbt = self.wload(wb[:, :, dt_ * 128:(dt_ + 1) * 128], 4, 128)
                bcol = self.vcol(li, "b_gate", bi * 8 + dt_)
                for tc in range(NTC):
                    ts = slice(tc * 512, (tc + 1) * 512)
                    pa, pat = self.bank()
                    for cc in range(KC):
                        self.mm(pa[:], wgv[:, cc, :], self.hT[:, cc, ts], cc == 0, cc == KC - 1,
                                reads=(wgt, self.hT_tok[tc]), writes=(pat,))
                    pb, pbt = self.bank()
                    for cc in range(4):
                        self.mm(pb[:], wbv[:, cc, :], self.yT[:, cc, ts], cc == 0, cc == 3,
                                reads=(wbt, self.yT_tok), writes=(pbt,))
                    sg, sgt = self.nscr()
                    c.op("act", lambda e: e.activation(sg[:], pa[:], AF.Sigmoid, bias=bcol), reads=(pat, self.vec_tok),
                         writes=(sgt,))
                    c.op("dve", lambda e: e.tensor_tensor(mT[:, dt_, ts], sg[:], pb[:], ALU.mult), reads=(sgt, pbt),
                         writes=(mtok,))
            for do in range(KC):
                wov, wot = self.wload(wo[:, :, do * 128:(do + 1) * 128], KC, 128)
                for tc in range(NTC):
                    ts = slice(tc * 512, (tc + 1) * 512)
                    pb, pbt = self.bank()
                    for cc in range(KC):
                        self.mm(pb[:], wov[:, cc, :], mT[:, cc, ts], cc == 0, cc == KC - 1, reads=(wot, mtok), writes=(pbt,))
                    c.op("dve", lambda e: e.tensor_tensor(self.xT[:, do, ts], self.xT[:, do, ts], pb[:], ALU.add),
                         reads=(pbt,), writes=(self.xT_tok[tc],))
            c.barrier()


    def sbmix(self, li):
        c = self.c
        with ExitStack() as st:
            qT = self.sb(st, "sb_qT", [128, 4, S], BF16)
            kT = self.sb(st, "sb_kT", [128, 4, S], BF16)
            V = self.sb(st, "sb_V", [128, NT, 8, 64], BF16)
            ytk = self.sb(st, "sb_y", [128, 4, 512], BF16)
            spb = [self.sb(st, f"sb_sp{i}", [128, 512], BF16) for i in range(2)]
            spt = [Tok(), Tok()]
            pT = [self.sb(st, f"sb_pT{i}", [128, 512], BF16) for i in range(2)]
            pTt = [Tok(), Tok()]
            suf = self.sb(st, "sb_suf", [1, 512], F32)
            sufh = self.sb(st, "sb_sufh", [1, 512], BF16)
            sufl = self.sb(st, "sb_sufl", [1, 512], BF16)
            suft = Tok()
            qtok, ktok, vtok, ytok = Tok(), Tok(), Tok(), Tok()
            self.proj_feat(li, O_SQ, 512, self.evac_featT(qT, qtok, 0.125))
            self.proj_feat(li, O_SK, 512, self.evac_featT(kT, ktok, 1.0))
            for q4 in range(4):
                def evac_vh(t, pb, pt, q4=q4):
                    c.op("act", lambda e: e.copy(V[:, t, q4 * 2:q4 * 2 + 2, :],
                                                 pb[:, 0:128].rearrange("p (h d) -> p h d", h=2)),
                         reads=(pt,), writes=(vtok,))
                self.proj_tok(li, O_SV + q4 * 128, 128, evac_vh)
            ti = 0
            for qc in range(NTC):
                for h in range(8):
                    hp = slice((h % 2) * 64, (h % 2) * 64 + 64)
                    hc = h // 2
                    ob, ot = self.bank_acc()
                    O = ob[:, 0:256].rearrange("p (j d) -> p j d", j=4)
                    nkt = 4 * qc + 4
                    c.op("dve", lambda e: e.memset(suf[:], 0.0), writes=(suft,))
                    c.op("dve", lambda e: e.memset(sufh[:], 0.0), writes=(suft,))
                    c.op("dve", lambda e: e.memset(sufl[:], 0.0), writes=(suft,))
                    first = True
                    for kt in range(nkt - 1, -1, -1):
                        j0 = max(0, kt - 4 * qc)
                        q0 = qc * 512 + j0 * 128
                        ncol = 512 - j0 * 128
                        diag = kt >= 4 * qc
                        ksl = kT[hp, hc, kt * 128:(kt + 1) * 128]
                        qsl = qT[hp, hc, q0:q0 + ncol]
                        pa, pat = self.bank()
                        self.mm(pa[:, 0:ncol], ksl, qsl, True, not diag, reads=(ktok, qtok), writes=(pat,))
                        if diag:
                            self.mm(pa[:, 0:128], self.ident_b[:], self.cnegs_b[:], False, True, reads=(self.const_tok,),
                                    writes=(pat,), skip_group_check=True)
                        e_, et = self.nscr()
                        sp, spk = spb[ti % 2], spt[ti % 2]
                        p, ptk = pT[ti % 2], pTt[ti % 2]
                        ti += 1
                        c.op("act", lambda e: e.activation(e_[:, 0:ncol], pa[:, 0:ncol], AF.Exp), reads=(pat,), writes=(et,))
                        c.op("act", lambda e: e.activation(sp[:, 0:ncol], e_[:, 0:ncol], AF.Ln, bias=1.0), reads=(et,),
                             writes=(spk,))
                        pb, pbt = self.bank()
                        self.mm(pb[:, 0:ncol], ksl, qsl, True, False, reads=(ktok, qtok), writes=(pbt,))
                        if diag:
                            self.mm(pb[:, 0:128], self.ident_b[:], self.cnegs_b[:], False, False, reads=(self.const_tok,),
                                    writes=(pbt,), skip_group_check=True)
                        self.mm(pb[:, 0:ncol], self.nones_b[0:1, :], sufh[0:1, 512 - ncol:512], False, False,
                                reads=(suft, self.const_tok), writes=(pbt,), skip_group_check=True)
                        self.mm(pb[:, 0:ncol], self.nones_b[0:1, :], sufl[0:1, 512 - ncol:512], False, False,
                                reads=(suft, self.const_tok), writes=(pbt,), skip_group_check=True)
                        self.mm(pb[:, 0:ncol], self.ntri_b[:], sp[:, 0:ncol], False, True, reads=(spk, self.const_tok),
                                writes=(pbt,), skip_group_check=True)
                        c.op("act", lambda e: e.activation(p[:, 0:ncol], pb[:, 0:ncol], AF.Exp), reads=(pbt,), writes=(ptk,))
                        for j in range(j0, 4):
                            cs = slice((j - j0) * 128, (j - j0 + 1) * 128)
                            self.mm(O[:, j, :], p[:, cs], V[:, kt, h, :], first, kt == 0, reads=(ptk, vtok), writes=(ot,),
                                    skip_group_check=True)
                            first = False
                        if kt > 0:
                            pc, pct = self.bank()
                            self.mm(pc[0:1, 0:ncol], self.ones_b[:, 0:1], sp[:, 0:ncol], True, True,
                                    reads=(spk, self.const_tok), writes=(pct,))
                            sl = slice(512 - ncol, 512)
                            c.op("dve", lambda e: e.tensor_tensor(suf[0:1, sl], suf[0:1, sl], pc[0:1, 0:ncol], ALU.add),
                                 reads=(pct,), writes=(suft,))
                            c.op("dve", lambda e: e.tensor_copy(sufh[0:1, sl], suf[0:1, sl]), reads=(suft,), writes=(suft,))
                            c.op("dve", lambda e: e.tensor_tensor(sufl[0:1, sl], suf[0:1, sl], sufh[0:1, sl], ALU.subtract),
                                 reads=(suft,), writes=(suft,))
                    for j in range(4):
                        c.op("dve", lambda e: e.tensor_copy(ytk[:, j, h * 64:(h + 1) * 64], O[:, j, :]), reads=(ot,),
                             writes=(ytok,))
                self.ytok_to_yT(ytk, ytok, qc)
            c.barrier()


    def gla(self, li):
        c = self.c
        ct = self.const_tok
        with ExitStack() as st:
            qeT = self.sb(st, "g_qe", [64, 4, S], BF16)
            keT = self.sb(st, "g_ke", [64, 4, S], BF16)
            k2 = self.sb(st, "g_k2", [128, NT, 256], BF16)
            vtk = self.sb(st, "g_v", [128, NT, 512], BF16)
            gnorm = self.sb(st, "g_norm", [128, 1], F32)
            dec = self.sb(st, "g_dec", [64, 4, 32], F32)
            tblk = self.sb(st, "g_tblk", [128, 128], F32)
            sp_ = ExitStack()
            alrT = self.sb(sp_, "g_alr", [16, S], BF16)
            balb = self.sb(sp_, "g_bal", [128, 256], F32)
            wal_f = self.sb(sp_, "g_walf", [16, 256], F32)
            wal_b = self.sb(sp_, "g_walb", [16, 256], BF16)
            n16 = self.sb(sp_, "g_n16", [128, 128], F32)
            m1 = self.sb(sp_, "g_m1", [128, 128], F32)
            m2 = self.sb(sp_, "g_m2", [128, 128], F32)
            att_tok = [Tok(), Tok()]
            mtok, qtok, ktok, vtok, k2tok, atok, ptok, stok, otok, ontok = [Tok() for _ in range(10)]
            c.op("pool", lambda e: e.memset(n16[:], -1.0 / 16.0), writes=(mtok,))
            c.op("pool", lambda e: e.affine_select(m1[:], n16[:], [[1, 128]], ALU.is_ge, 0.0, base=0, channel_multiplier=-1),
                 reads=(mtok,), writes=(mtok,))
            c.op("pool", lambda e: e.memset(m1[0:64, 64:128], 0.0), reads=(mtok,), writes=(mtok,))
            c.op("pool", lambda e: e.affine_select(m2[:], n16[:], [[-1, 128]], ALU.is_gt, 0.0, base=0, channel_multiplier=1),
                 reads=(mtok,), writes=(mtok,))
            c.op("pool", lambda e: e.memset(m2[64:128, 0:64], 0.0), reads=(mtok,), writes=(mtok,))
            c.op("pool", lambda e: e.tensor_copy(tblk[:], self.tri_f[:]), reads=(ct, mtok), writes=(mtok,))
            c.op("pool", lambda e: e.memset(tblk[0:64, 64:128], 0.0), reads=(mtok,), writes=(mtok,))
            c.dma(balb[:], self.A["gla_b_alpha"][li:li + 1, :].partition_broadcast(128), writes=(ptok,))
            c.dma(wal_f[:], self.A["gla_w_alpha"][li], writes=(ptok,))
            c.dma(gnorm[:], self.A["gla_norm"][li].rearrange("(p o) -> p o", o=1), writes=(ptok,))
            c.op("pool", lambda e: e.tensor_copy(wal_b[:], wal_f[:]), reads=(ptok,), writes=(ptok,))
            for h in range(4):
                def ev_q(mt, tc, pb, pt, h=h):
                    ts = slice(tc * 512, (tc + 1) * 512)
                    c.op("act", lambda e: e.activation(qeT[0:64, h, ts], pb[0:64, :], AF.Copy, scale=0.125), reads=(pt,),
                         writes=(qtok,))

                def ev_k(mt, tc, pb, pt, h=h):
                    ts = slice(tc * 512, (tc + 1) * 512)
                    c.op("dve", lambda e: e.tensor_copy(keT[0:64, h, ts], pb[0:64, :]), reads=(pt,), writes=(ktok,))
                self.proj_feat(li, O_GQ + h * 64, 64, ev_q)
                self.proj_feat(li, O_GK + h * 64, 64, ev_k)

            def ev_a(mt, tc, pb, pt):
                ts = slice(tc * 512, (tc + 1) * 512)
                c.op("act", lambda e: e.copy(alrT[0:16, ts], pb[0:16, :]), reads=(pt,), writes=(atok,))
            self.proj_feat(li, O_GA, 16, ev_a)
            for i in range(4):
                def ev_v(t, pb, pt, i=i):
                    c.op("act", lambda e: e.copy(vtk[:, t, i * 128:(i + 1) * 128], pb[:, 0:128]), reads=(pt,), writes=(vtok,))
                self.proj_tok(li, O_GV + i * 128, 128, ev_v)
            import os
            gstop = int(os.environ.get("GLA_STOP", "99"))
            if gstop == 1:
                c.barrier()
                return
            for t in range(NT):
                tl = slice(t * 128, (t + 1) * 128)
                pa, pat = self.bank()
                self.mm(pa[:, 0:256], alrT[0:16, tl], wal_b[0:16, :], True, True, reads=(atok, ptok), writes=(pat,))
                xs, xst = self.nscr()
                c.op("dve", lambda e: e.tensor_tensor(xs[:, 0:256], pa[:, 0:256], balb[:], ALU.add), reads=(pat, ptok),
                     writes=(xst,))
                c.op("act", lambda e: e.activation(xs[:, 0:256], xs[:, 0:256], AF.Exp, scale=-1.0), reads=(xst,), writes=(xst,))
                c.op("act", lambda e: e.activation(xs[:, 0:256], xs[:, 0:256], AF.Ln, bias=1.0), reads=(xst,), writes=(xst,))
                pw, pwt = self.bank()
                self.mm(pw[:, 0:256], m2[:], xs[:, 0:256], True, True, reads=(mtok, xst), writes=(pwt,))
                c.op("act", lambda e: e.activation(k2[:, t, :], pw[:, 0:256], AF.Exp), reads=(pwt,), writes=(k2tok,))
                pbT, pbTt = self.bank()
                for h in range(4):
                    self.mm(pbT[0:64, h * 128:(h + 1) * 128], xs[:, h * 64:(h + 1) * 64], m1[:], h == 0, h == 3,
                            reads=(mtok, xst), writes=(pbTt,), skip_group_check=True)
                ebp, ebpt = self.nscr()
                ebn, ebnt = self.nscr()
                c.op("act", lambda e: e.activation(ebp[0:64, :], pbT[0:64, :], AF.Exp), reads=(pbTt,), writes=(ebpt,))
                c.op("act", lambda e: e.activation(ebn[0:64, :], pbT[0:64, :], AF.Exp, scale=-1.0), reads=(pbTt,), writes=(ebnt,))
                c.op("dve", lambda e: e.tensor_tensor(qeT[0:64, :, tl], qeT[0:64, :, tl],
                                                      ebp[0:64, :].rearrange("p (h n) -> p h n", h=4), ALU.mult),
                     reads=(ebpt,), writes=(qtok,))
                c.op("dve", lambda e: e.tensor_tensor(keT[0:64, :, tl], keT[0:64, :, tl],
                                                      ebn[0:64, :].rearrange("p (h n) -> p h n", h=4), ALU.mult),
                     reads=(ebnt,), writes=(ktok,))
                c.op("dve", lambda e: e.tensor_copy(dec[0:64, :, 2 * t:2 * t + 2],
                                                    ebp[0:64, :].rearrange("p (h c s) -> p h c s", h=4, c=2)[:, :, :, 63]),
                     reads=(ebpt,), writes=(stok,))
            c.barrier()
            sp_.close()
            if gstop == 2:
                return
            st_f = self.sb(st, "g_stf", [64, 4, 128], F32)
            st_b = self.sb(st, "g_stb", [64, 4, 128], BF16)
            oT = self.sb(st, "g_oT", [128, 512], F32)
            onh = self.sb(st, "g_on", [128, S], BF16)
            attb = [self.sb(st, f"g_att{i}", [128, 128], BF16) for i in range(2)]
            for i in range(2):
                def ev_k2(t, pb, pt, i=i):
                    c.op("dve", lambda e: e.tensor_tensor(k2[:, t, i * 128:(i + 1) * 128], k2[:, t, i * 128:(i + 1) * 128],
                                                          pb[:, 0:128], ALU.mult), reads=(pt,), writes=(k2tok,))
                self.proj_tok(li, O_GK + i * 128, 128, ev_k2)
            if gstop == 3:
                c.barrier()
                return
            ai = 0
            for h in range(4):
                c.op("dve", lambda e: e.memset(st_f[0:64, h, :], 0.0), writes=(stok,))
                c.op("dve", lambda e: e.memset(st_b[0:64, h, :], 0.0), writes=(stok,))
                for t in range(NT):
                    tl = slice(t * 128, (t + 1) * 128)
                    pa, pat = self.bank()
                    self.mm(pa[:, 0:128], keT[0:64, h, tl], qeT[0:64, h, tl], True, True, reads=(ktok, qtok), writes=(pat,))
                    ab, abt = attb[ai % 2], att_tok[ai % 2]
                    ai += 1
                    c.op("dve", lambda e: e.tensor_tensor(ab[:], pa[:, 0:128], tblk[:], ALU.mult), reads=(pat, mtok),
                         writes=(abt,))
                    for half in range(2):
                        cn = 2 * t + half
                        rs = slice(half * 64, half * 64 + 64)
                        cs = slice(cn * 64, (cn + 1) * 64)
                        vsl = vtk[rs, t, h * 128:(h + 1) * 128]
                        po, pot = self.bank()
                        inter = cn > 0 and gstop != 4
                        self.mm(po[:, 0:64], vsl, ab[rs, rs], True, True, reads=(vtok, abt), writes=(pot,))
                        oc = (t % 4) * 128 + half * 64
                        c.op("act", lambda e: e.copy(oT[:, oc:oc + 64], po[:, 0:64]), reads=(pot,), writes=(otok,))
                        if inter:
                            pi_, pit = self.bank()
                            self.mm(pi_[:, 0:64], st_b[0:64, h, :], qeT[0:64, h, cs], True, True, reads=(stok, qtok),
                                    writes=(pit,))
                            c.op("dve", lambda e: e.tensor_tensor(oT[:, oc:oc + 64], oT[:, oc:oc + 64], pi_[:, 0:64], ALU.add),
                                 reads=(pit, otok), writes=(otok,))
                        if gstop == 5:
                            continue
                        ps_, pst = self.bank()
                        self.mm(ps_[0:64, 0:128], k2[rs, t, h * 64:(h + 1) * 64], vsl, True, True, reads=(k2tok, vtok),
                                writes=(pst,))
                        c.op("dve", lambda e: e.scalar_tensor_tensor(st_f[0:64, h, :], st_f[0:64, h, :], dec[0:64, h, cn:cn + 1],
                                                                     ps_[0:64, 0:128], ALU.mult, ALU.add),
                             reads=(pst, stok), writes=(stok,))
                        c.op("dve", lambda e: e.tensor_copy(st_b[0:64, h, :], st_f[0:64, h, :]), reads=(stok,), writes=(stok,))
                    if t % 4 == 3:
                        tc = t // 4
                        ts = slice(tc * 512, (tc + 1) * 512)
                        sq, sqt = self.nscr()
                        c.op("act", lambda e: e.activation(sq[:], oT[:], AF.Square), reads=(otok,), writes=(sqt,))
                        pn, pnt = self.bank()
                        self.mm(pn[:], self.ones_f[:], sq[:], True, True, reads=(sqt, ct), writes=(pnt,))
                        rr, rrt = self.nscr()
                        c.op("dve", lambda e: e.tensor_scalar(rr[:], pn[:], 1.0 / 128.0, EPS, ALU.mult, ALU.add), reads=(pnt,),
                             writes=(rrt,))
                        c.op("act", lambda e: e.activation(rr[:], rr[:], AF.Sqrt), reads=(rrt,), writes=(rrt,))
                        c.op("dve", lambda e: e.reciprocal(rr[:], rr[:]), reads=(rrt,), writes=(rrt,))
                        c.op("dve", lambda e: e.scalar_tensor_tensor(onh[:, ts], oT[:], gnorm[:, 0:1], rr[:], ALU.mult, ALU.mult),
                             reads=(otok, rrt, ptok), writes=(ontok,))

                def ev_g(mt, tc, pb, pt, h=h):
                    ts = slice(tc * 512, (tc + 1) * 512)
                    sg, sgt = self.nscr()
                    c.op("act", lambda e: e.activation(sg[:], pb[:], AF.Silu), reads=(pt,), writes=(sgt,))
                    c.op("dve", lambda e: e.tensor_tensor(self.yT[:, h, ts], sg[:], onh[:, ts], ALU.mult), reads=(sgt, ontok),
                         writes=(self.yT_tok,))
                self.proj_feat(li, O_GG + h * 128, 128, ev_g)
            c.barrier()


    def proj_feat_dup(self, li, col0, evac):
        c = self.c
        w = self.A["w_in"][li].rearrange("(c p) n -> p c n", p=128)
        wv, wt = self.wload(w[:, :, col0:col0 + 64], KC, 64)
        wd, wdt = self.wdup, self.wdup_tok
        c.op("pool", lambda e: e.tensor_copy(wd[:, :, 0:64], wv), reads=(wt,), writes=(wdt,))
        c.op("pool", lambda e: e.tensor_copy(wd[:, :, 64:128], wv), reads=(wt,), writes=(wdt,))
        for tc in range(NTC):
            ts = slice(tc * 512, (tc + 1) * 512)
            pb, pt = self.bank()
            for cc in range(KC):
                self.mm(pb[:], wd[:, cc, :], self.hT[:, cc, ts], cc == 0, cc == KC - 1, reads=(wdt, self.hT_tok[tc]),
                        writes=(pt,))
            evac(0, tc, pb, pt)

    def rope_apply(self, dst, pb, pt, n, scale, cos_ap, sin_ap, dtok):
        c = self.c
        raw, rawt = self.rraw[self.rr_i % 2], self.rraw_tok[self.rr_i % 2]
        self.rr_i += 1
        c.op("act", lambda e: e.activation(raw[:, 0:n], pb, AF.Copy, scale=scale), reads=(pt,), writes=(rawt,))
        p2, p2t = self.bank()
        self.mm(p2[:, 0:n], self.Pm[:], raw[:, 0:n], True, True, reads=(rawt, self.tbl_tok), writes=(p2t,))
        t1, t1t = self.nscr()
        c.op("pool", lambda e: e.tensor_tensor(t1[:, 0:n], raw[:, 0:n], cos_ap, ALU.mult), reads=(rawt, self.tbl_tok),
             writes=(t1t,))
        t2, t2t = self.nscr()
        c.op("dve", lambda e: e.tensor_tensor(t2[:, 0:n], p2[:, 0:n], sin_ap, ALU.mult), reads=(p2t, self.tbl_tok),
             writes=(t2t,))
        c.op("dve", lambda e: e.tensor_tensor(dst, t1[:, 0:n], t2[:, 0:n], ALU.add), reads=(t1t, t2t), writes=(dtok,))

    def nsa_tables(self, cosT, sinT):
        c = self.c
        tbl = self.tbl_tok
        PI = float(np.pi)
        C1 = 6.28125
        C2 = float(2 * np.pi - 6.28125)
        with ExitStack() as s2:
            pidx = self.sb(s2, "n_pi", [128, 1], I32)
            f = self.sb(s2, "n_f", [128, 8], F32)
            posi = self.sb(s2, "n_posi", [128, 512], I32)
            ki = self.sb(s2, "n_ki", [128, 512], I32)
            ftok, ptok = Tok(), Tok()
            c.op("pool", lambda e: e.iota(pidx[:], [[0, 1]], base=0, channel_multiplier=1), writes=(ftok,))
            PF, GE, DD, G8, II, ACTV, SGN, INV = [f[:, i:i + 1] for i in range(8)]
            V = lambda fn: c.op("dve", fn, reads=(ftok,), writes=(ftok,))
            V(lambda e: e.tensor_copy(PF, pidx[:]))
            V(lambda e: e.tensor_single_scalar(GE, PF, 64.0, ALU.is_ge))
            V(lambda e: e.scalar_tensor_tensor(DD, GE, -64.0, PF, ALU.mult, ALU.add))
            V(lambda e: e.tensor_single_scalar(G8, DD, 8.0, ALU.is_ge))
            V(lambda e: e.scalar_tensor_tensor(II, G8, -8.0, DD, ALU.mult, ALU.add))
            V(lambda e: e.tensor_single_scalar(ACTV, DD, 16.0, ALU.is_lt))
            V(lambda e: e.tensor_scalar(SGN, G8, 2.0, -1.0, ALU.mult, ALU.add))
            V(lambda e: e.memset(INV, 0.0))
            for i in range(8):
                ci = float(np.float32(500000.0) ** np.float32(-i / 8.0))
                V(lambda e: e.tensor_scalar(GE, II, float(i), ci, ALU.is_equal, ALU.mult))
                V(lambda e: e.tensor_tensor(INV, INV, GE, ALU.add))
            V(lambda e: e.tensor_tensor(INV, INV, ACTV, ALU.mult))
            for tc in range(NTC):
                ts = slice(tc * 512, (tc + 1) * 512)
                c.dma(posi[:], self.A["positions"][0:1, ts].partition_broadcast(128), writes=(ptok,))
                ang, angt = self.nscr()
                c.op("dve", lambda e: e.tensor_copy(ang[:], posi[:]), reads=(ptok,), writes=(angt,))
                c.op("dve", lambda e: e.tensor_scalar(ang[:], ang[:], INV, None, ALU.mult), reads=(angt, ftok), writes=(angt,))
                for phase, dstT, use_sign in ((0.0, sinT, True), (PI / 2, cosT, False)):
                    u, ut = self.nscr()
                    r, rt = self.nscr()
                    c.op("dve", lambda e: e.tensor_scalar(u[:], ang[:], phase, 1.0 / (2 * PI), ALU.add, ALU.mult),
                         reads=(angt,), writes=(ut,))
                    c.op("dve", lambda e: e.tensor_copy(ki[:], u[:]), reads=(ut,), writes=(ptok,))
                    c.op("dve", lambda e: e.tensor_copy(u[:], ki[:]), reads=(ptok,), writes=(ut,))
                    c.op("dve", lambda e: e.scalar_tensor_tensor(r[:], u[:], -C1, ang[:], ALU.mult, ALU.add),
                         reads=(ut, angt), writes=(rt,))
                    c.op("dve", lambda e: e.scalar_tensor_tensor(r[:], u[:], -C2, r[:], ALU.mult, ALU.add), reads=(ut, rt),
                         writes=(rt,))
                    if phase != 0.0:
                        c.op("dve", lambda e: e.tensor_scalar(r[:], r[:], phase, None, ALU.add), reads=(rt,), writes=(rt,))
                    c.op("dve", lambda e: e.tensor_single_scalar(u[:], r[:], PI, ALU.is_gt), reads=(rt,), writes=(ut,))
                    c.op("dve", lambda e: e.scalar_tensor_tensor(r[:], u[:], -2 * PI, r[:], ALU.mult, ALU.add), reads=(ut, rt),
                         writes=(rt,))
                    c.op("dve", lambda e: e.tensor_single_scalar(u[:], r[:], -PI, ALU.is_lt), reads=(rt,), writes=(ut,))
                    c.op("dve", lambda e: e.scalar_tensor_tensor(r[:], u[:], 2 * PI, r[:], ALU.mult, ALU.add), reads=(ut, rt),
                         writes=(rt,))
                    c.op("dve", lambda e: e.tensor_scalar(r[:], r[:], PI, -PI, ALU.min, ALU.max), reads=(rt,), writes=(rt,))
                    c.op("act", lambda e: e.activation(r[:], r[:], AF.Sin), reads=(rt,), writes=(rt,))
                    if use_sign:
                        c.op("dve", lambda e: e.tensor_scalar(dstT[:, ts], r[:], SGN, None, ALU.mult), reads=(rt, ftok),
                             writes=(tbl,))
                    else:
                        c.op("dve", lambda e: e.tensor_copy(dstT[:, ts], r[:]), reads=(rt,), writes=(tbl,))
            c.barrier()

    def nsa(self, li):
        c = self.c
        ct = self.const_tok
        import os
        nstop = int(os.environ.get("NSA_STOP", "99"))
        with ExitStack() as st:
            kcT2 = self.sb(st, "n_kcT", [128, 2, 128], BF16)
            VCX = self.sb(st, "n_vcx", [128, 2, 97], BF16)
            cmptok = Tok()

            def open_tables(sx):
                cosT = self.sb(sx, "n_cos", [128, S], BF16)
                sinT = self.sb(sx, "n_sin", [128, S], BF16)
                self.Pm = self.sb(sx, "n_Pm", [128, 128], BF16)
                self.tbl_tok = Tok()
                self.rraw = [self.sb(sx, f"n_raw{i}", [128, 512], BF16) for i in range(2)]
                self.rraw_tok = [Tok(), Tok()]
                self.rr_i = 0
                self.wdup = self.sb(sx, "n_wdup", [128, KC, 128], BF16)
                self.wdup_tok = Tok()
                self.nsa_tables(cosT, sinT)
                c.op("pool", lambda e: e.memset(self.Pm[:], 0.0), writes=(self.tbl_tok,))
                for (d0, s0) in ((0, 8), (8, 0), (64, 72), (72, 64)):
                    c.op("pool", lambda e: e.tensor_copy(self.Pm[:, d0:d0 + 8], self.ident_b[:, s0:s0 + 8]),
                         reads=(ct, self.tbl_tok), writes=(self.tbl_tok,))
                return cosT, sinT
            sA = ExitStack()
            cosT, sinT = open_tables(sA)
            tbl = self.tbl_tok
            if "cosT" in self.dbg_out:
                self.dump_featmajor_bf16(cosT[:].rearrange("p (c s) -> p c s", c=1), [tbl], self.dbg_out["cosT"])
                self.dump_featmajor_bf16(sinT[:].rearrange("p (c s) -> p c s", c=1), [tbl], self.dbg_out["sinT"])
            with ExitStack() as s3:
                xcT = [self.sb(s3, "n_xk", [128, S], BF16), self.sb(s3, "n_xv", [128, S], BF16)]
                xtok = Tok()
                for kv, col0 in ((0, O_NKC), (1, O_NVC)):
                    def ev_x(mt, tc, pb, pt, kv=kv):
                        ts = slice(tc * 512, (tc + 1) * 512)
                        c.op("act", lambda e: e.copy(xcT[kv][:, ts], pb[:]), reads=(pt,), writes=(xtok,))
                    self.proj_feat(li, col0, 128, ev_x)
                W1 = self.sb(s3, "n_w1", [128, 32, 256], BF16)
                stg = self.sb(s3, "n_stg", [128, 2048], F32)
                W2f = self.sb(s3, "n_w2f", [128, 2, 64], F32)
                W2d = self.sb(s3, "n_w2d", [128, 2, 128], BF16)
                pe2 = self.sb(s3, "n_pe2", [32, 128], F32)
                peb = self.sb(s3, "n_peb", [128, 32], BF16)
                gh = self.sb(s3, "n_gh", [128, 2, 128], BF16)
                hb = self.sb(s3, "n_hb", [128, 2], F32)
                ovf = self.sb(s3, "n_ovf", [128, 3, 32], F32)
                stgt, w1t, w2t, pet, ght, hbt, ovt = [Tok() for _ in range(7)]
                c.op("pool", lambda e: e.memset(ovf[:], 0.5), writes=(ovt,))
                for k_, off in ((0, 0), (1, 16)):
                    c.op("pool", lambda e: e.affine_select(ovf[:, k_, :], ovf[:, k_, :], [[-64, 32]], ALU.is_ge, 0.0, base=off,
                                                           channel_multiplier=16), reads=(ovt,), writes=(ovt,))
                    c.op("pool", lambda e: e.affine_select(ovf[:, k_, :], ovf[:, k_, :], [[64, 32]], ALU.is_ge, 0.0,
                                                           base=63 - off, channel_multiplier=-16), reads=(ovt,), writes=(ovt,))
                c.op("pool", lambda e: e.tensor_tensor(ovf[:, 2, :], ovf[:, 0, :], ovf[:, 1, :], ALU.add), reads=(ovt,),
                     writes=(ovt,))
                for g in range(2):
                    c.op("pool", lambda e: e.tensor_copy(VCX[:, g, 65:97], ovf[:, 2, :]), reads=(ovt,), writes=(cmptok,))
                c.op("pool", lambda e: e.memset(VCX[:, :, 64:65], 1.0), writes=(cmptok,))
                for kv in range(2):
                    w1 = self.A["cmp_wk1" if kv == 0 else "cmp_wv1"][li].rearrange("(l d) n -> d l n", d=64)
                    for piece in range(4):
                        for half in range(2):
                            c.dma(stg[half * 64:(half + 1) * 64, :].rearrange("p (l n) -> p l n", l=8),
                                  w1[:, piece * 8:(piece + 1) * 8, :], writes=(stgt,))
                        c.op("pool", lambda e: e.tensor_copy(W1[:, piece * 8:(piece + 1) * 8, :],
                                                             stg[:].rearrange("p (l n) -> p l n", l=8)),
                             reads=(stgt,), writes=(w1t,))
                    w2 = self.A["cmp_wk2" if kv == 0 else "cmp_wv2"][li].rearrange("(c p) n -> p c n", p=128)
                    c.dma(W2f[:], w2, writes=(w2t,))
                    c.op("pool", lambda e: e.tensor_copy(W2d[:, :, 0:64], W2f[:]), reads=(w2t,), writes=(w2t,))
                    c.op("pool", lambda e: e.tensor_copy(W2d[:, :, 64:128], W2f[:]), reads=(w2t,), writes=(w2t,))
                    pe = self.A["cmp_pos_k" if kv == 0 else "cmp_pos_v"][li]
                    c.dma(pe2[:, 0:64], pe, writes=(pet,))
                    c.dma(pe2[:, 64:128], pe, writes=(pet,))
                    pp, ppt = self.bank()
                    c.op("pe", lambda e: e.transpose(pp[:, 0:32], pe2[:], self.ident_f[0:32, 0:32]), reads=(pet, ct),
                         writes=(ppt,))
                    c.op("dve", lambda e: e.tensor_copy(peb[:], pp[:, 0:32]), reads=(ppt,), writes=(pet,))
                    for half in range(2):
                        pk_, pkt = self.bank()
                        for l in range(32):
                            self.mm(pk_[:, 0:1], W1[0:64, l, half * 128:(half + 1) * 128], peb[0:64, l:l + 1], l == 0, l == 31,
                                    reads=(w1t, pet), writes=(pkt,))
                        c.op("dve", lambda e: e.tensor_copy(hb[:, half:half + 1], pk_[:, 0:1]), reads=(pkt,), writes=(hbt,))
                    for g in range(2):
                        gs = slice(g * 64, g * 64 + 64)
                        for half in range(2):
                            ph, pht = self.bank()
                            for l in range(32):
                                self.mm(ph[:, 0:127], W1[gs, l, half * 128:(half + 1) * 128],
                                        xcT[kv][gs, l:l + 16 * 126 + 1:16], l == 0, l == 31, reads=(w1t, xtok), writes=(pht,))
                            x, xt = self.nscr()
                            x2, x2t = self.nscr()
                            N_ = slice(0, 127)
                            c.op("dve", lambda e: e.tensor_scalar(x[:, N_], ph[:, N_], hb[:, half:half + 1], None, ALU.add),
                                 reads=(pht, hbt), writes=(xt,))
                            c.op("dve", lambda e: e.tensor_tensor(x2[:, N_], x[:, N_], x[:, N_], ALU.mult), reads=(xt,),
                                 writes=(x2t,))
                            c.op("dve", lambda e: e.tensor_scalar(x2[:, N_], x2[:, N_], 0.044715, 1.0, ALU.mult, ALU.add),
                                 reads=(x2t,), writes=(x2t,))
                            c.op("dve", lambda e: e.tensor_tensor(x2[:, N_], x2[:, N_], x[:, N_], ALU.mult), reads=(x2t, xt),
                                 writes=(x2t,))
                            c.op("act", lambda e: e.activation(x2[:, N_], x2[:, N_], AF.Tanh, scale=0.7978845608028654),
                                 reads=(x2t,), writes=(x2t,))
                            c.op("dve", lambda e: e.tensor_scalar(x[:, N_], x[:, N_], 0.5, None, ALU.mult), reads=(xt,),
                                 writes=(xt,))
                            c.op("dve", lambda e: e.scalar_tensor_tensor(gh[:, half, 0:127], x2[:, N_], 1.0, x[:, N_], ALU.add,
                                                                         ALU.mult), reads=(x2t, xt), writes=(ght,))
                        if kv == 0:
                            pk, pkt2 = self.bank()
                            for half in range(2):
                                self.mm(pk[:, 0:127], W2d[:, half, :], gh[:, half, 0:127], half == 0, half == 1,
                                        reads=(w2t, ght), writes=(pkt2,))
                            self.rope_apply(kcT2[:, g, 0:127], pk[:, 0:127], pkt2, 127, 1.0,
                                            cosT[:, 31:31 + 16 * 126 + 1:16], sinT[:, 31:31 + 16 * 126 + 1:16], cmptok)
                        else:
                            pv, pvt = self.bank()
                            for half in range(2):
                                self.mm(pv[0:127, 0:64], gh[:, half, 0:127], W2d[:, half, 0:64], half == 0, half == 1,
                                        reads=(w2t, ght), writes=(pvt,))
                            c.op("act", lambda e: e.copy(VCX[0:127, g, 0:64], pv[0:127, 0:64]), reads=(pvt,), writes=(cmptok,))
                if "kcT" in self.dbg_out:
                    self.dump2d("kcT", kcT2[:].rearrange("p g n -> p (g n)"), [cmptok])
                    self.dump2d("vcx", VCX[:].rearrange("p g n -> p (g n)"), [cmptok])
                c.barrier()
            sA.close()
            if nstop == 1:
                return
            qT = self.sb(st, "n_qT", [128, 4, S], BF16)
            ksT2 = self.sb(st, "n_ksT", [128, 2, S], BF16)
            kwT2 = self.sb(st, "n_kwT", [128, 2, S], BF16)
            vs = self.sb(st, "n_vs", [128, NT, 2, 65], BF16)
            vw = self.sb(st, "n_vw", [128, NT, 2, 65], BF16)
            sg = self.sb(st, "n_sg", [128, NT, 24], F32)
            sB = ExitStack()
            cosT, sinT = open_tables(sB)
            qtok, kstok, kwtok, vstok, vwtok, sgtok = [Tok() for _ in range(6)]

            def ev_q(mt, tc, pb, pt):
                ts = slice(tc * 512, (tc + 1) * 512)
                self.rope_apply(qT[:, mt, ts], pb[:], pt, 512, 0.125, cosT[:, ts], sinT[:, ts], qtok)
            self.proj_feat(li, O_NQ, 512, ev_q)
            for g in range(2):
                def ev_ks(mt, tc, pb, pt, g=g):
                    ts = slice(tc * 512, (tc + 1) * 512)
                    self.rope_apply(ksT2[:, g, ts], pb[:], pt, 512, 1.0, cosT[:, ts], sinT[:, ts], kstok)

                def ev_kw(mt, tc, pb, pt, g=g):
                    ts = slice(tc * 512, (tc + 1) * 512)
                    self.rope_apply(kwT2[:, g, ts], pb[:], pt, 512, 1.0, cosT[:, ts], sinT[:, ts], kwtok)
                self.proj_feat_dup(li, O_NKS + g * 64, ev_ks)
                self.proj_feat_dup(li, O_NKW + g * 64, ev_kw)
            c.op("pool", lambda e: e.memset(vs[:, :, :, 64:65], 1.0), writes=(vstok,))
            c.op("pool", lambda e: e.memset(vw[:, :, :, 64:65], 1.0), writes=(vwtok,))

            def ev_vs(t, pb, pt):
                c.op("act", lambda e: e.copy(vs[:, t, :, 0:64], pb[:, 0:128].rearrange("p (g d) -> p g d", g=2)), reads=(pt,),
                     writes=(vstok,))

            def ev_vw(t, pb, pt):
                c.op("act", lambda e: e.copy(vw[:, t, :, 0:64], pb[:, 0:128].rearrange("p (g d) -> p g d", g=2)), reads=(pt,),
                     writes=(vwtok,))

            def ev_sg(t, pb, pt):
                c.op("act", lambda e: e.activation(sg[:, t, :], pb[:, 0:24], AF.Sigmoid), reads=(pt,), writes=(sgtok,))
            self.proj_tok(li, O_NVS, 128, ev_vs)
            self.proj_tok(li, O_NVW, 128, ev_vw)
            self.proj_tok(li, O_NG, 24, ev_sg)
            if "qT" in self.dbg_out:
                self.dump_featmajor_bf16(qT, [qtok], self.dbg_out["qT"])
            c.barrier()
            sB.close()
            if nstop == 2:
                return
            am = self.sb(st, "n_am", [128, NT, 32], F32)
            Esel = self.sb(st, "n_E", [32, NT, 128], BF16)
            wneg = self.sb(st, "n_wneg", [128, 128], BF16)
            cm = self.sb(st, "n_cm", [128, 512], BF16)
            negT = self.sb(st, "n_negT", [32, 2, 512], BF16)
            acc = self.sb(st, "n_acc", [128, 4, 512], F32)
            ybf = self.sb(st, "n_ybf", [128, 512], BF16)
            pT = [self.sb(st, f"n_pT{i}", [128, 512], BF16) for i in range(3)]
            pT_tok = [Tok() for _ in range(3)]
            imp = self.sb(st, "n_imp", [128, 4, 2, 32], F32)
            sm = self.sb(st, "n_sm", [128, 16], F32)
            impm = self.sb(st, "n_impm", [128, 32], F32)
            top8 = self.sb(st, "n_top8", [128, 8], F32)
            nselb = self.sb(st, "n_nsel", [128, 32], BF16)
            mtok, cmtok, negtok, acctok, ytok, imptok, smtok, tktok = [Tok() for _ in range(8)]
            tA, tAt = self.nscr()
            tAv = tA[:].rearrange("p (t j) -> p t j", t=NT)
            c.op("pool", lambda e: e.memset(am[:], 0.0), writes=(mtok,))
            c.op("pool", lambda e: e.affine_select(am[:], am[:], [[128, NT], [-64, 32]], ALU.is_ge, -100.0, base=0,
                                                   channel_multiplier=1), reads=(mtok,), writes=(mtok,))
            c.op("pool", lambda e: e.memset(tA[:], 100.0), writes=(tAt,))
            c.op("pool", lambda e: e.affine_select(tAv, tAv, [[128, NT], [-64, 32]], ALU.is_ge, 0.0, base=0,
                                                   channel_multiplier=1), reads=(tAt,), writes=(tAt,))
            c.op("pool", lambda e: e.affine_select(tAv, tAv, [[-128, NT], [64, 32]], ALU.is_ge, 0.0, base=63,
                                                   channel_multiplier=-1), reads=(tAt,), writes=(tAt,))
            c.op("pool", lambda e: e.memset(tAv[:, :, 0:1], 100.0), reads=(tAt,), writes=(tAt,))
            c.op("pool", lambda e: e.tensor_tensor(am[:], am[:], tAv, ALU.add), reads=(tAt, mtok), writes=(mtok,))
            c.op("pool", lambda e: e.memset(Esel[:], 1.0), writes=(mtok,))
            c.op("pool", lambda e: e.affine_select(Esel[:], Esel[:], [[128, NT], [1, 128]], ALU.is_ge, 0.0, base=0,
                                                   channel_multiplier=-64), reads=(mtok,), writes=(mtok,))
            c.op("pool", lambda e: e.affine_select(Esel[:], Esel[:], [[-128, NT], [-1, 128]], ALU.is_ge, 0.0, base=63,
                                                   channel_multiplier=64), reads=(mtok,), writes=(mtok,))
            c.op("pool", lambda e: e.affine_select(wneg[:], self.zer_f[:], [[-1, 128]], ALU.is_gt, NEG, base=0,
                                                   channel_multiplier=1), reads=(ct,), writes=(mtok,))
            pi = 0
            for qc in range(NTC):
                qs = slice(qc * 512, (qc + 1) * 512)
                c.op("pool", lambda e: e.memset(cm[:], 0.0), writes=(cmtok,))
                c.op("pool", lambda e: e.affine_select(cm[:], cm[:], [[1, 512]], ALU.is_ge, NEG, base=qc * 512 - 31,
                                                       channel_multiplier=-16), reads=(cmtok,), writes=(cmtok,))
                c.op("dve", lambda e: e.memset(imp[:], 0.0), writes=(imptok,))
                for h in range(8):
                    g = h // 4
                    hp = slice((h % 2) * 64, (h % 2) * 64 + 64)
                    hc = h // 2
                    hcol = slice(h * 64, (h + 1) * 64)
                    sb_, stk = self.bank()
                    self.mm(sb_[0:127, :], kcT2[hp, g, 0:127], qT[hp, hc, qs], True, False, reads=(cmptok, qtok), writes=(stk,))
                    self.mm(sb_[0:127, :], self.ident_b[0:127, 0:127], cm[0:127, :], False, True, reads=(ct, cmtok),
                            writes=(stk,), skip_group_check=True)
                    p, ptk = pT[pi % 3], pT_tok[pi % 3]
                    pi += 1
                    c.op("act", lambda e: e.activation(p[0:127, :], sb_[0:127, :], AF.Exp), reads=(stk,), writes=(ptk,))
                    ob, ot = self.bank_acc()
                    O = ob[:, 0:388].rearrange("p (j d) -> p j d", j=4)
                    for j in range(4):
                        self.mm(O[:, j, :], p[0:127, j * 128:(j + 1) * 128], VCX[0:127, g, :], j == 0, True,
                                reads=(ptk, cmptok), writes=(ot,), skip_group_check=True)
                    for j in range(4):
                        qt = qc * 4 + j
                        rcv, wv_ = sm[:, 2 * j:2 * j + 1], sm[:, 2 * j + 1:2 * j + 2]
                        c.op("dve", lambda e: e.tensor_scalar(rcv, O[:, j, 64:65], 1e-30, None, ALU.max), reads=(ot,),
                             writes=(smtok,))
                        c.op("dve", lambda e: e.reciprocal(rcv, rcv), reads=(smtok,), writes=(smtok,))
                        c.op("dve", lambda e: e.tensor_tensor(wv_, rcv, sg[:, qt, 3 * h:3 * h + 1], ALU.mult),
                             reads=(smtok, sgtok), writes=(smtok,))
                        c.op("dve", lambda e: e.tensor_scalar(acc[:, j, hcol], O[:, j, 0:64], wv_, None, ALU.mult),
                             reads=(ot, smtok), writes=(acctok,))
                        c.op("dve", lambda e: e.scalar_tensor_tensor(imp[:, j, g, :], O[:, j, 65:97], rcv, imp[:, j, g, :],
                                                                     ALU.mult, ALU.add), reads=(ot, smtok, imptok),
                             writes=(imptok,))
                for j in range(4):
                    qt = qc * 4 + j
                    for g in range(2):
                        c.op("dve", lambda e: e.tensor_tensor(impm[:], imp[:, j, g, :], am[:, qt, :], ALU.add),
                             reads=(imptok, mtok), writes=(tktok,))
                        c.op("dve", lambda e: e.max(top8[:], impm[:]), reads=(tktok,), writes=(tktok,))
                        c.op("dve", lambda e: e.tensor_scalar(impm[:], impm[:], top8[:, 7:8], None, ALU.is_ge), reads=(tktok,),
                             writes=(tktok,))
                        c.op("dve", lambda e: e.tensor_scalar(nselb[:], impm[:], -1.0, 30000.0, ALU.add, ALU.mult),
                             reads=(tktok,), writes=(tktok,))
                        pb, pt = self.bank()
                        pbb = pb[:].bitcast(BF16)
                        c.op("pe", lambda e: e.transpose(pbb[0:32, 0:128], nselb[:], self.ident_b[:]), reads=(tktok, ct),
                             writes=(pt,))
                        c.op("act", lambda e: e.copy(negT[0:32, g, j * 128:(j + 1) * 128], pbb[0:32, 0:128]), reads=(pt,),
                             writes=(negtok,))
                if "negT" in self.dbg_out and qc == 1:
                    self.dump2d("negT", negT[:].rearrange("p g n -> p (g n)"), [negtok])
                for br in (1, 2):
                    for h in range(8):
                        g = h // 4
                        hp = slice((h % 2) * 64, (h % 2) * 64 + 64)
                        hc = h // 2
                        hcol = slice(h * 64, (h + 1) * 64)
                        ob, ot = self.bank_acc()
                        O = ob[:, 0:260].rearrange("p (j d) -> p j d", j=4)
                        first = True
                        kt0 = 0 if br == 1 else max(0, 4 * qc - 2)
                        for kt in range(kt0, 4 * qc + 4):
                            rel = kt - 4 * qc
                            jlo = max(0, rel)
                            jhi = 3 if br == 1 else min(3, rel + 2)
                            ncol = (jhi - jlo + 1) * 128
                            q0 = qc * 512 + jlo * 128
                            kl = slice(kt * 128, (kt + 1) * 128)
                            KT = ksT2 if br == 1 else kwT2
                            ktk = kstok if br == 1 else kwtok
                            extra = []
                            if br == 1:
                                extra.append((slice(0, ncol), Esel[0:32, kt, :], negT[0:32, g, jlo * 128:jlo * 128 + ncol],
                                              (mtok, negtok)))
                            if rel >= 0:
                                extra.append((slice(0, 128), self.ident_b[:], self.cneg_b[:], (ct,)))
                            if br == 2 and 0 <= rel + 2 <= 3:
                                o2 = (rel + 2 - jlo) * 128
                                extra.append((slice(o2, o2 + 128), self.ident_b[:], wneg[:], (ct, mtok)))
                            sb_, stk = self.bank()
                            self.mm(sb_[:, 0:ncol], KT[hp, g, kl], qT[hp, hc, q0:q0 + ncol], True, len(extra) == 0,
                                    reads=(ktk, qtok), writes=(stk,))
                            for ei, (csl, lh, rh, rd) in enumerate(extra):
                                self.mm(sb_[:, csl], lh, rh, False, ei == len(extra) - 1, reads=rd, writes=(stk,),
                                        skip_group_check=True)
                            p, ptk = pT[pi % 3], pT_tok[pi % 3]
                            pi += 1
                            c.op("act", lambda e: e.activation(p[:, 0:ncol], sb_[:, 0:ncol], AF.Exp), reads=(stk,), writes=(ptk,))
                            VV = vs if br == 1 else vw
                            vtk_ = vstok if br == 1 else vwtok
                            for j in range(jlo, jhi + 1):
                                qt = qc * 4 + j
                                cs = slice((j - jlo) * 128, (j - jlo + 1) * 128)
                                self.mm(O[:, j, :], p[:, cs], VV[:, kt, g, :], first, kt == qt, reads=(ptk, vtk_), writes=(ot,),
                                        skip_group_check=True)
                                first = False
                        for j in range(4):
                            qt = qc * 4 + j
                            rcv, wv_ = sm[:, 2 * j:2 * j + 1], sm[:, 2 * j + 1:2 * j + 2]
                            c.op("dve", lambda e: e.reciprocal(rcv, O[:, j, 64:65]), reads=(ot,), writes=(smtok,))
                            c.op("dve", lambda e: e.tensor_tensor(wv_, rcv, sg[:, qt, 3 * h + br:3 * h + br + 1], ALU.mult),
                                 reads=(smtok, sgtok), writes=(smtok,))
                            c.op("dve", lambda e: e.scalar_tensor_tensor(acc[:, j, hcol], O[:, j, 0:64], wv_, acc[:, j, hcol],
                                                                         ALU.mult, ALU.add), reads=(ot, smtok, acctok),
                                 writes=(acctok,))
                for j in range(4):
                    t = qc * 4 + j
                    c.op("act", lambda e: e.copy(ybf[:], acc[:, j, :]), reads=(acctok,), writes=(ytok,))
                    pb, pt = self.bank()
                    pbb = pb[:].bitcast(BF16)
                    for jj in range(4):
                        c.op("pe", lambda e: e.transpose(pbb[:, jj * 128:(jj + 1) * 128], ybf[:, jj * 128:(jj + 1) * 128],
                                                         self.ident_b[:]), reads=(ytok, ct), writes=(pt,))
                    c.op("dve", lambda e: e.tensor_copy(self.yT[:, :, t * 128:(t + 1) * 128],
                                                        pbb[:, 0:512].rearrange("p (j n) -> p j n", j=4)),
                         reads=(pt,), writes=(self.yT_tok,))
            c.barrier()

    def fox(self, li):
        c = self.c
        with ExitStack() as st:
            qT = self.sb(st, "fx_qT", [128, 4, S], BF16)
            kT = self.sb(st, "fx_kT", [128, 4, S], BF16)
            V = self.sb(st, "fx_V", [128, NT, 8, 65], BF16)
            ytk = self.sb(st, "fx_y", [128, 4, 512], BF16)
            fl = self.sb(st, "fx_f", [128, NT, 8], F32)
            ncum = self.sb(st, "fx_ncum", [128, NT, 8], F32)
            nref = self.sb(st, "fx_nref", [128, NT, 8], F32)
            btab = self.sb(st, "fx_btab", [128, NT, NT, 8], F32)
            bfb = self.sb(st, "fx_bf", [128, 8], F32)
            qtok, ktok, vtok, ytok, ftok = Tok(), Tok(), Tok(), Tok(), Tok()
            self.aux_tok = Tok()
            self.proj_feat(li, O_FQ, 512, self.evac_featT(qT, qtok, 0.125))
            self.proj_feat(li, O_FK, 512, self.evac_featT(kT, ktok, 1.0))
            c.op("pool", lambda e: e.memset(V[:, :, :, 64:65], 1.0), writes=(vtok,))

            def evac_v(t, pb, pt):
                c.op("act", lambda e: e.copy(V[:, t, :, 0:64], pb[:, 0:512].rearrange("p (h d) -> p h d", h=8)),
                     reads=(pt,), writes=(vtok,))
            for half in range(4):
                def evac_vh(t, pb, pt, half=half):
                    c.op("act", lambda e: e.copy(V[:, t, half * 2:half * 2 + 2, 0:64],
                                                 pb[:, 0:128].rearrange("p (h d) -> p h d", h=2)),
                         reads=(pt,), writes=(vtok,))
                self.proj_tok(li, O_FV + half * 128, 128, evac_vh)
            c.dma(bfb[:], self.A["fox_b_f"][li:li + 1, :].partition_broadcast(128), writes=(ftok,))

            def evac_f(t, pb, pt):
                c.op("dve", lambda e: e.tensor_tensor(fl[:, t, :], pb[:, 0:8], bfb[:], ALU.add), reads=(pt, ftok),
                     writes=(ftok,))
            self.proj_tok(li, O_FF, 8, evac_f)
            flat = fl[:].rearrange("p t h -> p (t h)")
            c.op("act", lambda e: e.activation(flat, flat, AF.Exp, scale=-1.0), reads=(ftok,), writes=(ftok,))
            c.op("act", lambda e: e.activation(flat, flat, AF.Ln, bias=1.0), reads=(ftok,), writes=(ftok,))
            for t in range(NT):
                pb, pt = self.bank()
                for j in range(t):
                    self.mm(pb[:, 0:8], self.ones_f[:], fl[:, j, :], j == 0, False, reads=(ftok, self.const_tok),
                            writes=(pt,))
                self.mm(pb[:, 0:8], self.tri_f[:], fl[:, t, :], t == 0, True, reads=(ftok, self.const_tok), writes=(pt,))
                c.op("dve", lambda e: e.tensor_copy(ncum[:, t, :], pb[:, 0:8]), reads=(pt,), writes=(self.aux_tok,))
                if t > 0:
                    pb2, pt2 = self.bank()
                    for j in range(t):
                        self.mm(pb2[:, 0:8], self.ones_f[:], fl[:, j, :], j == 0, j == t - 1,
                                reads=(ftok, self.const_tok), writes=(pt2,))
                    c.op("dve", lambda e: e.tensor_copy(nref[:, t, :], pb2[:, 0:8]), reads=(pt2,), writes=(self.aux_tok,))
                else:
                    c.op("dve", lambda e: e.memset(nref[:, 0, :], 0.0), writes=(self.aux_tok,))
            for kt in range(NT):
                for qt in range(kt, NT):
                    c.op("pool", lambda e: e.tensor_tensor(btab[:, kt, qt, :], ncum[:, kt, :], nref[:, qt, :], ALU.subtract),
                         reads=(self.aux_tok,), writes=(self.aux_tok,))
            self.dump2d("ncum", ncum[:].rearrange("p t h -> p (t h)"), [self.aux_tok])
            self.dump2d("nref", nref[:].rearrange("p t h -> p (t h)"), [self.aux_tok])
            self.dump2d("fl", fl[:].rearrange("p t h -> p (t h)"), [ftok])
            self.attention(st, "fx", 8, qT, qtok, kT, ktok, V, vtok, ytk, ytok,
                           bias_fn=lambda h, kt, qt: btab[:, kt, qt, h:h + 1],
                           post_qc=lambda qc: self.ytok_to_yT(ytk, ytok, qc))
            c.barrier()

    def final_norm_store(self, out):
        self.store_tok_major(out, normed=True)

    def store_tok_major(self, out, normed):
        c = self.c
        L = len(self.layers)
        with ExitStack() as st:
            if normed:
                for tc in range(NTC):
                    ts = slice(tc * 512, (tc + 1) * 512)
                    pb, pt = self.bank()
                    for cc in range(KC):
                        sq, sqt = self.nscr()
                        c.op("act", lambda e: e.activation(sq[:], self.xT[:, cc, ts], AF.Square),
                             reads=(self.xT_tok[tc],), writes=(sqt,))
                        self.mm(pb[:], self.ones_f[:], sq[:], cc == 0, cc == KC - 1, reads=(sqt, self.const_tok),
                                writes=(pt,))
                    rs, rst = self.nscr()
                    c.op("dve", lambda e: e.tensor_scalar(rs[:], pb[:], 1.0 / D, EPS, ALU.mult, ALU.add), reads=(pt,),
                         writes=(rst,))
                    c.op("act", lambda e: e.activation(rs[:], rs[:], AF.Sqrt), reads=(rst,), writes=(rst,))
                    c.op("dve", lambda e: e.reciprocal(rs[:], rs[:]), reads=(rst,), writes=(rst,))
                    for cc in range(KC):
                        g = self.vecT[:, L * 72 + cc:L * 72 + cc + 1]
                        c.op("dve", lambda e: e.scalar_tensor_tensor(self.xT[:, cc, ts], self.xT[:, cc, ts], g, rs[:],
                                                                     ALU.mult, ALU.mult),
                             reads=(rst, self.vec_tok), writes=(self.xT_tok[tc],))
            os_ = [self.sb(st, f"os{i}", [128, D], F32) for i in range(2)]
            os_tok = [Tok(), Tok()]
            for t in range(NT):
                b = t % 2
                for half in range(2):
                    pb, pt = self.bank()
                    for j in range(4):
                        cc = half * 4 + j
                        c.op("pe", lambda e: e.transpose(pb[:, j * 128:(j + 1) * 128],
                                                         self.xT[:, cc, t * 128:(t + 1) * 128], self.ident_f[:]),
                             reads=(self.xT_tok[t // 4], self.const_tok), writes=(pt,), inc=(j == 3))
                    dst = os_[b][:, half * 512:(half + 1) * 512]
                    if half == 0:
                        c.op("dve", lambda e: e.tensor_copy(dst, pb[:]), reads=(pt,), writes=(os_tok[b],))
                    else:
                        c.op("act", lambda e: e.copy(dst, pb[:]), reads=(pt,), writes=(os_tok[b],))
                c.dma(out[t * 128:(t + 1) * 128, :], os_[b][:], reads=(os_tok[b],))
            c.barrier()

    def dump2d(self, name, ap, toks):
        if name in self.dbg_out:
            self.c.barrier()
            self.c.dma(self.dbg_out[name], ap, reads=tuple(toks), q="pool")
            self.c.barrier()

    def dump_featmajor_bf16(self, tT, toks, dst):
        c = self.c
        with ExitStack() as st:
            tmp = self.sb(st, "dmp", [128, S], F32)
            tt = Tok()
            for cc in range(tT.shape[1]):
                c.op("dve", lambda e: e.tensor_copy(tmp[:], tT[:, cc, :]), reads=tuple(toks), writes=(tt,))
                c.dma(dst[cc * 128:(cc + 1) * 128, :], tmp[:], reads=(tt,))
            c.barrier()


def _prep_inputs(inputs, layers, b):
    L = len(layers)
    vecs = np.zeros((L, 72, 128), np.float32)
    for i, l in enumerate(layers):
        vecs[i, 0:8] = inputs["norm_mix"][l].reshape(8, 128)
        vecs[i, 8:16] = inputs["norm_ff"][l].reshape(8, 128)
        vecs[i, 16:48] = inputs["b_gate"][l].reshape(32, 128)
    m = {
        "x": np.ascontiguousarray(inputs["x"][b]),
        "w_in": np.ascontiguousarray(inputs["w_in"][layers]),
        "vecs": vecs,
        "norm_final": np.ascontiguousarray(inputs["norm_final"].reshape(8, 128)),
        "positions": np.ascontiguousarray(inputs["positions"][b:b + 1]).astype(np.int32),
        "cmp_pos_k": np.ascontiguousarray(inputs["nsa_cmp_pos_k"][layers]),
        "cmp_pos_v": np.ascontiguousarray(inputs["nsa_cmp_pos_v"][layers]),
        "cmp_wk1": np.ascontiguousarray(inputs["nsa_cmp_wk1"][layers]),
        "cmp_wk2": np.ascontiguousarray(inputs["nsa_cmp_wk2"][layers]),
        "cmp_wv1": np.ascontiguousarray(inputs["nsa_cmp_wv1"][layers]),
        "cmp_wv2": np.ascontiguousarray(inputs["nsa_cmp_wv2"][layers]),
        "fox_b_f": np.ascontiguousarray(inputs["fox_b_f"][layers]),
        "gla_w_alpha": np.ascontiguousarray(inputs["gla_w_alpha"][layers]),
        "gla_b_alpha": np.ascontiguousarray(inputs["gla_b_alpha"][layers]),
        "gla_norm": np.ascontiguousarray(inputs["gla_norm"][layers]),
        "w_branch": np.ascontiguousarray(inputs["w_branch"][layers]),
        "w_out": np.ascontiguousarray(inputs["w_out"][layers]),
        "w_ff1": np.ascontiguousarray(inputs["w_ff1"][layers]),
        "w_ff2": np.ascontiguousarray(inputs["w_ff2"][layers]),
    }
    return m


def run(inputs, layers=(0, 1, 2, 3), debug=(), ncores=8, trace=False, stage=99):
    layers = list(layers)
    bld = Builder(layers, first=True, last=True, debug=debug, stage=stage)
    nc = bld.build()
    in_maps = [_prep_inputs(inputs, layers, b) for b in range(ncores)]
    res = run_bass_kernel_spmd(nc, in_maps, core_ids=list(range(ncores)), trace=trace)
    return res


def kernel(**inputs):
    inputs = {k: np.asarray(v) for k, v in inputs.items()}
    res = run(inputs)
    out = np.stack([np.asarray(r["out"]) for r in res.results], axis=0)
    return out.astype(np.float32)
```

```python
import numpy as np
from contextlib import ExitStack
import concourse.bass as bass
import concourse.mybir as mybir
from concourse.bass_utils import run_bass_kernel_spmd

F32 = mybir.dt.float32
BF16 = mybir.dt.bfloat16
I32 = mybir.dt.int32
ALU = mybir.AluOpType
AF = mybir.ActivationFunctionType
AX = mybir.AxisListType

S = 2048
D = 1024
NT = S // 128
NTC = S // 512
KC = D // 128
DEPTH = 4
DFF = 4096
D_IN = 10032
EPS = 1e-6
NEG = -30000.0

SPLITS = (512, 128, 128, 128, 128, 128, 128, 24, 512, 512, 512, 256, 256, 512, 16, 512, 512, 512, 512, 8, 4096)
OFFS = np.concatenate([[0], np.cumsum(SPLITS)]).tolist()
(O_NQ, O_NKC, O_NVC, O_NKS, O_NVS, O_NKW, O_NVW, O_NG, O_SQ, O_SK, O_SV, O_GQ, O_GK, O_GV, O_GA, O_GG,
 O_FQ, O_FK, O_FV, O_FF, O_GATE) = OFFS[:21]

EPOCH = 4000


class Tok:
    __slots__ = ("w", "r", "name")

    def __init__(self, name=""):
        self.w = None
        self.r = {}
        self.name = name


class Ctx:
    def __init__(self, nc, es):
        self.nc = nc
        self.es = es
        self.eng = dict(pe=nc.tensor, act=nc.scalar, dve=nc.vector, pool=nc.gpsimd, sp=nc.sync)
        self.cur = {}
        self.nsem = 0
        self.waited = {e: {} for e in self.eng}
        for e in ("pe", "act", "dve", "pool"):
            self.cur[e] = [self._newsem(e), 0]
        self.own = {e: set() for e in self.eng}
        for e in ("pe", "act", "dve", "pool"):
            self.own[e].add(id(self.cur[e][0]))
        self.dq = {}
        for q in ("sp", "act", "pool"):
            sems = [self._newsem("d" + q) for _ in range(8 if q == "sp" else 4)]
            self.dq[q] = dict(sems=sems, tgt=[0] * len(sems), i=0)
        self.all_dma = []

    def _newsem(self, name):
        self.nsem += 1
        return self.es.enter_context(self.nc.semaphore(f"s_{name}_{self.nsem}"))

    def _wait(self, e, deps):
        w = self.waited[e]
        for (sem, val) in deps:
            if val <= 0:
                continue
            k = id(sem)
            if e == "pe" and k in self.own["pe"]:
                continue
            if w.get(k, 0) >= val:
                continue
            self.eng[e].wait_ge(sem, val)
            w[k] = val

    @staticmethod
    def _deps(reads, writes):
        deps = []
        for t in reads:
            if t.w is not None:
                deps.append(t.w)
        for t in writes:
            if t.w is not None:
                deps.append(t.w)
            deps.extend(t.r.values())
        return deps

    @staticmethod
    def _record(stamp, reads, writes):
        sem, val = stamp
        for t in reads:
            t.r[id(sem)] = stamp
        for t in writes:
            t.w = stamp
            t.r = {}

    def op(self, e, fn, reads=(), writes=(), inc=True):
        self._wait(e, self._deps(reads, writes))
        ins = fn(self.eng[e])
        sem, cnt = self.cur[e]
        stamp = (sem, cnt + 1)
        if inc:
            ins.then_inc(sem, 1)
            self.cur[e][1] = cnt + 1
        self._record(stamp, reads, writes)
        if inc and cnt + 1 >= EPOCH:
            ns = self._newsem(e)
            self.own[e].add(id(ns))
            self.cur[e] = [ns, 0]
        return ins

    def dma(self, out, in_, reads=(), writes=(), q="sp", **kw):
        dq = self.dq[q]
        i = dq["i"] % len(dq["sems"])
        dq["i"] += 1
        sem = dq["sems"][i]
        deps = self._deps(reads, writes)
        deps.append((sem, dq["tgt"][i]))
        self._wait(q, deps)
        self.eng[q].dma_start(out=out, in_=in_, **kw).then_inc(sem, 16)
        dq["tgt"][i] += 16
        stamp = (sem, dq["tgt"][i])
        self._record(stamp, reads, writes)
        return stamp

    def barrier(self):
        stamps = []
        for e in ("pe", "act", "dve", "pool"):
            sem, cnt = self.cur[e]
            stamps.append((sem, cnt))
        for q, dq in self.dq.items():
            for s, t in zip(dq["sems"], dq["tgt"]):
                stamps.append((s, t))
        for e in ("pe", "act", "dve", "pool", "sp"):
            self._wait_all(e, stamps)

    def _wait_all(self, e, stamps):
        w = self.waited[e]
        for (sem, val) in stamps:
            if val <= 0:
                continue
            k = id(sem)
            if k in self.own.get(e, ()) and (e == "pe"):
                continue
            if w.get(k, 0) >= val:
                continue
            self.eng[e].wait_ge(sem, val)
            w[k] = val


class Builder:
    def __init__(self, layers, first, last, debug=(), stage=99):
        self.stage = stage
        self.layers = layers
        self.first = first
        self.last = last
        self.debug = debug
        self.nc = bass.Bass("TRN2", target_bir_lowering=False)
        self.dbg_out = {}

    def sb(self, st, name, shape, dt):
        self._uid = getattr(self, "_uid", 0) + 1
        return st.enter_context(self.nc.sbuf_tensor(f"{name}_{self._uid}", shape, dt))

    def dram_in(self, name, shape, dt=F32):
        return self.nc.dram_tensor(name, list(shape), dt, kind="ExternalInput").ap()

    def mm(self, out, lhsT, rhs, start, stop, reads, writes, inc=None, **kw):
        if inc is None:
            inc = True
        return self.c.op("pe", lambda e: e.matmul(out, lhsT, rhs, start=start, stop=stop, **kw),
                         reads=reads, writes=writes, inc=inc)

    def bank(self):
        i = self.bank_i % 6
        self.bank_i += 1
        return self.ps[i], self.ps_tok[i]

    def bank_acc(self):
        i = 6 + self.bank_j % 2
        self.bank_j += 1
        return self.ps[i], self.ps_tok[i]

    def wload(self, src3, kc, ncols, eng="pool"):
        i = self.w_i % 2
        self.w_i += 1
        stg, stok = self.wstg[i], self.wstg_tok[i]
        wb, wtok = self.wbf[i], self.wbf_tok[i]
        n = kc * ncols
        assert n <= self.WMAX
        sv = stg[:, 0:n].rearrange("p (c n) -> p c n", c=kc)
        wv = wb[:, 0:n].rearrange("p (c n) -> p c n", c=kc)
        self.c.dma(sv, src3, reads=(), writes=(stok,))
        self.c.op(eng, lambda e: e.tensor_copy(wb[:, 0:n], stg[:, 0:n]), reads=(stok,), writes=(wtok,))
        return wv, wtok

    def build(self):
        nc = self.nc
        L = len(self.layers)
        A = {}
        A["x"] = self.dram_in("x", [S, D])
        A["w_in"] = self.dram_in("w_in", [L, D, D_IN])
        A["vecs"] = self.dram_in("vecs", [L, 72, 128])
        A["norm_final"] = self.dram_in("norm_final", [8, 128])
        A["positions"] = self.dram_in("positions", [1, S], I32)
        A["cmp_pos_k"] = self.dram_in("cmp_pos_k", [L, 32, 64])
        A["cmp_pos_v"] = self.dram_in("cmp_pos_v", [L, 32, 64])
        A["cmp_wk1"] = self.dram_in("cmp_wk1", [L, 2048, 256])
        A["cmp_wk2"] = self.dram_in("cmp_wk2", [L, 256, 64])
        A["cmp_wv1"] = self.dram_in("cmp_wv1", [L, 2048, 256])
        A["cmp_wv2"] = self.dram_in("cmp_wv2", [L, 256, 64])
        A["fox_b_f"] = self.dram_in("fox_b_f", [L, 8])
        A["gla_w_alpha"] = self.dram_in("gla_w_alpha", [L, 16, 256])
        A["gla_b_alpha"] = self.dram_in("gla_b_alpha", [L, 256])
        A["gla_norm"] = self.dram_in("gla_norm", [L, 128])
        A["w_branch"] = self.dram_in("w_branch", [L, 4, 512, D])
        A["w_out"] = self.dram_in("w_out", [L, D, D])
        A["w_ff1"] = self.dram_in("w_ff1", [L, D, DFF])
        A["w_ff2"] = self.dram_in("w_ff2", [L, DFF, D])
        self.A = A
        out = nc.dram_tensor("out", [S, D], F32, kind="ExternalOutput").ap()
        for name, shape in self.debug:
            self.dbg_out[name] = nc.dram_tensor("dbg_" + name, list(shape), F32, kind="ExternalOutput").ap()

        with ExitStack() as es:
            self.es = es
            c = self.c = Ctx(nc, es)
            self.xT = self.sb(es, "xT", [128, KC, S], F32)
            self.hT = self.sb(es, "hT", [128, KC, S], BF16)
            self.xT_tok = [Tok(f"xT{i}") for i in range(NTC)]
            self.hT_tok = [Tok(f"hT{i}") for i in range(NTC)]
            self.ident_f = self.sb(es, "ident_f", [128, 128], F32)
            self.ident_b = self.sb(es, "ident_b", [128, 128], BF16)
            self.ones_f = self.sb(es, "ones_f", [128, 128], F32)
            self.const_tok = Tok("const")
            self.vecT = self.sb(es, "vecT", [128, L * 72 + 8], F32)
            self.vec_tok = Tok("vec")
            self.WMAX = 1024
            self.wstg = [self.sb(es, f"wstg{i}", [128, self.WMAX], F32) for i in range(2)]
            self.wbf = [self.sb(es, f"wbf{i}", [128, self.WMAX], BF16) for i in range(2)]
            self.wstg_tok = [Tok() for _ in range(2)]
            self.wbf_tok = [Tok() for _ in range(2)]
            self.w_i = 0
            self.scr = [self.sb(es, f"scr{i}", [128, 512], F32) for i in range(4)]
            self.scr_tok = [Tok() for _ in range(4)]
            self.scr_i = 0
            self.ps = [es.enter_context(nc.psum_tensor(f"ps{i}", [128, 512], F32)) for i in range(8)]
            self.ps_tok = [Tok(f"ps{i}") for i in range(8)]
            self.bank_i = 0
            self.bank_j = 0

            self.make_consts()
            if self.first:
                self.load_x()
            else:
                self.load_xT()
            for li in range(L):
                if self.stage >= 1:
                    self.layer(li)
            if self.last and self.stage >= 3:
                self.final_norm_store(out)
            else:
                self.store_xT(out)
            c.barrier()
        return nc

    def run_streams(self, gens, k=2):
        gens = iter(gens)
        active = []
        for g in gens:
            active.append(g)
            if len(active) == k:
                break
        while active:
            for g in list(active):
                try:
                    next(g)
                except StopIteration:
                    active.remove(g)
                    nxt = next(gens, None)
                    if nxt is not None:
                        active.append(nxt)

    def nscr(self):
        i = self.scr_i % len(self.scr)
        self.scr_i += 1
        return self.scr[i], self.scr_tok[i]

    def make_consts(self):
        c = self.c
        nc = self.nc
        ct = self.const_tok
        c.op("pool", lambda e: e.memset(self.ones_f[:], 1.0), writes=(ct,))
        c.op("pool", lambda e: e.affine_select(self.ident_f[:], self.ones_f[:], [[-1, 128]], ALU.is_equal, 0.0,
                                               base=0, channel_multiplier=1), reads=(ct,), writes=(ct,))
        c.op("pool", lambda e: e.tensor_copy(self.ident_b[:], self.ident_f[:]), reads=(ct,), writes=(ct,))
        self.tri_f = self.sb(self.es, "tri_f", [128, 128], F32)
        c.op("pool", lambda e: e.affine_select(self.tri_f[:], self.ones_f[:], [[1, 128]], ALU.is_ge, 0.0,
                                               base=0, channel_multiplier=-1), reads=(ct,), writes=(ct,))
        self.zer_f = self.sb(self.es, "zer_f", [128, 128], F32)
        self.cneg_b = self.sb(self.es, "cneg_b", [128, 128], BF16)
        c.op("pool", lambda e: e.memset(self.zer_f[:], 0.0), writes=(ct,))
        self.nones_f = self.sb(self.es, "nones_f", [128, 128], F32)
        self.ones_b = self.sb(self.es, "ones_b", [128, 128], BF16)
        self.nones_b = self.sb(self.es, "nones_b", [128, 128], BF16)
        self.cnegs_b = self.sb(self.es, "cnegs_b", [128, 128], BF16)
        self.ntri_b = self.sb(self.es, "ntri_b", [128, 128], BF16)
        c.op("pool", lambda e: e.memset(self.nones_f[:], -1.0), writes=(ct,))
        c.op("pool", lambda e: e.memset(self.ones_b[:], 1.0), writes=(ct,))
        c.op("pool", lambda e: e.memset(self.nones_b[:], -1.0), writes=(ct,))
        c.op("pool", lambda e: e.affine_select(self.cnegs_b[:], self.zer_f[:], [[1, 128]], ALU.is_gt, NEG,
                                               base=0, channel_multiplier=-1), reads=(ct,), writes=(ct,))
        c.op("pool", lambda e: e.affine_select(self.ntri_b[:], self.nones_f[:], [[-1, 128]], ALU.is_ge, 0.0,
                                               base=0, channel_multiplier=1), reads=(ct,), writes=(ct,))
        c.op("pool", lambda e: e.affine_select(self.cneg_b[:], self.zer_f[:], [[1, 128]], ALU.is_ge, NEG,
                                               base=0, channel_multiplier=-1), reads=(ct,), writes=(ct,))
        L = len(self.layers)
        nrow = L * 72 + 8
        with ExitStack() as st:
            tmp = self.sb(st, "vtmp", [128, 4, 128], F32)
            tt = Tok()
            r0 = 0
            chunks = []
            while r0 < nrow:
                n = min(128, nrow - r0)
                chunks.append((r0, n))
                r0 += n
            for ci, (r0, n) in enumerate(chunks):
                a = r0
                while a < r0 + n:
                    if a < L * 72:
                        b = min(r0 + n, L * 72)
                        src = self.A["vecs"].rearrange("l r p -> (l r) p")[a:b, :]
                    else:
                        b = r0 + n
                        src = self.A["norm_final"][a - L * 72:b - L * 72, :]
                    c.dma(tmp[a - r0:b - r0, ci, :], src, writes=(tt,))
                    a = b
                pb, pt = self.bank()
                c.op("pe", lambda e: e.transpose(pb[:, 0:n], tmp[0:n, ci, :], self.ident_f[0:n, 0:n]),
                     reads=(tt, ct), writes=(pt,))
                c.op("dve", lambda e: e.tensor_copy(self.vecT[:, r0:r0 + n], pb[:, 0:n]), reads=(pt,),
                     writes=(self.vec_tok,))
            c.barrier()

    def vcol(self, li, kind, j):
        base = li * 72 + {"norm_mix": 0, "norm_ff": 8, "b_gate": 16}[kind]
        return self.vecT[:, base + j:base + j + 1]

    def load_x(self):
        c = self.c
        x = self.A["x"]
        with ExitStack() as st:
            xs = [self.sb(st, f"xs{i}", [128, D], F32) for i in range(2)]
            xs_tok = [Tok(), Tok()]
            for t in range(NT):
                b = t % 2
                c.dma(xs[b][:], x[t * 128:(t + 1) * 128, :], writes=(xs_tok[b],))
                for half in range(2):
                    pb, pt = self.bank()
                    for j in range(4):
                        cc = half * 4 + j
                        c.op("pe", lambda e: e.transpose(pb[:, j * 128:(j + 1) * 128], xs[b][:, cc * 128:(cc + 1) * 128],
                                                         self.ident_f[:]),
                             reads=(xs_tok[b], self.const_tok), writes=(pt,), inc=(j == 3))
                    dst = self.xT[:, half * 4:half * 4 + 4, t * 128:(t + 1) * 128]
                    src = pb[:].rearrange("p (j n) -> p j n", j=4)
                    eng = "dve" if half == 0 else "act"
                    if eng == "dve":
                        c.op("dve", lambda e: e.tensor_copy(dst, src), reads=(pt,), writes=(self.xT_tok[t // 4],))
                    else:
                        c.op("act", lambda e: e.copy(dst, src), reads=(pt,), writes=(self.xT_tok[t // 4],))
            c.barrier()

    def load_xT(self):
        raise NotImplementedError

    def store_xT(self, out):
        self.store_tok_major(out, normed=False)

    def rmsnorm_to_hT(self, gcol):
        c = self.c
        for tc in range(NTC):
            ts = slice(tc * 512, (tc + 1) * 512)
            pb, pt = self.bank()
            for cc in range(KC):
                sq, sqt = self.nscr()
                c.op("act", lambda e: e.activation(sq[:], self.xT[:, cc, ts], AF.Square),
                     reads=(self.xT_tok[tc],), writes=(sqt,))
                self.mm(pb[:], self.ones_f[:], sq[:], cc == 0, cc == KC - 1, reads=(sqt, self.const_tok), writes=(pt,))
            rs, rst = self.nscr()
            c.op("dve", lambda e: e.tensor_scalar(rs[:], pb[:], 1.0 / D, EPS, ALU.mult, ALU.add), reads=(pt,),
                 writes=(rst,))
            c.op("act", lambda e: e.activation(rs[:], rs[:], AF.Sqrt), reads=(rst,), writes=(rst,))
            c.op("dve", lambda e: e.reciprocal(rs[:], rs[:]), reads=(rst,), writes=(rst,))
            for cc in range(KC):
                c.op("dve", lambda e: e.scalar_tensor_tensor(self.hT[:, cc, ts], self.xT[:, cc, ts], gcol(cc), rs[:],
                                                             ALU.mult, ALU.mult),
                     reads=(self.xT_tok[tc], rst, self.vec_tok), writes=(self.hT_tok[tc],))

    def layer(self, li):
        self.rmsnorm_to_hT(lambda cc: self.vcol(li, "norm_mix", cc))
        if "hT" in self.dbg_out and li == 0:
            self.dump_featmajor_bf16(self.hT, self.hT_tok, self.dbg_out["hT"])
        if self.stage >= 4:
            self.yT = self.sb(self.es, f"yT{li}", [128, 4, S], BF16) if not hasattr(self, "yT") else self.yT
            self.yT_tok = Tok("yT")
            if self.stage >= 8:
                self.nsa(li)
                if "ynsa" in self.dbg_out and li == 0:
                    self.dump_featmajor_bf16(self.yT, [self.yT_tok], self.dbg_out["ynsa"])
                self.combine(li, 0)
            if self.stage == 8:
                return
            if self.stage >= 7:
                self.gla(li)
                if "ygla" in self.dbg_out and li == 0:
                    self.dump_featmajor_bf16(self.yT, [self.yT_tok], self.dbg_out["ygla"])
                self.combine(li, 2)
            if self.stage >= 6 and self.stage != 7:
                self.sbmix(li)
                if "ysb" in self.dbg_out and li == 0:
                    self.dump_featmajor_bf16(self.yT, [self.yT_tok], self.dbg_out["ysb"])
                self.combine(li, 1)
            if self.stage == 7:
                return
            self.fox(li)
            if "yT" in self.dbg_out and li == 0:
                self.dump_featmajor_bf16(self.yT, [self.yT_tok], self.dbg_out["yT"])
            if self.stage >= 5:
                self.combine(li, 3)
        if self.stage >= 2:
            self.rmsnorm_to_hT(lambda cc: self.vcol(li, "norm_ff", cc))
            self.ffn(li)

    def ffn(self, li):
        c = self.c
        w1 = self.A["w_ff1"][li].rearrange("(c p) n -> p c n", p=128)
        w2 = self.A["w_ff2"][li].rearrange("(f p) n -> p f n", p=128)
        G = 4
        with ExitStack() as st:
            aT = [self.sb(st, f"aT{i}", [128, G, S], BF16) for i in range(2)]
            aT_tok = [Tok(), Tok()]
            import os
            for g in range(int(os.environ.get('FFN_G', DFF // (128 * G)))):
                ab, abt = aT[g % 2], aT_tok[g % 2]
                for half in range(G):
                    f0 = g * G + half
                    wv, wt = self.wload(w1[:, :, f0 * 128:(f0 + 1) * 128], KC, 128)
                    for j in range(1):
                        for tc in range(NTC):
                            ts = slice(tc * 512, (tc + 1) * 512)
                            pb, pt = self.bank()
                            for cc in range(KC):
                                self.mm(pb[:], wv[:, cc, j * 128:(j + 1) * 128], self.hT[:, cc, ts], cc == 0, cc == KC - 1,
                                        reads=(wt, self.hT_tok[tc]), writes=(pt,))
                            r, rt = self.nscr()
                            c.op("act", lambda e: e.activation(r[:], pb[:], AF.Relu), reads=(pt,), writes=(rt,))
                            c.op("dve", lambda e: e.tensor_tensor(ab[:, half, ts], r[:], r[:], ALU.mult),
                                 reads=(rt,), writes=(abt,))
                for dh in range(4):
                    wv, wt = self.wload(w2[:, g * G:(g + 1) * G, dh * 256:(dh + 1) * 256], G, 256)
                    for j in range(2):
                        dt_ = dh * 2 + j
                        for tc in range(NTC):
                            ts = slice(tc * 512, (tc + 1) * 512)
                            pb, pt = self.bank()
                            for f in range(G):
                                self.mm(pb[:], wv[:, f, j * 128:(j + 1) * 128], ab[:, f, ts], f == 0, f == G - 1,
                                        reads=(wt, abt), writes=(pt,))
                            c.op("dve", lambda e: e.tensor_tensor(self.xT[:, dt_, ts], self.xT[:, dt_, ts], pb[:], ALU.add),
                                 reads=(pt,), writes=(self.xT_tok[tc],))
            c.barrier()


    def proj_feat(self, li, col0, ncols, evac):
        w = self.A["w_in"][li].rearrange("(c p) n -> p c n", p=128)
        n0 = 0
        while n0 < ncols:
            nn = min(128, ncols - n0)
            wv, wt = self.wload(w[:, :, col0 + n0:col0 + n0 + nn], KC, nn)
            for j in range((nn + 127) // 128):
                m = min(128, nn - j * 128)
                for tc in range(NTC):
                    ts = slice(tc * 512, (tc + 1) * 512)
                    pb, pt = self.bank()
                    for cc in range(KC):
                        self.mm(pb[0:m, :], wv[:, cc, j * 128:j * 128 + m], self.hT[:, cc, ts], cc == 0, cc == KC - 1,
                                reads=(wt, self.hT_tok[tc]), writes=(pt,))
                    evac((n0 + j * 128) // 128, tc, pb, pt)
            n0 += nn

    def proj_tok(self, li, col0, ncols, evac):
        w = self.A["w_in"][li].rearrange("(c p) n -> p c n", p=128)
        wv, wt = self.wload(w[:, :, col0:col0 + ncols], KC, ncols)
        for t in range(NT):
            pb, pt = self.bank()
            for cc in range(KC):
                self.mm(pb[:, 0:ncols], self.hT[:, cc, t * 128:(t + 1) * 128], wv[:, cc, :], cc == 0, cc == KC - 1,
                        reads=(wt, self.hT_tok[t // 4]), writes=(pt,))
            evac(t, pb, pt)

    def evac_featT(self, dst, dtok, scale=1.0):
        c = self.c
        cnt = [0]

        def f(mt, tc, pb, pt):
            ts = slice(tc * 512, (tc + 1) * 512)
            cnt[0] += 1
            if cnt[0] % 2 == 0:
                c.op("dve", lambda e: e.tensor_scalar(dst[:, mt, ts], pb[:], scale, None, ALU.mult), reads=(pt,),
                     writes=(dtok,))
            else:
                c.op("act", lambda e: e.activation(dst[:, mt, ts], pb[:], AF.Copy, scale=scale), reads=(pt,),
                     writes=(dtok,))
        return f

    def attention(self, st, name, nheads, qT, qtok, kT, ktok, V, vtok, ytok_t, ytok_tok, bias_fn=None, ycol0=0, post_qc=None):
        c = self.c
        pT = [self.sb(st, f"{name}_pT{i}", [128, 512], BF16) for i in range(4)]
        pT_tok = [Tok() for _ in range(4)]
        rc = self.sb(st, f"{name}_rc", [128, 8], F32)
        rc_tok = [Tok(), Tok()]
        pi = [0]

        def head_stream(qc, h):
            hp = slice((h % 2) * 64, (h % 2) * 64 + 64)
            hc = h // 2
            ob, ot = self.bank_acc()
            O = ob[:, 0:260].rearrange("p (j d) -> p j d", j=4)
            nkt = 4 * qc + 4
            for kt in range(nkt):
                j0 = max(0, kt - 4 * qc)
                q0 = qc * 512 + j0 * 128
                ncol = 512 - j0 * 128
                sb_, stk = self.bank()
                diag = kt >= 4 * qc
                self.mm(sb_[:, 0:ncol], kT[hp, hc, kt * 128:(kt + 1) * 128], qT[hp, hc, q0:q0 + ncol], True, not diag,
                        reads=(ktok, qtok), writes=(stk,))
                if diag:
                    self.mm(sb_[:, 0:128], self.ident_b[:], self.cneg_b[:], False, True,
                            reads=(self.const_tok,), writes=(stk,), skip_group_check=True)
                p, ptk = pT[pi[0] % 4], pT_tok[pi[0] % 4]
                pi[0] += 1
                for j in range(j0, 4):
                    qt = qc * 4 + j
                    cs = slice((j - j0) * 128, (j - j0 + 1) * 128)
                    b = bias_fn(h, kt, qt) if bias_fn is not None else 0.0
                    c.op("act", lambda e: e.activation(p[:, cs], sb_[:, cs], AF.Exp, bias=b), reads=(stk, self.aux_tok),
                         writes=(ptk,))
                yield
                for j in range(j0, 4):
                    qt = qc * 4 + j
                    cs = slice((j - j0) * 128, (j - j0 + 1) * 128)
                    self.mm(O[:, j, :], p[:, cs], V[:, kt, h, :], kt == 0 and j == 0, kt == qt, reads=(ptk, vtok), writes=(ot,),
                            skip_group_check=True)
            rct = rc_tok[h % 2]
            for j in range(4):
                rcj = rc[:, (h % 2) * 4 + j:(h % 2) * 4 + j + 1]
                c.op("dve", lambda e: e.reciprocal(rcj, O[:, j, 64:65]), reads=(ot,), writes=(rct,))
                c.op("dve", lambda e: e.tensor_scalar(ytok_t[:, j, ycol0 + h * 64:ycol0 + (h + 1) * 64], O[:, j, 0:64],
                                                      rcj, None, ALU.mult),
                     reads=(ot, rct), writes=(ytok_tok,))

        for qc in range(NTC):
            self.run_streams([head_stream(qc, h) for h in range(nheads)], 2)
            if post_qc is not None:
                post_qc(qc)

    def ytok_to_yT(self, ytok_t, ytok_tok, qc):
        c = self.c
        for tl in range(4):
            t = qc * 4 + tl
            pb, pt = self.bank()
            pbb = pb[:].bitcast(BF16)
            for j in range(4):
                c.op("pe", lambda e: e.transpose(pbb[:, j * 128:(j + 1) * 128], ytok_t[:, tl, j * 128:(j + 1) * 128],
                                                 self.ident_b[:]),
                     reads=(ytok_tok, self.const_tok), writes=(pt,))
            c.op("dve", lambda e: e.tensor_copy(self.yT[:, :, t * 128:(t + 1) * 128],
                                                pbb[:, 0:512].rearrange("p (j n) -> p j n", j=4)),
                 reads=(pt,), writes=(self.yT_tok,))


    def combine(self, li, bi):
        c = self.c
        wg = self.A["w_in"][li].rearrange("(c p) n -> p c n", p=128)
        wb = self.A["w_branch"][li, bi].rearrange("(c p) n -> p c n", p=128)
        wo = self.A["w_out"][li].rearrange("(c p) n -> p c n", p=128)
        with ExitStack() as st:
            mT = self.sb(st, "mT", [128, KC, S], BF16)
            mtok = Tok()
            for dt_ in range(KC):
                g0 = O_GATE + bi * D + dt_ * 128
                wgv, wgt = self.wload(wg[:, :, g0:g0 + 128], KC, 128)
                wbv, wbt = self.wload(wb[:, :, dt_ * 128:(dt_ + 1) * 128], 4, 128)
                bcol = self.vcol(li, "b_gate", bi * 8 + dt_)
                for tc in range(NTC):
                    ts = slice(tc * 512, (tc + 1) * 512)
                    pa, pat = self.bank()
                    for cc in range(KC):
                        self.mm(pa[:], wgv[:, cc, :], self.hT[:, cc, ts], cc == 0, cc == KC - 1,
                                reads=(wgt, self.hT_tok[tc]), writes=(pat,))
                    pb, pbt = self.bank()
                    for cc in range(4):
                        self.mm(pb[:], wbv[:, cc, :], self.yT[:, cc, ts], cc == 0, cc == 3,
                                reads=(wbt, self.yT_tok), writes=(pbt,))
                    sg, sgt = self.nscr()
                    c.op("act", lambda e: e.activation(sg[:], pa[:], AF.Sigmoid, bias=bcol), reads=(pat, self.vec_tok),
                         writes=(sgt,))
                    c.op("dve", lambda e: e.tensor_tensor(mT[:, dt_, ts], sg[:], pb[:], ALU.mult), reads=(sgt, pbt),
                         writes=(mtok,))
            for do in range(KC):
                wov, wot = self.wload(wo[:, :, do * 128:(do + 1) * 128], KC, 128)
                for tc in range(NTC):
                    ts = slice(tc * 512, (tc + 1) * 512)
                    pb, pbt = self.bank()
                    for cc in range(KC):
                        self.mm(pb[:], wov[:, cc, :], mT[:, cc, ts], cc == 0, cc == KC - 1, reads=(wot, mtok), writes=(pbt,))
                    c.op("dve", lambda e: e.tensor_tensor(self.xT[:, do, ts], self.xT[:, do, ts], pb[:], ALU.add),
                         reads=(pbt,), writes=(self.xT_tok[tc],))
            c.barrier()


    def sbmix(self, li):
        c = self.c
        with ExitStack() as st:
            qT = self.sb(st, "sb_qT", [128, 4, S], BF16)
            kT = self.sb(st, "sb_kT", [128, 4, S], BF16)
            V = self.sb(st, "sb_V", [128, NT, 8, 64], BF16)
            ytk = self.sb(st, "sb_y", [128, 4, 512], BF16)
            spb = [[self.sb(st, f"sb_sp{k}{i}", [128, 512], BF16) for i in range(2)] for k in range(2)]
            spt = [[Tok(), Tok()] for k in range(2)]
            pT = [[self.sb(st, f"sb_pT{k}{i}", [128, 512], BF16) for i in range(2)] for k in range(2)]
            pTt = [[Tok(), Tok()] for k in range(2)]
            sufs = [(self.sb(st, f"sb_suf{k}", [1, 512], F32), self.sb(st, f"sb_sufh{k}", [1, 512], BF16),
                     self.sb(st, f"sb_sufl{k}", [1, 512], BF16), Tok()) for k in range(2)]
            qtok, ktok, vtok, ytok = Tok(), Tok(), Tok(), Tok()
            self.proj_feat(li, O_SQ, 512, self.evac_featT(qT, qtok, 0.125))
            self.proj_feat(li, O_SK, 512, self.evac_featT(kT, ktok, 1.0))
            for q4 in range(4):
                def evac_vh(t, pb, pt, q4=q4):
                    c.op("act", lambda e: e.copy(V[:, t, q4 * 2:q4 * 2 + 2, :],
                                                 pb[:, 0:128].rearrange("p (h d) -> p h d", h=2)),
                         reads=(pt,), writes=(vtok,))
                self.proj_tok(li, O_SV + q4 * 128, 128, evac_vh)
            def head_stream(qc, h):
                s_ = h % 2
                hp = slice((h % 2) * 64, (h % 2) * 64 + 64)
                hc = h // 2
                ob, ot = self.bank_acc()
                O = ob[:, 0:256].rearrange("p (j d) -> p j d", j=4)
                nkt = 4 * qc + 4
                suf, sufh, sufl, suft = sufs[s_]
                c.op("dve", lambda e: e.memset(suf[:], 0.0), writes=(suft,))
                c.op("dve", lambda e: e.memset(sufh[:], 0.0), writes=(suft,))
                c.op("dve", lambda e: e.memset(sufl[:], 0.0), writes=(suft,))
                first = True
                ti = 0
                for kt in range(nkt - 1, -1, -1):
                    j0 = max(0, kt - 4 * qc)
                    q0 = qc * 512 + j0 * 128
                    ncol = 512 - j0 * 128
                    diag = kt >= 4 * qc
                    ksl = kT[hp, hc, kt * 128:(kt + 1) * 128]
                    qsl = qT[hp, hc, q0:q0 + ncol]
                    pa, pat = self.bank()
                    self.mm(pa[:, 0:ncol], ksl, qsl, True, not diag, reads=(ktok, qtok), writes=(pat,))
                    if diag:
                        self.mm(pa[:, 0:128], self.ident_b[:], self.cnegs_b[:], False, True, reads=(self.const_tok,),
                                writes=(pat,), skip_group_check=True)
                    e_, et = self.nscr()
                    sp, spk = spb[s_][ti % 2], spt[s_][ti % 2]
                    p, ptk = pT[s_][ti % 2], pTt[s_][ti % 2]
                    ti += 1
                    c.op("act", lambda e: e.activation(e_[:, 0:ncol], pa[:, 0:ncol], AF.Exp), reads=(pat,), writes=(et,))
                    c.op("act", lambda e: e.activation(sp[:, 0:ncol], e_[:, 0:ncol], AF.Ln, bias=1.0), reads=(et,),
                         writes=(spk,))
                    yield
                    pb, pbt = self.bank()
                    self.mm(pb[:, 0:ncol], ksl, qsl, True, False, reads=(ktok, qtok), writes=(pbt,))
                    if diag:
                        self.mm(pb[:, 0:128], self.ident_b[:], self.cnegs_b[:], False, False, reads=(self.const_tok,),
                                writes=(pbt,), skip_group_check=True)
                    self.mm(pb[:, 0:ncol], self.nones_b[0:1, :], sufh[0:1, 512 - ncol:512], False, False,
                            reads=(suft, self.const_tok), writes=(pbt,), skip_group_check=True)
                    self.mm(pb[:, 0:ncol], self.nones_b[0:1, :], sufl[0:1, 512 - ncol:512], False, False,
                            reads=(suft, self.const_tok), writes=(pbt,), skip_group_check=True)
                    self.mm(pb[:, 0:ncol], self.ntri_b[:], sp[:, 0:ncol], False, True, reads=(spk, self.const_tok),
                            writes=(pbt,), skip_group_check=True)
                    if kt > 0:
                        pc, pct = self.bank()
                        self.mm(pc[0:1, 0:ncol], self.ones_b[:, 0:1], sp[:, 0:ncol], True, True,
                                reads=(spk, self.const_tok), writes=(pct,))
                        sl = slice(512 - ncol, 512)
                        c.op("dve", lambda e: e.tensor_tensor(suf[0:1, sl], suf[0:1, sl], pc[0:1, 0:ncol], ALU.add),
                             reads=(pct,), writes=(suft,))
                        c.op("dve", lambda e: e.tensor_copy(sufh[0:1, sl], suf[0:1, sl]), reads=(suft,), writes=(suft,))
                        c.op("dve", lambda e: e.tensor_tensor(sufl[0:1, sl], suf[0:1, sl], sufh[0:1, sl], ALU.subtract),
                             reads=(suft,), writes=(suft,))
                    c.op("act", lambda e: e.activation(p[:, 0:ncol], pb[:, 0:ncol], AF.Exp), reads=(pbt,), writes=(ptk,))
                    yield
                    for j in range(j0, 4):
                        cs = slice((j - j0) * 128, (j - j0 + 1) * 128)
                        self.mm(O[:, j, :], p[:, cs], V[:, kt, h, :], first, kt == 0, reads=(ptk, vtok), writes=(ot,),
                                skip_group_check=True)
                        first = False
                for j in range(4):
                    c.op("dve", lambda e: e.tensor_copy(ytk[:, j, h * 64:(h + 1) * 64], O[:, j, :]), reads=(ot,),
                         writes=(ytok,))

            for qc in range(NTC):
                self.run_streams([head_stream(qc, h) for h in range(8)], 2)
                self.ytok_to_yT(ytk, ytok, qc)
            c.barrier()

    def gla(self, li):
        c = self.c
        ct = self.const_tok
        with ExitStack() as st:
            qeT = self.sb(st, "g_qe", [64, 4, S], BF16)
            keT = self.sb(st, "g_ke", [64, 4, S], BF16)
            k2 = self.sb(st, "g_k2", [128, NT, 256], BF16)
            vtk = self.sb(st, "g_v", [128, NT, 512], BF16)
            gnorm = self.sb(st, "g_norm", [128, 1], F32)
            dec = self.sb(st, "g_dec", [64, 4, 32], F32)
            tblk = self.sb(st, "g_tblk", [128, 128], F32)
            sp_ = ExitStack()
            alrT = self.sb(sp_, "g_alr", [16, S], BF16)
            balb = self.sb(sp_, "g_bal", [128, 256], F32)
            wal_f = self.sb(sp_, "g_walf", [16, 256], F32)
            wal_b = self.sb(sp_, "g_walb", [16, 256], BF16)
            n16 = self.sb(sp_, "g_n16", [128, 128], F32)
            m1 = self.sb(sp_, "g_m1", [128, 128], F32)
            m2 = self.sb(sp_, "g_m2", [128, 128], F32)
            att_tok = [Tok(), Tok()]
            mtok, qtok, ktok, vtok, k2tok, atok, ptok, stok, otok, ontok = [Tok() for _ in range(10)]
            c.op("pool", lambda e: e.memset(n16[:], -1.0 / 16.0), writes=(mtok,))
            c.op("pool", lambda e: e.affine_select(m1[:], n16[:], [[1, 128]], ALU.is_ge, 0.0, base=0, channel_multiplier=-1),
                 reads=(mtok,), writes=(mtok,))
            c.op("pool", lambda e: e.memset(m1[0:64, 64:128], 0.0), reads=(mtok,), writes=(mtok,))
            c.op("pool", lambda e: e.affine_select(m2[:], n16[:], [[-1, 128]], ALU.is_gt, 0.0, base=0, channel_multiplier=1),
                 reads=(mtok,), writes=(mtok,))
            c.op("pool", lambda e: e.memset(m2[64:128, 0:64], 0.0), reads=(mtok,), writes=(mtok,))
            c.op("pool", lambda e: e.tensor_copy(tblk[:], self.tri_f[:]), reads=(ct, mtok), writes=(mtok,))
            c.op("pool", lambda e: e.memset(tblk[0:64, 64:128], 0.0), reads=(mtok,), writes=(mtok,))
            c.dma(balb[:], self.A["gla_b_alpha"][li:li + 1, :].partition_broadcast(128), writes=(ptok,))
            c.dma(wal_f[:], self.A["gla_w_alpha"][li], writes=(ptok,))
            c.dma(gnorm[:], self.A["gla_norm"][li].rearrange("(p o) -> p o", o=1), writes=(ptok,))
            c.op("pool", lambda e: e.tensor_copy(wal_b[:], wal_f[:]), reads=(ptok,), writes=(ptok,))
            for h in range(4):
                def ev_q(mt, tc, pb, pt, h=h):
                    ts = slice(tc * 512, (tc + 1) * 512)
                    c.op("act", lambda e: e.activation(qeT[0:64, h, ts], pb[0:64, :], AF.Copy, scale=0.125), reads=(pt,),
                         writes=(qtok,))

                def ev_k(mt, tc, pb, pt, h=h):
                    ts = slice(tc * 512, (tc + 1) * 512)
                    c.op("dve", lambda e: e.tensor_copy(keT[0:64, h, ts], pb[0:64, :]), reads=(pt,), writes=(ktok,))
                self.proj_feat(li, O_GQ + h * 64, 64, ev_q)
                self.proj_feat(li, O_GK + h * 64, 64, ev_k)

            def ev_a(mt, tc, pb, pt):
                ts = slice(tc * 512, (tc + 1) * 512)
                c.op("act", lambda e: e.copy(alrT[0:16, ts], pb[0:16, :]), reads=(pt,), writes=(atok,))
            self.proj_feat(li, O_GA, 16, ev_a)
            for i in range(4):
                def ev_v(t, pb, pt, i=i):
                    c.op("act", lambda e: e.copy(vtk[:, t, i * 128:(i + 1) * 128], pb[:, 0:128]), reads=(pt,), writes=(vtok,))
                self.proj_tok(li, O_GV + i * 128, 128, ev_v)
            import os
            gstop = int(os.environ.get("GLA_STOP", "99"))
            if gstop == 1:
                c.barrier()
                return
            for t in range(NT):
                tl = slice(t * 128, (t + 1) * 128)
                pa, pat = self.bank()
                self.mm(pa[:, 0:256], alrT[0:16, tl], wal_b[0:16, :], True, True, reads=(atok, ptok), writes=(pat,))
                xs, xst = self.nscr()
                c.op("dve", lambda e: e.tensor_tensor(xs[:, 0:256], pa[:, 0:256], balb[:], ALU.add), reads=(pat, ptok),
                     writes=(xst,))
                c.op("act", lambda e: e.activation(xs[:, 0:256], xs[:, 0:256], AF.Exp, scale=-1.0), reads=(xst,), writes=(xst,))
                c.op("act", lambda e: e.activation(xs[:, 0:256], xs[:, 0:256], AF.Ln, bias=1.0), reads=(xst,), writes=(xst,))
                pw, pwt = self.bank()
                self.mm(pw[:, 0:256], m2[:], xs[:, 0:256], True, True, reads=(mtok, xst), writes=(pwt,))
                c.op("act", lambda e: e.activation(k2[:, t, :], pw[:, 0:256], AF.Exp), reads=(pwt,), writes=(k2tok,))
                pbT, pbTt = self.bank()
                for h in range(4):
                    self.mm(pbT[0:64, h * 128:(h + 1) * 128], xs[:, h * 64:(h + 1) * 64], m1[:], h == 0, h == 3,
                            reads=(mtok, xst), writes=(pbTt,), skip_group_check=True)
                ebp, ebpt = self.nscr()
                ebn, ebnt = self.nscr()
                c.op("act", lambda e: e.activation(ebp[0:64, :], pbT[0:64, :], AF.Exp), reads=(pbTt,), writes=(ebpt,))
                c.op("act", lambda e: e.activation(ebn[0:64, :], pbT[0:64, :], AF.Exp, scale=-1.0), reads=(pbTt,), writes=(ebnt,))
                c.op("dve", lambda e: e.tensor_tensor(qeT[0:64, :, tl], qeT[0:64, :, tl],
                                                      ebp[0:64, :].rearrange("p (h n) -> p h n", h=4), ALU.mult),
                     reads=(ebpt,), writes=(qtok,))
                c.op("dve", lambda e: e.tensor_tensor(keT[0:64, :, tl], keT[0:64, :, tl],
                                                      ebn[0:64, :].rearrange("p (h n) -> p h n", h=4), ALU.mult),
                     reads=(ebnt,), writes=(ktok,))
                c.op("dve", lambda e: e.tensor_copy(dec[0:64, :, 2 * t:2 * t + 2],
                                                    ebp[0:64, :].rearrange("p (h c s) -> p h c s", h=4, c=2)[:, :, :, 63]),
                     reads=(ebpt,), writes=(stok,))
            c.barrier()
            sp_.close()
            if gstop == 2:
                return
            st_f = self.sb(st, "g_stf", [64, 4, 128], F32)
            st_b = self.sb(st, "g_stb", [64, 4, 128], BF16)
            oT = self.sb(st, "g_oT", [128, 512], F32)
            onh = self.sb(st, "g_on", [128, S], BF16)
            attb = [self.sb(st, f"g_att{i}", [128, 128], BF16) for i in range(2)]
            for i in range(2):
                def ev_k2(t, pb, pt, i=i):
                    c.op("dve", lambda e: e.tensor_tensor(k2[:, t, i * 128:(i + 1) * 128], k2[:, t, i * 128:(i + 1) * 128],
                                                          pb[:, 0:128], ALU.mult), reads=(pt,), writes=(k2tok,))
                self.proj_tok(li, O_GK + i * 128, 128, ev_k2)
            if gstop == 3:
                c.barrier()
                return
            ai = 0
            for h in range(4):
                c.op("dve", lambda e: e.memset(st_f[0:64, h, :], 0.0), writes=(stok,))
                c.op("dve", lambda e: e.memset(st_b[0:64, h, :], 0.0), writes=(stok,))
                for t in range(NT):
                    tl = slice(t * 128, (t + 1) * 128)
                    pa, pat = self.bank()
                    self.mm(pa[:, 0:128], keT[0:64, h, tl], qeT[0:64, h, tl], True, True, reads=(ktok, qtok), writes=(pat,))
                    ab, abt = attb[ai % 2], att_tok[ai % 2]
                    ai += 1
                    c.op("dve", lambda e: e.tensor_tensor(ab[:], pa[:, 0:128], tblk[:], ALU.mult), reads=(pat, mtok),
                         writes=(abt,))
                    for half in range(2):
                        cn = 2 * t + half
                        rs = slice(half * 64, half * 64 + 64)
                        cs = slice(cn * 64, (cn + 1) * 64)
                        vsl = vtk[rs, t, h * 128:(h + 1) * 128]
                        po, pot = self.bank()
                        inter = cn > 0 and gstop != 4
                        self.mm(po[:, 0:64], vsl, ab[rs, rs], True, True, reads=(vtok, abt), writes=(pot,))
                        oc = (t % 4) * 128 + half * 64
                        c.op("act", lambda e: e.copy(oT[:, oc:oc + 64], po[:, 0:64]), reads=(pot,), writes=(otok,))
                        if inter:
                            pi_, pit = self.bank()
                            self.mm(pi_[:, 0:64], st_b[0:64, h, :], qeT[0:64, h, cs], True, True, reads=(stok, qtok),
                                    writes=(pit,))
                            c.op("dve", lambda e: e.tensor_tensor(oT[:, oc:oc + 64], oT[:, oc:oc + 64], pi_[:, 0:64], ALU.add),
                                 reads=(pit, otok), writes=(otok,))
                        if gstop == 5:
                            continue
                        ps_, pst = self.bank()
                        self.mm(ps_[0:64, 0:128], k2[rs, t, h * 64:(h + 1) * 64], vsl, True, True, reads=(k2tok, vtok),
                                writes=(pst,))
                        c.op("dve", lambda e: e.scalar_tensor_tensor(st_f[0:64, h, :], st_f[0:64, h, :], dec[0:64, h, cn:cn + 1],
                                                                     ps_[0:64, 0:128], ALU.mult, ALU.add),
                             reads=(pst, stok), writes=(stok,))
                        c.op("dve", lambda e: e.tensor_copy(st_b[0:64, h, :], st_f[0:64, h, :]), reads=(stok,), writes=(stok,))
                    if t % 4 == 3:
                        tc = t // 4
                        ts = slice(tc * 512, (tc + 1) * 512)
                        sq, sqt = self.nscr()
                        c.op("act", lambda e: e.activation(sq[:], oT[:], AF.Square), reads=(otok,), writes=(sqt,))
                        pn, pnt = self.bank()
                        self.mm(pn[:], self.ones_f[:], sq[:], True, True, reads=(sqt, ct), writes=(pnt,))
                        rr, rrt = self.nscr()
                        c.op("dve", lambda e: e.tensor_scalar(rr[:], pn[:], 1.0 / 128.0, EPS, ALU.mult, ALU.add), reads=(pnt,),
                             writes=(rrt,))
                        c.op("act", lambda e: e.activation(rr[:], rr[:], AF.Sqrt), reads=(rrt,), writes=(rrt,))
                        c.op("dve", lambda e: e.reciprocal(rr[:], rr[:]), reads=(rrt,), writes=(rrt,))
                        c.op("dve", lambda e: e.scalar_tensor_tensor(onh[:, ts], oT[:], gnorm[:, 0:1], rr[:], ALU.mult, ALU.mult),
                             reads=(otok, rrt, ptok), writes=(ontok,))

                def ev_g(mt, tc, pb, pt, h=h):
                    ts = slice(tc * 512, (tc + 1) * 512)
                    sg, sgt = self.nscr()
                    c.op("act", lambda e: e.activation(sg[:], pb[:], AF.Silu), reads=(pt,), writes=(sgt,))
                    c.op("dve", lambda e: e.tensor_tensor(self.yT[:, h, ts], sg[:], onh[:, ts], ALU.mult), reads=(sgt, ontok),
                         writes=(self.yT_tok,))
                self.proj_feat(li, O_GG + h * 128, 128, ev_g)
            c.barrier()


    def proj_feat_dup(self, li, col0, evac):
        c = self.c
        w = self.A["w_in"][li].rearrange("(c p) n -> p c n", p=128)
        wv, wt = self.wload(w[:, :, col0:col0 + 64], KC, 64)
        wd, wdt = self.wdup, self.wdup_tok
        c.op("pool", lambda e: e.tensor_copy(wd[:, :, 0:64], wv), reads=(wt,), writes=(wdt,))
        c.op("pool", lambda e: e.tensor_copy(wd[:, :, 64:128], wv), reads=(wt,), writes=(wdt,))
        for tc in range(NTC):
            ts = slice(tc * 512, (tc + 1) * 512)
            pb, pt = self.bank()
            for cc in range(KC):
                self.mm(pb[:], wd[:, cc, :], self.hT[:, cc, ts], cc == 0, cc == KC - 1, reads=(wdt, self.hT_tok[tc]),
                        writes=(pt,))
            evac(0, tc, pb, pt)

    def rope_apply(self, dst, pb, pt, n, scale, cos_ap, sin_ap, dtok):
        c = self.c
        raw, rawt = self.rraw[self.rr_i % 2], self.rraw_tok[self.rr_i % 2]
        self.rr_i += 1
        c.op("act", lambda e: e.activation(raw[:, 0:n], pb, AF.Copy, scale=scale), reads=(pt,), writes=(rawt,))
        p2, p2t = self.bank()
        self.mm(p2[:, 0:n], self.Pm[:], raw[:, 0:n], True, True, reads=(rawt, self.tbl_tok), writes=(p2t,))
        t1, t1t = self.nscr()
        c.op("pool", lambda e: e.tensor_tensor(t1[:, 0:n], raw[:, 0:n], cos_ap, ALU.mult), reads=(rawt, self.tbl_tok),
             writes=(t1t,))
        t2, t2t = self.nscr()
        c.op("dve", lambda e: e.tensor_tensor(t2[:, 0:n], p2[:, 0:n], sin_ap, ALU.mult), reads=(p2t, self.tbl_tok),
             writes=(t2t,))
        c.op("dve", lambda e: e.tensor_tensor(dst, t1[:, 0:n], t2[:, 0:n], ALU.add), reads=(t1t, t2t), writes=(dtok,))

    def nsa_tables(self, cosT, sinT):
        c = self.c
        tbl = self.tbl_tok
        PI = float(np.pi)
        C1 = 6.28125
        C2 = float(2 * np.pi - 6.28125)
        with ExitStack() as s2:
            pidx = self.sb(s2, "n_pi", [128, 1], I32)
            f = self.sb(s2, "n_f", [128, 8], F32)
            posi = self.sb(s2, "n_posi", [128, 512], I32)
            ki = self.sb(s2, "n_ki", [128, 512], I32)
            ftok, ptok = Tok(), Tok()
            c.op("pool", lambda e: e.iota(pidx[:], [[0, 1]], base=0, channel_multiplier=1), writes=(ftok,))
            PF, GE, DD, G8, II, ACTV, SGN, INV = [f[:, i:i + 1] for i in range(8)]
            V = lambda fn: c.op("dve", fn, reads=(ftok,), writes=(ftok,))
            V(lambda e: e.tensor_copy(PF, pidx[:]))
            V(lambda e: e.tensor_single_scalar(GE, PF, 64.0, ALU.is_ge))
            V(lambda e: e.scalar_tensor_tensor(DD, GE, -64.0, PF, ALU.mult, ALU.add))
            V(lambda e: e.tensor_single_scalar(G8, DD, 8.0, ALU.is_ge))
            V(lambda e: e.scalar_tensor_tensor(II, G8, -8.0, DD, ALU.mult, ALU.add))
            V(lambda e: e.tensor_single_scalar(ACTV, DD, 16.0, ALU.is_lt))
            V(lambda e: e.tensor_scalar(SGN, G8, 2.0, -1.0, ALU.mult, ALU.add))
            V(lambda e: e.memset(INV, 0.0))
            for i in range(8):
                ci = float(np.float32(500000.0) ** np.float32(-i / 8.0))
                V(lambda e: e.tensor_scalar(GE, II, float(i), ci, ALU.is_equal, ALU.mult))
                V(lambda e: e.tensor_tensor(INV, INV, GE, ALU.add))
            V(lambda e: e.tensor_tensor(INV, INV, ACTV, ALU.mult))
            for tc in range(NTC):
                ts = slice(tc * 512, (tc + 1) * 512)
                c.dma(posi[:], self.A["positions"][0:1, ts].partition_broadcast(128), writes=(ptok,))
                ang, angt = self.nscr()
                c.op("dve", lambda e: e.tensor_copy(ang[:], posi[:]), reads=(ptok,), writes=(angt,))
                c.op("dve", lambda e: e.tensor_scalar(ang[:], ang[:], INV, None, ALU.mult), reads=(angt, ftok), writes=(angt,))
                for phase, dstT, use_sign in ((0.0, sinT, True), (PI / 2, cosT, False)):
                    u, ut = self.nscr()
                    r, rt = self.nscr()
                    c.op("dve", lambda e: e.tensor_scalar(u[:], ang[:], phase, 1.0 / (2 * PI), ALU.add, ALU.mult),
                         reads=(angt,), writes=(ut,))
                    c.op("dve", lambda e: e.tensor_copy(ki[:], u[:]), reads=(ut,), writes=(ptok,))
                    c.op("dve", lambda e: e.tensor_copy(u[:], ki[:]), reads=(ptok,), writes=(ut,))
                    c.op("dve", lambda e: e.scalar_tensor_tensor(r[:], u[:], -C1, ang[:], ALU.mult, ALU.add),
                         reads=(ut, angt), writes=(rt,))
                    c.op("dve", lambda e: e.scalar_tensor_tensor(r[:], u[:], -C2, r[:], ALU.mult, ALU.add), reads=(ut, rt),
                         writes=(rt,))
                    if phase != 0.0:
                        c.op("dve", lambda e: e.tensor_scalar(r[:], r[:], phase, None, ALU.add), reads=(rt,), writes=(rt,))
                    c.op("dve", lambda e: e.tensor_single_scalar(u[:], r[:], PI, ALU.is_gt), reads=(rt,), writes=(ut,))
                    c.op("dve", lambda e: e.scalar_tensor_tensor(r[:], u[:], -2 * PI, r[:], ALU.mult, ALU.add), reads=(ut, rt),
                         writes=(rt,))
                    c.op("dve", lambda e: e.tensor_single_scalar(u[:], r[:], -PI, ALU.is_lt), reads=(rt,), writes=(ut,))
                    c.op("dve", lambda e: e.scalar_tensor_tensor(r[:], u[:], 2 * PI, r[:], ALU.mult, ALU.add), reads=(ut, rt),
                         writes=(rt,))
                    c.op("dve", lambda e: e.tensor_scalar(r[:], r[:], PI, -PI, ALU.min, ALU.max), reads=(rt,), writes=(rt,))
                    c.op("act", lambda e: e.activation(r[:], r[:], AF.Sin), reads=(rt,), writes=(rt,))
                    if use_sign:
                        c.op("dve", lambda e: e.tensor_scalar(dstT[:, ts], r[:], SGN, None, ALU.mult), reads=(rt, ftok),
                             writes=(tbl,))
                    else:
                        c.op("dve", lambda e: e.tensor_copy(dstT[:, ts], r[:]), reads=(rt,), writes=(tbl,))
            c.barrier()

    def nsa(self, li):
        c = self.c
        ct = self.const_tok
        import os
        nstop = int(os.environ.get("NSA_STOP", "99"))
        with ExitStack() as st:
            kcT2 = self.sb(st, "n_kcT", [128, 2, 128], BF16)
            VCX = self.sb(st, "n_vcx", [128, 2, 97], BF16)
            cmptok = Tok()

            def open_tables(sx):
                cosT = self.sb(sx, "n_cos", [128, S], BF16)
                sinT = self.sb(sx, "n_sin", [128, S], BF16)
                self.Pm = self.sb(sx, "n_Pm", [128, 128], BF16)
                self.tbl_tok = Tok()
                self.rraw = [self.sb(sx, f"n_raw{i}", [128, 512], BF16) for i in range(2)]
                self.rraw_tok = [Tok(), Tok()]
                self.rr_i = 0
                self.wdup = self.sb(sx, "n_wdup", [128, KC, 128], BF16)
                self.wdup_tok = Tok()
                self.nsa_tables(cosT, sinT)
                c.op("pool", lambda e: e.memset(self.Pm[:], 0.0), writes=(self.tbl_tok,))
                for (d0, s0) in ((0, 8), (8, 0), (64, 72), (72, 64)):
                    c.op("pool", lambda e: e.tensor_copy(self.Pm[:, d0:d0 + 8], self.ident_b[:, s0:s0 + 8]),
                         reads=(ct, self.tbl_tok), writes=(self.tbl_tok,))
                return cosT, sinT
            sA = ExitStack()
            cosT, sinT = open_tables(sA)
            tbl = self.tbl_tok
            if "cosT" in self.dbg_out:
                self.dump_featmajor_bf16(cosT[:].rearrange("p (c s) -> p c s", c=1), [tbl], self.dbg_out["cosT"])
                self.dump_featmajor_bf16(sinT[:].rearrange("p (c s) -> p c s", c=1), [tbl], self.dbg_out["sinT"])
            with ExitStack() as s3:
                xcT = [self.sb(s3, "n_xk", [128, S], BF16), self.sb(s3, "n_xv", [128, S], BF16)]
                xtok = Tok()
                for kv, col0 in ((0, O_NKC), (1, O_NVC)):
                    def ev_x(mt, tc, pb, pt, kv=kv):
                        ts = slice(tc * 512, (tc + 1) * 512)
                        c.op("act", lambda e: e.copy(xcT[kv][:, ts], pb[:]), reads=(pt,), writes=(xtok,))
                    self.proj_feat(li, col0, 128, ev_x)
                W1 = self.sb(s3, "n_w1", [128, 32, 256], BF16)
                stg = self.sb(s3, "n_stg", [128, 2048], F32)
                W2f = self.sb(s3, "n_w2f", [128, 2, 64], F32)
                W2d = self.sb(s3, "n_w2d", [128, 2, 128], BF16)
                pe2 = self.sb(s3, "n_pe2", [32, 128], F32)
                peb = self.sb(s3, "n_peb", [128, 32], BF16)
                gh = self.sb(s3, "n_gh", [128, 2, 128], BF16)
                hb = self.sb(s3, "n_hb", [128, 2], F32)
                ovf = self.sb(s3, "n_ovf", [128, 3, 32], F32)
                stgt, w1t, w2t, pet, ght, hbt, ovt = [Tok() for _ in range(7)]
                c.op("pool", lambda e: e.memset(ovf[:], 0.5), writes=(ovt,))
                for k_, off in ((0, 0), (1, 16)):
                    c.op("pool", lambda e: e.affine_select(ovf[:, k_, :], ovf[:, k_, :], [[-64, 32]], ALU.is_ge, 0.0, base=off,
                                                           channel_multiplier=16), reads=(ovt,), writes=(ovt,))
                    c.op("pool", lambda e: e.affine_select(ovf[:, k_, :], ovf[:, k_, :], [[64, 32]], ALU.is_ge, 0.0,
                                                           base=63 - off, channel_multiplier=-16), reads=(ovt,), writes=(ovt,))
                c.op("pool", lambda e: e.tensor_tensor(ovf[:, 2, :], ovf[:, 0, :], ovf[:, 1, :], ALU.add), reads=(ovt,),
                     writes=(ovt,))
                for g in range(2):
                    c.op("pool", lambda e: e.tensor_copy(VCX[:, g, 65:97], ovf[:, 2, :]), reads=(ovt,), writes=(cmptok,))
                c.op("pool", lambda e: e.memset(VCX[:, :, 64:65], 1.0), writes=(cmptok,))
                for kv in range(2):
                    w1 = self.A["cmp_wk1" if kv == 0 else "cmp_wv1"][li].rearrange("(l d) n -> d l n", d=64)
                    for piece in range(4):
                        for half in range(2):
                            c.dma(stg[half * 64:(half + 1) * 64, :].rearrange("p (l n) -> p l n", l=8),
                                  w1[:, piece * 8:(piece + 1) * 8, :], writes=(stgt,))
                        c.op("pool", lambda e: e.tensor_copy(W1[:, piece * 8:(piece + 1) * 8, :],
                                                             stg[:].rearrange("p (l n) -> p l n", l=8)),
                             reads=(stgt,), writes=(w1t,))
                    w2 = self.A["cmp_wk2" if kv == 0 else "cmp_wv2"][li].rearrange("(c p) n -> p c n", p=128)
                    c.dma(W2f[:], w2, writes=(w2t,))
                    c.op("pool", lambda e: e.tensor_copy(W2d[:, :, 0:64], W2f[:]), reads=(w2t,), writes=(w2t,))
                    c.op("pool", lambda e: e.tensor_copy(W2d[:, :, 64:128], W2f[:]), reads=(w2t,), writes=(w2t,))
                    pe = self.A["cmp_pos_k" if kv == 0 else "cmp_pos_v"][li]
                    c.dma(pe2[:, 0:64], pe, writes=(pet,))
                    c.dma(pe2[:, 64:128], pe, writes=(pet,))
                    pp, ppt = self.bank()
                    c.op("pe", lambda e: e.transpose(pp[:, 0:32], pe2[:], self.ident_f[0:32, 0:32]), reads=(pet, ct),
                         writes=(ppt,))
                    c.op("dve", lambda e: e.tensor_copy(peb[:], pp[:, 0:32]), reads=(ppt,), writes=(pet,))
                    for half in range(2):
                        pk_, pkt = self.bank()
                        for l in range(32):
                            self.mm(pk_[:, 0:1], W1[0:64, l, half * 128:(half + 1) * 128], peb[0:64, l:l + 1], l == 0, l == 31,
                                    reads=(w1t, pet), writes=(pkt,))
                        c.op("dve", lambda e: e.tensor_copy(hb[:, half:half + 1], pk_[:, 0:1]), reads=(pkt,), writes=(hbt,))
                    for g in range(2):
                        gs = slice(g * 64, g * 64 + 64)
                        for half in range(2):
                            ph, pht = self.bank()
                            for l in range(32):
                                self.mm(ph[:, 0:127], W1[gs, l, half * 128:(half + 1) * 128],
                                        xcT[kv][gs, l:l + 16 * 126 + 1:16], l == 0, l == 31, reads=(w1t, xtok), writes=(pht,))
                            x, xt = self.nscr()
                            x2, x2t = self.nscr()
                            N_ = slice(0, 127)
                            c.op("dve", lambda e: e.tensor_scalar(x[:, N_], ph[:, N_], hb[:, half:half + 1], None, ALU.add),
                                 reads=(pht, hbt), writes=(xt,))
                            c.op("dve", lambda e: e.tensor_tensor(x2[:, N_], x[:, N_], x[:, N_], ALU.mult), reads=(xt,),
                                 writes=(x2t,))
                            c.op("dve", lambda e: e.tensor_scalar(x2[:, N_], x2[:, N_], 0.044715, 1.0, ALU.mult, ALU.add),
                                 reads=(x2t,), writes=(x2t,))
                            c.op("dve", lambda e: e.tensor_tensor(x2[:, N_], x2[:, N_], x[:, N_], ALU.mult), reads=(x2t, xt),
                                 writes=(x2t,))
                            c.op("act", lambda e: e.activation(x2[:, N_], x2[:, N_], AF.Tanh, scale=0.7978845608028654),
                                 reads=(x2t,), writes=(x2t,))
                            c.op("dve", lambda e: e.tensor_scalar(x[:, N_], x[:, N_], 0.5, None, ALU.mult), reads=(xt,),
                                 writes=(xt,))
                            c.op("dve", lambda e: e.scalar_tensor_tensor(gh[:, half, 0:127], x2[:, N_], 1.0, x[:, N_], ALU.add,
                                                                         ALU.mult), reads=(x2t, xt), writes=(ght,))
                        if kv == 0:
                            pk, pkt2 = self.bank()
                            for half in range(2):
                                self.mm(pk[:, 0:127], W2d[:, half, :], gh[:, half, 0:127], half == 0, half == 1,
                                        reads=(w2t, ght), writes=(pkt2,))
                            self.rope_apply(kcT2[:, g, 0:127], pk[:, 0:127], pkt2, 127, 1.0,
                                            cosT[:, 31:31 + 16 * 126 + 1:16], sinT[:, 31:31 + 16 * 126 + 1:16], cmptok)
                        else:
                            pv, pvt = self.bank()
                            for half in range(2):
                                self.mm(pv[0:127, 0:64], gh[:, half, 0:127], W2d[:, half, 0:64], half == 0, half == 1,
                                        reads=(w2t, ght), writes=(pvt,))
                            c.op("act", lambda e: e.copy(VCX[0:127, g, 0:64], pv[0:127, 0:64]), reads=(pvt,), writes=(cmptok,))
                if "kcT" in self.dbg_out:
                    self.dump2d("kcT", kcT2[:].rearrange("p g n -> p (g n)"), [cmptok])
                    self.dump2d("vcx", VCX[:].rearrange("p g n -> p (g n)"), [cmptok])
                c.barrier()
            sA.close()
            if nstop == 1:
                return
            qT = self.sb(st, "n_qT", [128, 4, S], BF16)
            ksT2 = self.sb(st, "n_ksT", [128, 2, S], BF16)
            kwT2 = self.sb(st, "n_kwT", [128, 2, S], BF16)
            vs = self.sb(st, "n_vs", [128, NT, 2, 65], BF16)
            vw = self.sb(st, "n_vw", [128, NT, 2, 65], BF16)
            sg = self.sb(st, "n_sg", [128, NT, 24], F32)
            sB = ExitStack()
            cosT, sinT = open_tables(sB)
            qtok, kstok, kwtok, vstok, vwtok, sgtok = [Tok() for _ in range(6)]

            def ev_q(mt, tc, pb, pt):
                ts = slice(tc * 512, (tc + 1) * 512)
                self.rope_apply(qT[:, mt, ts], pb[:], pt, 512, 0.125, cosT[:, ts], sinT[:, ts], qtok)
            self.proj_feat(li, O_NQ, 512, ev_q)
            for g in range(2):
                def ev_ks(mt, tc, pb, pt, g=g):
                    ts = slice(tc * 512, (tc + 1) * 512)
                    self.rope_apply(ksT2[:, g, ts], pb[:], pt, 512, 1.0, cosT[:, ts], sinT[:, ts], kstok)

                def ev_kw(mt, tc, pb, pt, g=g):
                    ts = slice(tc * 512, (tc + 1) * 512)
                    self.rope_apply(kwT2[:, g, ts], pb[:], pt, 512, 1.0, cosT[:, ts], sinT[:, ts], kwtok)
                self.proj_feat_dup(li, O_NKS + g * 64, ev_ks)
                self.proj_feat_dup(li, O_NKW + g * 64, ev_kw)
            c.op("pool", lambda e: e.memset(vs[:, :, :, 64:65], 1.0), writes=(vstok,))
            c.op("pool", lambda e: e.memset(vw[:, :, :, 64:65], 1.0), writes=(vwtok,))

            def ev_vs(t, pb, pt):
                c.op("act", lambda e: e.copy(vs[:, t, :, 0:64], pb[:, 0:128].rearrange("p (g d) -> p g d", g=2)), reads=(pt,),
                     writes=(vstok,))

            def ev_vw(t, pb, pt):
                c.op("act", lambda e: e.copy(vw[:, t, :, 0:64], pb[:, 0:128].rearrange("p (g d) -> p g d", g=2)), reads=(pt,),
                     writes=(vwtok,))

            def ev_sg(t, pb, pt):
                c.op("act", lambda e: e.activation(sg[:, t, :], pb[:, 0:24], AF.Sigmoid), reads=(pt,), writes=(sgtok,))
            self.proj_tok(li, O_NVS, 128, ev_vs)
            self.proj_tok(li, O_NVW, 128, ev_vw)
            self.proj_tok(li, O_NG, 24, ev_sg)
            if "qT" in self.dbg_out:
                self.dump_featmajor_bf16(qT, [qtok], self.dbg_out["qT"])
            c.barrier()
            sB.close()
            if nstop == 2:
                return
            am = self.sb(st, "n_am", [128, NT, 32], F32)
            Esel = self.sb(st, "n_E", [32, NT, 128], BF16)
            wneg = self.sb(st, "n_wneg", [128, 128], BF16)
            cm = self.sb(st, "n_cm", [128, 512], BF16)
            negT = self.sb(st, "n_negT", [32, 2, 512], BF16)
            acc = self.sb(st, "n_acc", [128, 4, 512], F32)
            ybf = self.sb(st, "n_ybf", [128, 512], BF16)
            pT = [self.sb(st, f"n_pT{i}", [128, 512], BF16) for i in range(4)]
            pT_tok = [Tok() for _ in range(4)]
            imp = self.sb(st, "n_imp", [128, 4, 2, 32], F32)
            sm = self.sb(st, "n_sm", [128, 16], F32)
            impm = self.sb(st, "n_impm", [128, 32], F32)
            top8 = self.sb(st, "n_top8", [128, 8], F32)
            nselb = self.sb(st, "n_nsel", [128, 32], BF16)
            mtok, cmtok, negtok, acctok, ytok, imptok, tktok = [Tok() for _ in range(7)]
            smtok = [Tok(), Tok()]
            tA, tAt = self.nscr()
            tAv = tA[:].rearrange("p (t j) -> p t j", t=NT)
            c.op("pool", lambda e: e.memset(am[:], 0.0), writes=(mtok,))
            c.op("pool", lambda e: e.affine_select(am[:], am[:], [[128, NT], [-64, 32]], ALU.is_ge, -100.0, base=0,
                                                   channel_multiplier=1), reads=(mtok,), writes=(mtok,))
            c.op("pool", lambda e: e.memset(tA[:], 100.0), writes=(tAt,))
            c.op("pool", lambda e: e.affine_select(tAv, tAv, [[128, NT], [-64, 32]], ALU.is_ge, 0.0, base=0,
                                                   channel_multiplier=1), reads=(tAt,), writes=(tAt,))
            c.op("pool", lambda e: e.affine_select(tAv, tAv, [[-128, NT], [64, 32]], ALU.is_ge, 0.0, base=63,
                                                   channel_multiplier=-1), reads=(tAt,), writes=(tAt,))
            c.op("pool", lambda e: e.memset(tAv[:, :, 0:1], 100.0), reads=(tAt,), writes=(tAt,))
            c.op("pool", lambda e: e.tensor_tensor(am[:], am[:], tAv, ALU.add), reads=(tAt, mtok), writes=(mtok,))
            c.op("pool", lambda e: e.memset(Esel[:], 1.0), writes=(mtok,))
            c.op("pool", lambda e: e.affine_select(Esel[:], Esel[:], [[128, NT], [1, 128]], ALU.is_ge, 0.0, base=0,
                                                   channel_multiplier=-64), reads=(mtok,), writes=(mtok,))
            c.op("pool", lambda e: e.affine_select(Esel[:], Esel[:], [[-128, NT], [-1, 128]], ALU.is_ge, 0.0, base=63,
                                                   channel_multiplier=64), reads=(mtok,), writes=(mtok,))
            c.op("pool", lambda e: e.affine_select(wneg[:], self.zer_f[:], [[-1, 128]], ALU.is_gt, NEG, base=0,
                                                   channel_multiplier=1), reads=(ct,), writes=(mtok,))
            pi = [0]
            for qc in range(NTC):
                qs = slice(qc * 512, (qc + 1) * 512)
                c.op("pool", lambda e: e.memset(cm[:], 0.0), writes=(cmtok,))
                c.op("pool", lambda e: e.affine_select(cm[:], cm[:], [[1, 512]], ALU.is_ge, NEG, base=qc * 512 - 31,
                                                       channel_multiplier=-16), reads=(cmtok,), writes=(cmtok,))
                c.op("dve", lambda e: e.memset(imp[:], 0.0), writes=(imptok,))
                def cmp_stream(h):
                    g = h // 4
                    hp = slice((h % 2) * 64, (h % 2) * 64 + 64)
                    hc = h // 2
                    hcol = slice(h * 64, (h + 1) * 64)
                    sb_, stk = self.bank()
                    self.mm(sb_[0:127, :], kcT2[hp, g, 0:127], qT[hp, hc, qs], True, False, reads=(cmptok, qtok), writes=(stk,))
                    self.mm(sb_[0:127, :], self.ident_b[0:127, 0:127], cm[0:127, :], False, True, reads=(ct, cmtok),
                            writes=(stk,), skip_group_check=True)
                    p, ptk = pT[pi[0] % 4], pT_tok[pi[0] % 4]
                    pi[0] += 1
                    c.op("act", lambda e: e.activation(p[0:127, :], sb_[0:127, :], AF.Exp), reads=(stk,), writes=(ptk,))
                    yield
                    ob, ot = self.bank_acc()
                    O = ob[:, 0:388].rearrange("p (j d) -> p j d", j=4)
                    for j in range(4):
                        self.mm(O[:, j, :], p[0:127, j * 128:(j + 1) * 128], VCX[0:127, g, :], j == 0, True,
                                reads=(ptk, cmptok), writes=(ot,), skip_group_check=True)
                    yield
                    smt = smtok[h % 2]
                    for j in range(4):
                        qt = qc * 4 + j
                        o_ = (h % 2) * 8 + 2 * j
                        rcv, wv_ = sm[:, o_:o_ + 1], sm[:, o_ + 1:o_ + 2]
                        c.op("dve", lambda e: e.tensor_scalar(rcv, O[:, j, 64:65], 1e-30, None, ALU.max), reads=(ot,),
                             writes=(smt,))
                        c.op("dve", lambda e: e.reciprocal(rcv, rcv), reads=(smt,), writes=(smt,))
                        c.op("dve", lambda e: e.tensor_tensor(wv_, rcv, sg[:, qt, 3 * h:3 * h + 1], ALU.mult),
                             reads=(smt, sgtok), writes=(smt,))
                        c.op("dve", lambda e: e.tensor_scalar(acc[:, j, hcol], O[:, j, 0:64], wv_, None, ALU.mult),
                             reads=(ot, smt), writes=(acctok,))
                        c.op("dve", lambda e: e.scalar_tensor_tensor(imp[:, j, g, :], O[:, j, 65:97], rcv, imp[:, j, g, :],
                                                                     ALU.mult, ALU.add), reads=(ot, smt, imptok),
                             writes=(imptok,))
                self.run_streams([cmp_stream(h) for h in range(8)], 2)
                for j in range(4):
                    qt = qc * 4 + j
                    for g in range(2):
                        c.op("dve", lambda e: e.tensor_tensor(impm[:], imp[:, j, g, :], am[:, qt, :], ALU.add),
                             reads=(imptok, mtok), writes=(tktok,))
                        c.op("dve", lambda e: e.max(top8[:], impm[:]), reads=(tktok,), writes=(tktok,))
                        c.op("dve", lambda e: e.tensor_scalar(impm[:], impm[:], top8[:, 7:8], None, ALU.is_ge), reads=(tktok,),
                             writes=(tktok,))
                        c.op("dve", lambda e: e.tensor_scalar(nselb[:], impm[:], -1.0, 30000.0, ALU.add, ALU.mult),
                             reads=(tktok,), writes=(tktok,))
                        pb, pt = self.bank()
                        pbb = pb[:].bitcast(BF16)
                        c.op("pe", lambda e: e.transpose(pbb[0:32, 0:128], nselb[:], self.ident_b[:]), reads=(tktok, ct),
                             writes=(pt,))
                        c.op("act", lambda e: e.copy(negT[0:32, g, j * 128:(j + 1) * 128], pbb[0:32, 0:128]), reads=(pt,),
                             writes=(negtok,))
                if "negT" in self.dbg_out and qc == 1:
                    self.dump2d("negT", negT[:].rearrange("p g n -> p (g n)"), [negtok])
                def sw_stream(br, h):
                    g = h // 4
                    hp = slice((h % 2) * 64, (h % 2) * 64 + 64)
                    hc = h // 2
                    hcol = slice(h * 64, (h + 1) * 64)
                    ob, ot = self.bank_acc()
                    O = ob[:, 0:260].rearrange("p (j d) -> p j d", j=4)
                    first = True
                    kt0 = 0 if br == 1 else max(0, 4 * qc - 2)
                    for kt in range(kt0, 4 * qc + 4):
                        rel = kt - 4 * qc
                        jlo = max(0, rel)
                        jhi = 3 if br == 1 else min(3, rel + 2)
                        ncol = (jhi - jlo + 1) * 128
                        q0 = qc * 512 + jlo * 128
                        kl = slice(kt * 128, (kt + 1) * 128)
                        KT = ksT2 if br == 1 else kwT2
                        ktk = kstok if br == 1 else kwtok
                        extra = []
                        if br == 1:
                            extra.append((slice(0, ncol), Esel[0:32, kt, :], negT[0:32, g, jlo * 128:jlo * 128 + ncol],
                                          (mtok, negtok)))
                        if rel >= 0:
                            extra.append((slice(0, 128), self.ident_b[:], self.cneg_b[:], (ct,)))
                        if br == 2 and 0 <= rel + 2 <= 3:
                            o2 = (rel + 2 - jlo) * 128
                            extra.append((slice(o2, o2 + 128), self.ident_b[:], wneg[:], (ct, mtok)))
                        sb_, stk = self.bank()
                        self.mm(sb_[:, 0:ncol], KT[hp, g, kl], qT[hp, hc, q0:q0 + ncol], True, len(extra) == 0,
                                reads=(ktk, qtok), writes=(stk,))
                        for ei, (csl, lh, rh, rd) in enumerate(extra):
                            self.mm(sb_[:, csl], lh, rh, False, ei == len(extra) - 1, reads=rd, writes=(stk,),
                                    skip_group_check=True)
                        p, ptk = pT[pi[0] % 4], pT_tok[pi[0] % 4]
                        pi[0] += 1
                        c.op("act", lambda e: e.activation(p[:, 0:ncol], sb_[:, 0:ncol], AF.Exp), reads=(stk,), writes=(ptk,))
                        yield
                        VV = vs if br == 1 else vw
                        vtk_ = vstok if br == 1 else vwtok
                        for j in range(jlo, jhi + 1):
                            qt = qc * 4 + j
                            cs = slice((j - jlo) * 128, (j - jlo + 1) * 128)
                            self.mm(O[:, j, :], p[:, cs], VV[:, kt, g, :], first, kt == qt, reads=(ptk, vtk_), writes=(ot,),
                                    skip_group_check=True)
                            first = False
                    smt = smtok[h % 2]
                    for j in range(4):
                        qt = qc * 4 + j
                        o_ = (h % 2) * 8 + 2 * j
                        rcv, wv_ = sm[:, o_:o_ + 1], sm[:, o_ + 1:o_ + 2]
                        c.op("dve", lambda e: e.reciprocal(rcv, O[:, j, 64:65]), reads=(ot,), writes=(smt,))
                        c.op("dve", lambda e: e.tensor_tensor(wv_, rcv, sg[:, qt, 3 * h + br:3 * h + br + 1], ALU.mult),
                             reads=(smt, sgtok), writes=(smt,))
                        c.op("dve", lambda e: e.scalar_tensor_tensor(acc[:, j, hcol], O[:, j, 0:64], wv_, acc[:, j, hcol],
                                                                     ALU.mult, ALU.add), reads=(ot, smt, acctok),
                             writes=(acctok,))
                self.run_streams([sw_stream(br, h) for br in (1, 2) for h in range(8)], 2)
                for j in range(4):
                    t = qc * 4 + j
                    c.op("act", lambda e: e.copy(ybf[:], acc[:, j, :]), reads=(acctok,), writes=(ytok,))
                    pb, pt = self.bank()
                    pbb = pb[:].bitcast(BF16)
                    for jj in range(4):
                        c.op("pe", lambda e: e.transpose(pbb[:, jj * 128:(jj + 1) * 128], ybf[:, jj * 128:(jj + 1) * 128],
                                                         self.ident_b[:]), reads=(ytok, ct), writes=(pt,))
                    c.op("dve", lambda e: e.tensor_copy(self.yT[:, :, t * 128:(t + 1) * 128],
                                                        pbb[:, 0:512].rearrange("p (j n) -> p j n", j=4)),
                         reads=(pt,), writes=(self.yT_tok,))
            c.barrier()

    def fox(self, li):
        c = self.c
        with ExitStack() as st:
            qT = self.sb(st, "fx_qT", [128, 4, S], BF16)
            kT = self.sb(st, "fx_kT", [128, 4, S], BF16)
            V = self.sb(st, "fx_V", [128, NT, 8, 65], BF16)
            ytk = self.sb(st, "fx_y", [128, 4, 512], BF16)
            fl = self.sb(st, "fx_f", [128, NT, 8], F32)
            ncum = self.sb(st, "fx_ncum", [128, NT, 8], F32)
            nref = self.sb(st, "fx_nref", [128, NT, 8], F32)
            btab = self.sb(st, "fx_btab", [128, NT, NT, 8], F32)
            bfb = self.sb(st, "fx_bf", [128, 8], F32)
            qtok, ktok, vtok, ytok, ftok = Tok(), Tok(), Tok(), Tok(), Tok()
            self.aux_tok = Tok()
            self.proj_feat(li, O_FQ, 512, self.evac_featT(qT, qtok, 0.125))
            self.proj_feat(li, O_FK, 512, self.evac_featT(kT, ktok, 1.0))
            c.op("pool", lambda e: e.memset(V[:, :, :, 64:65], 1.0), writes=(vtok,))

            def evac_v(t, pb, pt):
                c.op("act", lambda e: e.copy(V[:, t, :, 0:64], pb[:, 0:512].rearrange("p (h d) -> p h d", h=8)),
                     reads=(pt,), writes=(vtok,))
            for half in range(4):
                def evac_vh(t, pb, pt, half=half):
                    c.op("act", lambda e: e.copy(V[:, t, half * 2:half * 2 + 2, 0:64],
                                                 pb[:, 0:128].rearrange("p (h d) -> p h d", h=2)),
                         reads=(pt,), writes=(vtok,))
                self.proj_tok(li, O_FV + half * 128, 128, evac_vh)
            c.dma(bfb[:], self.A["fox_b_f"][li:li + 1, :].partition_broadcast(128), writes=(ftok,))

            def evac_f(t, pb, pt):
                c.op("dve", lambda e: e.tensor_tensor(fl[:, t, :], pb[:, 0:8], bfb[:], ALU.add), reads=(pt, ftok),
                     writes=(ftok,))
            self.proj_tok(li, O_FF, 8, evac_f)
            flat = fl[:].rearrange("p t h -> p (t h)")
            c.op("act", lambda e: e.activation(flat, flat, AF.Exp, scale=-1.0), reads=(ftok,), writes=(ftok,))
            c.op("act", lambda e: e.activation(flat, flat, AF.Ln, bias=1.0), reads=(ftok,), writes=(ftok,))
            for t in range(NT):
                pb, pt = self.bank()
                for j in range(t):
                    self.mm(pb[:, 0:8], self.ones_f[:], fl[:, j, :], j == 0, False, reads=(ftok, self.const_tok),
                            writes=(pt,))
                self.mm(pb[:, 0:8], self.tri_f[:], fl[:, t, :], t == 0, True, reads=(ftok, self.const_tok), writes=(pt,))
                c.op("dve", lambda e: e.tensor_copy(ncum[:, t, :], pb[:, 0:8]), reads=(pt,), writes=(self.aux_tok,))
                if t > 0:
                    pb2, pt2 = self.bank()
                    for j in range(t):
                        self.mm(pb2[:, 0:8], self.ones_f[:], fl[:, j, :], j == 0, j == t - 1,
                                reads=(ftok, self.const_tok), writes=(pt2,))
                    c.op("dve", lambda e: e.tensor_copy(nref[:, t, :], pb2[:, 0:8]), reads=(pt2,), writes=(self.aux_tok,))
                else:
                    c.op("dve", lambda e: e.memset(nref[:, 0, :], 0.0), writes=(self.aux_tok,))
            for kt in range(NT):
                for qt in range(kt, NT):
                    c.op("pool", lambda e: e.tensor_tensor(btab[:, kt, qt, :], ncum[:, kt, :], nref[:, qt, :], ALU.subtract),
                         reads=(self.aux_tok,), writes=(self.aux_tok,))
            self.dump2d("ncum", ncum[:].rearrange("p t h -> p (t h)"), [self.aux_tok])
            self.dump2d("nref", nref[:].rearrange("p t h -> p (t h)"), [self.aux_tok])
            self.dump2d("fl", fl[:].rearrange("p t h -> p (t h)"), [ftok])
            self.attention(st, "fx", 8, qT, qtok, kT, ktok, V, vtok, ytk, ytok,
                           bias_fn=lambda h, kt, qt: btab[:, kt, qt, h:h + 1],
                           post_qc=lambda qc: self.ytok_to_yT(ytk, ytok, qc))
            c.barrier()

    def final_norm_store(self, out):
        self.store_tok_major(out, normed=True)

    def store_tok_major(self, out, normed):
        c = self.c
        L = len(self.layers)
        with ExitStack() as st:
            if normed:
                for tc in range(NTC):
                    ts = slice(tc * 512, (tc + 1) * 512)
                    pb, pt = self.bank()
                    for cc in range(KC):
                        sq, sqt = self.nscr()
                        c.op("act", lambda e: e.activation(sq[:], self.xT[:, cc, ts], AF.Square),
                             reads=(self.xT_tok[tc],), writes=(sqt,))
                        self.mm(pb[:], self.ones_f[:], sq[:], cc == 0, cc == KC - 1, reads=(sqt, self.const_tok),
                                writes=(pt,))
                    rs, rst = self.nscr()
                    c.op("dve", lambda e: e.tensor_scalar(rs[:], pb[:], 1.0 / D, EPS, ALU.mult, ALU.add), reads=(pt,),
                         writes=(rst,))
                    c.op("act", lambda e: e.activation(rs[:], rs[:], AF.Sqrt), reads=(rst,), writes=(rst,))
                    c.op("dve", lambda e: e.reciprocal(rs[:], rs[:]), reads=(rst,), writes=(rst,))
                    for cc in range(KC):
                        g = self.vecT[:, L * 72 + cc:L * 72 + cc + 1]
                        c.op("dve", lambda e: e.scalar_tensor_tensor(self.xT[:, cc, ts], self.xT[:, cc, ts], g, rs[:],
                                                                     ALU.mult, ALU.mult),
                             reads=(rst, self.vec_tok), writes=(self.xT_tok[tc],))
            os_ = [self.sb(st, f"os{i}", [128, D], F32) for i in range(2)]
            os_tok = [Tok(), Tok()]
            for t in range(NT):
                b = t % 2
                for half in range(2):
                    pb, pt = self.bank()
                    for j in range(4):
                        cc = half * 4 + j
                        c.op("pe", lambda e: e.transpose(pb[:, j * 128:(j + 1) * 128],
                                                         self.xT[:, cc, t * 128:(t + 1) * 128], self.ident_f[:]),
                             reads=(self.xT_tok[t // 4], self.const_tok), writes=(pt,), inc=(j == 3))
                    dst = os_[b][:, half * 512:(half + 1) * 512]
                    if half == 0:
                        c.op("dve", lambda e: e.tensor_copy(dst, pb[:]), reads=(pt,), writes=(os_tok[b],))
                    else:
                        c.op("act", lambda e: e.copy(dst, pb[:]), reads=(pt,), writes=(os_tok[b],))
                c.dma(out[t * 128:(t + 1) * 128, :], os_[b][:], reads=(os_tok[b],))
            c.barrier()

    def dump2d(self, name, ap, toks):
        if name in self.dbg_out:
            self.c.barrier()
            self.c.dma(self.dbg_out[name], ap, reads=tuple(toks), q="pool")
            self.c.barrier()

    def dump_featmajor_bf16(self, tT, toks, dst):
        c = self.c
        with ExitStack() as st:
            tmp = self.sb(st, "dmp", [128, S], F32)
            tt = Tok()
            for cc in range(tT.shape[1]):
                c.op("dve", lambda e: e.tensor_copy(tmp[:], tT[:, cc, :]), reads=tuple(toks), writes=(tt,))
                c.dma(dst[cc * 128:(cc + 1) * 128, :], tmp[:], reads=(tt,))
            c.barrier()


def _prep_inputs(inputs, layers, b):
    L = len(layers)
    vecs = np.zeros((L, 72, 128), np.float32)
    for i, l in enumerate(layers):
        vecs[i, 0:8] = inputs["norm_mix"][l].reshape(8, 128)
        vecs[i, 8:16] = inputs["norm_ff"][l].reshape(8, 128)
        vecs[i, 16:48] = inputs["b_gate"][l].reshape(32, 128)
    m = {
        "x": np.ascontiguousarray(inputs["x"][b]),
        "w_in": np.ascontiguousarray(inputs["w_in"][layers]),
        "vecs": vecs,
        "norm_final": np.ascontiguousarray(inputs["norm_final"].reshape(8, 128)),
        "positions": np.ascontiguousarray(inputs["positions"][b:b + 1]).astype(np.int32),
        "cmp_pos_k": np.ascontiguousarray(inputs["nsa_cmp_pos_k"][layers]),
        "cmp_pos_v": np.ascontiguousarray(inputs["nsa_cmp_pos_v"][layers]),
        "cmp_wk1": np.ascontiguousarray(inputs["nsa_cmp_wk1"][layers]),
        "cmp_wk2": np.ascontiguousarray(inputs["nsa_cmp_wk2"][layers]),
        "cmp_wv1": np.ascontiguousarray(inputs["nsa_cmp_wv1"][layers]),
        "cmp_wv2": np.ascontiguousarray(inputs["nsa_cmp_wv2"][layers]),
        "fox_b_f": np.ascontiguousarray(inputs["fox_b_f"][layers]),
        "gla_w_alpha": np.ascontiguousarray(inputs["gla_w_alpha"][layers]),
        "gla_b_alpha": np.ascontiguousarray(inputs["gla_b_alpha"][layers]),
        "gla_norm": np.ascontiguousarray(inputs["gla_norm"][layers]),
        "w_branch": np.ascontiguousarray(inputs["w_branch"][layers]),
        "w_out": np.ascontiguousarray(inputs["w_out"][layers]),
        "w_ff1": np.ascontiguousarray(inputs["w_ff1"][layers]),
        "w_ff2": np.ascontiguousarray(inputs["w_ff2"][layers]),
    }
    return m


def run(inputs, layers=(0, 1, 2, 3), debug=(), ncores=8, trace=False, stage=99):
    layers = list(layers)
    bld = Builder(layers, first=True, last=True, debug=debug, stage=stage)
    nc = bld.build()
    in_maps = [_prep_inputs(inputs, layers, b) for b in range(ncores)]
    res = run_bass_kernel_spmd(nc, in_maps, core_ids=list(range(ncores)), trace=trace)
    return res


def kernel(**inputs):
    inputs = {k: np.asarray(v) for k, v in inputs.items()}
    res = run(inputs)
    out = np.stack([np.asarray(r["out"]) for r in res.results], axis=0)
    return out.astype(np.float32)
```

```python
import numpy as np
from contextlib import ExitStack
import concourse.bass as bass
import concourse.mybir as mybir
from concourse.bass_utils import run_bass_kernel_spmd

F32 = mybir.dt.float32
BF16 = mybir.dt.bfloat16
I32 = mybir.dt.int32
ALU = mybir.AluOpType
AF = mybir.ActivationFunctionType
AX = mybir.AxisListType

S = 2048
D = 1024
NT = S // 128
NTC = S // 512
KC = D // 128
DEPTH = 4
DFF = 4096
D_IN = 10032
EPS = 1e-6
NEG = -30000.0

SPLITS = (512, 128, 128, 128, 128, 128, 128, 24, 512, 512, 512, 256, 256, 512, 16, 512, 512, 512, 512, 8, 4096)
OFFS = np.concatenate([[0], np.cumsum(SPLITS)]).tolist()
(O_NQ, O_NKC, O_NVC, O_NKS, O_NVS, O_NKW, O_NVW, O_NG, O_SQ, O_SK, O_SV, O_GQ, O_GK, O_GV, O_GA, O_GG,
 O_FQ, O_FK, O_FV, O_FF, O_GATE) = OFFS[:21]

EPOCH = 4000


class Tok:
    __slots__ = ("w", "r", "name")

    def __init__(self, name=""):
        self.w = None
        self.r = {}
        self.name = name


class Ctx:
    def __init__(self, nc, es):
        self.nc = nc
        self.es = es
        self.eng = dict(pe=nc.tensor, act=nc.scalar, dve=nc.vector, pool=nc.gpsimd, sp=nc.sync)
        self.cur = {}
        self.nsem = 0
        self.waited = {e: {} for e in self.eng}
        for e in ("pe", "act", "dve", "pool"):
            self.cur[e] = [self._newsem(e), 0]
        self.own = {e: set() for e in self.eng}
        for e in ("pe", "act", "dve", "pool"):
            self.own[e].add(id(self.cur[e][0]))
        self.dq = {}
        for q in ("sp", "act", "pool"):
            sems = [self._newsem("d" + q) for _ in range(8 if q == "sp" else 4)]
            self.dq[q] = dict(sems=sems, tgt=[0] * len(sems), i=0)
        self.all_dma = []

    def _newsem(self, name):
        self.nsem += 1
        return self.es.enter_context(self.nc.semaphore(f"s_{name}_{self.nsem}"))

    def _wait(self, e, deps):
        w = self.waited[e]
        for (sem, val) in deps:
            if val <= 0:
                continue
            k = id(sem)
            if e == "pe" and k in self.own["pe"]:
                continue
            if w.get(k, 0) >= val:
                continue
            self.eng[e].wait_ge(sem, val)
            w[k] = val

    @staticmethod
    def _deps(reads, writes):
        deps = []
        for t in reads:
            if t.w is not None:
                deps.append(t.w)
        for t in writes:
            if t.w is not None:
                deps.append(t.w)
            deps.extend(t.r.values())
        return deps

    @staticmethod
    def _record(stamp, reads, writes):
        sem, val = stamp
        for t in reads:
            t.r[id(sem)] = stamp
        for t in writes:
            t.w = stamp
            t.r = {}

    def op(self, e, fn, reads=(), writes=(), inc=True):
        self._wait(e, self._deps(reads, writes))
        ins = fn(self.eng[e])
        sem, cnt = self.cur[e]
        stamp = (sem, cnt + 1)
        if inc:
            ins.then_inc(sem, 1)
            self.cur[e][1] = cnt + 1
        self._record(stamp, reads, writes)
        if inc and cnt + 1 >= EPOCH:
            ns = self._newsem(e)
            self.own[e].add(id(ns))
            self.cur[e] = [ns, 0]
        return ins

    def dma(self, out, in_, reads=(), writes=(), q="sp", **kw):
        dq = self.dq[q]
        i = dq["i"] % len(dq["sems"])
        dq["i"] += 1
        sem = dq["sems"][i]
        deps = self._deps(reads, writes)
        deps.append((sem, dq["tgt"][i]))
        self._wait(q, deps)
        self.eng[q].dma_start(out=out, in_=in_, **kw).then_inc(sem, 16)
        dq["tgt"][i] += 16
        stamp = (sem, dq["tgt"][i])
        self._record(stamp, reads, writes)
        return stamp

    def barrier(self):
        stamps = []
        for e in ("pe", "act", "dve", "pool"):
            sem, cnt = self.cur[e]
            stamps.append((sem, cnt))
        for q, dq in self.dq.items():
            for s, t in zip(dq["sems"], dq["tgt"]):
                stamps.append((s, t))
        for e in ("pe", "act", "dve", "pool", "sp"):
            self._wait_all(e, stamps)

    def _wait_all(self, e, stamps):
        w = self.waited[e]
        for (sem, val) in stamps:
            if val <= 0:
                continue
            k = id(sem)
            if k in self.own.get(e, ()) and (e == "pe"):
                continue
            if w.get(k, 0) >= val:
                continue
            self.eng[e].wait_ge(sem, val)
            w[k] = val


class Builder:
    def __init__(self, layers, first, last, debug=(), stage=99):
        self.stage = stage
        self.layers = layers
        self.first = first
        self.last = last
        self.debug = debug
        self.nc = bass.Bass("TRN2", target_bir_lowering=False)
        self.dbg_out = {}

    def sb(self, st, name, shape, dt):
        self._uid = getattr(self, "_uid", 0) + 1
        return st.enter_context(self.nc.sbuf_tensor(f"{name}_{self._uid}", shape, dt))

    def dram_in(self, name, shape, dt=F32):
        return self.nc.dram_tensor(name, list(shape), dt, kind="ExternalInput").ap()

    def mm(self, out, lhsT, rhs, start, stop, reads, writes, inc=None, **kw):
        if inc is None:
            inc = True
        return self.c.op("pe", lambda e: e.matmul(out, lhsT, rhs, start=start, stop=stop, **kw),
                         reads=reads, writes=writes, inc=inc)

    def bank(self):
        i = self.bank_i % 6
        self.bank_i += 1
        return self.ps[i], self.ps_tok[i]

    def bank_acc(self):
        i = 6 + self.bank_j % 2
        self.bank_j += 1
        return self.ps[i], self.ps_tok[i]

    def wload(self, src3, kc, ncols, eng="pool"):
        i = self.w_i % 2
        self.w_i += 1
        stg, stok = self.wstg[i], self.wstg_tok[i]
        wb, wtok = self.wbf[i], self.wbf_tok[i]
        n = kc * ncols
        assert n <= self.WMAX
        sv = stg[:, 0:n].rearrange("p (c n) -> p c n", c=kc)
        wv = wb[:, 0:n].rearrange("p (c n) -> p c n", c=kc)
        self.c.dma(sv, src3, reads=(), writes=(stok,))
        self.c.op(eng, lambda e: e.tensor_copy(wb[:, 0:n], stg[:, 0:n]), reads=(stok,), writes=(wtok,))
        return wv, wtok

    def build(self):
        nc = self.nc
        L = len(self.layers)
        A = {}
        A["x"] = self.dram_in("x", [S, D])
        A["w_in"] = self.dram_in("w_in", [L, D, D_IN])
        A["vecs"] = self.dram_in("vecs", [L, 72, 128])
        A["norm_final"] = self.dram_in("norm_final", [8, 128])
        A["positions"] = self.dram_in("positions", [1, S], I32)
        A["cmp_pos_k"] = self.dram_in("cmp_pos_k", [L, 32, 64])
        A["cmp_pos_v"] = self.dram_in("cmp_pos_v", [L, 32, 64])
        A["cmp_wk1"] = self.dram_in("cmp_wk1", [L, 2048, 256])
        A["cmp_wk2"] = self.dram_in("cmp_wk2", [L, 256, 64])
        A["cmp_wv1"] = self.dram_in("cmp_wv1", [L, 2048, 256])
        A["cmp_wv2"] = self.dram_in("cmp_wv2", [L, 256, 64])
        A["fox_b_f"] = self.dram_in("fox_b_f", [L, 8])
        A["gla_w_alpha"] = self.dram_in("gla_w_alpha", [L, 16, 256])
        A["gla_b_alpha"] = self.dram_in("gla_b_alpha", [L, 256])
        A["gla_norm"] = self.dram_in("gla_norm", [L, 128])
        A["w_branch"] = self.dram_in("w_branch", [L, 4, 512, D])
        A["w_out"] = self.dram_in("w_out", [L, D, D])
        A["w_ff1"] = self.dram_in("w_ff1", [L, D, DFF])
        A["w_ff2"] = self.dram_in("w_ff2", [L, DFF, D])
        self.A = A
        out = nc.dram_tensor("out", [S, D], F32, kind="ExternalOutput").ap()
        for name, shape in self.debug:
            self.dbg_out[name] = nc.dram_tensor("dbg_" + name, list(shape), F32, kind="ExternalOutput").ap()

        with ExitStack() as es:
            self.es = es
            c = self.c = Ctx(nc, es)
            self.xT = self.sb(es, "xT", [128, KC, S], F32)
            self.hT = self.sb(es, "hT", [128, KC, S], BF16)
            self.xT_tok = [Tok(f"xT{i}") for i in range(NTC)]
            self.hT_tok = [Tok(f"hT{i}") for i in range(NTC)]
            self.ident_f = self.sb(es, "ident_f", [128, 128], F32)
            self.ident_b = self.sb(es, "ident_b", [128, 128], BF16)
            self.ones_f = self.sb(es, "ones_f", [128, 128], F32)
            self.const_tok = Tok("const")
            self.vecT = self.sb(es, "vecT", [128, L * 72 + 8], F32)
            self.vec_tok = Tok("vec")
            self.WMAX = 1024
            self.wstg = [self.sb(es, f"wstg{i}", [128, self.WMAX], F32) for i in range(2)]
            self.wbf = [self.sb(es, f"wbf{i}", [128, self.WMAX], BF16) for i in range(2)]
            self.wstg_tok = [Tok() for _ in range(2)]
            self.wbf_tok = [Tok() for _ in range(2)]
            self.w_i = 0
            self.scr = [self.sb(es, f"scr{i}", [128, 512], F32) for i in range(4)]
            self.scr_tok = [Tok() for _ in range(4)]
            self.scr_i = 0
            self.ps = [es.enter_context(nc.psum_tensor(f"ps{i}", [128, 512], F32)) for i in range(8)]
            self.ps_tok = [Tok(f"ps{i}") for i in range(8)]
            self.bank_i = 0
            self.bank_j = 0

            self.make_consts()
            if self.first:
                self.load_x()
            else:
                self.load_xT()
            for li in range(L):
                if self.stage >= 1:
                    self.layer(li)
            if self.last and self.stage >= 3:
                self.final_norm_store(out)
            else:
                self.store_xT(out)
            c.barrier()
        return nc

    def run_streams(self, gens, k=2):
        gens = iter(gens)
        active = []
        for g in gens:
            active.append(g)
            if len(active) == k:
                break
        while active:
            for g in list(active):
                try:
                    next(g)
                except StopIteration:
                    active.remove(g)
                    nxt = next(gens, None)
                    if nxt is not None:
                        active.append(nxt)

    def nscr(self):
        i = self.scr_i % len(self.scr)
        self.scr_i += 1
        return self.scr[i], self.scr_tok[i]

    def make_consts(self):
        c = self.c
        nc = self.nc
        ct = self.const_tok
        c.op("pool", lambda e: e.memset(self.ones_f[:], 1.0), writes=(ct,))
        c.op("pool", lambda e: e.affine_select(self.ident_f[:], self.ones_f[:], [[-1, 128]], ALU.is_equal, 0.0,
                                               base=0, channel_multiplier=1), reads=(ct,), writes=(ct,))
        c.op("pool", lambda e: e.tensor_copy(self.ident_b[:], self.ident_f[:]), reads=(ct,), writes=(ct,))
        self.tri_f = self.sb(self.es, "tri_f", [128, 128], F32)
        c.op("pool", lambda e: e.affine_select(self.tri_f[:], self.ones_f[:], [[1, 128]], ALU.is_ge, 0.0,
                                               base=0, channel_multiplier=-1), reads=(ct,), writes=(ct,))
        self.zer_f = self.sb(self.es, "zer_f", [128, 128], F32)
        self.cneg_b = self.sb(self.es, "cneg_b", [128, 128], BF16)
        c.op("pool", lambda e: e.memset(self.zer_f[:], 0.0), writes=(ct,))
        self.nones_f = self.sb(self.es, "nones_f", [128, 128], F32)
        self.ones_b = self.sb(self.es, "ones_b", [128, 128], BF16)
        self.nones_b = self.sb(self.es, "nones_b", [128, 128], BF16)
        self.cnegs_b = self.sb(self.es, "cnegs_b", [128, 128], BF16)
        self.ntri_b = self.sb(self.es, "ntri_b", [128, 128], BF16)
        c.op("pool", lambda e: e.memset(self.nones_f[:], -1.0), writes=(ct,))
        c.op("pool", lambda e: e.memset(self.ones_b[:], 1.0), writes=(ct,))
        c.op("pool", lambda e: e.memset(self.nones_b[:], -1.0), writes=(ct,))
        c.op("pool", lambda e: e.affine_select(self.cnegs_b[:], self.zer_f[:], [[1, 128]], ALU.is_gt, NEG,
                                               base=0, channel_multiplier=-1), reads=(ct,), writes=(ct,))
        c.op("pool", lambda e: e.affine_select(self.ntri_b[:], self.nones_f[:], [[-1, 128]], ALU.is_ge, 0.0,
                                               base=0, channel_multiplier=1), reads=(ct,), writes=(ct,))
        c.op("pool", lambda e: e.affine_select(self.cneg_b[:], self.zer_f[:], [[1, 128]], ALU.is_ge, NEG,
                                               base=0, channel_multiplier=-1), reads=(ct,), writes=(ct,))
        L = len(self.layers)
        nrow = L * 72 + 8
        with ExitStack() as st:
            tmp = self.sb(st, "vtmp", [128, 4, 128], F32)
            tt = Tok()
            r0 = 0
            chunks = []
            while r0 < nrow:
                n = min(128, nrow - r0)
                chunks.append((r0, n))
                r0 += n
            for ci, (r0, n) in enumerate(chunks):
                a = r0
                while a < r0 + n:
                    if a < L * 72:
                        b = min(r0 + n, L * 72)
                        src = self.A["vecs"].rearrange("l r p -> (l r) p")[a:b, :]
                    else:
                        b = r0 + n
                        src = self.A["norm_final"][a - L * 72:b - L * 72, :]
                    c.dma(tmp[a - r0:b - r0, ci, :], src, writes=(tt,))
                    a = b
                pb, pt = self.bank()
                c.op("pe", lambda e: e.transpose(pb[:, 0:n], tmp[0:n, ci, :], self.ident_f[0:n, 0:n]),
                     reads=(tt, ct), writes=(pt,))
                c.op("dve", lambda e: e.tensor_copy(self.vecT[:, r0:r0 + n], pb[:, 0:n]), reads=(pt,),
                     writes=(self.vec_tok,))
            c.barrier()

    def vcol(self, li, kind, j):
        base = li * 72 + {"norm_mix": 0, "norm_ff": 8, "b_gate": 16}[kind]
        return self.vecT[:, base + j:base + j + 1]

    def load_x(self):
        c = self.c
        x = self.A["x"]
        with ExitStack() as st:
            xs = [self.sb(st, f"xs{i}", [128, D], F32) for i in range(2)]
            xs_tok = [Tok(), Tok()]
            for t in range(NT):
                b = t % 2
                c.dma(xs[b][:], x[t * 128:(t + 1) * 128, :], writes=(xs_tok[b],))
                for half in range(2):
                    pb, pt = self.bank()
                    for j in range(4):
                        cc = half * 4 + j
                        c.op("pe", lambda e: e.transpose(pb[:, j * 128:(j + 1) * 128], xs[b][:, cc * 128:(cc + 1) * 128],
                                                         self.ident_f[:]),
                             reads=(xs_tok[b], self.const_tok), writes=(pt,), inc=(j == 3))
                    dst = self.xT[:, half * 4:half * 4 + 4, t * 128:(t + 1) * 128]
                    src = pb[:].rearrange("p (j n) -> p j n", j=4)
                    eng = "dve" if half == 0 else "act"
                    if eng == "dve":
                        c.op("dve", lambda e: e.tensor_copy(dst, src), reads=(pt,), writes=(self.xT_tok[t // 4],))
                    else:
                        c.op("act", lambda e: e.copy(dst, src), reads=(pt,), writes=(self.xT_tok[t // 4],))
            c.barrier()

    def load_xT(self):
        raise NotImplementedError

    def store_xT(self, out):
        self.store_tok_major(out, normed=False)

    def rmsnorm_to_hT(self, gcol):
        c = self.c
        for tc in range(NTC):
            ts = slice(tc * 512, (tc + 1) * 512)
            pb, pt = self.bank()
            for cc in range(KC):
                sq, sqt = self.nscr()
                c.op("act", lambda e: e.activation(sq[:], self.xT[:, cc, ts], AF.Square),
                     reads=(self.xT_tok[tc],), writes=(sqt,))
                self.mm(pb[:], self.ones_f[:], sq[:], cc == 0, cc == KC - 1, reads=(sqt, self.const_tok), writes=(pt,))
            rs, rst = self.nscr()
            c.op("dve", lambda e: e.tensor_scalar(rs[:], pb[:], 1.0 / D, EPS, ALU.mult, ALU.add), reads=(pt,),
                 writes=(rst,))
            c.op("act", lambda e: e.activation(rs[:], rs[:], AF.Sqrt), reads=(rst,), writes=(rst,))
            c.op("dve", lambda e: e.reciprocal(rs[:], rs[:]), reads=(rst,), writes=(rst,))
            for cc in range(KC):
                c.op("dve", lambda e: e.scalar_tensor_tensor(self.hT[:, cc, ts], self.xT[:, cc, ts], gcol(cc), rs[:],
                                                             ALU.mult, ALU.mult),
                     reads=(self.xT_tok[tc], rst, self.vec_tok), writes=(self.hT_tok[tc],))

    def layer(self, li):
        self.rmsnorm_to_hT(lambda cc: self.vcol(li, "norm_mix", cc))
        if "hT" in self.dbg_out and li == 0:
            self.dump_featmajor_bf16(self.hT, self.hT_tok, self.dbg_out["hT"])
        if self.stage >= 4:
            self.yT = self.sb(self.es, f"yT{li}", [128, 4, S], BF16) if not hasattr(self, "yT") else self.yT
            self.yT_tok = Tok("yT")
            if self.stage >= 8:
                self.nsa(li)
                if "ynsa" in self.dbg_out and li == 0:
                    self.dump_featmajor_bf16(self.yT, [self.yT_tok], self.dbg_out["ynsa"])
                self.combine(li, 0)
            if self.stage == 8:
                return
            if self.stage >= 7:
                self.gla(li)
                if "ygla" in self.dbg_out and li == 0:
                    self.dump_featmajor_bf16(self.yT, [self.yT_tok], self.dbg_out["ygla"])
                self.combine(li, 2)
            if self.stage >= 6 and self.stage != 7:
                self.sbmix(li)
                if "ysb" in self.dbg_out and li == 0:
                    self.dump_featmajor_bf16(self.yT, [self.yT_tok], self.dbg_out["ysb"])
                self.combine(li, 1)
            if self.stage == 7:
                return
            self.fox(li)
            if "yT" in self.dbg_out and li == 0:
                self.dump_featmajor_bf16(self.yT, [self.yT_tok], self.dbg_out["yT"])
            if self.stage >= 5:
                self.combine(li, 3)
        if self.stage >= 2:
            self.rmsnorm_to_hT(lambda cc: self.vcol(li, "norm_ff", cc))
            self.ffn(li)

    def ffn(self, li):
        c = self.c
        w1 = self.A["w_ff1"][li].rearrange("(c p) n -> p c n", p=128)
        w2 = self.A["w_ff2"][li].rearrange("(f p) n -> p f n", p=128)
        G = 4
        with ExitStack() as st:
            aT = [self.sb(st, f"aT{i}", [128, G, S], BF16) for i in range(2)]
            aT_tok = [Tok(), Tok()]
            import os
            for g in range(int(os.environ.get('FFN_G', DFF // (128 * G)))):
                ab, abt = aT[g % 2], aT_tok[g % 2]
                for half in range(G):
                    f0 = g * G + half
                    wv, wt = self.wload(w1[:, :, f0 * 128:(f0 + 1) * 128], KC, 128)
                    for j in range(1):
                        for tc in range(NTC):
                            ts = slice(tc * 512, (tc + 1) * 512)
                            pb, pt = self.bank()
                            for cc in range(KC):
                                self.mm(pb[:], wv[:, cc, j * 128:(j + 1) * 128], self.hT[:, cc, ts], cc == 0, cc == KC - 1,
                                        reads=(wt, self.hT_tok[tc]), writes=(pt,))
                            r, rt = self.nscr()
                            c.op("act", lambda e: e.activation(r[:], pb[:], AF.Relu), reads=(pt,), writes=(rt,))
                            c.op("dve", lambda e: e.tensor_tensor(ab[:, half, ts], r[:], r[:], ALU.mult),
                                 reads=(rt,), writes=(abt,))
                for dh in range(4):
                    wv, wt = self.wload(w2[:, g * G:(g + 1) * G, dh * 256:(dh + 1) * 256], G, 256)
                    for j in range(2):
                        dt_ = dh * 2 + j
                        for tc in range(NTC):
                            ts = slice(tc * 512, (tc + 1) * 512)
                            pb, pt = self.bank()
                            for f in range(G):
                                self.mm(pb[:], wv[:, f, j * 128:(j + 1) * 128], ab[:, f, ts], f == 0, f == G - 1,
                                        reads=(wt, abt), writes=(pt,))
                            c.op("dve", lambda e: e.tensor_tensor(self.xT[:, dt_, ts], self.xT[:, dt_, ts], pb[:], ALU.add),
                                 reads=(pt,), writes=(self.xT_tok[tc],))
            c.barrier()


    def proj_feat(self, li, col0, ncols, evac):
        w = self.A["w_in"][li].rearrange("(c p) n -> p c n", p=128)
        n0 = 0
        while n0 < ncols:
            nn = min(128, ncols - n0)
            wv, wt = self.wload(w[:, :, col0 + n0:col0 + n0 + nn], KC, nn)
            for j in range((nn + 127) // 128):
                m = min(128, nn - j * 128)
                for tc in range(NTC):
                    ts = slice(tc * 512, (tc + 1) * 512)
                    pb, pt = self.bank()
                    for cc in range(KC):
                        self.mm(pb[0:m, :], wv[:, cc, j * 128:j * 128 + m], self.hT[:, cc, ts], cc == 0, cc == KC - 1,
                                reads=(wt, self.hT_tok[tc]), writes=(pt,))
                    evac((n0 + j * 128) // 128, tc, pb, pt)
            n0 += nn

    def proj_tok(self, li, col0, ncols, evac):
        w = self.A["w_in"][li].rearrange("(c p) n -> p c n", p=128)
        wv, wt = self.wload(w[:, :, col0:col0 + ncols], KC, ncols)
        for t in range(NT):
            pb, pt = self.bank()
            for cc in range(KC):
                self.mm(pb[:, 0:ncols], self.hT[:, cc, t * 128:(t + 1) * 128], wv[:, cc, :], cc == 0, cc == KC - 1,
                        reads=(wt, self.hT_tok[t // 4]), writes=(pt,))
            evac(t, pb, pt)

    def evac_featT(self, dst, dtok, scale=1.0):
        c = self.c
        cnt = [0]

        def f(mt, tc, pb, pt):
            ts = slice(tc * 512, (tc + 1) * 512)
            cnt[0] += 1
            if cnt[0] % 2 == 0:
                c.op("dve", lambda e: e.tensor_scalar(dst[:, mt, ts], pb[:], scale, None, ALU.mult), reads=(pt,),
                     writes=(dtok,))
            else:
                c.op("act", lambda e: e.activation(dst[:, mt, ts], pb[:], AF.Copy, scale=scale), reads=(pt,),
                     writes=(dtok,))
        return f

    def attention(self, st, name, nheads, qT, qtok, kT, ktok, V, vtok, ytok_t, ytok_tok, bias_fn=None, ycol0=0, post_qc=None):
        c = self.c
        pT = [self.sb(st, f"{name}_pT{i}", [128, 512], BF16) for i in range(4)]
        pT_tok = [Tok() for _ in range(4)]
        rc = self.sb(st, f"{name}_rc", [128, 8], F32)
        rc_tok = [Tok(), Tok()]
        pi = [0]

        def head_stream(qc, h):
            hp = slice((h % 2) * 64, (h % 2) * 64 + 64)
            hc = h // 2
            ob, ot = self.bank_acc()
            O = ob[:, 0:260].rearrange("p (j d) -> p j d", j=4)
            nkt = 4 * qc + 4
            for kt in range(nkt):
                j0 = max(0, kt - 4 * qc)
                q0 = qc * 512 + j0 * 128
                ncol = 512 - j0 * 128
                sb_, stk = self.bank()
                diag = kt >= 4 * qc
                self.mm(sb_[:, 0:ncol], kT[hp, hc, kt * 128:(kt + 1) * 128], qT[hp, hc, q0:q0 + ncol], True, not diag,
                        reads=(ktok, qtok), writes=(stk,))
                if diag:
                    self.mm(sb_[:, 0:128], self.ident_b[:], self.cneg_b[:], False, True,
                            reads=(self.const_tok,), writes=(stk,), skip_group_check=True)
                p, ptk = pT[pi[0] % 4], pT_tok[pi[0] % 4]
                pi[0] += 1
                for half in range(2):
                    jl, jh = max(j0, 2 * half), 2 * half + 1
                    if jl > jh:
                        continue
                    cs = slice((jl - j0) * 128, (jh - j0 + 1) * 128)
                    b = bias_fn(h, kt, qc * 4 + 2 * half + 1) if bias_fn is not None else 0.0
                    c.op("act", lambda e: e.activation(p[:, cs], sb_[:, cs], AF.Exp, bias=b), reads=(stk, self.aux_tok),
                         writes=(ptk,))
                yield
                for j in range(j0, 4):
                    qt = qc * 4 + j
                    cs = slice((j - j0) * 128, (j - j0 + 1) * 128)
                    self.mm(O[:, j, :], p[:, cs], V[:, kt, h, :], kt == 0 and j == 0, kt == qt, reads=(ptk, vtok), writes=(ot,),
                            skip_group_check=True)
            rct = rc_tok[h % 2]
            for j in range(4):
                rcj = rc[:, (h % 2) * 4 + j:(h % 2) * 4 + j + 1]
                c.op("dve", lambda e: e.reciprocal(rcj, O[:, j, 64:65]), reads=(ot,), writes=(rct,))
                c.op("dve", lambda e: e.tensor_scalar(ytok_t[:, j, ycol0 + h * 64:ycol0 + (h + 1) * 64], O[:, j, 0:64],
                                                      rcj, None, ALU.mult),
                     reads=(ot, rct), writes=(ytok_tok,))

        for qc in range(NTC):
            self.run_streams([head_stream(qc, h) for h in range(nheads)], 2)
            if post_qc is not None:
                post_qc(qc)

    def ytok_to_yT(self, ytok_t, ytok_tok, qc):
        c = self.c
        for tl in range(4):
            t = qc * 4 + tl
            pb, pt = self.bank()
            pbb = pb[:].bitcast(BF16)
            for j in range(4):
                c.op("pe", lambda e: e.transpose(pbb[:, j * 128:(j + 1) * 128], ytok_t[:, tl, j * 128:(j + 1) * 128],
                                                 self.ident_b[:]),
                     reads=(ytok_tok, self.const_tok), writes=(pt,))
            c.op("dve", lambda e: e.tensor_copy(self.yT[:, :, t * 128:(t + 1) * 128],
                                                pbb[:, 0:512].rearrange("p (j n) -> p j n", j=4)),
                 reads=(pt,), writes=(self.yT_tok,))


    def combine(self, li, bi):
        c = self.c
        wg = self.A["w_in"][li].rearrange("(c p) n -> p c n", p=128)
        wb = self.A["w_branch"][li, bi].rearrange("(c p) n -> p c n", p=128)
        wo = self.A["w_out"][li].rearrange("(c p) n -> p c n", p=128)
        with ExitStack() as st:
            mT = self.sb(st, "mT", [128, KC, S], BF16)
            mtok = Tok()
            for dt_ in range(KC):
                g0 = O_GATE + bi * D + dt_ * 128
                wgv, wgt = self.wload(wg[:, :, g0:g0 + 128], KC, 128)
                wbv, wbt = self.wload(wb[:, :, dt_ * 128:(dt_ + 1) * 128], 4, 128)
                bcol = self.vcol(li, "b_gate", bi * 8 + dt_)
                for tc in range(NTC):
                    ts = slice(tc * 512, (tc + 1) * 512)
                    pa, pat = self.bank()
                    for cc in range(KC):
                        self.mm(pa[:], wgv[:, cc, :], self.hT[:, cc, ts], cc == 0, cc == KC - 1,
                                reads=(wgt, self.hT_tok[tc]), writes=(pat,))
                    pb, pbt = self.bank()
                    for cc in range(4):
                        self.mm(pb[:], wbv[:, cc, :], self.yT[:, cc, ts], cc == 0, cc == 3,
                                reads=(wbt, self.yT_tok), writes=(pbt,))
                    sg, sgt = self.nscr()
                    c.op("act", lambda e: e.activation(sg[:], pa[:], AF.Sigmoid, bias=bcol), reads=(pat, self.vec_tok),
                         writes=(sgt,))
                    c.op("dve", lambda e: e.tensor_tensor(mT[:, dt_, ts], sg[:], pb[:], ALU.mult), reads=(sgt, pbt),
                         writes=(mtok,))
            for do in range(KC):
                wov, wot = self.wload(wo[:, :, do * 128:(do + 1) * 128], KC, 128)
                for tc in range(NTC):
                    ts = slice(tc * 512, (tc + 1) * 512)
                    pb, pbt = self.bank()
                    for cc in range(KC):
                        self.mm(pb[:], wov[:, cc, :], mT[:, cc, ts], cc == 0, cc == KC - 1, reads=(wot, mtok), writes=(pbt,))
                    c.op("dve", lambda e: e.tensor_tensor(self.xT[:, do, ts], self.xT[:, do, ts], pb[:], ALU.add),
                         reads=(pbt,), writes=(self.xT_tok[tc],))
            c.barrier()


    def sbmix(self, li):
        c = self.c
        with ExitStack() as st:
            qT = self.sb(st, "sb_qT", [128, 4, S], BF16)
            kT = self.sb(st, "sb_kT", [128, 4, S], BF16)
            V = self.sb(st, "sb_V", [128, NT, 8, 64], BF16)
            ytk = self.sb(st, "sb_y", [128, 4, 512], BF16)
            spb = [[self.sb(st, f"sb_sp{k}{i}", [128, 512], BF16) for i in range(2)] for k in range(2)]
            spt = [[Tok(), Tok()] for k in range(2)]
            pT = [[self.sb(st, f"sb_pT{k}{i}", [128, 512], BF16) for i in range(2)] for k in range(2)]
            pTt = [[Tok(), Tok()] for k in range(2)]
            sufs = [(self.sb(st, f"sb_suf{k}", [1, 512], F32), self.sb(st, f"sb_sufh{k}", [1, 512], BF16),
                     self.sb(st, f"sb_sufl{k}", [1, 512], BF16), Tok()) for k in range(2)]
            qtok, ktok, vtok, ytok = Tok(), Tok(), Tok(), Tok()
            self.proj_feat(li, O_SQ, 512, self.evac_featT(qT, qtok, 0.125))
            self.proj_feat(li, O_SK, 512, self.evac_featT(kT, ktok, 1.0))
            for q4 in range(4):
                def evac_vh(t, pb, pt, q4=q4):
                    c.op("act", lambda e: e.copy(V[:, t, q4 * 2:q4 * 2 + 2, :],
                                                 pb[:, 0:128].rearrange("p (h d) -> p h d", h=2)),
                         reads=(pt,), writes=(vtok,))
                self.proj_tok(li, O_SV + q4 * 128, 128, evac_vh)
            def head_stream(qc, h):
                s_ = h % 2
                hp = slice((h % 2) * 64, (h % 2) * 64 + 64)
                hc = h // 2
                ob, ot = self.bank_acc()
                O = ob[:, 0:256].rearrange("p (j d) -> p j d", j=4)
                nkt = 4 * qc + 4
                suf, sufh, sufl, suft = sufs[s_]
                c.op("dve", lambda e: e.memset(suf[:], 0.0), writes=(suft,))
                c.op("dve", lambda e: e.memset(sufh[:], 0.0), writes=(suft,))
                c.op("dve", lambda e: e.memset(sufl[:], 0.0), writes=(suft,))
                first = True
                ti = 0
                for kt in range(nkt - 1, -1, -1):
                    j0 = max(0, kt - 4 * qc)
                    q0 = qc * 512 + j0 * 128
                    ncol = 512 - j0 * 128
                    diag = kt >= 4 * qc
                    ksl = kT[hp, hc, kt * 128:(kt + 1) * 128]
                    qsl = qT[hp, hc, q0:q0 + ncol]
                    pa, pat = self.bank()
                    self.mm(pa[:, 0:ncol], ksl, qsl, True, not diag, reads=(ktok, qtok), writes=(pat,))
                    if diag:
                        self.mm(pa[:, 0:128], self.ident_b[:], self.cnegs_b[:], False, True, reads=(self.const_tok,),
                                writes=(pat,), skip_group_check=True)
                    e_, et = self.nscr()
                    sp, spk = spb[s_][ti % 2], spt[s_][ti % 2]
                    p, ptk = pT[s_][ti % 2], pTt[s_][ti % 2]
                    ti += 1
                    c.op("act", lambda e: e.activation(e_[:, 0:ncol], pa[:, 0:ncol], AF.Exp), reads=(pat,), writes=(et,))
                    c.op("act", lambda e: e.activation(sp[:, 0:ncol], e_[:, 0:ncol], AF.Ln, bias=1.0), reads=(et,),
                         writes=(spk,))
                    yield
                    pb, pbt = self.bank()
                    self.mm(pb[:, 0:ncol], ksl, qsl, True, False, reads=(ktok, qtok), writes=(pbt,))
                    if diag:
                        self.mm(pb[:, 0:128], self.ident_b[:], self.cnegs_b[:], False, False, reads=(self.const_tok,),
                                writes=(pbt,), skip_group_check=True)
                    self.mm(pb[:, 0:ncol], self.nones_b[0:1, :], sufh[0:1, 512 - ncol:512], False, False,
                            reads=(suft, self.const_tok), writes=(pbt,), skip_group_check=True)
                    self.mm(pb[:, 0:ncol], self.nones_b[0:1, :], sufl[0:1, 512 - ncol:512], False, False,
                            reads=(suft, self.const_tok), writes=(pbt,), skip_group_check=True)
                    self.mm(pb[:, 0:ncol], self.ntri_b[:], sp[:, 0:ncol], False, True, reads=(spk, self.const_tok),
                            writes=(pbt,), skip_group_check=True)
                    if kt > 0:
                        pc, pct = self.bank()
                        self.mm(pc[0:1, 0:ncol], self.ones_b[:, 0:1], sp[:, 0:ncol], True, True,
                                reads=(spk, self.const_tok), writes=(pct,))
                        sl = slice(512 - ncol, 512)
                        c.op("dve", lambda e: e.tensor_tensor(suf[0:1, sl], suf[0:1, sl], pc[0:1, 0:ncol], ALU.add),
                             reads=(pct,), writes=(suft,))
                        c.op("dve", lambda e: e.tensor_copy(sufh[0:1, sl], suf[0:1, sl]), reads=(suft,), writes=(suft,))
                        c.op("dve", lambda e: e.tensor_tensor(sufl[0:1, sl], suf[0:1, sl], sufh[0:1, sl], ALU.subtract),
                             reads=(suft,), writes=(suft,))
                    c.op("act", lambda e: e.activation(p[:, 0:ncol], pb[:, 0:ncol], AF.Exp), reads=(pbt,), writes=(ptk,))
                    yield
                    for j in range(j0, 4):
                        cs = slice((j - j0) * 128, (j - j0 + 1) * 128)
                        self.mm(O[:, j, :], p[:, cs], V[:, kt, h, :], first, kt == 0, reads=(ptk, vtok), writes=(ot,),
                                skip_group_check=True)
                        first = False
                for j in range(4):
                    c.op("dve", lambda e: e.tensor_copy(ytk[:, j, h * 64:(h + 1) * 64], O[:, j, :]), reads=(ot,),
                         writes=(ytok,))

            for qc in range(NTC):
                self.run_streams([head_stream(qc, h) for h in range(8)], 2)
                self.ytok_to_yT(ytk, ytok, qc)
            c.barrier()

    def gla(self, li):
        c = self.c
        ct = self.const_tok
        with ExitStack() as st:
            qeT = self.sb(st, "g_qe", [64, 4, S], BF16)
            keT = self.sb(st, "g_ke", [64, 4, S], BF16)
            k2 = self.sb(st, "g_k2", [128, NT, 256], BF16)
            vtk = self.sb(st, "g_v", [128, NT, 512], BF16)
            gnorm = self.sb(st, "g_norm", [128, 1], F32)
            dec = self.sb(st, "g_dec", [64, 4, 32], F32)
            tblk = self.sb(st, "g_tblk", [128, 128], F32)
            sp_ = ExitStack()
            alrT = self.sb(sp_, "g_alr", [16, S], BF16)
            balb = self.sb(sp_, "g_bal", [128, 256], F32)
            wal_f = self.sb(sp_, "g_walf", [16, 256], F32)
            wal_b = self.sb(sp_, "g_walb", [16, 256], BF16)
            n16 = self.sb(sp_, "g_n16", [128, 128], F32)
            m1 = self.sb(sp_, "g_m1", [128, 128], F32)
            m2 = self.sb(sp_, "g_m2", [128, 128], F32)
            att_tok = [Tok(), Tok()]
            mtok, qtok, ktok, vtok, k2tok, atok, ptok, stok, otok, ontok = [Tok() for _ in range(10)]
            c.op("pool", lambda e: e.memset(n16[:], -1.0 / 16.0), writes=(mtok,))
            c.op("pool", lambda e: e.affine_select(m1[:], n16[:], [[1, 128]], ALU.is_ge, 0.0, base=0, channel_multiplier=-1),
                 reads=(mtok,), writes=(mtok,))
            c.op("pool", lambda e: e.memset(m1[0:64, 64:128], 0.0), reads=(mtok,), writes=(mtok,))
            c.op("pool", lambda e: e.affine_select(m2[:], n16[:], [[-1, 128]], ALU.is_gt, 0.0, base=0, channel_multiplier=1),
                 reads=(mtok,), writes=(mtok,))
            c.op("pool", lambda e: e.memset(m2[64:128, 0:64], 0.0), reads=(mtok,), writes=(mtok,))
            c.op("pool", lambda e: e.tensor_copy(tblk[:], self.tri_f[:]), reads=(ct, mtok), writes=(mtok,))
            c.op("pool", lambda e: e.memset(tblk[0:64, 64:128], 0.0), reads=(mtok,), writes=(mtok,))
            c.dma(balb[:], self.A["gla_b_alpha"][li:li + 1, :].partition_broadcast(128), writes=(ptok,))
            c.dma(wal_f[:], self.A["gla_w_alpha"][li], writes=(ptok,))
            c.dma(gnorm[:], self.A["gla_norm"][li].rearrange("(p o) -> p o", o=1), writes=(ptok,))
            c.op("pool", lambda e: e.tensor_copy(wal_b[:], wal_f[:]), reads=(ptok,), writes=(ptok,))
            for h in range(4):
                def ev_q(mt, tc, pb, pt, h=h):
                    ts = slice(tc * 512, (tc + 1) * 512)
                    c.op("act", lambda e: e.activation(qeT[0:64, h, ts], pb[0:64, :], AF.Copy, scale=0.125), reads=(pt,),
                         writes=(qtok,))

                def ev_k(mt, tc, pb, pt, h=h):
                    ts = slice(tc * 512, (tc + 1) * 512)
                    c.op("dve", lambda e: e.tensor_copy(keT[0:64, h, ts], pb[0:64, :]), reads=(pt,), writes=(ktok,))
                self.proj_feat(li, O_GQ + h * 64, 64, ev_q)
                self.proj_feat(li, O_GK + h * 64, 64, ev_k)

            def ev_a(mt, tc, pb, pt):
                ts = slice(tc * 512, (tc + 1) * 512)
                c.op("act", lambda e: e.copy(alrT[0:16, ts], pb[0:16, :]), reads=(pt,), writes=(atok,))
            self.proj_feat(li, O_GA, 16, ev_a)
            for i in range(4):
                def ev_v(t, pb, pt, i=i):
                    c.op("act", lambda e: e.copy(vtk[:, t, i * 128:(i + 1) * 128], pb[:, 0:128]), reads=(pt,), writes=(vtok,))
                self.proj_tok(li, O_GV + i * 128, 128, ev_v)
            import os
            gstop = int(os.environ.get("GLA_STOP", "99"))
            if gstop == 1:
                c.barrier()
                return
            for t in range(NT):
                tl = slice(t * 128, (t + 1) * 128)
                pa, pat = self.bank()
                self.mm(pa[:, 0:256], alrT[0:16, tl], wal_b[0:16, :], True, True, reads=(atok, ptok), writes=(pat,))
                xs, xst = self.nscr()
                c.op("dve", lambda e: e.tensor_tensor(xs[:, 0:256], pa[:, 0:256], balb[:], ALU.add), reads=(pat, ptok),
                     writes=(xst,))
                c.op("act", lambda e: e.activation(xs[:, 0:256], xs[:, 0:256], AF.Exp, scale=-1.0), reads=(xst,), writes=(xst,))
                c.op("act", lambda e: e.activation(xs[:, 0:256], xs[:, 0:256], AF.Ln, bias=1.0), reads=(xst,), writes=(xst,))
                pw, pwt = self.bank()
                self.mm(pw[:, 0:256], m2[:], xs[:, 0:256], True, True, reads=(mtok, xst), writes=(pwt,))
                c.op("act", lambda e: e.activation(k2[:, t, :], pw[:, 0:256], AF.Exp), reads=(pwt,), writes=(k2tok,))
                pbT, pbTt = self.bank()
                for h in range(4):
                    self.mm(pbT[0:64, h * 128:(h + 1) * 128], xs[:, h * 64:(h + 1) * 64], m1[:], h == 0, h == 3,
                            reads=(mtok, xst), writes=(pbTt,), skip_group_check=True)
                ebp, ebpt = self.nscr()
                ebn, ebnt = self.nscr()
                c.op("act", lambda e: e.activation(ebp[0:64, :], pbT[0:64, :], AF.Exp), reads=(pbTt,), writes=(ebpt,))
                c.op("act", lambda e: e.activation(ebn[0:64, :], pbT[0:64, :], AF.Exp, scale=-1.0), reads=(pbTt,), writes=(ebnt,))
                c.op("dve", lambda e: e.tensor_tensor(qeT[0:64, :, tl], qeT[0:64, :, tl],
                                                      ebp[0:64, :].rearrange("p (h n) -> p h n", h=4), ALU.mult),
                     reads=(ebpt,), writes=(qtok,))
                c.op("dve", lambda e: e.tensor_tensor(keT[0:64, :, tl], keT[0:64, :, tl],
                                                      ebn[0:64, :].rearrange("p (h n) -> p h n", h=4), ALU.mult),
                     reads=(ebnt,), writes=(ktok,))
                c.op("dve", lambda e: e.tensor_copy(dec[0:64, :, 2 * t:2 * t + 2],
                                                    ebp[0:64, :].rearrange("p (h c s) -> p h c s", h=4, c=2)[:, :, :, 63]),
                     reads=(ebpt,), writes=(stok,))
            c.barrier()
            sp_.close()
            if gstop == 2:
                return
            st_f = self.sb(st, "g_stf", [64, 4, 128], F32)
            st_b = self.sb(st, "g_stb", [64, 4, 128], BF16)
            oT = self.sb(st, "g_oT", [128, 512], F32)
            onh = self.sb(st, "g_on", [128, S], BF16)
            attb = [self.sb(st, f"g_att{i}", [128, 128], BF16) for i in range(2)]
            for i in range(2):
                def ev_k2(t, pb, pt, i=i):
                    c.op("dve", lambda e: e.tensor_tensor(k2[:, t, i * 128:(i + 1) * 128], k2[:, t, i * 128:(i + 1) * 128],
                                                          pb[:, 0:128], ALU.mult), reads=(pt,), writes=(k2tok,))
                self.proj_tok(li, O_GK + i * 128, 128, ev_k2)
            if gstop == 3:
                c.barrier()
                return
            ai = 0
            for h in range(4):
                c.op("dve", lambda e: e.memset(st_f[0:64, h, :], 0.0), writes=(stok,))
                c.op("dve", lambda e: e.memset(st_b[0:64, h, :], 0.0), writes=(stok,))
                for t in range(NT):
                    tl = slice(t * 128, (t + 1) * 128)
                    pa, pat = self.bank()
                    self.mm(pa[:, 0:128], keT[0:64, h, tl], qeT[0:64, h, tl], True, True, reads=(ktok, qtok), writes=(pat,))
                    ab, abt = attb[ai % 2], att_tok[ai % 2]
                    ai += 1
                    c.op("dve", lambda e: e.tensor_tensor(ab[:], pa[:, 0:128], tblk[:], ALU.mult), reads=(pat, mtok),
                         writes=(abt,))
                    for half in range(2):
                        cn = 2 * t + half
                        rs = slice(half * 64, half * 64 + 64)
                        cs = slice(cn * 64, (cn + 1) * 64)
                        vsl = vtk[rs, t, h * 128:(h + 1) * 128]
                        po, pot = self.bank()
                        inter = cn > 0 and gstop != 4
                        self.mm(po[:, 0:64], vsl, ab[rs, rs], True, True, reads=(vtok, abt), writes=(pot,))
                        oc = (t % 4) * 128 + half * 64
                        c.op("act", lambda e: e.copy(oT[:, oc:oc + 64], po[:, 0:64]), reads=(pot,), writes=(otok,))
                        if inter:
                            pi_, pit = self.bank()
                            self.mm(pi_[:, 0:64], st_b[0:64, h, :], qeT[0:64, h, cs], True, True, reads=(stok, qtok),
                                    writes=(pit,))
                            c.op("dve", lambda e: e.tensor_tensor(oT[:, oc:oc + 64], oT[:, oc:oc + 64], pi_[:, 0:64], ALU.add),
                                 reads=(pit, otok), writes=(otok,))
                        if gstop == 5:
                            continue
                        ps_, pst = self.bank()
                        self.mm(ps_[0:64, 0:128], k2[rs, t, h * 64:(h + 1) * 64], vsl, True, True, reads=(k2tok, vtok),
                                writes=(pst,))
                        c.op("dve", lambda e: e.scalar_tensor_tensor(st_f[0:64, h, :], st_f[0:64, h, :], dec[0:64, h, cn:cn + 1],
                                                                     ps_[0:64, 0:128], ALU.mult, ALU.add),
                             reads=(pst, stok), writes=(stok,))
                        c.op("dve", lambda e: e.tensor_copy(st_b[0:64, h, :], st_f[0:64, h, :]), reads=(stok,), writes=(stok,))
                    if t % 4 == 3:
                        tc = t // 4
                        ts = slice(tc * 512, (tc + 1) * 512)
                        sq, sqt = self.nscr()
                        c.op("act", lambda e: e.activation(sq[:], oT[:], AF.Square), reads=(otok,), writes=(sqt,))
                        pn, pnt = self.bank()
                        self.mm(pn[:], self.ones_f[:], sq[:], True, True, reads=(sqt, ct), writes=(pnt,))
                        rr, rrt = self.nscr()
                        c.op("dve", lambda e: e.tensor_scalar(rr[:], pn[:], 1.0 / 128.0, EPS, ALU.mult, ALU.add), reads=(pnt,),
                             writes=(rrt,))
                        c.op("act", lambda e: e.activation(rr[:], rr[:], AF.Sqrt), reads=(rrt,), writes=(rrt,))
                        c.op("dve", lambda e: e.reciprocal(rr[:], rr[:]), reads=(rrt,), writes=(rrt,))
                        c.op("dve", lambda e: e.scalar_tensor_tensor(onh[:, ts], oT[:], gnorm[:, 0:1], rr[:], ALU.mult, ALU.mult),
                             reads=(otok, rrt, ptok), writes=(ontok,))

                def ev_g(mt, tc, pb, pt, h=h):
                    ts = slice(tc * 512, (tc + 1) * 512)
                    sg, sgt = self.nscr()
                    c.op("act", lambda e: e.activation(sg[:], pb[:], AF.Silu), reads=(pt,), writes=(sgt,))
                    c.op("dve", lambda e: e.tensor_tensor(self.yT[:, h, ts], sg[:], onh[:, ts], ALU.mult), reads=(sgt, ontok),
                         writes=(self.yT_tok,))
                self.proj_feat(li, O_GG + h * 128, 128, ev_g)
            c.barrier()


    def proj_feat_dup(self, li, col0, evac):
        c = self.c
        w = self.A["w_in"][li].rearrange("(c p) n -> p c n", p=128)
        wv, wt = self.wload(w[:, :, col0:col0 + 64], KC, 64)
        wd, wdt = self.wdup, self.wdup_tok
        c.op("pool", lambda e: e.tensor_copy(wd[:, :, 0:64], wv), reads=(wt,), writes=(wdt,))
        c.op("pool", lambda e: e.tensor_copy(wd[:, :, 64:128], wv), reads=(wt,), writes=(wdt,))
        for tc in range(NTC):
            ts = slice(tc * 512, (tc + 1) * 512)
            pb, pt = self.bank()
            for cc in range(KC):
                self.mm(pb[:], wd[:, cc, :], self.hT[:, cc, ts], cc == 0, cc == KC - 1, reads=(wdt, self.hT_tok[tc]),
                        writes=(pt,))
            evac(0, tc, pb, pt)

    def rope_apply(self, dst, pb, pt, n, scale, cos_ap, sin_ap, dtok):
        c = self.c
        raw, rawt = self.rraw[self.rr_i % 2], self.rraw_tok[self.rr_i % 2]
        self.rr_i += 1
        c.op("act", lambda e: e.activation(raw[:, 0:n], pb, AF.Copy, scale=scale), reads=(pt,), writes=(rawt,))
        p2, p2t = self.bank()
        self.mm(p2[:, 0:n], self.Pm[:], raw[:, 0:n], True, True, reads=(rawt, self.tbl_tok), writes=(p2t,))
        t1, t1t = self.nscr()
        c.op("pool", lambda e: e.tensor_tensor(t1[:, 0:n], raw[:, 0:n], cos_ap, ALU.mult), reads=(rawt, self.tbl_tok),
             writes=(t1t,))
        t2, t2t = self.nscr()
        c.op("dve", lambda e: e.tensor_tensor(t2[:, 0:n], p2[:, 0:n], sin_ap, ALU.mult), reads=(p2t, self.tbl_tok),
             writes=(t2t,))
        c.op("dve", lambda e: e.tensor_tensor(dst, t1[:, 0:n], t2[:, 0:n], ALU.add), reads=(t1t, t2t), writes=(dtok,))

    def nsa_tables(self, cosT, sinT):
        c = self.c
        tbl = self.tbl_tok
        PI = float(np.pi)
        C1 = 6.28125
        C2 = float(2 * np.pi - 6.28125)
        with ExitStack() as s2:
            pidx = self.sb(s2, "n_pi", [128, 1], I32)
            f = self.sb(s2, "n_f", [128, 8], F32)
            posi = self.sb(s2, "n_posi", [128, 512], I32)
            ki = self.sb(s2, "n_ki", [128, 512], I32)
            ftok, ptok = Tok(), Tok()
            c.op("pool", lambda e: e.iota(pidx[:], [[0, 1]], base=0, channel_multiplier=1), writes=(ftok,))
            PF, GE, DD, G8, II, ACTV, SGN, INV = [f[:, i:i + 1] for i in range(8)]
            V = lambda fn: c.op("dve", fn, reads=(ftok,), writes=(ftok,))
            V(lambda e: e.tensor_copy(PF, pidx[:]))
            V(lambda e: e.tensor_single_scalar(GE, PF, 64.0, ALU.is_ge))
            V(lambda e: e.scalar_tensor_tensor(DD, GE, -64.0, PF, ALU.mult, ALU.add))
            V(lambda e: e.tensor_single_scalar(G8, DD, 8.0, ALU.is_ge))
            V(lambda e: e.scalar_tensor_tensor(II, G8, -8.0, DD, ALU.mult, ALU.add))
            V(lambda e: e.tensor_single_scalar(ACTV, DD, 16.0, ALU.is_lt))
            V(lambda e: e.tensor_scalar(SGN, G8, 2.0, -1.0, ALU.mult, ALU.add))
            V(lambda e: e.memset(INV, 0.0))
            for i in range(8):
                ci = float(np.float32(500000.0) ** np.float32(-i / 8.0))
                V(lambda e: e.tensor_scalar(GE, II, float(i), ci, ALU.is_equal, ALU.mult))
                V(lambda e: e.tensor_tensor(INV, INV, GE, ALU.add))
            V(lambda e: e.tensor_tensor(INV, INV, ACTV, ALU.mult))
            for tc in range(NTC):
                ts = slice(tc * 512, (tc + 1) * 512)
                c.dma(posi[:], self.A["positions"][0:1, ts].partition_broadcast(128), writes=(ptok,))
                ang, angt = self.nscr()
                c.op("dve", lambda e: e.tensor_copy(ang[:], posi[:]), reads=(ptok,), writes=(angt,))
                c.op("dve", lambda e: e.tensor_scalar(ang[:], ang[:], INV, None, ALU.mult), reads=(angt, ftok), writes=(angt,))
                for phase, dstT, use_sign in ((0.0, sinT, True), (PI / 2, cosT, False)):
                    u, ut = self.nscr()
                    r, rt = self.nscr()
                    c.op("dve", lambda e: e.tensor_scalar(u[:], ang[:], phase, 1.0 / (2 * PI), ALU.add, ALU.mult),
                         reads=(angt,), writes=(ut,))
                    c.op("dve", lambda e: e.tensor_copy(ki[:], u[:]), reads=(ut,), writes=(ptok,))
                    c.op("dve", lambda e: e.tensor_copy(u[:], ki[:]), reads=(ptok,), writes=(ut,))
                    c.op("dve", lambda e: e.scalar_tensor_tensor(r[:], u[:], -C1, ang[:], ALU.mult, ALU.add),
                         reads=(ut, angt), writes=(rt,))
                    c.op("dve", lambda e: e.scalar_tensor_tensor(r[:], u[:], -C2, r[:], ALU.mult, ALU.add), reads=(ut, rt),
                         writes=(rt,))
                    if phase != 0.0:
                        c.op("dve", lambda e: e.tensor_scalar(r[:], r[:], phase, None, ALU.add), reads=(rt,), writes=(rt,))
                    c.op("dve", lambda e: e.tensor_single_scalar(u[:], r[:], PI, ALU.is_gt), reads=(rt,), writes=(ut,))
                    c.op("dve", lambda e: e.scalar_tensor_tensor(r[:], u[:], -2 * PI, r[:], ALU.mult, ALU.add), reads=(ut, rt),
                         writes=(rt,))
                    c.op("dve", lambda e: e.tensor_single_scalar(u[:], r[:], -PI, ALU.is_lt), reads=(rt,), writes=(ut,))
                    c.op("dve", lambda e: e.scalar_tensor_tensor(r[:], u[:], 2 * PI, r[:], ALU.mult, ALU.add), reads=(ut, rt),
                         writes=(rt,))
                    c.op("dve", lambda e: e.tensor_scalar(r[:], r[:], PI, -PI, ALU.min, ALU.max), reads=(rt,), writes=(rt,))
                    c.op("act", lambda e: e.activation(r[:], r[:], AF.Sin), reads=(rt,), writes=(rt,))
                    if use_sign:
                        c.op("dve", lambda e: e.tensor_scalar(dstT[:, ts], r[:], SGN, None, ALU.mult), reads=(rt, ftok),
                             writes=(tbl,))
                    else:
                        c.op("dve", lambda e: e.tensor_copy(dstT[:, ts], r[:]), reads=(rt,), writes=(tbl,))
            c.barrier()

    def nsa(self, li):
        c = self.c
        ct = self.const_tok
        import os
        nstop = int(os.environ.get("NSA_STOP", "99"))
        with ExitStack() as st:
            kcT2 = self.sb(st, "n_kcT", [128, 2, 128], BF16)
            VCX = self.sb(st, "n_vcx", [128, 2, 97], BF16)
            cmptok = Tok()

            def open_tables(sx):
                cosT = self.sb(sx, "n_cos", [128, S], BF16)
                sinT = self.sb(sx, "n_sin", [128, S], BF16)
                self.Pm = self.sb(sx, "n_Pm", [128, 128], BF16)
                self.tbl_tok = Tok()
                self.rraw = [self.sb(sx, f"n_raw{i}", [128, 512], BF16) for i in range(2)]
                self.rraw_tok = [Tok(), Tok()]
                self.rr_i = 0
                self.wdup = self.sb(sx, "n_wdup", [128, KC, 128], BF16)
                self.wdup_tok = Tok()
                self.nsa_tables(cosT, sinT)
                c.op("pool", lambda e: e.memset(self.Pm[:], 0.0), writes=(self.tbl_tok,))
                for (d0, s0) in ((0, 8), (8, 0), (64, 72), (72, 64)):
                    c.op("pool", lambda e: e.tensor_copy(self.Pm[:, d0:d0 + 8], self.ident_b[:, s0:s0 + 8]),
                         reads=(ct, self.tbl_tok), writes=(self.tbl_tok,))
                return cosT, sinT
            sA = ExitStack()
            cosT, sinT = open_tables(sA)
            tbl = self.tbl_tok
            if "cosT" in self.dbg_out:
                self.dump_featmajor_bf16(cosT[:].rearrange("p (c s) -> p c s", c=1), [tbl], self.dbg_out["cosT"])
                self.dump_featmajor_bf16(sinT[:].rearrange("p (c s) -> p c s", c=1), [tbl], self.dbg_out["sinT"])
            with ExitStack() as s3:
                xcT = [self.sb(s3, "n_xk", [128, S], BF16), self.sb(s3, "n_xv", [128, S], BF16)]
                xtok = Tok()
                for kv, col0 in ((0, O_NKC), (1, O_NVC)):
                    def ev_x(mt, tc, pb, pt, kv=kv):
                        ts = slice(tc * 512, (tc + 1) * 512)
                        c.op("act", lambda e: e.copy(xcT[kv][:, ts], pb[:]), reads=(pt,), writes=(xtok,))
                    self.proj_feat(li, col0, 128, ev_x)
                W1 = self.sb(s3, "n_w1", [128, 32, 256], BF16)
                stg = self.sb(s3, "n_stg", [128, 2048], F32)
                W2f = self.sb(s3, "n_w2f", [128, 2, 64], F32)
                W2d = self.sb(s3, "n_w2d", [128, 2, 128], BF16)
                pe2 = self.sb(s3, "n_pe2", [32, 128], F32)
                peb = self.sb(s3, "n_peb", [128, 32], BF16)
                gh = self.sb(s3, "n_gh", [128, 2, 128], BF16)
                hb = self.sb(s3, "n_hb", [128, 2], F32)
                ovf = self.sb(s3, "n_ovf", [128, 3, 32], F32)
                stgt, w1t, w2t, pet, ght, hbt, ovt = [Tok() for _ in range(7)]
                c.op("pool", lambda e: e.memset(ovf[:], 0.5), writes=(ovt,))
                for k_, off in ((0, 0), (1, 16)):
                    c.op("pool", lambda e: e.affine_select(ovf[:, k_, :], ovf[:, k_, :], [[-64, 32]], ALU.is_ge, 0.0, base=off,
                                                           channel_multiplier=16), reads=(ovt,), writes=(ovt,))
                    c.op("pool", lambda e: e.affine_select(ovf[:, k_, :], ovf[:, k_, :], [[64, 32]], ALU.is_ge, 0.0,
                                                           base=63 - off, channel_multiplier=-16), reads=(ovt,), writes=(ovt,))
                c.op("pool", lambda e: e.tensor_tensor(ovf[:, 2, :], ovf[:, 0, :], ovf[:, 1, :], ALU.add), reads=(ovt,),
                     writes=(ovt,))
                for g in range(2):
                    c.op("pool", lambda e: e.tensor_copy(VCX[:, g, 65:97], ovf[:, 2, :]), reads=(ovt,), writes=(cmptok,))
                c.op("pool", lambda e: e.memset(VCX[:, :, 64:65], 1.0), writes=(cmptok,))
                for kv in range(2):
                    w1 = self.A["cmp_wk1" if kv == 0 else "cmp_wv1"][li].rearrange("(l d) n -> d l n", d=64)
                    for piece in range(4):
                        for half in range(2):
                            c.dma(stg[half * 64:(half + 1) * 64, :].rearrange("p (l n) -> p l n", l=8),
                                  w1[:, piece * 8:(piece + 1) * 8, :], writes=(stgt,))
                        c.op("pool", lambda e: e.tensor_copy(W1[:, piece * 8:(piece + 1) * 8, :],
                                                             stg[:].rearrange("p (l n) -> p l n", l=8)),
                             reads=(stgt,), writes=(w1t,))
                    w2 = self.A["cmp_wk2" if kv == 0 else "cmp_wv2"][li].rearrange("(c p) n -> p c n", p=128)
                    c.dma(W2f[:], w2, writes=(w2t,))
                    c.op("pool", lambda e: e.tensor_copy(W2d[:, :, 0:64], W2f[:]), reads=(w2t,), writes=(w2t,))
                    c.op("pool", lambda e: e.tensor_copy(W2d[:, :, 64:128], W2f[:]), reads=(w2t,), writes=(w2t,))
                    pe = self.A["cmp_pos_k" if kv == 0 else "cmp_pos_v"][li]
                    c.dma(pe2[:, 0:64], pe, writes=(pet,))
                    c.dma(pe2[:, 64:128], pe, writes=(pet,))
                    pp, ppt = self.bank()
                    c.op("pe", lambda e: e.transpose(pp[:, 0:32], pe2[:], self.ident_f[0:32, 0:32]), reads=(pet, ct),
                         writes=(ppt,))
                    c.op("dve", lambda e: e.tensor_copy(peb[:], pp[:, 0:32]), reads=(ppt,), writes=(pet,))
                    for half in range(2):
                        pk_, pkt = self.bank()
                        for l in range(32):
                            self.mm(pk_[:, 0:1], W1[0:64, l, half * 128:(half + 1) * 128], peb[0:64, l:l + 1], l == 0, l == 31,
                                    reads=(w1t, pet), writes=(pkt,))
                        c.op("dve", lambda e: e.tensor_copy(hb[:, half:half + 1], pk_[:, 0:1]), reads=(pkt,), writes=(hbt,))
                    for g in range(2):
                        gs = slice(g * 64, g * 64 + 64)
                        for half in range(2):
                            ph, pht = self.bank()
                            for l in range(32):
                                self.mm(ph[:, 0:127], W1[gs, l, half * 128:(half + 1) * 128],
                                        xcT[kv][gs, l:l + 16 * 126 + 1:16], l == 0, l == 31, reads=(w1t, xtok), writes=(pht,))
                            x, xt = self.nscr()
                            x2, x2t = self.nscr()
                            N_ = slice(0, 127)
                            c.op("dve", lambda e: e.tensor_scalar(x[:, N_], ph[:, N_], hb[:, half:half + 1], None, ALU.add),
                                 reads=(pht, hbt), writes=(xt,))
                            c.op("dve", lambda e: e.tensor_tensor(x2[:, N_], x[:, N_], x[:, N_], ALU.mult), reads=(xt,),
                                 writes=(x2t,))
                            c.op("dve", lambda e: e.tensor_scalar(x2[:, N_], x2[:, N_], 0.044715, 1.0, ALU.mult, ALU.add),
                                 reads=(x2t,), writes=(x2t,))
                            c.op("dve", lambda e: e.tensor_tensor(x2[:, N_], x2[:, N_], x[:, N_], ALU.mult), reads=(x2t, xt),
                                 writes=(x2t,))
                            c.op("act", lambda e: e.activation(x2[:, N_], x2[:, N_], AF.Tanh, scale=0.7978845608028654),
                                 reads=(x2t,), writes=(x2t,))
                            c.op("dve", lambda e: e.tensor_scalar(x[:, N_], x[:, N_], 0.5, None, ALU.mult), reads=(xt,),
                                 writes=(xt,))
                            c.op("dve", lambda e: e.scalar_tensor_tensor(gh[:, half, 0:127], x2[:, N_], 1.0, x[:, N_], ALU.add,
                                                                         ALU.mult), reads=(x2t, xt), writes=(ght,))
                        if kv == 0:
                            pk, pkt2 = self.bank()
                            for half in range(2):
                                self.mm(pk[:, 0:127], W2d[:, half, :], gh[:, half, 0:127], half == 0, half == 1,
                                        reads=(w2t, ght), writes=(pkt2,))
                            self.rope_apply(kcT2[:, g, 0:127], pk[:, 0:127], pkt2, 127, 1.0,
                                            cosT[:, 31:31 + 16 * 126 + 1:16], sinT[:, 31:31 + 16 * 126 + 1:16], cmptok)
                        else:
                            pv, pvt = self.bank()
                            for half in range(2):
                                self.mm(pv[0:127, 0:64], gh[:, half, 0:127], W2d[:, half, 0:64], half == 0, half == 1,
                                        reads=(w2t, ght), writes=(pvt,))
                            c.op("act", lambda e: e.copy(VCX[0:127, g, 0:64], pv[0:127, 0:64]), reads=(pvt,), writes=(cmptok,))
                if "kcT" in self.dbg_out:
                    self.dump2d("kcT", kcT2[:].rearrange("p g n -> p (g n)"), [cmptok])
                    self.dump2d("vcx", VCX[:].rearrange("p g n -> p (g n)"), [cmptok])
                c.barrier()
            sA.close()
            if nstop == 1:
                return
            qT = self.sb(st, "n_qT", [128, 4, S], BF16)
            ksT2 = self.sb(st, "n_ksT", [128, 2, S], BF16)
            kwT2 = self.sb(st, "n_kwT", [128, 2, S], BF16)
            vs = self.sb(st, "n_vs", [128, NT, 2, 65], BF16)
            vw = self.sb(st, "n_vw", [128, NT, 2, 65], BF16)
            sg = self.sb(st, "n_sg", [128, NT, 24], F32)
            sB = ExitStack()
            cosT, sinT = open_tables(sB)
            qtok, kstok, kwtok, vstok, vwtok, sgtok = [Tok() for _ in range(6)]

            def ev_q(mt, tc, pb, pt):
                ts = slice(tc * 512, (tc + 1) * 512)
                self.rope_apply(qT[:, mt, ts], pb[:], pt, 512, 0.125, cosT[:, ts], sinT[:, ts], qtok)
            self.proj_feat(li, O_NQ, 512, ev_q)
            for g in range(2):
                def ev_ks(mt, tc, pb, pt, g=g):
                    ts = slice(tc * 512, (tc + 1) * 512)
                    self.rope_apply(ksT2[:, g, ts], pb[:], pt, 512, 1.0, cosT[:, ts], sinT[:, ts], kstok)

                def ev_kw(mt, tc, pb, pt, g=g):
                    ts = slice(tc * 512, (tc + 1) * 512)
                    self.rope_apply(kwT2[:, g, ts], pb[:], pt, 512, 1.0, cosT[:, ts], sinT[:, ts], kwtok)
                self.proj_feat_dup(li, O_NKS + g * 64, ev_ks)
                self.proj_feat_dup(li, O_NKW + g * 64, ev_kw)
            c.op("pool", lambda e: e.memset(vs[:, :, :, 64:65], 1.0), writes=(vstok,))
            c.op("pool", lambda e: e.memset(vw[:, :, :, 64:65], 1.0), writes=(vwtok,))

            def ev_vs(t, pb, pt):
                c.op("act", lambda e: e.copy(vs[:, t, :, 0:64], pb[:, 0:128].rearrange("p (g d) -> p g d", g=2)), reads=(pt,),
                     writes=(vstok,))

            def ev_vw(t, pb, pt):
                c.op("act", lambda e: e.copy(vw[:, t, :, 0:64], pb[:, 0:128].rearrange("p (g d) -> p g d", g=2)), reads=(pt,),
                     writes=(vwtok,))

            def ev_sg(t, pb, pt):
                c.op("act", lambda e: e.activation(sg[:, t, :], pb[:, 0:24], AF.Sigmoid), reads=(pt,), writes=(sgtok,))
            self.proj_tok(li, O_NVS, 128, ev_vs)
            self.proj_tok(li, O_NVW, 128, ev_vw)
            self.proj_tok(li, O_NG, 24, ev_sg)
            if "qT" in self.dbg_out:
                self.dump_featmajor_bf16(qT, [qtok], self.dbg_out["qT"])
            c.barrier()
            sB.close()
            if nstop == 2:
                return
            am = self.sb(st, "n_am", [128, NT, 32], F32)
            Esel = self.sb(st, "n_E", [32, NT, 128], BF16)
            wneg = self.sb(st, "n_wneg", [128, 128], BF16)
            cm = self.sb(st, "n_cm", [128, 512], BF16)
            negT = self.sb(st, "n_negT", [32, 2, 512], BF16)
            acc = self.sb(st, "n_acc", [128, 4, 512], F32)
            ybf = self.sb(st, "n_ybf", [128, 512], BF16)
            pT = [self.sb(st, f"n_pT{i}", [128, 512], BF16) for i in range(4)]
            pT_tok = [Tok() for _ in range(4)]
            imp = self.sb(st, "n_imp", [128, 4, 2, 32], F32)
            sm = self.sb(st, "n_sm", [128, 16], F32)
            impm = self.sb(st, "n_impm", [128, 32], F32)
            top8 = self.sb(st, "n_top8", [128, 8], F32)
            nselb = self.sb(st, "n_nsel", [128, 32], BF16)
            mtok, cmtok, negtok, acctok, ytok, imptok, tktok = [Tok() for _ in range(7)]
            smtok = [Tok(), Tok()]
            tA, tAt = self.nscr()
            tAv = tA[:].rearrange("p (t j) -> p t j", t=NT)
            c.op("pool", lambda e: e.memset(am[:], 0.0), writes=(mtok,))
            c.op("pool", lambda e: e.affine_select(am[:], am[:], [[128, NT], [-64, 32]], ALU.is_ge, -100.0, base=0,
                                                   channel_multiplier=1), reads=(mtok,), writes=(mtok,))
            c.op("pool", lambda e: e.memset(tA[:], 100.0), writes=(tAt,))
            c.op("pool", lambda e: e.affine_select(tAv, tAv, [[128, NT], [-64, 32]], ALU.is_ge, 0.0, base=0,
                                                   channel_multiplier=1), reads=(tAt,), writes=(tAt,))
            c.op("pool", lambda e: e.affine_select(tAv, tAv, [[-128, NT], [64, 32]], ALU.is_ge, 0.0, base=63,
                                                   channel_multiplier=-1), reads=(tAt,), writes=(tAt,))
            c.op("pool", lambda e: e.memset(tAv[:, :, 0:1], 100.0), reads=(tAt,), writes=(tAt,))
            c.op("pool", lambda e: e.tensor_tensor(am[:], am[:], tAv, ALU.add), reads=(tAt, mtok), writes=(mtok,))
            c.op("pool", lambda e: e.memset(Esel[:], 1.0), writes=(mtok,))
            c.op("pool", lambda e: e.affine_select(Esel[:], Esel[:], [[128, NT], [1, 128]], ALU.is_ge, 0.0, base=0,
                                                   channel_multiplier=-64), reads=(mtok,), writes=(mtok,))
            c.op("pool", lambda e: e.affine_select(Esel[:], Esel[:], [[-128, NT], [-1, 128]], ALU.is_ge, 0.0, base=63,
                                                   channel_multiplier=64), reads=(mtok,), writes=(mtok,))
            c.op("pool", lambda e: e.affine_select(wneg[:], self.zer_f[:], [[-1, 128]], ALU.is_gt, NEG, base=0,
                                                   channel_multiplier=1), reads=(ct,), writes=(mtok,))
            pi = [0]
            for qc in range(NTC):
                qs = slice(qc * 512, (qc + 1) * 512)
                c.op("pool", lambda e: e.memset(cm[:], 0.0), writes=(cmtok,))
                c.op("pool", lambda e: e.affine_select(cm[:], cm[:], [[1, 512]], ALU.is_ge, NEG, base=qc * 512 - 31,
                                                       channel_multiplier=-16), reads=(cmtok,), writes=(cmtok,))
                c.op("dve", lambda e: e.memset(imp[:], 0.0), writes=(imptok,))
                def cmp_stream(h):
                    g = h // 4
                    hp = slice((h % 2) * 64, (h % 2) * 64 + 64)
                    hc = h // 2
                    hcol = slice(h * 64, (h + 1) * 64)
                    sb_, stk = self.bank()
                    self.mm(sb_[0:127, :], kcT2[hp, g, 0:127], qT[hp, hc, qs], True, False, reads=(cmptok, qtok), writes=(stk,))
                    self.mm(sb_[0:127, :], self.ident_b[0:127, 0:127], cm[0:127, :], False, True, reads=(ct, cmtok),
                            writes=(stk,), skip_group_check=True)
                    p, ptk = pT[pi[0] % 4], pT_tok[pi[0] % 4]
                    pi[0] += 1
                    c.op("act", lambda e: e.activation(p[0:127, :], sb_[0:127, :], AF.Exp), reads=(stk,), writes=(ptk,))
                    yield
                    ob, ot = self.bank_acc()
                    O = ob[:, 0:388].rearrange("p (j d) -> p j d", j=4)
                    for j in range(4):
                        self.mm(O[:, j, :], p[0:127, j * 128:(j + 1) * 128], VCX[0:127, g, :], j == 0, True,
                                reads=(ptk, cmptok), writes=(ot,), skip_group_check=True)
                    yield
                    smt = smtok[h % 2]
                    for j in range(4):
                        qt = qc * 4 + j
                        o_ = (h % 2) * 8 + 2 * j
                        rcv, wv_ = sm[:, o_:o_ + 1], sm[:, o_ + 1:o_ + 2]
                        c.op("dve", lambda e: e.tensor_scalar(rcv, O[:, j, 64:65], 1e-30, None, ALU.max), reads=(ot,),
                             writes=(smt,))
                        c.op("dve", lambda e: e.reciprocal(rcv, rcv), reads=(smt,), writes=(smt,))
                        c.op("dve", lambda e: e.tensor_tensor(wv_, rcv, sg[:, qt, 3 * h:3 * h + 1], ALU.mult),
                             reads=(smt, sgtok), writes=(smt,))
                        c.op("dve", lambda e: e.tensor_scalar(acc[:, j, hcol], O[:, j, 0:64], wv_, None, ALU.mult),
                             reads=(ot, smt), writes=(acctok,))
                        c.op("dve", lambda e: e.scalar_tensor_tensor(imp[:, j, g, :], O[:, j, 65:97], rcv, imp[:, j, g, :],
                                                                     ALU.mult, ALU.add), reads=(ot, smt, imptok),
                             writes=(imptok,))
                self.run_streams([cmp_stream(h) for h in range(8)], 2)
                for j in range(4):
                    qt = qc * 4 + j
                    for g in range(2):
                        c.op("dve", lambda e: e.tensor_tensor(impm[:], imp[:, j, g, :], am[:, qt, :], ALU.add),
                             reads=(imptok, mtok), writes=(tktok,))
                        c.op("dve", lambda e: e.max(top8[:], impm[:]), reads=(tktok,), writes=(tktok,))
                        c.op("dve", lambda e: e.tensor_scalar(impm[:], impm[:], top8[:, 7:8], None, ALU.is_ge), reads=(tktok,),
                             writes=(tktok,))
                        c.op("dve", lambda e: e.tensor_scalar(nselb[:], impm[:], -1.0, 30000.0, ALU.add, ALU.mult),
                             reads=(tktok,), writes=(tktok,))
                        pb, pt = self.bank()
                        pbb = pb[:].bitcast(BF16)
                        c.op("pe", lambda e: e.transpose(pbb[0:32, 0:128], nselb[:], self.ident_b[:]), reads=(tktok, ct),
                             writes=(pt,))
                        c.op("act", lambda e: e.copy(negT[0:32, g, j * 128:(j + 1) * 128], pbb[0:32, 0:128]), reads=(pt,),
                             writes=(negtok,))
                if "negT" in self.dbg_out and qc == 1:
                    self.dump2d("negT", negT[:].rearrange("p g n -> p (g n)"), [negtok])
                def sw_stream(br, h):
                    g = h // 4
                    hp = slice((h % 2) * 64, (h % 2) * 64 + 64)
                    hc = h // 2
                    hcol = slice(h * 64, (h + 1) * 64)
                    ob, ot = self.bank_acc()
                    O = ob[:, 0:260].rearrange("p (j d) -> p j d", j=4)
                    first = True
                    kt0 = 0 if br == 1 else max(0, 4 * qc - 2)
                    for kt in range(kt0, 4 * qc + 4):
                        rel = kt - 4 * qc
                        jlo = max(0, rel)
                        jhi = 3 if br == 1 else min(3, rel + 2)
                        ncol = (jhi - jlo + 1) * 128
                        q0 = qc * 512 + jlo * 128
                        kl = slice(kt * 128, (kt + 1) * 128)
                        KT = ksT2 if br == 1 else kwT2
                        ktk = kstok if br == 1 else kwtok
                        extra = []
                        if br == 1:
                            extra.append((slice(0, ncol), Esel[0:32, kt, :], negT[0:32, g, jlo * 128:jlo * 128 + ncol],
                                          (mtok, negtok)))
                        if rel >= 0:
                            extra.append((slice(0, 128), self.ident_b[:], self.cneg_b[:], (ct,)))
                        if br == 2 and 0 <= rel + 2 <= 3:
                            o2 = (rel + 2 - jlo) * 128
                            extra.append((slice(o2, o2 + 128), self.ident_b[:], wneg[:], (ct, mtok)))
                        sb_, stk = self.bank()
                        self.mm(sb_[:, 0:ncol], KT[hp, g, kl], qT[hp, hc, q0:q0 + ncol], True, len(extra) == 0,
                                reads=(ktk, qtok), writes=(stk,))
                        for ei, (csl, lh, rh, rd) in enumerate(extra):
                            self.mm(sb_[:, csl], lh, rh, False, ei == len(extra) - 1, reads=rd, writes=(stk,),
                                    skip_group_check=True)
                        p, ptk = pT[pi[0] % 4], pT_tok[pi[0] % 4]
                        pi[0] += 1
                        c.op("act", lambda e: e.activation(p[:, 0:ncol], sb_[:, 0:ncol], AF.Exp), reads=(stk,), writes=(ptk,))
                        yield
                        VV = vs if br == 1 else vw
                        vtk_ = vstok if br == 1 else vwtok
                        for j in range(jlo, jhi + 1):
                            qt = qc * 4 + j
                            cs = slice((j - jlo) * 128, (j - jlo + 1) * 128)
                            self.mm(O[:, j, :], p[:, cs], VV[:, kt, g, :], first, kt == qt, reads=(ptk, vtk_), writes=(ot,),
                                    skip_group_check=True)
                            first = False
                    smt = smtok[h % 2]
                    for j in range(4):
                        qt = qc * 4 + j
                        o_ = (h % 2) * 8 + 2 * j
                        rcv, wv_ = sm[:, o_:o_ + 1], sm[:, o_ + 1:o_ + 2]
                        c.op("dve", lambda e: e.reciprocal(rcv, O[:, j, 64:65]), reads=(ot,), writes=(smt,))
                        c.op("dve", lambda e: e.tensor_tensor(wv_, rcv, sg[:, qt, 3 * h + br:3 * h + br + 1], ALU.mult),
                             reads=(smt, sgtok), writes=(smt,))
                        c.op("dve", lambda e: e.scalar_tensor_tensor(acc[:, j, hcol], O[:, j, 0:64], wv_, acc[:, j, hcol],
                                                                     ALU.mult, ALU.add), reads=(ot, smt, acctok),
                             writes=(acctok,))
                self.run_streams([sw_stream(br, h) for br in (1, 2) for h in range(8)], 2)
                for j in range(4):
                    t = qc * 4 + j
                    c.op("act", lambda e: e.copy(ybf[:], acc[:, j, :]), reads=(acctok,), writes=(ytok,))
                    pb, pt = self.bank()
                    pbb = pb[:].bitcast(BF16)
                    for jj in range(4):
                        c.op("pe", lambda e: e.transpose(pbb[:, jj * 128:(jj + 1) * 128], ybf[:, jj * 128:(jj + 1) * 128],
                                                         self.ident_b[:]), reads=(ytok, ct), writes=(pt,))
                    c.op("dve", lambda e: e.tensor_copy(self.yT[:, :, t * 128:(t + 1) * 128],
                                                        pbb[:, 0:512].rearrange("p (j n) -> p j n", j=4)),
                         reads=(pt,), writes=(self.yT_tok,))
            c.barrier()

    def fox(self, li):
        c = self.c
        with ExitStack() as st:
            qT = self.sb(st, "fx_qT", [128, 4, S], BF16)
            kT = self.sb(st, "fx_kT", [128, 4, S], BF16)
            V = self.sb(st, "fx_V", [128, NT, 8, 65], BF16)
            ytk = self.sb(st, "fx_y", [128, 4, 512], BF16)
            fl = self.sb(st, "fx_f", [128, NT, 8], F32)
            ncum = self.sb(st, "fx_ncum", [128, NT, 8], F32)
            nref = self.sb(st, "fx_nref", [128, NT, 8], F32)
            btab = self.sb(st, "fx_btab", [128, NT, NT, 8], F32)
            bfb = self.sb(st, "fx_bf", [128, 8], F32)
            qtok, ktok, vtok, ytok, ftok = Tok(), Tok(), Tok(), Tok(), Tok()
            self.aux_tok = Tok()
            self.proj_feat(li, O_FQ, 512, self.evac_featT(qT, qtok, 0.125))
            self.proj_feat(li, O_FK, 512, self.evac_featT(kT, ktok, 1.0))
            c.op("pool", lambda e: e.memset(V[:, :, :, 64:65], 1.0), writes=(vtok,))

            def evac_v(t, pb, pt):
                c.op("act", lambda e: e.copy(V[:, t, :, 0:64], pb[:, 0:512].rearrange("p (h d) -> p h d", h=8)),
                     reads=(pt,), writes=(vtok,))
            for half in range(4):
                def evac_vh(t, pb, pt, half=half):
                    c.op("act", lambda e: e.copy(V[:, t, half * 2:half * 2 + 2, 0:64],
                                                 pb[:, 0:128].rearrange("p (h d) -> p h d", h=2)),
                         reads=(pt,), writes=(vtok,))
                self.proj_tok(li, O_FV + half * 128, 128, evac_vh)
            c.dma(bfb[:], self.A["fox_b_f"][li:li + 1, :].partition_broadcast(128), writes=(ftok,))

            def evac_f(t, pb, pt):
                c.op("dve", lambda e: e.tensor_tensor(fl[:, t, :], pb[:, 0:8], bfb[:], ALU.add), reads=(pt, ftok),
                     writes=(ftok,))
            self.proj_tok(li, O_FF, 8, evac_f)
            flat = fl[:].rearrange("p t h -> p (t h)")
            c.op("act", lambda e: e.activation(flat, flat, AF.Exp, scale=-1.0), reads=(ftok,), writes=(ftok,))
            c.op("act", lambda e: e.activation(flat, flat, AF.Ln, bias=1.0), reads=(ftok,), writes=(ftok,))
            for t in range(NT):
                pb, pt = self.bank()
                for j in range(t):
                    self.mm(pb[:, 0:8], self.ones_f[:], fl[:, j, :], j == 0, False, reads=(ftok, self.const_tok),
                            writes=(pt,))
                self.mm(pb[:, 0:8], self.tri_f[:], fl[:, t, :], t == 0, True, reads=(ftok, self.const_tok), writes=(pt,))
                c.op("dve", lambda e: e.tensor_copy(ncum[:, t, :], pb[:, 0:8]), reads=(pt,), writes=(self.aux_tok,))
                if t > 0:
                    pb2, pt2 = self.bank()
                    for j in range(t):
                        self.mm(pb2[:, 0:8], self.ones_f[:], fl[:, j, :], j == 0, j == t - 1,
                                reads=(ftok, self.const_tok), writes=(pt2,))
                    c.op("dve", lambda e: e.tensor_copy(nref[:, t, :], pb2[:, 0:8]), reads=(pt2,), writes=(self.aux_tok,))
                else:
                    c.op("dve", lambda e: e.memset(nref[:, 0, :], 0.0), writes=(self.aux_tok,))
            for kt in range(NT):
                for qt in range(1, NT, 2):
                    if qt >= kt:
                        c.op("pool", lambda e: e.tensor_tensor(btab[:, kt, qt, :], ncum[:, kt, :], nref[:, qt, :],
                                                               ALU.subtract), reads=(self.aux_tok,), writes=(self.aux_tok,))
            self.dump2d("ncum", ncum[:].rearrange("p t h -> p (t h)"), [self.aux_tok])
            self.dump2d("nref", nref[:].rearrange("p t h -> p (t h)"), [self.aux_tok])
            self.dump2d("fl", fl[:].rearrange("p t h -> p (t h)"), [ftok])
            self.attention(st, "fx", 8, qT, qtok, kT, ktok, V, vtok, ytk, ytok,
                           bias_fn=lambda h, kt, qt: btab[:, kt, qt, h:h + 1],
                           post_qc=lambda qc: self.ytok_to_yT(ytk, ytok, qc))
            c.barrier()

    def final_norm_store(self, out):
        self.store_tok_major(out, normed=True)

    def store_tok_major(self, out, normed):
        c = self.c
        L = len(self.layers)
        with ExitStack() as st:
            if normed:
                for tc in range(NTC):
                    ts = slice(tc * 512, (tc + 1) * 512)
                    pb, pt = self.bank()
                    for cc in range(KC):
                        sq, sqt = self.nscr()
                        c.op("act", lambda e: e.activation(sq[:], self.xT[:, cc, ts], AF.Square),
                             reads=(self.xT_tok[tc],), writes=(sqt,))
                        self.mm(pb[:], self.ones_f[:], sq[:], cc == 0, cc == KC - 1, reads=(sqt, self.const_tok),
                                writes=(pt,))
                    rs, rst = self.nscr()
                    c.op("dve", lambda e: e.tensor_scalar(rs[:], pb[:], 1.0 / D, EPS, ALU.mult, ALU.add), reads=(pt,),
                         writes=(rst,))
                    c.op("act", lambda e: e.activation(rs[:], rs[:], AF.Sqrt), reads=(rst,), writes=(rst,))
                    c.op("dve", lambda e: e.reciprocal(rs[:], rs[:]), reads=(rst,), writes=(rst,))
                    for cc in range(KC):
                        g = self.vecT[:, L * 72 + cc:L * 72 + cc + 1]
                        c.op("dve", lambda e: e.scalar_tensor_tensor(self.xT[:, cc, ts], self.xT[:, cc, ts], g, rs[:],
                                                                     ALU.mult, ALU.mult),
                             reads=(rst, self.vec_tok), writes=(self.xT_tok[tc],))
            os_ = [self.sb(st, f"os{i}", [128, D], F32) for i in range(2)]
            os_tok = [Tok(), Tok()]
            for t in range(NT):
                b = t % 2
                for half in range(2):
                    pb, pt = self.bank()
                    for j in range(4):
                        cc = half * 4 + j
                        c.op("pe", lambda e: e.transpose(pb[:, j * 128:(j + 1) * 128],
                                                         self.xT[:, cc, t * 128:(t + 1) * 128], self.ident_f[:]),
                             reads=(self.xT_tok[t // 4], self.const_tok), writes=(pt,), inc=(j == 3))
                    dst = os_[b][:, half * 512:(half + 1) * 512]
                    if half == 0:
                        c.op("dve", lambda e: e.tensor_copy(dst, pb[:]), reads=(pt,), writes=(os_tok[b],))
                    else:
                        c.op("act", lambda e: e.copy(dst, pb[:]), reads=(pt,), writes=(os_tok[b],))
                c.dma(out[t * 128:(t + 1) * 128, :], os_[b][:], reads=(os_tok[b],))
            c.barrier()

    def dump2d(self, name, ap, toks):
        if name in self.dbg_out:
            self.c.barrier()
            self.c.dma(self.dbg_out[name], ap, reads=tuple(toks), q="pool")
            self.c.barrier()

    def dump_featmajor_bf16(self, tT, toks, dst):
        c = self.c
        with ExitStack() as st:
            tmp = self.sb(st, "dmp", [128, S], F32)
            tt = Tok()
            for cc in range(tT.shape[1]):
                c.op("dve", lambda e: e.tensor_copy(tmp[:], tT[:, cc, :]), reads=tuple(toks), writes=(tt,))
                c.dma(dst[cc * 128:(cc + 1) * 128, :], tmp[:], reads=(tt,))
            c.barrier()


def _prep_inputs(inputs, layers, b):
    L = len(layers)
    vecs = np.zeros((L, 72, 128), np.float32)
    for i, l in enumerate(layers):
        vecs[i, 0:8] = inputs["norm_mix"][l].reshape(8, 128)
        vecs[i, 8:16] = inputs["norm_ff"][l].reshape(8, 128)
        vecs[i, 16:48] = inputs["b_gate"][l].reshape(32, 128)
    m = {
        "x": np.ascontiguousarray(inputs["x"][b]),
        "w_in": np.ascontiguousarray(inputs["w_in"][layers]),
        "vecs": vecs,
        "norm_final": np.ascontiguousarray(inputs["norm_final"].reshape(8, 128)),
        "positions": np.ascontiguousarray(inputs["positions"][b:b + 1]).astype(np.int32),
        "cmp_pos_k": np.ascontiguousarray(inputs["nsa_cmp_pos_k"][layers]),
        "cmp_pos_v": np.ascontiguousarray(inputs["nsa_cmp_pos_v"][layers]),
        "cmp_wk1": np.ascontiguousarray(inputs["nsa_cmp_wk1"][layers]),
        "cmp_wk2": np.ascontiguousarray(inputs["nsa_cmp_wk2"][layers]),
        "cmp_wv1": np.ascontiguousarray(inputs["nsa_cmp_wv1"][layers]),
        "cmp_wv2": np.ascontiguousarray(inputs["nsa_cmp_wv2"][layers]),
        "fox_b_f": np.ascontiguousarray(inputs["fox_b_f"][layers]),
        "gla_w_alpha": np.ascontiguousarray(inputs["gla_w_alpha"][layers]),
        "gla_b_alpha": np.ascontiguousarray(inputs["gla_b_alpha"][layers]),
        "gla_norm": np.ascontiguousarray(inputs["gla_norm"][layers]),
        "w_branch": np.ascontiguousarray(inputs["w_branch"][layers]),
        "w_out": np.ascontiguousarray(inputs["w_out"][layers]),
        "w_ff1": np.ascontiguousarray(inputs["w_ff1"][layers]),
        "w_ff2": np.ascontiguousarray(inputs["w_ff2"][layers]),
    }
    return m


def run(inputs, layers=(0, 1, 2, 3), debug=(), ncores=8, trace=False, stage=99):
    layers = list(layers)
    bld = Builder(layers, first=True, last=True, debug=debug, stage=stage)
    nc = bld.build()
    in_maps = [_prep_inputs(inputs, layers, b) for b in range(ncores)]
    res = run_bass_kernel_spmd(nc, in_maps, core_ids=list(range(ncores)), trace=trace)
    return res


def kernel(**inputs):
    inputs = {k: np.asarray(v) for k, v in inputs.items()}
    res = run(inputs)
    out = np.stack([np.asarray(r["out"]) for r in res.results], axis=0)
    return out.astype(np.float32)
```

```python
import numpy as np
from contextlib import ExitStack
import concourse.bass as bass
import concourse.mybir as mybir
from concourse.bass_utils import run_bass_kernel_spmd

F32 = mybir.dt.float32
BF16 = mybir.dt.bfloat16
I32 = mybir.dt.int32
ALU = mybir.AluOpType
AF = mybir.ActivationFunctionType
AX = mybir.AxisListType

S = 2048
D = 1024
NT = S // 128
NTC = S // 512
KC = D // 128
DEPTH = 4
DFF = 4096
D_IN = 10032
EPS = 1e-6
NEG = -30000.0

SPLITS = (512, 128, 128, 128, 128, 128, 128, 24, 512, 512, 512, 256, 256, 512, 16, 512, 512, 512, 512, 8, 4096)
OFFS = np.concatenate([[0], np.cumsum(SPLITS)]).tolist()
(O_NQ, O_NKC, O_NVC, O_NKS, O_NVS, O_NKW, O_NVW, O_NG, O_SQ, O_SK, O_SV, O_GQ, O_GK, O_GV, O_GA, O_GG,
 O_FQ, O_FK, O_FV, O_FF, O_GATE) = OFFS[:21]

EPOCH = 4000


class Tok:
    __slots__ = ("w", "r", "name")

    def __init__(self, name=""):
        self.w = None
        self.r = {}
        self.name = name


class Ctx:
    def __init__(self, nc, es):
        self.nc = nc
        self.es = es
        self.eng = dict(pe=nc.tensor, act=nc.scalar, dve=nc.vector, pool=nc.gpsimd, sp=nc.sync)
        self.cur = {}
        self.nsem = 0
        self.waited = {e: {} for e in self.eng}
        for e in ("pe", "act", "dve", "pool"):
            self.cur[e] = [self._newsem(e), 0]
        self.own = {e: set() for e in self.eng}
        for e in ("pe", "act", "dve", "pool"):
            self.own[e].add(id(self.cur[e][0]))
        self.dq = {}
        for q in ("sp", "act", "pool"):
            sems = [self._newsem("d" + q) for _ in range(8 if q == "sp" else 4)]
            self.dq[q] = dict(sems=sems, tgt=[0] * len(sems), i=0)
        self.all_dma = []

    def _newsem(self, name):
        self.nsem += 1
        return self.es.enter_context(self.nc.semaphore(f"s_{name}_{self.nsem}"))

    def _wait(self, e, deps):
        w = self.waited[e]
        for (sem, val) in deps:
            if val <= 0:
                continue
            k = id(sem)
            if e == "pe" and k in self.own["pe"]:
                continue
            if w.get(k, 0) >= val:
                continue
            self.eng[e].wait_ge(sem, val)
            w[k] = val

    @staticmethod
    def _deps(reads, writes):
        deps = []
        for t in reads:
            if t.w is not None:
                deps.append(t.w)
        for t in writes:
            if t.w is not None:
                deps.append(t.w)
            deps.extend(t.r.values())
        return deps

    @staticmethod
    def _record(stamp, reads, writes):
        sem, val = stamp
        for t in reads:
            t.r[id(sem)] = stamp
        for t in writes:
            t.w = stamp
            t.r = {}

    def op(self, e, fn, reads=(), writes=(), inc=True):
        self._wait(e, self._deps(reads, writes))
        ins = fn(self.eng[e])
        sem, cnt = self.cur[e]
        stamp = (sem, cnt + 1)
        if inc:
            ins.then_inc(sem, 1)
            self.cur[e][1] = cnt + 1
        self._record(stamp, reads, writes)
        if inc and cnt + 1 >= EPOCH:
            ns = self._newsem(e)
            self.own[e].add(id(ns))
            self.cur[e] = [ns, 0]
        return ins

    def dma(self, out, in_, reads=(), writes=(), q="sp", **kw):
        dq = self.dq[q]
        i = dq["i"] % len(dq["sems"])
        dq["i"] += 1
        sem = dq["sems"][i]
        deps = self._deps(reads, writes)
        deps.append((sem, dq["tgt"][i]))
        self._wait(q, deps)
        self.eng[q].dma_start(out=out, in_=in_, **kw).then_inc(sem, 16)
        dq["tgt"][i] += 16
        stamp = (sem, dq["tgt"][i])
        self._record(stamp, reads, writes)
        return stamp

    def barrier(self):
        stamps = []
        for e in ("pe", "act", "dve", "pool"):
            sem, cnt = self.cur[e]
            stamps.append((sem, cnt))
        for q, dq in self.dq.items():
            for s, t in zip(dq["sems"], dq["tgt"]):
                stamps.append((s, t))
        for e in ("pe", "act", "dve", "pool", "sp"):
            self._wait_all(e, stamps)

    def _wait_all(self, e, stamps):
        w = self.waited[e]
        for (sem, val) in stamps:
            if val <= 0:
                continue
            k = id(sem)
            if k in self.own.get(e, ()) and (e == "pe"):
                continue
            if w.get(k, 0) >= val:
                continue
            self.eng[e].wait_ge(sem, val)
            w[k] = val


class Builder:
    def __init__(self, layers, first, last, debug=(), stage=99):
        self.stage = stage
        self.layers = layers
        self.first = first
        self.last = last
        self.debug = debug
        self.nc = bass.Bass("TRN2", target_bir_lowering=False)
        self.dbg_out = {}

    def sb(self, st, name, shape, dt):
        self._uid = getattr(self, "_uid", 0) + 1
        return st.enter_context(self.nc.sbuf_tensor(f"{name}_{self._uid}", shape, dt))

    def dram_in(self, name, shape, dt=F32):
        return self.nc.dram_tensor(name, list(shape), dt, kind="ExternalInput").ap()

    def mm(self, out, lhsT, rhs, start, stop, reads, writes, inc=None, **kw):
        if inc is None:
            inc = True
        return self.c.op("pe", lambda e: e.matmul(out, lhsT, rhs, start=start, stop=stop, **kw),
                         reads=reads, writes=writes, inc=inc)

    def bank(self):
        i = self.bank_i % self.n_scr_banks
        self.bank_i += 1
        return self.ps[i], self.ps_tok[i]

    def bank_acc(self):
        busy = self.acc_busy
        for i in range(self.n_scr_banks, 8):
            if i not in busy:
                busy.add(i)
                self.last_acc = i
                return self.ps[i], self.ps_tok[i]
        raise RuntimeError("no free accumulator bank")

    def release_acc(self, ob):
        for i in range(8):
            if self.ps[i] is ob:
                self.acc_busy.discard(i)

    def wload(self, src3, kc, ncols, eng="pool"):
        i = self.w_i % 2
        self.w_i += 1
        stg, stok = self.wstg[i], self.wstg_tok[i]
        wb, wtok = self.wbf[i], self.wbf_tok[i]
        n = kc * ncols
        assert n <= self.WMAX
        sv = stg[:, 0:n].rearrange("p (c n) -> p c n", c=kc)
        wv = wb[:, 0:n].rearrange("p (c n) -> p c n", c=kc)
        self.c.dma(sv, src3, reads=(), writes=(stok,))
        self.c.op(eng, lambda e: e.tensor_copy(wb[:, 0:n], stg[:, 0:n]), reads=(stok,), writes=(wtok,))
        return wv, wtok

    def build(self):
        nc = self.nc
        L = len(self.layers)
        A = {}
        A["x"] = self.dram_in("x", [S, D])
        A["w_in"] = self.dram_in("w_in", [L, D, D_IN])
        A["vecs"] = self.dram_in("vecs", [L, 72, 128])
        A["norm_final"] = self.dram_in("norm_final", [8, 128])
        A["positions"] = self.dram_in("positions", [1, S], I32)
        A["cmp_pos_k"] = self.dram_in("cmp_pos_k", [L, 32, 64])
        A["cmp_pos_v"] = self.dram_in("cmp_pos_v", [L, 32, 64])
        A["cmp_wk1"] = self.dram_in("cmp_wk1", [L, 2048, 256])
        A["cmp_wk2"] = self.dram_in("cmp_wk2", [L, 256, 64])
        A["cmp_wv1"] = self.dram_in("cmp_wv1", [L, 2048, 256])
        A["cmp_wv2"] = self.dram_in("cmp_wv2", [L, 256, 64])
        A["fox_b_f"] = self.dram_in("fox_b_f", [L, 8])
        A["gla_w_alpha"] = self.dram_in("gla_w_alpha", [L, 16, 256])
        A["gla_b_alpha"] = self.dram_in("gla_b_alpha", [L, 256])
        A["gla_norm"] = self.dram_in("gla_norm", [L, 128])
        A["w_branch"] = self.dram_in("w_branch", [L, 4, 512, D])
        A["w_out"] = self.dram_in("w_out", [L, D, D])
        A["w_ff1"] = self.dram_in("w_ff1", [L, D, DFF])
        A["w_ff2"] = self.dram_in("w_ff2", [L, DFF, D])
        self.A = A
        out = nc.dram_tensor("out", [S, D], F32, kind="ExternalOutput").ap()
        for name, shape in self.debug:
            self.dbg_out[name] = nc.dram_tensor("dbg_" + name, list(shape), F32, kind="ExternalOutput").ap()

        with ExitStack() as es:
            self.es = es
            c = self.c = Ctx(nc, es)
            self.xT = self.sb(es, "xT", [128, KC, S], F32)
            self.hT = self.sb(es, "hT", [128, KC, S], BF16)
            self.xT_tok = [Tok(f"xT{i}") for i in range(NTC)]
            self.hT_tok = [Tok(f"hT{i}") for i in range(NTC)]
            self.ident_f = self.sb(es, "ident_f", [128, 128], F32)
            self.ident_b = self.sb(es, "ident_b", [128, 128], BF16)
            self.ones_f = self.sb(es, "ones_f", [128, 128], F32)
            self.const_tok = Tok("const")
            self.vecT = self.sb(es, "vecT", [128, L * 72 + 8], F32)
            self.vec_tok = Tok("vec")
            self.WMAX = 1024
            self.wstg = [self.sb(es, f"wstg{i}", [128, self.WMAX], F32) for i in range(2)]
            self.wbf = [self.sb(es, f"wbf{i}", [128, self.WMAX], BF16) for i in range(2)]
            self.wstg_tok = [Tok() for _ in range(2)]
            self.wbf_tok = [Tok() for _ in range(2)]
            self.w_i = 0
            self.scr = [self.sb(es, f"scr{i}", [128, 512], F32) for i in range(4)]
            self.scr_tok = [Tok() for _ in range(4)]
            self.scr_i = 0
            self.ps = [es.enter_context(nc.psum_tensor(f"ps{i}", [128, 512], F32)) for i in range(8)]
            self.ps_tok = [Tok(f"ps{i}") for i in range(8)]
            self.bank_i = 0
            self.bank_j = 0
            self.acc_busy = set()
            self.n_scr_banks = 6

            self.make_consts()
            if self.first:
                self.load_x()
            else:
                self.load_xT()
            for li in range(L):
                if self.stage >= 1:
                    self.layer(li)
            if self.last and self.stage >= 3:
                self.final_norm_store(out)
            else:
                self.store_xT(out)
            c.barrier()
        return nc

    def run_streams(self, gens, k=2):
        gens = iter(gens)
        active = []
        for g in gens:
            active.append(g)
            if len(active) == k:
                break
        while active:
            for g in list(active):
                try:
                    next(g)
                except StopIteration:
                    active.remove(g)
                    nxt = next(gens, None)
                    if nxt is not None:
                        active.append(nxt)

    def nscr(self):
        i = self.scr_i % len(self.scr)
        self.scr_i += 1
        return self.scr[i], self.scr_tok[i]

    def make_consts(self):
        c = self.c
        nc = self.nc
        ct = self.const_tok
        c.op("pool", lambda e: e.memset(self.ones_f[:], 1.0), writes=(ct,))
        c.op("pool", lambda e: e.affine_select(self.ident_f[:], self.ones_f[:], [[-1, 128]], ALU.is_equal, 0.0,
                                               base=0, channel_multiplier=1), reads=(ct,), writes=(ct,))
        c.op("pool", lambda e: e.tensor_copy(self.ident_b[:], self.ident_f[:]), reads=(ct,), writes=(ct,))
        self.tri_f = self.sb(self.es, "tri_f", [128, 128], F32)
        c.op("pool", lambda e: e.affine_select(self.tri_f[:], self.ones_f[:], [[1, 128]], ALU.is_ge, 0.0,
                                               base=0, channel_multiplier=-1), reads=(ct,), writes=(ct,))
        self.zer_f = self.sb(self.es, "zer_f", [128, 128], F32)
        self.cneg_b = self.sb(self.es, "cneg_b", [128, 128], BF16)
        c.op("pool", lambda e: e.memset(self.zer_f[:], 0.0), writes=(ct,))
        self.nones_f = self.sb(self.es, "nones_f", [128, 128], F32)
        self.ones_b = self.sb(self.es, "ones_b", [128, 128], BF16)
        self.nones_b = self.sb(self.es, "nones_b", [128, 128], BF16)
        self.cnegs_b = self.sb(self.es, "cnegs_b", [128, 128], BF16)
        self.ntri_b = self.sb(self.es, "ntri_b", [128, 128], BF16)
        c.op("pool", lambda e: e.memset(self.nones_f[:], -1.0), writes=(ct,))
        c.op("pool", lambda e: e.memset(self.ones_b[:], 1.0), writes=(ct,))
        c.op("pool", lambda e: e.memset(self.nones_b[:], -1.0), writes=(ct,))
        c.op("pool", lambda e: e.affine_select(self.cnegs_b[:], self.zer_f[:], [[1, 128]], ALU.is_gt, NEG,
                                               base=0, channel_multiplier=-1), reads=(ct,), writes=(ct,))
        c.op("pool", lambda e: e.affine_select(self.ntri_b[:], self.nones_f[:], [[-1, 128]], ALU.is_ge, 0.0,
                                               base=0, channel_multiplier=1), reads=(ct,), writes=(ct,))
        c.op("pool", lambda e: e.affine_select(self.cneg_b[:], self.zer_f[:], [[1, 128]], ALU.is_ge, NEG,
                                               base=0, channel_multiplier=-1), reads=(ct,), writes=(ct,))
        L = len(self.layers)
        nrow = L * 72 + 8
        with ExitStack() as st:
            tmp = self.sb(st, "vtmp", [128, 4, 128], F32)
            tt = Tok()
            r0 = 0
            chunks = []
            while r0 < nrow:
                n = min(128, nrow - r0)
                chunks.append((r0, n))
                r0 += n
            for ci, (r0, n) in enumerate(chunks):
                a = r0
                while a < r0 + n:
                    if a < L * 72:
                        b = min(r0 + n, L * 72)
                        src = self.A["vecs"].rearrange("l r p -> (l r) p")[a:b, :]
                    else:
                        b = r0 + n
                        src = self.A["norm_final"][a - L * 72:b - L * 72, :]
                    c.dma(tmp[a - r0:b - r0, ci, :], src, writes=(tt,))
                    a = b
                pb, pt = self.bank()
                c.op("pe", lambda e: e.transpose(pb[:, 0:n], tmp[0:n, ci, :], self.ident_f[0:n, 0:n]),
                     reads=(tt, ct), writes=(pt,))
                c.op("dve", lambda e: e.tensor_copy(self.vecT[:, r0:r0 + n], pb[:, 0:n]), reads=(pt,),
                     writes=(self.vec_tok,))
            c.barrier()

    def vcol(self, li, kind, j):
        base = li * 72 + {"norm_mix": 0, "norm_ff": 8, "b_gate": 16}[kind]
        return self.vecT[:, base + j:base + j + 1]

    def load_x(self):
        c = self.c
        x = self.A["x"]
        with ExitStack() as st:
            xs = [self.sb(st, f"xs{i}", [128, D], F32) for i in range(2)]
            xs_tok = [Tok(), Tok()]
            for t in range(NT):
                b = t % 2
                c.dma(xs[b][:], x[t * 128:(t + 1) * 128, :], writes=(xs_tok[b],))
                for half in range(2):
                    pb, pt = self.bank()
                    for j in range(4):
                        cc = half * 4 + j
                        c.op("pe", lambda e: e.transpose(pb[:, j * 128:(j + 1) * 128], xs[b][:, cc * 128:(cc + 1) * 128],
                                                         self.ident_f[:]),
                             reads=(xs_tok[b], self.const_tok), writes=(pt,), inc=(j == 3))
                    dst = self.xT[:, half * 4:half * 4 + 4, t * 128:(t + 1) * 128]
                    src = pb[:].rearrange("p (j n) -> p j n", j=4)
                    eng = "dve" if half == 0 else "act"
                    if eng == "dve":
                        c.op("dve", lambda e: e.tensor_copy(dst, src), reads=(pt,), writes=(self.xT_tok[t // 4],))
                    else:
                        c.op("act", lambda e: e.copy(dst, src), reads=(pt,), writes=(self.xT_tok[t // 4],))
            c.barrier()

    def load_xT(self):
        raise NotImplementedError

    def store_xT(self, out):
        self.store_tok_major(out, normed=False)

    def rmsnorm_to_hT(self, gcol):
        c = self.c
        for tc in range(NTC):
            ts = slice(tc * 512, (tc + 1) * 512)
            pb, pt = self.bank()
            for cc in range(KC):
                sq, sqt = self.nscr()
                c.op("act", lambda e: e.activation(sq[:], self.xT[:, cc, ts], AF.Square),
                     reads=(self.xT_tok[tc],), writes=(sqt,))
                self.mm(pb[:], self.ones_f[:], sq[:], cc == 0, cc == KC - 1, reads=(sqt, self.const_tok), writes=(pt,))
            rs, rst = self.nscr()
            c.op("dve", lambda e: e.tensor_scalar(rs[:], pb[:], 1.0 / D, EPS, ALU.mult, ALU.add), reads=(pt,),
                 writes=(rst,))
            c.op("act", lambda e: e.activation(rs[:], rs[:], AF.Sqrt), reads=(rst,), writes=(rst,))
            c.op("dve", lambda e: e.reciprocal(rs[:], rs[:]), reads=(rst,), writes=(rst,))
            for cc in range(KC):
                c.op("dve", lambda e: e.scalar_tensor_tensor(self.hT[:, cc, ts], self.xT[:, cc, ts], gcol(cc), rs[:],
                                                             ALU.mult, ALU.mult),
                     reads=(self.xT_tok[tc], rst, self.vec_tok), writes=(self.hT_tok[tc],))

    def layer(self, li):
        self.rmsnorm_to_hT(lambda cc: self.vcol(li, "norm_mix", cc))
        if "hT" in self.dbg_out and li == 0:
            self.dump_featmajor_bf16(self.hT, self.hT_tok, self.dbg_out["hT"])
        if self.stage >= 4:
            self.yT = self.sb(self.es, f"yT{li}", [128, 4, S], BF16) if not hasattr(self, "yT") else self.yT
            self.yT_tok = Tok("yT")
            if self.stage >= 8:
                self.nsa(li)
                if "ynsa" in self.dbg_out and li == 0:
                    self.dump_featmajor_bf16(self.yT, [self.yT_tok], self.dbg_out["ynsa"])
                self.combine(li, 0)
            if self.stage == 8:
                return
            if self.stage >= 7:
                self.gla(li)
                if "ygla" in self.dbg_out and li == 0:
                    self.dump_featmajor_bf16(self.yT, [self.yT_tok], self.dbg_out["ygla"])
                self.combine(li, 2)
            if self.stage >= 6 and self.stage != 7:
                self.sbmix(li)
                if "ysb" in self.dbg_out and li == 0:
                    self.dump_featmajor_bf16(self.yT, [self.yT_tok], self.dbg_out["ysb"])
                self.combine(li, 1)
            if self.stage == 7:
                return
            self.fox(li)
            if "yT" in self.dbg_out and li == 0:
                self.dump_featmajor_bf16(self.yT, [self.yT_tok], self.dbg_out["yT"])
            if self.stage >= 5:
                self.combine(li, 3)
        if self.stage >= 2:
            self.rmsnorm_to_hT(lambda cc: self.vcol(li, "norm_ff", cc))
            self.ffn(li)

    def ffn(self, li):
        c = self.c
        w1 = self.A["w_ff1"][li].rearrange("(c p) n -> p c n", p=128)
        w2 = self.A["w_ff2"][li].rearrange("(f p) n -> p f n", p=128)
        G = 4
        with ExitStack() as st:
            aT = [self.sb(st, f"aT{i}", [128, G, S], BF16) for i in range(2)]
            aT_tok = [Tok(), Tok()]
            import os
            for g in range(int(os.environ.get('FFN_G', DFF // (128 * G)))):
                ab, abt = aT[g % 2], aT_tok[g % 2]
                for half in range(G):
                    f0 = g * G + half
                    wv, wt = self.wload(w1[:, :, f0 * 128:(f0 + 1) * 128], KC, 128)
                    for j in range(1):
                        for tc in range(NTC):
                            ts = slice(tc * 512, (tc + 1) * 512)
                            pb, pt = self.bank()
                            for cc in range(KC):
                                self.mm(pb[:], wv[:, cc, j * 128:(j + 1) * 128], self.hT[:, cc, ts], cc == 0, cc == KC - 1,
                                        reads=(wt, self.hT_tok[tc]), writes=(pt,))
                            r, rt = self.nscr()
                            c.op("act", lambda e: e.activation(r[:], pb[:], AF.Relu), reads=(pt,), writes=(rt,))
                            c.op("dve", lambda e: e.tensor_tensor(ab[:, half, ts], r[:], r[:], ALU.mult),
                                 reads=(rt,), writes=(abt,))
                for dh in range(4):
                    wv, wt = self.wload(w2[:, g * G:(g + 1) * G, dh * 256:(dh + 1) * 256], G, 256)
                    for j in range(2):
                        dt_ = dh * 2 + j
                        for tc in range(NTC):
                            ts = slice(tc * 512, (tc + 1) * 512)
                            pb, pt = self.bank()
                            for f in range(G):
                                self.mm(pb[:], wv[:, f, j * 128:(j + 1) * 128], ab[:, f, ts], f == 0, f == G - 1,
                                        reads=(wt, abt), writes=(pt,))
                            c.op("dve", lambda e: e.tensor_tensor(self.xT[:, dt_, ts], self.xT[:, dt_, ts], pb[:], ALU.add),
                                 reads=(pt,), writes=(self.xT_tok[tc],))
            c.barrier()


    def proj_feat(self, li, col0, ncols, evac):
        w = self.A["w_in"][li].rearrange("(c p) n -> p c n", p=128)
        n0 = 0
        while n0 < ncols:
            nn = min(128, ncols - n0)
            wv, wt = self.wload(w[:, :, col0 + n0:col0 + n0 + nn], KC, nn)
            for j in range((nn + 127) // 128):
                m = min(128, nn - j * 128)
                for tc in range(NTC):
                    ts = slice(tc * 512, (tc + 1) * 512)
                    pb, pt = self.bank()
                    for cc in range(KC):
                        self.mm(pb[0:m, :], wv[:, cc, j * 128:j * 128 + m], self.hT[:, cc, ts], cc == 0, cc == KC - 1,
                                reads=(wt, self.hT_tok[tc]), writes=(pt,))
                    evac((n0 + j * 128) // 128, tc, pb, pt)
            n0 += nn

    def proj_tok(self, li, col0, ncols, evac):
        w = self.A["w_in"][li].rearrange("(c p) n -> p c n", p=128)
        wv, wt = self.wload(w[:, :, col0:col0 + ncols], KC, ncols)
        for t in range(NT):
            pb, pt = self.bank()
            for cc in range(KC):
                self.mm(pb[:, 0:ncols], self.hT[:, cc, t * 128:(t + 1) * 128], wv[:, cc, :], cc == 0, cc == KC - 1,
                        reads=(wt, self.hT_tok[t // 4]), writes=(pt,))
            evac(t, pb, pt)

    def evac_featT(self, dst, dtok, scale=1.0):
        c = self.c
        cnt = [0]

        def f(mt, tc, pb, pt):
            ts = slice(tc * 512, (tc + 1) * 512)
            cnt[0] += 1
            if cnt[0] % 2 == 0:
                c.op("dve", lambda e: e.tensor_scalar(dst[:, mt, ts], pb[:], scale, None, ALU.mult), reads=(pt,),
                     writes=(dtok,))
            else:
                c.op("act", lambda e: e.activation(dst[:, mt, ts], pb[:], AF.Copy, scale=scale), reads=(pt,),
                     writes=(dtok,))
        return f

    def attention(self, st, name, nheads, qT, qtok, kT, ktok, V, vtok, ytok_t, ytok_tok, bias_fn=None, ycol0=0, post_qc=None):
        c = self.c
        pT = [self.sb(st, f"{name}_pT{i}", [128, 512], BF16) for i in range(6)]
        pT_tok = [Tok() for _ in range(6)]
        rc = self.sb(st, f"{name}_rc", [128, 12], F32)
        rc_tok = [Tok(), Tok(), Tok()]
        pi = [0]

        def head_stream(qc, h):
            hp = slice((h % 2) * 64, (h % 2) * 64 + 64)
            hc = h // 2
            ob, ot = self.bank_acc()
            O = ob[:, 0:260].rearrange("p (j d) -> p j d", j=4)
            nkt = 4 * qc + 4
            for kt in range(nkt):
                j0 = max(0, kt - 4 * qc)
                q0 = qc * 512 + j0 * 128
                ncol = 512 - j0 * 128
                sb_, stk = self.bank()
                diag = kt >= 4 * qc
                self.mm(sb_[:, 0:ncol], kT[hp, hc, kt * 128:(kt + 1) * 128], qT[hp, hc, q0:q0 + ncol], True, not diag,
                        reads=(ktok, qtok), writes=(stk,))
                if diag:
                    self.mm(sb_[:, 0:128], self.ident_b[:], self.cneg_b[:], False, True,
                            reads=(self.const_tok,), writes=(stk,), skip_group_check=True)
                p, ptk = pT[pi[0] % 6], pT_tok[pi[0] % 6]
                pi[0] += 1
                for half in range(2):
                    jl, jh = max(j0, 2 * half), 2 * half + 1
                    if jl > jh:
                        continue
                    cs = slice((jl - j0) * 128, (jh - j0 + 1) * 128)
                    b = bias_fn(h, kt, qc * 4 + 2 * half + 1) if bias_fn is not None else 0.0
                    c.op("act", lambda e: e.activation(p[:, cs], sb_[:, cs], AF.Exp, bias=b), reads=(stk, self.aux_tok),
                         writes=(ptk,))
                yield
                for j in range(j0, 4):
                    qt = qc * 4 + j
                    cs = slice((j - j0) * 128, (j - j0 + 1) * 128)
                    self.mm(O[:, j, :], p[:, cs], V[:, kt, h, :], kt == 0 and j == 0, kt == qt, reads=(ptk, vtok), writes=(ot,),
                            skip_group_check=True)
            rct = rc_tok[h % 3]
            for j in range(4):
                rcj = rc[:, (h % 3) * 4 + j:(h % 3) * 4 + j + 1]
                c.op("dve", lambda e: e.reciprocal(rcj, O[:, j, 64:65]), reads=(ot,), writes=(rct,))
                c.op("dve", lambda e: e.tensor_scalar(ytok_t[:, j, ycol0 + h * 64:ycol0 + (h + 1) * 64], O[:, j, 0:64],
                                                      rcj, None, ALU.mult),
                     reads=(ot, rct), writes=(ytok_tok,))
            self.release_acc(ob)

        c.barrier()
        self.n_scr_banks = 5
        for qc in range(NTC):
            self.run_streams([head_stream(qc, h) for h in range(nheads)], 3)
            if post_qc is not None:
                post_qc(qc)
        c.barrier()
        self.n_scr_banks = 6

    def ytok_to_yT(self, ytok_t, ytok_tok, qc):
        c = self.c
        for tl in range(4):
            t = qc * 4 + tl
            pb, pt = self.bank()
            pbb = pb[:].bitcast(BF16)
            for j in range(4):
                c.op("pe", lambda e: e.transpose(pbb[:, j * 128:(j + 1) * 128], ytok_t[:, tl, j * 128:(j + 1) * 128],
                                                 self.ident_b[:]),
                     reads=(ytok_tok, self.const_tok), writes=(pt,))
            c.op("dve", lambda e: e.tensor_copy(self.yT[:, :, t * 128:(t + 1) * 128],
                                                pbb[:, 0:512].rearrange("p (j n) -> p j n", j=4)),
                 reads=(pt,), writes=(self.yT_tok,))


    def combine(self, li, bi):
        c = self.c
        wg = self.A["w_in"][li].rearrange("(c p) n -> p c n", p=128)
        wb = self.A["w_branch"][li, bi].rearrange("(c p) n -> p c n", p=128)
        wo = self.A["w_out"][li].rearrange("(c p) n -> p c n", p=128)
        with ExitStack() as st:
            mT = self.sb(st, "mT", [128, KC, S], BF16)
            mtok = Tok()
            for dt_ in range(KC):
                g0 = O_GATE + bi * D + dt_ * 128
                wgv, wgt = self.wload(wg[:, :, g0:g0 + 128], KC, 128)
                wbv, wbt = self.wload(wb[:, :, dt_ * 128:(dt_ + 1) * 128], 4, 128)
                bcol = self.vcol(li, "b_gate", bi * 8 + dt_)
                for tc in range(NTC):
                    ts = slice(tc * 512, (tc + 1) * 512)
                    pa, pat = self.bank()
                    for cc in range(KC):
                        self.mm(pa[:], wgv[:, cc, :], self.hT[:, cc, ts], cc == 0, cc == KC - 1,
                                reads=(wgt, self.hT_tok[tc]), writes=(pat,))
                    pb, pbt = self.bank()
                    for cc in range(4):
                        self.mm(pb[:], wbv[:, cc, :], self.yT[:, cc, ts], cc == 0, cc == 3,
                                reads=(wbt, self.yT_tok), writes=(pbt,))
                    sg, sgt = self.nscr()
                    c.op("act", lambda e: e.activation(sg[:], pa[:], AF.Sigmoid, bias=bcol), reads=(pat, self.vec_tok),
                         writes=(sgt,))
                    c.op("dve", lambda e: e.tensor_tensor(mT[:, dt_, ts], sg[:], pb[:], ALU.mult), reads=(sgt, pbt),
                         writes=(mtok,))
            for do in range(KC):
                wov, wot = self.wload(wo[:, :, do * 128:(do + 1) * 128], KC, 128)
                for tc in range(NTC):
                    ts = slice(tc * 512, (tc + 1) * 512)
                    pb, pbt = self.bank()
                    for cc in range(KC):
                        self.mm(pb[:], wov[:, cc, :], mT[:, cc, ts], cc == 0, cc == KC - 1, reads=(wot, mtok), writes=(pbt,))
                    c.op("dve", lambda e: e.tensor_tensor(self.xT[:, do, ts], self.xT[:, do, ts], pb[:], ALU.add),
                         reads=(pbt,), writes=(self.xT_tok[tc],))
            c.barrier()


    def sbmix(self, li):
        c = self.c
        with ExitStack() as st:
            qT = self.sb(st, "sb_qT", [128, 4, S], BF16)
            kT = self.sb(st, "sb_kT", [128, 4, S], BF16)
            V = self.sb(st, "sb_V", [128, NT, 8, 64], BF16)
            ytk = self.sb(st, "sb_y", [128, 4, 512], BF16)
            spb = [[self.sb(st, f"sb_sp{k}{i}", [128, 512], BF16) for i in range(2)] for k in range(2)]
            spt = [[Tok(), Tok()] for k in range(2)]
            pT = [[self.sb(st, f"sb_pT{k}{i}", [128, 512], BF16) for i in range(2)] for k in range(2)]
            pTt = [[Tok(), Tok()] for k in range(2)]
            sufs = [(self.sb(st, f"sb_suf{k}", [1, 512], F32), self.sb(st, f"sb_sufh{k}", [1, 512], BF16),
                     self.sb(st, f"sb_sufl{k}", [1, 512], BF16), Tok()) for k in range(2)]
            qtok, ktok, vtok, ytok = Tok(), Tok(), Tok(), Tok()
            self.proj_feat(li, O_SQ, 512, self.evac_featT(qT, qtok, 0.125))
            self.proj_feat(li, O_SK, 512, self.evac_featT(kT, ktok, 1.0))
            for q4 in range(4):
                def evac_vh(t, pb, pt, q4=q4):
                    c.op("act", lambda e: e.copy(V[:, t, q4 * 2:q4 * 2 + 2, :],
                                                 pb[:, 0:128].rearrange("p (h d) -> p h d", h=2)),
                         reads=(pt,), writes=(vtok,))
                self.proj_tok(li, O_SV + q4 * 128, 128, evac_vh)
            def head_stream(qc, h):
                s_ = h % 2
                hp = slice((h % 2) * 64, (h % 2) * 64 + 64)
                hc = h // 2
                ob, ot = self.bank_acc()
                O = ob[:, 0:256].rearrange("p (j d) -> p j d", j=4)
                nkt = 4 * qc + 4
                suf, sufh, sufl, suft = sufs[s_]
                c.op("dve", lambda e: e.memset(suf[:], 0.0), writes=(suft,))
                c.op("dve", lambda e: e.memset(sufh[:], 0.0), writes=(suft,))
                c.op("dve", lambda e: e.memset(sufl[:], 0.0), writes=(suft,))
                first = True
                ti = 0
                for kt in range(nkt - 1, -1, -1):
                    j0 = max(0, kt - 4 * qc)
                    q0 = qc * 512 + j0 * 128
                    ncol = 512 - j0 * 128
                    diag = kt >= 4 * qc
                    ksl = kT[hp, hc, kt * 128:(kt + 1) * 128]
                    qsl = qT[hp, hc, q0:q0 + ncol]
                    pa, pat = self.bank()
                    self.mm(pa[:, 0:ncol], ksl, qsl, True, not diag, reads=(ktok, qtok), writes=(pat,))
                    if diag:
                        self.mm(pa[:, 0:128], self.ident_b[:], self.cnegs_b[:], False, True, reads=(self.const_tok,),
                                writes=(pat,), skip_group_check=True)
                    e_, et = self.nscr()
                    sp, spk = spb[s_][ti % 2], spt[s_][ti % 2]
                    p, ptk = pT[s_][ti % 2], pTt[s_][ti % 2]
                    ti += 1
                    c.op("act", lambda e: e.activation(e_[:, 0:ncol], pa[:, 0:ncol], AF.Exp), reads=(pat,), writes=(et,))
                    c.op("act", lambda e: e.activation(sp[:, 0:ncol], e_[:, 0:ncol], AF.Ln, bias=1.0), reads=(et,),
                         writes=(spk,))
                    yield
                    pb, pbt = self.bank()
                    self.mm(pb[:, 0:ncol], ksl, qsl, True, False, reads=(ktok, qtok), writes=(pbt,))
                    if diag:
                        self.mm(pb[:, 0:128], self.ident_b[:], self.cnegs_b[:], False, False, reads=(self.const_tok,),
                                writes=(pbt,), skip_group_check=True)
                    self.mm(pb[:, 0:ncol], self.nones_b[0:1, :], sufh[0:1, 512 - ncol:512], False, False,
                            reads=(suft, self.const_tok), writes=(pbt,), skip_group_check=True)
                    self.mm(pb[:, 0:ncol], self.nones_b[0:1, :], sufl[0:1, 512 - ncol:512], False, False,
                            reads=(suft, self.const_tok), writes=(pbt,), skip_group_check=True)
                    self.mm(pb[:, 0:ncol], self.ntri_b[:], sp[:, 0:ncol], False, True, reads=(spk, self.const_tok),
                            writes=(pbt,), skip_group_check=True)
                    if kt > 0:
                        pc, pct = self.bank()
                        self.mm(pc[0:1, 0:ncol], self.ones_b[:, 0:1], sp[:, 0:ncol], True, True,
                                reads=(spk, self.const_tok), writes=(pct,))
                        sl = slice(512 - ncol, 512)
                        c.op("dve", lambda e: e.tensor_tensor(suf[0:1, sl], suf[0:1, sl], pc[0:1, 0:ncol], ALU.add),
                             reads=(pct,), writes=(suft,))
                        c.op("dve", lambda e: e.tensor_copy(sufh[0:1, sl], suf[0:1, sl]), reads=(suft,), writes=(suft,))
                        c.op("dve", lambda e: e.tensor_tensor(sufl[0:1, sl], suf[0:1, sl], sufh[0:1, sl], ALU.subtract),
                             reads=(suft,), writes=(suft,))
                    c.op("act", lambda e: e.activation(p[:, 0:ncol], pb[:, 0:ncol], AF.Exp), reads=(pbt,), writes=(ptk,))
                    yield
                    for j in range(j0, 4):
                        cs = slice((j - j0) * 128, (j - j0 + 1) * 128)
                        self.mm(O[:, j, :], p[:, cs], V[:, kt, h, :], first, kt == 0, reads=(ptk, vtok), writes=(ot,),
                                skip_group_check=True)
                        first = False
                for j in range(4):
                    c.op("dve", lambda e: e.tensor_copy(ytk[:, j, h * 64:(h + 1) * 64], O[:, j, :]), reads=(ot,),
                         writes=(ytok,))
                self.release_acc(ob)

            for qc in range(NTC):
                self.run_streams([head_stream(qc, h) for h in range(8)], 2)
                self.ytok_to_yT(ytk, ytok, qc)
            c.barrier()

    def gla(self, li):
        c = self.c
        ct = self.const_tok
        with ExitStack() as st:
            qeT = self.sb(st, "g_qe", [64, 4, S], BF16)
            keT = self.sb(st, "g_ke", [64, 4, S], BF16)
            k2 = self.sb(st, "g_k2", [128, NT, 256], BF16)
            vtk = self.sb(st, "g_v", [128, NT, 512], BF16)
            gnorm = self.sb(st, "g_norm", [128, 1], F32)
            dec = self.sb(st, "g_dec", [64, 4, 32], F32)
            tblk = self.sb(st, "g_tblk", [128, 128], F32)
            sp_ = ExitStack()
            alrT = self.sb(sp_, "g_alr", [16, S], BF16)
            balb = self.sb(sp_, "g_bal", [128, 256], F32)
            wal_f = self.sb(sp_, "g_walf", [16, 256], F32)
            wal_b = self.sb(sp_, "g_walb", [16, 256], BF16)
            n16 = self.sb(sp_, "g_n16", [128, 128], F32)
            m1 = self.sb(sp_, "g_m1", [128, 128], F32)
            m2 = self.sb(sp_, "g_m2", [128, 128], F32)
            att_tok = [Tok(), Tok()]
            mtok, qtok, ktok, vtok, k2tok, atok, ptok, stok, otok, ontok = [Tok() for _ in range(10)]
            c.op("pool", lambda e: e.memset(n16[:], -1.0 / 16.0), writes=(mtok,))
            c.op("pool", lambda e: e.affine_select(m1[:], n16[:], [[1, 128]], ALU.is_ge, 0.0, base=0, channel_multiplier=-1),
                 reads=(mtok,), writes=(mtok,))
            c.op("pool", lambda e: e.memset(m1[0:64, 64:128], 0.0), reads=(mtok,), writes=(mtok,))
            c.op("pool", lambda e: e.affine_select(m2[:], n16[:], [[-1, 128]], ALU.is_gt, 0.0, base=0, channel_multiplier=1),
                 reads=(mtok,), writes=(mtok,))
            c.op("pool", lambda e: e.memset(m2[64:128, 0:64], 0.0), reads=(mtok,), writes=(mtok,))
            c.op("pool", lambda e: e.tensor_copy(tblk[:], self.tri_f[:]), reads=(ct, mtok), writes=(mtok,))
            c.op("pool", lambda e: e.memset(tblk[0:64, 64:128], 0.0), reads=(mtok,), writes=(mtok,))
            c.dma(balb[:], self.A["gla_b_alpha"][li:li + 1, :].partition_broadcast(128), writes=(ptok,))
            c.dma(wal_f[:], self.A["gla_w_alpha"][li], writes=(ptok,))
            c.dma(gnorm[:], self.A["gla_norm"][li].rearrange("(p o) -> p o", o=1), writes=(ptok,))
            c.op("pool", lambda e: e.tensor_copy(wal_b[:], wal_f[:]), reads=(ptok,), writes=(ptok,))
            for h in range(4):
                def ev_q(mt, tc, pb, pt, h=h):
                    ts = slice(tc * 512, (tc + 1) * 512)
                    c.op("act", lambda e: e.activation(qeT[0:64, h, ts], pb[0:64, :], AF.Copy, scale=0.125), reads=(pt,),
                         writes=(qtok,))

                def ev_k(mt, tc, pb, pt, h=h):
                    ts = slice(tc * 512, (tc + 1) * 512)
                    c.op("dve", lambda e: e.tensor_copy(keT[0:64, h, ts], pb[0:64, :]), reads=(pt,), writes=(ktok,))
                self.proj_feat(li, O_GQ + h * 64, 64, ev_q)
                self.proj_feat(li, O_GK + h * 64, 64, ev_k)

            def ev_a(mt, tc, pb, pt):
                ts = slice(tc * 512, (tc + 1) * 512)
                c.op("act", lambda e: e.copy(alrT[0:16, ts], pb[0:16, :]), reads=(pt,), writes=(atok,))
            self.proj_feat(li, O_GA, 16, ev_a)
            for i in range(4):
                def ev_v(t, pb, pt, i=i):
                    c.op("act", lambda e: e.copy(vtk[:, t, i * 128:(i + 1) * 128], pb[:, 0:128]), reads=(pt,), writes=(vtok,))
                self.proj_tok(li, O_GV + i * 128, 128, ev_v)
            import os
            gstop = int(os.environ.get("GLA_STOP", "99"))
            if gstop == 1:
                c.barrier()
                return
            for t in range(NT):
                tl = slice(t * 128, (t + 1) * 128)
                pa, pat = self.bank()
                self.mm(pa[:, 0:256], alrT[0:16, tl], wal_b[0:16, :], True, True, reads=(atok, ptok), writes=(pat,))
                xs, xst = self.nscr()
                c.op("dve", lambda e: e.tensor_tensor(xs[:, 0:256], pa[:, 0:256], balb[:], ALU.add), reads=(pat, ptok),
                     writes=(xst,))
                c.op("act", lambda e: e.activation(xs[:, 0:256], xs[:, 0:256], AF.Exp, scale=-1.0), reads=(xst,), writes=(xst,))
                c.op("act", lambda e: e.activation(xs[:, 0:256], xs[:, 0:256], AF.Ln, bias=1.0), reads=(xst,), writes=(xst,))
                pw, pwt = self.bank()
                self.mm(pw[:, 0:256], m2[:], xs[:, 0:256], True, True, reads=(mtok, xst), writes=(pwt,))
                c.op("act", lambda e: e.activation(k2[:, t, :], pw[:, 0:256], AF.Exp), reads=(pwt,), writes=(k2tok,))
                pbT, pbTt = self.bank()
                for h in range(4):
                    self.mm(pbT[0:64, h * 128:(h + 1) * 128], xs[:, h * 64:(h + 1) * 64], m1[:], h == 0, h == 3,
                            reads=(mtok, xst), writes=(pbTt,), skip_group_check=True)
                ebp, ebpt = self.nscr()
                ebn, ebnt = self.nscr()
                c.op("act", lambda e: e.activation(ebp[0:64, :], pbT[0:64, :], AF.Exp), reads=(pbTt,), writes=(ebpt,))
                c.op("act", lambda e: e.activation(ebn[0:64, :], pbT[0:64, :], AF.Exp, scale=-1.0), reads=(pbTt,), writes=(ebnt,))
                c.op("dve", lambda e: e.tensor_tensor(qeT[0:64, :, tl], qeT[0:64, :, tl],
                                                      ebp[0:64, :].rearrange("p (h n) -> p h n", h=4), ALU.mult),
                     reads=(ebpt,), writes=(qtok,))
                c.op("dve", lambda e: e.tensor_tensor(keT[0:64, :, tl], keT[0:64, :, tl],
                                                      ebn[0:64, :].rearrange("p (h n) -> p h n", h=4), ALU.mult),
                     reads=(ebnt,), writes=(ktok,))
                c.op("dve", lambda e: e.tensor_copy(dec[0:64, :, 2 * t:2 * t + 2],
                                                    ebp[0:64, :].rearrange("p (h c s) -> p h c s", h=4, c=2)[:, :, :, 63]),
                     reads=(ebpt,), writes=(stok,))
            c.barrier()
            sp_.close()
            if gstop == 2:
                return
            st_f = self.sb(st, "g_stf", [64, 4, 128], F32)
            st_b = self.sb(st, "g_stb", [64, 4, 128], BF16)
            oT = self.sb(st, "g_oT", [128, 512], F32)
            onh = self.sb(st, "g_on", [128, S], BF16)
            attb = [self.sb(st, f"g_att{i}", [128, 128], BF16) for i in range(2)]
            for i in range(2):
                def ev_k2(t, pb, pt, i=i):
                    c.op("dve", lambda e: e.tensor_tensor(k2[:, t, i * 128:(i + 1) * 128], k2[:, t, i * 128:(i + 1) * 128],
                                                          pb[:, 0:128], ALU.mult), reads=(pt,), writes=(k2tok,))
                self.proj_tok(li, O_GK + i * 128, 128, ev_k2)
            if gstop == 3:
                c.barrier()
                return
            ai = 0
            for h in range(4):
                c.op("dve", lambda e: e.memset(st_f[0:64, h, :], 0.0), writes=(stok,))
                c.op("dve", lambda e: e.memset(st_b[0:64, h, :], 0.0), writes=(stok,))
                for t in range(NT):
                    tl = slice(t * 128, (t + 1) * 128)
                    pa, pat = self.bank()
                    self.mm(pa[:, 0:128], keT[0:64, h, tl], qeT[0:64, h, tl], True, True, reads=(ktok, qtok), writes=(pat,))
                    ab, abt = attb[ai % 2], att_tok[ai % 2]
                    ai += 1
                    c.op("dve", lambda e: e.tensor_tensor(ab[:], pa[:, 0:128], tblk[:], ALU.mult), reads=(pat, mtok),
                         writes=(abt,))
                    for half in range(2):
                        cn = 2 * t + half
                        rs = slice(half * 64, half * 64 + 64)
                        cs = slice(cn * 64, (cn + 1) * 64)
                        vsl = vtk[rs, t, h * 128:(h + 1) * 128]
                        po, pot = self.bank()
                        inter = cn > 0 and gstop != 4
                        self.mm(po[:, 0:64], vsl, ab[rs, rs], True, True, reads=(vtok, abt), writes=(pot,))
                        oc = (t % 4) * 128 + half * 64
                        c.op("act", lambda e: e.copy(oT[:, oc:oc + 64], po[:, 0:64]), reads=(pot,), writes=(otok,))
                        if inter:
                            pi_, pit = self.bank()
                            self.mm(pi_[:, 0:64], st_b[0:64, h, :], qeT[0:64, h, cs], True, True, reads=(stok, qtok),
                                    writes=(pit,))
                            c.op("dve", lambda e: e.tensor_tensor(oT[:, oc:oc + 64], oT[:, oc:oc + 64], pi_[:, 0:64], ALU.add),
                                 reads=(pit, otok), writes=(otok,))
                        if gstop == 5:
                            continue
                        ps_, pst = self.bank()
                        self.mm(ps_[0:64, 0:128], k2[rs, t, h * 64:(h + 1) * 64], vsl, True, True, reads=(k2tok, vtok),
                                writes=(pst,))
                        c.op("dve", lambda e: e.scalar_tensor_tensor(st_f[0:64, h, :], st_f[0:64, h, :], dec[0:64, h, cn:cn + 1],
                                                                     ps_[0:64, 0:128], ALU.mult, ALU.add),
                             reads=(pst, stok), writes=(stok,))
                        c.op("dve", lambda e: e.tensor_copy(st_b[0:64, h, :], st_f[0:64, h, :]), reads=(stok,), writes=(stok,))
                    if t % 4 == 3:
                        tc = t // 4
                        ts = slice(tc * 512, (tc + 1) * 512)
                        sq, sqt = self.nscr()
                        c.op("act", lambda e: e.activation(sq[:], oT[:], AF.Square), reads=(otok,), writes=(sqt,))
                        pn, pnt = self.bank()
                        self.mm(pn[:], self.ones_f[:], sq[:], True, True, reads=(sqt, ct), writes=(pnt,))
                        rr, rrt = self.nscr()
                        c.op("dve", lambda e: e.tensor_scalar(rr[:], pn[:], 1.0 / 128.0, EPS, ALU.mult, ALU.add), reads=(pnt,),
                             writes=(rrt,))
                        c.op("act", lambda e: e.activation(rr[:], rr[:], AF.Sqrt), reads=(rrt,), writes=(rrt,))
                        c.op("dve", lambda e: e.reciprocal(rr[:], rr[:]), reads=(rrt,), writes=(rrt,))
                        c.op("dve", lambda e: e.scalar_tensor_tensor(onh[:, ts], oT[:], gnorm[:, 0:1], rr[:], ALU.mult, ALU.mult),
                             reads=(otok, rrt, ptok), writes=(ontok,))

                def ev_g(mt, tc, pb, pt, h=h):
                    ts = slice(tc * 512, (tc + 1) * 512)
                    sg, sgt = self.nscr()
                    c.op("act", lambda e: e.activation(sg[:], pb[:], AF.Silu), reads=(pt,), writes=(sgt,))
                    c.op("dve", lambda e: e.tensor_tensor(self.yT[:, h, ts], sg[:], onh[:, ts], ALU.mult), reads=(sgt, ontok),
                         writes=(self.yT_tok,))
                self.proj_feat(li, O_GG + h * 128, 128, ev_g)
            c.barrier()


    def proj_feat_dup(self, li, col0, evac):
        c = self.c
        w = self.A["w_in"][li].rearrange("(c p) n -> p c n", p=128)
        wv, wt = self.wload(w[:, :, col0:col0 + 64], KC, 64)
        wd, wdt = self.wdup, self.wdup_tok
        c.op("pool", lambda e: e.tensor_copy(wd[:, :, 0:64], wv), reads=(wt,), writes=(wdt,))
        c.op("pool", lambda e: e.tensor_copy(wd[:, :, 64:128], wv), reads=(wt,), writes=(wdt,))
        for tc in range(NTC):
            ts = slice(tc * 512, (tc + 1) * 512)
            pb, pt = self.bank()
            for cc in range(KC):
                self.mm(pb[:], wd[:, cc, :], self.hT[:, cc, ts], cc == 0, cc == KC - 1, reads=(wdt, self.hT_tok[tc]),
                        writes=(pt,))
            evac(0, tc, pb, pt)

    def rope_apply(self, dst, pb, pt, n, scale, cos_ap, sin_ap, dtok):
        c = self.c
        raw, rawt = self.rraw[self.rr_i % 2], self.rraw_tok[self.rr_i % 2]
        self.rr_i += 1
        c.op("act", lambda e: e.activation(raw[:, 0:n], pb, AF.Copy, scale=scale), reads=(pt,), writes=(rawt,))
        p2, p2t = self.bank()
        self.mm(p2[:, 0:n], self.Pm[:], raw[:, 0:n], True, True, reads=(rawt, self.tbl_tok), writes=(p2t,))
        t1, t1t = self.nscr()
        c.op("pool", lambda e: e.tensor_tensor(t1[:, 0:n], raw[:, 0:n], cos_ap, ALU.mult), reads=(rawt, self.tbl_tok),
             writes=(t1t,))
        t2, t2t = self.nscr()
        c.op("dve", lambda e: e.tensor_tensor(t2[:, 0:n], p2[:, 0:n], sin_ap, ALU.mult), reads=(p2t, self.tbl_tok),
             writes=(t2t,))
        c.op("dve", lambda e: e.tensor_tensor(dst, t1[:, 0:n], t2[:, 0:n], ALU.add), reads=(t1t, t2t), writes=(dtok,))

    def nsa_tables(self, cosT, sinT):
        c = self.c
        tbl = self.tbl_tok
        PI = float(np.pi)
        C1 = 6.28125
        C2 = float(2 * np.pi - 6.28125)
        with ExitStack() as s2:
            pidx = self.sb(s2, "n_pi", [128, 1], I32)
            f = self.sb(s2, "n_f", [128, 8], F32)
            posi = self.sb(s2, "n_posi", [128, 512], I32)
            ki = self.sb(s2, "n_ki", [128, 512], I32)
            ftok, ptok = Tok(), Tok()
            c.op("pool", lambda e: e.iota(pidx[:], [[0, 1]], base=0, channel_multiplier=1), writes=(ftok,))
            PF, GE, DD, G8, II, ACTV, SGN, INV = [f[:, i:i + 1] for i in range(8)]
            V = lambda fn: c.op("dve", fn, reads=(ftok,), writes=(ftok,))
            V(lambda e: e.tensor_copy(PF, pidx[:]))
            V(lambda e: e.tensor_single_scalar(GE, PF, 64.0, ALU.is_ge))
            V(lambda e: e.scalar_tensor_tensor(DD, GE, -64.0, PF, ALU.mult, ALU.add))
            V(lambda e: e.tensor_single_scalar(G8, DD, 8.0, ALU.is_ge))
            V(lambda e: e.scalar_tensor_tensor(II, G8, -8.0, DD, ALU.mult, ALU.add))
            V(lambda e: e.tensor_single_scalar(ACTV, DD, 16.0, ALU.is_lt))
            V(lambda e: e.tensor_scalar(SGN, G8, 2.0, -1.0, ALU.mult, ALU.add))
            V(lambda e: e.memset(INV, 0.0))
            for i in range(8):
                ci = float(np.float32(500000.0) ** np.float32(-i / 8.0))
                V(lambda e: e.tensor_scalar(GE, II, float(i), ci, ALU.is_equal, ALU.mult))
                V(lambda e: e.tensor_tensor(INV, INV, GE, ALU.add))
            V(lambda e: e.tensor_tensor(INV, INV, ACTV, ALU.mult))
            for tc in range(NTC):
                ts = slice(tc * 512, (tc + 1) * 512)
                c.dma(posi[:], self.A["positions"][0:1, ts].partition_broadcast(128), writes=(ptok,))
                ang, angt = self.nscr()
                c.op("dve", lambda e: e.tensor_copy(ang[:], posi[:]), reads=(ptok,), writes=(angt,))
                c.op("dve", lambda e: e.tensor_scalar(ang[:], ang[:], INV, None, ALU.mult), reads=(angt, ftok), writes=(angt,))
                for phase, dstT, use_sign in ((0.0, sinT, True), (PI / 2, cosT, False)):
                    u, ut = self.nscr()
                    r, rt = self.nscr()
                    c.op("dve", lambda e: e.tensor_scalar(u[:], ang[:], phase, 1.0 / (2 * PI), ALU.add, ALU.mult),
                         reads=(angt,), writes=(ut,))
                    c.op("dve", lambda e: e.tensor_copy(ki[:], u[:]), reads=(ut,), writes=(ptok,))
                    c.op("dve", lambda e: e.tensor_copy(u[:], ki[:]), reads=(ptok,), writes=(ut,))
                    c.op("dve", lambda e: e.scalar_tensor_tensor(r[:], u[:], -C1, ang[:], ALU.mult, ALU.add),
                         reads=(ut, angt), writes=(rt,))
                    c.op("dve", lambda e: e.scalar_tensor_tensor(r[:], u[:], -C2, r[:], ALU.mult, ALU.add), reads=(ut, rt),
                         writes=(rt,))
                    if phase != 0.0:
                        c.op("dve", lambda e: e.tensor_scalar(r[:], r[:], phase, None, ALU.add), reads=(rt,), writes=(rt,))
                    c.op("dve", lambda e: e.tensor_single_scalar(u[:], r[:], PI, ALU.is_gt), reads=(rt,), writes=(ut,))
                    c.op("dve", lambda e: e.scalar_tensor_tensor(r[:], u[:], -2 * PI, r[:], ALU.mult, ALU.add), reads=(ut, rt),
                         writes=(rt,))
                    c.op("dve", lambda e: e.tensor_single_scalar(u[:], r[:], -PI, ALU.is_lt), reads=(rt,), writes=(ut,))
                    c.op("dve", lambda e: e.scalar_tensor_tensor(r[:], u[:], 2 * PI, r[:], ALU.mult, ALU.add), reads=(ut, rt),
                         writes=(rt,))
                    c.op("dve", lambda e: e.tensor_scalar(r[:], r[:], PI, -PI, ALU.min, ALU.max), reads=(rt,), writes=(rt,))
                    c.op("act", lambda e: e.activation(r[:], r[:], AF.Sin), reads=(rt,), writes=(rt,))
                    if use_sign:
                        c.op("dve", lambda e: e.tensor_scalar(dstT[:, ts], r[:], SGN, None, ALU.mult), reads=(rt, ftok),
                             writes=(tbl,))
                    else:
                        c.op("dve", lambda e: e.tensor_copy(dstT[:, ts], r[:]), reads=(rt,), writes=(tbl,))
            c.barrier()

    def nsa(self, li):
        c = self.c
        ct = self.const_tok
        import os
        nstop = int(os.environ.get("NSA_STOP", "99"))
        with ExitStack() as st:
            kcT2 = self.sb(st, "n_kcT", [128, 2, 128], BF16)
            VCX = self.sb(st, "n_vcx", [128, 2, 97], BF16)
            cmptok = Tok()

            def open_tables(sx):
                cosT = self.sb(sx, "n_cos", [128, S], BF16)
                sinT = self.sb(sx, "n_sin", [128, S], BF16)
                self.Pm = self.sb(sx, "n_Pm", [128, 128], BF16)
                self.tbl_tok = Tok()
                self.rraw = [self.sb(sx, f"n_raw{i}", [128, 512], BF16) for i in range(2)]
                self.rraw_tok = [Tok(), Tok()]
                self.rr_i = 0
                self.wdup = self.sb(sx, "n_wdup", [128, KC, 128], BF16)
                self.wdup_tok = Tok()
                self.nsa_tables(cosT, sinT)
                c.op("pool", lambda e: e.memset(self.Pm[:], 0.0), writes=(self.tbl_tok,))
                for (d0, s0) in ((0, 8), (8, 0), (64, 72), (72, 64)):
                    c.op("pool", lambda e: e.tensor_copy(self.Pm[:, d0:d0 + 8], self.ident_b[:, s0:s0 + 8]),
                         reads=(ct, self.tbl_tok), writes=(self.tbl_tok,))
                return cosT, sinT
            sA = ExitStack()
            cosT, sinT = open_tables(sA)
            tbl = self.tbl_tok
            if "cosT" in self.dbg_out:
                self.dump_featmajor_bf16(cosT[:].rearrange("p (c s) -> p c s", c=1), [tbl], self.dbg_out["cosT"])
                self.dump_featmajor_bf16(sinT[:].rearrange("p (c s) -> p c s", c=1), [tbl], self.dbg_out["sinT"])
            with ExitStack() as s3:
                xcT = [self.sb(s3, "n_xk", [128, S], BF16), self.sb(s3, "n_xv", [128, S], BF16)]
                xtok = Tok()
                for kv, col0 in ((0, O_NKC), (1, O_NVC)):
                    def ev_x(mt, tc, pb, pt, kv=kv):
                        ts = slice(tc * 512, (tc + 1) * 512)
                        c.op("act", lambda e: e.copy(xcT[kv][:, ts], pb[:]), reads=(pt,), writes=(xtok,))
                    self.proj_feat(li, col0, 128, ev_x)
                W1 = self.sb(s3, "n_w1", [128, 32, 256], BF16)
                stg = self.sb(s3, "n_stg", [128, 2048], F32)
                W2f = self.sb(s3, "n_w2f", [128, 2, 64], F32)
                W2d = self.sb(s3, "n_w2d", [128, 2, 128], BF16)
                pe2 = self.sb(s3, "n_pe2", [32, 128], F32)
                peb = self.sb(s3, "n_peb", [128, 32], BF16)
                gh = self.sb(s3, "n_gh", [128, 2, 128], BF16)
                hb = self.sb(s3, "n_hb", [128, 2], F32)
                ovf = self.sb(s3, "n_ovf", [128, 3, 32], F32)
                stgt, w1t, w2t, pet, ght, hbt, ovt = [Tok() for _ in range(7)]
                c.op("pool", lambda e: e.memset(ovf[:], 0.5), writes=(ovt,))
                for k_, off in ((0, 0), (1, 16)):
                    c.op("pool", lambda e: e.affine_select(ovf[:, k_, :], ovf[:, k_, :], [[-64, 32]], ALU.is_ge, 0.0, base=off,
                                                           channel_multiplier=16), reads=(ovt,), writes=(ovt,))
                    c.op("pool", lambda e: e.affine_select(ovf[:, k_, :], ovf[:, k_, :], [[64, 32]], ALU.is_ge, 0.0,
                                                           base=63 - off, channel_multiplier=-16), reads=(ovt,), writes=(ovt,))
                c.op("pool", lambda e: e.tensor_tensor(ovf[:, 2, :], ovf[:, 0, :], ovf[:, 1, :], ALU.add), reads=(ovt,),
                     writes=(ovt,))
                for g in range(2):
                    c.op("pool", lambda e: e.tensor_copy(VCX[:, g, 65:97], ovf[:, 2, :]), reads=(ovt,), writes=(cmptok,))
                c.op("pool", lambda e: e.memset(VCX[:, :, 64:65], 1.0), writes=(cmptok,))
                for kv in range(2):
                    w1 = self.A["cmp_wk1" if kv == 0 else "cmp_wv1"][li].rearrange("(l d) n -> d l n", d=64)
                    for piece in range(4):
                        for half in range(2):
                            c.dma(stg[half * 64:(half + 1) * 64, :].rearrange("p (l n) -> p l n", l=8),
                                  w1[:, piece * 8:(piece + 1) * 8, :], writes=(stgt,))
                        c.op("pool", lambda e: e.tensor_copy(W1[:, piece * 8:(piece + 1) * 8, :],
                                                             stg[:].rearrange("p (l n) -> p l n", l=8)),
                             reads=(stgt,), writes=(w1t,))
                    w2 = self.A["cmp_wk2" if kv == 0 else "cmp_wv2"][li].rearrange("(c p) n -> p c n", p=128)
                    c.dma(W2f[:], w2, writes=(w2t,))
                    c.op("pool", lambda e: e.tensor_copy(W2d[:, :, 0:64], W2f[:]), reads=(w2t,), writes=(w2t,))
                    c.op("pool", lambda e: e.tensor_copy(W2d[:, :, 64:128], W2f[:]), reads=(w2t,), writes=(w2t,))
                    pe = self.A["cmp_pos_k" if kv == 0 else "cmp_pos_v"][li]
                    c.dma(pe2[:, 0:64], pe, writes=(pet,))
                    c.dma(pe2[:, 64:128], pe, writes=(pet,))
                    pp, ppt = self.bank()
                    c.op("pe", lambda e: e.transpose(pp[:, 0:32], pe2[:], self.ident_f[0:32, 0:32]), reads=(pet, ct),
                         writes=(ppt,))
                    c.op("dve", lambda e: e.tensor_copy(peb[:], pp[:, 0:32]), reads=(ppt,), writes=(pet,))
                    for half in range(2):
                        pk_, pkt = self.bank()
                        for l in range(32):
                            self.mm(pk_[:, 0:1], W1[0:64, l, half * 128:(half + 1) * 128], peb[0:64, l:l + 1], l == 0, l == 31,
                                    reads=(w1t, pet), writes=(pkt,))
                        c.op("dve", lambda e: e.tensor_copy(hb[:, half:half + 1], pk_[:, 0:1]), reads=(pkt,), writes=(hbt,))
                    for g in range(2):
                        gs = slice(g * 64, g * 64 + 64)
                        for half in range(2):
                            ph, pht = self.bank()
                            for l in range(32):
                                self.mm(ph[:, 0:127], W1[gs, l, half * 128:(half + 1) * 128],
                                        xcT[kv][gs, l:l + 16 * 126 + 1:16], l == 0, l == 31, reads=(w1t, xtok), writes=(pht,))
                            x, xt = self.nscr()
                            x2, x2t = self.nscr()
                            N_ = slice(0, 127)
                            c.op("dve", lambda e: e.tensor_scalar(x[:, N_], ph[:, N_], hb[:, half:half + 1], None, ALU.add),
                                 reads=(pht, hbt), writes=(xt,))
                            c.op("dve", lambda e: e.tensor_tensor(x2[:, N_], x[:, N_], x[:, N_], ALU.mult), reads=(xt,),
                                 writes=(x2t,))
                            c.op("dve", lambda e: e.tensor_scalar(x2[:, N_], x2[:, N_], 0.044715, 1.0, ALU.mult, ALU.add),
                                 reads=(x2t,), writes=(x2t,))
                            c.op("dve", lambda e: e.tensor_tensor(x2[:, N_], x2[:, N_], x[:, N_], ALU.mult), reads=(x2t, xt),
                                 writes=(x2t,))
                            c.op("act", lambda e: e.activation(x2[:, N_], x2[:, N_], AF.Tanh, scale=0.7978845608028654),
                                 reads=(x2t,), writes=(x2t,))
                            c.op("dve", lambda e: e.tensor_scalar(x[:, N_], x[:, N_], 0.5, None, ALU.mult), reads=(xt,),
                                 writes=(xt,))
                            c.op("dve", lambda e: e.scalar_tensor_tensor(gh[:, half, 0:127], x2[:, N_], 1.0, x[:, N_], ALU.add,
                                                                         ALU.mult), reads=(x2t, xt), writes=(ght,))
                        if kv == 0:
                            pk, pkt2 = self.bank()
                            for half in range(2):
                                self.mm(pk[:, 0:127], W2d[:, half, :], gh[:, half, 0:127], half == 0, half == 1,
                                        reads=(w2t, ght), writes=(pkt2,))
                            self.rope_apply(kcT2[:, g, 0:127], pk[:, 0:127], pkt2, 127, 1.0,
                                            cosT[:, 31:31 + 16 * 126 + 1:16], sinT[:, 31:31 + 16 * 126 + 1:16], cmptok)
                        else:
                            pv, pvt = self.bank()
                            for half in range(2):
                                self.mm(pv[0:127, 0:64], gh[:, half, 0:127], W2d[:, half, 0:64], half == 0, half == 1,
                                        reads=(w2t, ght), writes=(pvt,))
                            c.op("act", lambda e: e.copy(VCX[0:127, g, 0:64], pv[0:127, 0:64]), reads=(pvt,), writes=(cmptok,))
                if "kcT" in self.dbg_out:
                    self.dump2d("kcT", kcT2[:].rearrange("p g n -> p (g n)"), [cmptok])
                    self.dump2d("vcx", VCX[:].rearrange("p g n -> p (g n)"), [cmptok])
                c.barrier()
            sA.close()
            if nstop == 1:
                return
            qT = self.sb(st, "n_qT", [128, 4, S], BF16)
            ksT2 = self.sb(st, "n_ksT", [128, 2, S], BF16)
            kwT2 = self.sb(st, "n_kwT", [128, 2, S], BF16)
            vs = self.sb(st, "n_vs", [128, NT, 2, 65], BF16)
            vw = self.sb(st, "n_vw", [128, NT, 2, 65], BF16)
            sg = self.sb(st, "n_sg", [128, NT, 24], F32)
            sB = ExitStack()
            cosT, sinT = open_tables(sB)
            qtok, kstok, kwtok, vstok, vwtok, sgtok = [Tok() for _ in range(6)]

            def ev_q(mt, tc, pb, pt):
                ts = slice(tc * 512, (tc + 1) * 512)
                self.rope_apply(qT[:, mt, ts], pb[:], pt, 512, 0.125, cosT[:, ts], sinT[:, ts], qtok)
            self.proj_feat(li, O_NQ, 512, ev_q)
            for g in range(2):
                def ev_ks(mt, tc, pb, pt, g=g):
                    ts = slice(tc * 512, (tc + 1) * 512)
                    self.rope_apply(ksT2[:, g, ts], pb[:], pt, 512, 1.0, cosT[:, ts], sinT[:, ts], kstok)

                def ev_kw(mt, tc, pb, pt, g=g):
                    ts = slice(tc * 512, (tc + 1) * 512)
                    self.rope_apply(kwT2[:, g, ts], pb[:], pt, 512, 1.0, cosT[:, ts], sinT[:, ts], kwtok)
                self.proj_feat_dup(li, O_NKS + g * 64, ev_ks)
                self.proj_feat_dup(li, O_NKW + g * 64, ev_kw)
            c.op("pool", lambda e: e.memset(vs[:, :, :, 64:65], 1.0), writes=(vstok,))
            c.op("pool", lambda e: e.memset(vw[:, :, :, 64:65], 1.0), writes=(vwtok,))

            def ev_vs(t, pb, pt):
                c.op("act", lambda e: e.copy(vs[:, t, :, 0:64], pb[:, 0:128].rearrange("p (g d) -> p g d", g=2)), reads=(pt,),
                     writes=(vstok,))

            def ev_vw(t, pb, pt):
                c.op("act", lambda e: e.copy(vw[:, t, :, 0:64], pb[:, 0:128].rearrange("p (g d) -> p g d", g=2)), reads=(pt,),
                     writes=(vwtok,))

            def ev_sg(t, pb, pt):
                c.op("act", lambda e: e.activation(sg[:, t, :], pb[:, 0:24], AF.Sigmoid), reads=(pt,), writes=(sgtok,))
            self.proj_tok(li, O_NVS, 128, ev_vs)
            self.proj_tok(li, O_NVW, 128, ev_vw)
            self.proj_tok(li, O_NG, 24, ev_sg)
            if "qT" in self.dbg_out:
                self.dump_featmajor_bf16(qT, [qtok], self.dbg_out["qT"])
            c.barrier()
            sB.close()
            if nstop == 2:
                return
            am = self.sb(st, "n_am", [128, NT, 32], F32)
            Esel = self.sb(st, "n_E", [32, NT, 128], BF16)
            wneg = self.sb(st, "n_wneg", [128, 128], BF16)
            cm = self.sb(st, "n_cm", [128, 512], BF16)
            negT = self.sb(st, "n_negT", [32, 2, 512], BF16)
            acc = self.sb(st, "n_acc", [128, 4, 512], F32)
            ybf = self.sb(st, "n_ybf", [128, 512], BF16)
            pT = [self.sb(st, f"n_pT{i}", [128, 512], BF16) for i in range(5)]
            pT_tok = [Tok() for _ in range(5)]
            imp = self.sb(st, "n_imp", [128, 4, 2, 32], F32)
            sm = self.sb(st, "n_sm", [128, 24], F32)
            impm = self.sb(st, "n_impm", [128, 32], F32)
            top8 = self.sb(st, "n_top8", [128, 8], F32)
            nselb = self.sb(st, "n_nsel", [128, 32], BF16)
            mtok, cmtok, negtok, acctok, ytok, imptok, tktok = [Tok() for _ in range(7)]
            smtok = [Tok(), Tok(), Tok()]
            tA, tAt = self.nscr()
            tAv = tA[:].rearrange("p (t j) -> p t j", t=NT)
            c.op("pool", lambda e: e.memset(am[:], 0.0), writes=(mtok,))
            c.op("pool", lambda e: e.affine_select(am[:], am[:], [[128, NT], [-64, 32]], ALU.is_ge, -100.0, base=0,
                                                   channel_multiplier=1), reads=(mtok,), writes=(mtok,))
            c.op("pool", lambda e: e.memset(tA[:], 100.0), writes=(tAt,))
            c.op("pool", lambda e: e.affine_select(tAv, tAv, [[128, NT], [-64, 32]], ALU.is_ge, 0.0, base=0,
                                                   channel_multiplier=1), reads=(tAt,), writes=(tAt,))
            c.op("pool", lambda e: e.affine_select(tAv, tAv, [[-128, NT], [64, 32]], ALU.is_ge, 0.0, base=63,
                                                   channel_multiplier=-1), reads=(tAt,), writes=(tAt,))
            c.op("pool", lambda e: e.memset(tAv[:, :, 0:1], 100.0), reads=(tAt,), writes=(tAt,))
            c.op("pool", lambda e: e.tensor_tensor(am[:], am[:], tAv, ALU.add), reads=(tAt, mtok), writes=(mtok,))
            c.op("pool", lambda e: e.memset(Esel[:], 1.0), writes=(mtok,))
            c.op("pool", lambda e: e.affine_select(Esel[:], Esel[:], [[128, NT], [1, 128]], ALU.is_ge, 0.0, base=0,
                                                   channel_multiplier=-64), reads=(mtok,), writes=(mtok,))
            c.op("pool", lambda e: e.affine_select(Esel[:], Esel[:], [[-128, NT], [-1, 128]], ALU.is_ge, 0.0, base=63,
                                                   channel_multiplier=64), reads=(mtok,), writes=(mtok,))
            c.op("pool", lambda e: e.affine_select(wneg[:], self.zer_f[:], [[-1, 128]], ALU.is_gt, NEG, base=0,
                                                   channel_multiplier=1), reads=(ct,), writes=(mtok,))
            pi = [0]
            c.barrier()
            self.n_scr_banks = 5
            for qc in range(NTC):
                qs = slice(qc * 512, (qc + 1) * 512)
                c.op("pool", lambda e: e.memset(cm[:], 0.0), writes=(cmtok,))
                c.op("pool", lambda e: e.affine_select(cm[:], cm[:], [[1, 512]], ALU.is_ge, NEG, base=qc * 512 - 31,
                                                       channel_multiplier=-16), reads=(cmtok,), writes=(cmtok,))
                c.op("dve", lambda e: e.memset(imp[:], 0.0), writes=(imptok,))
                def cmp_stream(h):
                    g = h // 4
                    hp = slice((h % 2) * 64, (h % 2) * 64 + 64)
                    hc = h // 2
                    hcol = slice(h * 64, (h + 1) * 64)
                    sb_, stk = self.bank()
                    self.mm(sb_[0:127, :], kcT2[hp, g, 0:127], qT[hp, hc, qs], True, False, reads=(cmptok, qtok), writes=(stk,))
                    self.mm(sb_[0:127, :], self.ident_b[0:127, 0:127], cm[0:127, :], False, True, reads=(ct, cmtok),
                            writes=(stk,), skip_group_check=True)
                    p, ptk = pT[pi[0] % 5], pT_tok[pi[0] % 5]
                    pi[0] += 1
                    c.op("act", lambda e: e.activation(p[0:127, :], sb_[0:127, :], AF.Exp), reads=(stk,), writes=(ptk,))
                    yield
                    ob, ot = self.bank_acc()
                    O = ob[:, 0:388].rearrange("p (j d) -> p j d", j=4)
                    for j in range(4):
                        self.mm(O[:, j, :], p[0:127, j * 128:(j + 1) * 128], VCX[0:127, g, :], j == 0, True,
                                reads=(ptk, cmptok), writes=(ot,), skip_group_check=True)
                    yield
                    smt = smtok[h % 3]
                    for j in range(4):
                        qt = qc * 4 + j
                        o_ = (h % 3) * 8 + 2 * j
                        rcv, wv_ = sm[:, o_:o_ + 1], sm[:, o_ + 1:o_ + 2]
                        c.op("dve", lambda e: e.tensor_scalar(rcv, O[:, j, 64:65], 1e-30, None, ALU.max), reads=(ot,),
                             writes=(smt,))
                        c.op("dve", lambda e: e.reciprocal(rcv, rcv), reads=(smt,), writes=(smt,))
                        c.op("dve", lambda e: e.tensor_tensor(wv_, rcv, sg[:, qt, 3 * h:3 * h + 1], ALU.mult),
                             reads=(smt, sgtok), writes=(smt,))
                        c.op("dve", lambda e: e.tensor_scalar(acc[:, j, hcol], O[:, j, 0:64], wv_, None, ALU.mult),
                             reads=(ot, smt), writes=(acctok,))
                        c.op("dve", lambda e: e.scalar_tensor_tensor(imp[:, j, g, :], O[:, j, 65:97], rcv, imp[:, j, g, :],
                                                                     ALU.mult, ALU.add), reads=(ot, smt, imptok),
                             writes=(imptok,))
                    self.release_acc(ob)
                self.run_streams([cmp_stream(h) for h in range(8)], 3)
                for j in range(4):
                    qt = qc * 4 + j
                    for g in range(2):
                        c.op("dve", lambda e: e.tensor_tensor(impm[:], imp[:, j, g, :], am[:, qt, :], ALU.add),
                             reads=(imptok, mtok), writes=(tktok,))
                        c.op("dve", lambda e: e.max(top8[:], impm[:]), reads=(tktok,), writes=(tktok,))
                        c.op("dve", lambda e: e.tensor_scalar(impm[:], impm[:], top8[:, 7:8], None, ALU.is_ge), reads=(tktok,),
                             writes=(tktok,))
                        c.op("dve", lambda e: e.tensor_scalar(nselb[:], impm[:], -1.0, 30000.0, ALU.add, ALU.mult),
                             reads=(tktok,), writes=(tktok,))
                        pb, pt = self.bank()
                        pbb = pb[:].bitcast(BF16)
                        c.op("pe", lambda e: e.transpose(pbb[0:32, 0:128], nselb[:], self.ident_b[:]), reads=(tktok, ct),
                             writes=(pt,))
                        c.op("act", lambda e: e.copy(negT[0:32, g, j * 128:(j + 1) * 128], pbb[0:32, 0:128]), reads=(pt,),
                             writes=(negtok,))
                if "negT" in self.dbg_out and qc == 1:
                    self.dump2d("negT", negT[:].rearrange("p g n -> p (g n)"), [negtok])
                def sw_stream(br, h):
                    g = h // 4
                    hp = slice((h % 2) * 64, (h % 2) * 64 + 64)
                    hc = h // 2
                    hcol = slice(h * 64, (h + 1) * 64)
                    ob, ot = self.bank_acc()
                    O = ob[:, 0:260].rearrange("p (j d) -> p j d", j=4)
                    first = True
                    kt0 = 0 if br == 1 else max(0, 4 * qc - 2)
                    for kt in range(kt0, 4 * qc + 4):
                        rel = kt - 4 * qc
                        jlo = max(0, rel)
                        jhi = 3 if br == 1 else min(3, rel + 2)
                        ncol = (jhi - jlo + 1) * 128
                        q0 = qc * 512 + jlo * 128
                        kl = slice(kt * 128, (kt + 1) * 128)
                        KT = ksT2 if br == 1 else kwT2
                        ktk = kstok if br == 1 else kwtok
                        extra = []
                        if br == 1:
                            extra.append((slice(0, ncol), Esel[0:32, kt, :], negT[0:32, g, jlo * 128:jlo * 128 + ncol],
                                          (mtok, negtok)))
                        if rel >= 0:
                            extra.append((slice(0, 128), self.ident_b[:], self.cneg_b[:], (ct,)))
                        if br == 2 and 0 <= rel + 2 <= 3:
                            o2 = (rel + 2 - jlo) * 128
                            extra.append((slice(o2, o2 + 128), self.ident_b[:], wneg[:], (ct, mtok)))
                        sb_, stk = self.bank()
                        self.mm(sb_[:, 0:ncol], KT[hp, g, kl], qT[hp, hc, q0:q0 + ncol], True, len(extra) == 0,
                                reads=(ktk, qtok), writes=(stk,))
                        for ei, (csl, lh, rh, rd) in enumerate(extra):
                            self.mm(sb_[:, csl], lh, rh, False, ei == len(extra) - 1, reads=rd, writes=(stk,),
                                    skip_group_check=True)
                        p, ptk = pT[pi[0] % 5], pT_tok[pi[0] % 5]
                        pi[0] += 1
                        c.op("act", lambda e: e.activation(p[:, 0:ncol], sb_[:, 0:ncol], AF.Exp), reads=(stk,), writes=(ptk,))
                        yield
                        VV = vs if br == 1 else vw
                        vtk_ = vstok if br == 1 else vwtok
                        for j in range(jlo, jhi + 1):
                            qt = qc * 4 + j
                            cs = slice((j - jlo) * 128, (j - jlo + 1) * 128)
                            self.mm(O[:, j, :], p[:, cs], VV[:, kt, g, :], first, kt == qt, reads=(ptk, vtk_), writes=(ot,),
                                    skip_group_check=True)
                            first = False
                    smt = smtok[h % 3]
                    for j in range(4):
                        qt = qc * 4 + j
                        o_ = (h % 3) * 8 + 2 * j
                        rcv, wv_ = sm[:, o_:o_ + 1], sm[:, o_ + 1:o_ + 2]
                        c.op("dve", lambda e: e.reciprocal(rcv, O[:, j, 64:65]), reads=(ot,), writes=(smt,))
                        c.op("dve", lambda e: e.tensor_tensor(wv_, rcv, sg[:, qt, 3 * h + br:3 * h + br + 1], ALU.mult),
                             reads=(smt, sgtok), writes=(smt,))
                        c.op("dve", lambda e: e.scalar_tensor_tensor(acc[:, j, hcol], O[:, j, 0:64], wv_, acc[:, j, hcol],
                                                                     ALU.mult, ALU.add), reads=(ot, smt, acctok),
                             writes=(acctok,))
                    self.release_acc(ob)
                self.run_streams([sw_stream(br, h) for br in (1, 2) for h in range(8)], 3)
                for j in range(4):
                    t = qc * 4 + j
                    c.op("act", lambda e: e.copy(ybf[:], acc[:, j, :]), reads=(acctok,), writes=(ytok,))
                    pb, pt = self.bank()
                    pbb = pb[:].bitcast(BF16)
                    for jj in range(4):
                        c.op("pe", lambda e: e.transpose(pbb[:, jj * 128:(jj + 1) * 128], ybf[:, jj * 128:(jj + 1) * 128],
                                                         self.ident_b[:]), reads=(ytok, ct), writes=(pt,))
                    c.op("dve", lambda e: e.tensor_copy(self.yT[:, :, t * 128:(t + 1) * 128],
                                                        pbb[:, 0:512].rearrange("p (j n) -> p j n", j=4)),
                         reads=(pt,), writes=(self.yT_tok,))
            c.barrier()
            self.n_scr_banks = 6

    def fox(self, li):
        c = self.c
        with ExitStack() as st:
            qT = self.sb(st, "fx_qT", [128, 4, S], BF16)
            kT = self.sb(st, "fx_kT", [128, 4, S], BF16)
            V = self.sb(st, "fx_V", [128, NT, 8, 65], BF16)
            ytk = self.sb(st, "fx_y", [128, 4, 512], BF16)
            fl = self.sb(st, "fx_f", [128, NT, 8], F32)
            ncum = self.sb(st, "fx_ncum", [128, NT, 8], F32)
            nref = self.sb(st, "fx_nref", [128, NT, 8], F32)
            btab = self.sb(st, "fx_btab", [128, NT, NT, 8], F32)
            bfb = self.sb(st, "fx_bf", [128, 8], F32)
            qtok, ktok, vtok, ytok, ftok = Tok(), Tok(), Tok(), Tok(), Tok()
            self.aux_tok = Tok()
            self.proj_feat(li, O_FQ, 512, self.evac_featT(qT, qtok, 0.125))
            self.proj_feat(li, O_FK, 512, self.evac_featT(kT, ktok, 1.0))
            c.op("pool", lambda e: e.memset(V[:, :, :, 64:65], 1.0), writes=(vtok,))

            def evac_v(t, pb, pt):
                c.op("act", lambda e: e.copy(V[:, t, :, 0:64], pb[:, 0:512].rearrange("p (h d) -> p h d", h=8)),
                     reads=(pt,), writes=(vtok,))
            for half in range(4):
                def evac_vh(t, pb, pt, half=half):
                    c.op("act", lambda e: e.copy(V[:, t, half * 2:half * 2 + 2, 0:64],
                                                 pb[:, 0:128].rearrange("p (h d) -> p h d", h=2)),
                         reads=(pt,), writes=(vtok,))
                self.proj_tok(li, O_FV + half * 128, 128, evac_vh)
            c.dma(bfb[:], self.A["fox_b_f"][li:li + 1, :].partition_broadcast(128), writes=(ftok,))

            def evac_f(t, pb, pt):
                c.op("dve", lambda e: e.tensor_tensor(fl[:, t, :], pb[:, 0:8], bfb[:], ALU.add), reads=(pt, ftok),
                     writes=(ftok,))
            self.proj_tok(li, O_FF, 8, evac_f)
            flat = fl[:].rearrange("p t h -> p (t h)")
            c.op("act", lambda e: e.activation(flat, flat, AF.Exp, scale=-1.0), reads=(ftok,), writes=(ftok,))
            c.op("act", lambda e: e.activation(flat, flat, AF.Ln, bias=1.0), reads=(ftok,), writes=(ftok,))
            for t in range(NT):
                pb, pt = self.bank()
                for j in range(t):
                    self.mm(pb[:, 0:8], self.ones_f[:], fl[:, j, :], j == 0, False, reads=(ftok, self.const_tok),
                            writes=(pt,))
                self.mm(pb[:, 0:8], self.tri_f[:], fl[:, t, :], t == 0, True, reads=(ftok, self.const_tok), writes=(pt,))
                c.op("dve", lambda e: e.tensor_copy(ncum[:, t, :], pb[:, 0:8]), reads=(pt,), writes=(self.aux_tok,))
                if t > 0:
                    pb2, pt2 = self.bank()
                    for j in range(t):
                        self.mm(pb2[:, 0:8], self.ones_f[:], fl[:, j, :], j == 0, j == t - 1,
                                reads=(ftok, self.const_tok), writes=(pt2,))
                    c.op("dve", lambda e: e.tensor_copy(nref[:, t, :], pb2[:, 0:8]), reads=(pt2,), writes=(self.aux_tok,))
                else:
                    c.op("dve", lambda e: e.memset(nref[:, 0, :], 0.0), writes=(self.aux_tok,))
            for kt in range(NT):
                for qt in range(1, NT, 2):
                    if qt >= kt:
                        c.op("pool", lambda e: e.tensor_tensor(btab[:, kt, qt, :], ncum[:, kt, :], nref[:, qt, :],
                                                               ALU.subtract), reads=(self.aux_tok,), writes=(self.aux_tok,))
            self.dump2d("ncum", ncum[:].rearrange("p t h -> p (t h)"), [self.aux_tok])
            self.dump2d("nref", nref[:].rearrange("p t h -> p (t h)"), [self.aux_tok])
            self.dump2d("fl", fl[:].rearrange("p t h -> p (t h)"), [ftok])
            self.attention(st, "fx", 8, qT, qtok, kT, ktok, V, vtok, ytk, ytok,
                           bias_fn=lambda h, kt, qt: btab[:, kt, qt, h:h + 1],
                           post_qc=lambda qc: self.ytok_to_yT(ytk, ytok, qc))
            c.barrier()

    def final_norm_store(self, out):
        self.store_tok_major(out, normed=True)

    def store_tok_major(self, out, normed):
        c = self.c
        L = len(self.layers)
        with ExitStack() as st:
            if normed:
                for tc in range(NTC):
                    ts = slice(tc * 512, (tc + 1) * 512)
                    pb, pt = self.bank()
                    for cc in range(KC):
                        sq, sqt = self.nscr()
                        c.op("act", lambda e: e.activation(sq[:], self.xT[:, cc, ts], AF.Square),
                             reads=(self.xT_tok[tc],), writes=(sqt,))
                        self.mm(pb[:], self.ones_f[:], sq[:], cc == 0, cc == KC - 1, reads=(sqt, self.const_tok),
                                writes=(pt,))
                    rs, rst = self.nscr()
                    c.op("dve", lambda e: e.tensor_scalar(rs[:], pb[:], 1.0 / D, EPS, ALU.mult, ALU.add), reads=(pt,),
                         writes=(rst,))
                    c.op("act", lambda e: e.activation(rs[:], rs[:], AF.Sqrt), reads=(rst,), writes=(rst,))
                    c.op("dve", lambda e: e.reciprocal(rs[:], rs[:]), reads=(rst,), writes=(rst,))
                    for cc in range(KC):
                        g = self.vecT[:, L * 72 + cc:L * 72 + cc + 1]
                        c.op("dve", lambda e: e.scalar_tensor_tensor(self.xT[:, cc, ts], self.xT[:, cc, ts], g, rs[:],
                                                                     ALU.mult, ALU.mult),
                             reads=(rst, self.vec_tok), writes=(self.xT_tok[tc],))
            os_ = [self.sb(st, f"os{i}", [128, D], F32) for i in range(2)]
            os_tok = [Tok(), Tok()]
            for t in range(NT):
                b = t % 2
                for half in range(2):
                    pb, pt = self.bank()
                    for j in range(4):
                        cc = half * 4 + j
                        c.op("pe", lambda e: e.transpose(pb[:, j * 128:(j + 1) * 128],
                                                         self.xT[:, cc, t * 128:(t + 1) * 128], self.ident_f[:]),
                             reads=(self.xT_tok[t // 4], self.const_tok), writes=(pt,), inc=(j == 3))
                    dst = os_[b][:, half * 512:(half + 1) * 512]
                    if half == 0:
                        c.op("dve", lambda e: e.tensor_copy(dst, pb[:]), reads=(pt,), writes=(os_tok[b],))
                    else:
                        c.op("act", lambda e: e.copy(dst, pb[:]), reads=(pt,), writes=(os_tok[b],))
                c.dma(out[t * 128:(t + 1) * 128, :], os_[b][:], reads=(os_tok[b],))
            c.barrier()

    def dump2d(self, name, ap, toks):
        if name in self.dbg_out:
            self.c.barrier()
            self.c.dma(self.dbg_out[name], ap, reads=tuple(toks), q="pool")
            self.c.barrier()

    def dump_featmajor_bf16(self, tT, toks, dst):
        c = self.c
        with ExitStack() as st:
            tmp = self.sb(st, "dmp", [128, S], F32)
            tt = Tok()
            for cc in range(tT.shape[1]):
                c.op("dve", lambda e: e.tensor_copy(tmp[:], tT[:, cc, :]), reads=tuple(toks), writes=(tt,))
                c.dma(dst[cc * 128:(cc + 1) * 128, :], tmp[:], reads=(tt,))
            c.barrier()


def _prep_inputs(inputs, layers, b):
    L = len(layers)
    vecs = np.zeros((L, 72, 128), np.float32)
    for i, l in enumerate(layers):
        vecs[i, 0:8] = inputs["norm_mix"][l].reshape(8, 128)
        vecs[i, 8:16] = inputs["norm_ff"][l].reshape(8, 128)
        vecs[i, 16:48] = inputs["b_gate"][l].reshape(32, 128)
    m = {
        "x": np.ascontiguousarray(inputs["x"][b]),
        "w_in": np.ascontiguousarray(inputs["w_in"][layers]),
        "vecs": vecs,
        "norm_final": np.ascontiguousarray(inputs["norm_final"].reshape(8, 128)),
        "positions": np.ascontiguousarray(inputs["positions"][b:b + 1]).astype(np.int32),
        "cmp_pos_k": np.ascontiguousarray(inputs["nsa_cmp_pos_k"][layers]),
        "cmp_pos_v": np.ascontiguousarray(inputs["nsa_cmp_pos_v"][layers]),
        "cmp_wk1": np.ascontiguousarray(inputs["nsa_cmp_wk1"][layers]),
        "cmp_wk2": np.ascontiguousarray(inputs["nsa_cmp_wk2"][layers]),
        "cmp_wv1": np.ascontiguousarray(inputs["nsa_cmp_wv1"][layers]),
        "cmp_wv2": np.ascontiguousarray(inputs["nsa_cmp_wv2"][layers]),
        "fox_b_f": np.ascontiguousarray(inputs["fox_b_f"][layers]),
        "gla_w_alpha": np.ascontiguousarray(inputs["gla_w_alpha"][layers]),
        "gla_b_alpha": np.ascontiguousarray(inputs["gla_b_alpha"][layers]),
        "gla_norm": np.ascontiguousarray(inputs["gla_norm"][layers]),
        "w_branch": np.ascontiguousarray(inputs["w_branch"][layers]),
        "w_out": np.ascontiguousarray(inputs["w_out"][layers]),
        "w_ff1": np.ascontiguousarray(inputs["w_ff1"][layers]),
        "w_ff2": np.ascontiguousarray(inputs["w_ff2"][layers]),
    }
    return m


def run(inputs, layers=(0, 1, 2, 3), debug=(), ncores=8, trace=False, stage=99):
    layers = list(layers)
    bld = Builder(layers, first=True, last=True, debug=debug, stage=stage)
    nc = bld.build()
    in_maps = [_prep_inputs(inputs, layers, b) for b in range(ncores)]
    res = run_bass_kernel_spmd(nc, in_maps, core_ids=list(range(ncores)), trace=trace)
    return res


def kernel(**inputs):
    inputs = {k: np.asarray(v) for k, v in inputs.items()}
    res = run(inputs)
    out = np.stack([np.asarray(r["out"]) for r in res.results], axis=0)
    return out.astype(np.float32)
```

```python
import numpy as np
from contextlib import ExitStack
import concourse.bass as bass
import concourse.mybir as mybir
from concourse.bass_utils import run_bass_kernel_spmd

F32 = mybir.dt.float32
BF16 = mybir.dt.bfloat16
I32 = mybir.dt.int32
ALU = mybir.AluOpType
AF = mybir.ActivationFunctionType
AX = mybir.AxisListType

S = 2048
D = 1024
NT = S // 128
NTC = S // 512
KC = D // 128
DEPTH = 4
DFF = 4096
D_IN = 10032
EPS = 1e-6
NEG = -30000.0

SPLITS = (512, 128, 128, 128, 128, 128, 128, 24, 512, 512, 512, 256, 256, 512, 16, 512, 512, 512, 512, 8, 4096)
OFFS = np.concatenate([[0], np.cumsum(SPLITS)]).tolist()
(O_NQ, O_NKC, O_NVC, O_NKS, O_NVS, O_NKW, O_NVW, O_NG, O_SQ, O_SK, O_SV, O_GQ, O_GK, O_GV, O_GA, O_GG,
 O_FQ, O_FK, O_FV, O_FF, O_GATE) = OFFS[:21]

EPOCH = 4000


class Tok:
    __slots__ = ("w", "r", "name")

    def __init__(self, name=""):
        self.w = None
        self.r = {}
        self.name = name


class Ctx:
    def __init__(self, nc, es):
        self.nc = nc
        self.es = es
        self.eng = dict(pe=nc.tensor, act=nc.scalar, dve=nc.vector, pool=nc.gpsimd, sp=nc.sync)
        self.cur = {}
        self.nsem = 0
        self.waited = {e: {} for e in self.eng}
        for e in ("pe", "act", "dve", "pool"):
            self.cur[e] = [self._newsem(e), 0]
        self.own = {e: set() for e in self.eng}
        for e in ("pe", "act", "dve", "pool"):
            self.own[e].add(id(self.cur[e][0]))
        self.dq = {}
        for q in ("sp", "act", "pool"):
            sems = [self._newsem("d" + q) for _ in range(8 if q == "sp" else 4)]
            self.dq[q] = dict(sems=sems, tgt=[0] * len(sems), i=0)
        self.all_dma = []

    def _newsem(self, name):
        self.nsem += 1
        return self.es.enter_context(self.nc.semaphore(f"s_{name}_{self.nsem}"))

    def _wait(self, e, deps):
        w = self.waited[e]
        for (sem, val) in deps:
            if val <= 0:
                continue
            k = id(sem)
            if e == "pe" and k in self.own["pe"]:
                continue
            if w.get(k, 0) >= val:
                continue
            self.eng[e].wait_ge(sem, val)
            w[k] = val

    @staticmethod
    def _deps(reads, writes):
        deps = []
        for t in reads:
            if t.w is not None:
                deps.append(t.w)
        for t in writes:
            if t.w is not None:
                deps.append(t.w)
            deps.extend(t.r.values())
        return deps

    @staticmethod
    def _record(stamp, reads, writes):
        sem, val = stamp
        for t in reads:
            t.r[id(sem)] = stamp
        for t in writes:
            t.w = stamp
            t.r = {}

    def op(self, e, fn, reads=(), writes=(), inc=True):
        self._wait(e, self._deps(reads, writes))
        ins = fn(self.eng[e])
        sem, cnt = self.cur[e]
        stamp = (sem, cnt + 1)
        if inc:
            ins.then_inc(sem, 1)
            self.cur[e][1] = cnt + 1
        self._record(stamp, reads, writes)
        if inc and cnt + 1 >= EPOCH:
            ns = self._newsem(e)
            self.own[e].add(id(ns))
            self.cur[e] = [ns, 0]
        return ins

    def dma(self, out, in_, reads=(), writes=(), q="sp", **kw):
        dq = self.dq[q]
        i = dq["i"] % len(dq["sems"])
        dq["i"] += 1
        sem = dq["sems"][i]
        deps = self._deps(reads, writes)
        deps.append((sem, dq["tgt"][i]))
        self._wait(q, deps)
        self.eng[q].dma_start(out=out, in_=in_, **kw).then_inc(sem, 16)
        dq["tgt"][i] += 16
        stamp = (sem, dq["tgt"][i])
        self._record(stamp, reads, writes)
        return stamp

    def barrier(self):
        stamps = []
        for e in ("pe", "act", "dve", "pool"):
            sem, cnt = self.cur[e]
            stamps.append((sem, cnt))
        for q, dq in self.dq.items():
            for s, t in zip(dq["sems"], dq["tgt"]):
                stamps.append((s, t))
        for e in ("pe", "act", "dve", "pool", "sp"):
            self._wait_all(e, stamps)

    def _wait_all(self, e, stamps):
        w = self.waited[e]
        for (sem, val) in stamps:
            if val <= 0:
                continue
            k = id(sem)
            if k in self.own.get(e, ()) and (e == "pe"):
                continue
            if w.get(k, 0) >= val:
                continue
            self.eng[e].wait_ge(sem, val)
            w[k] = val


class Builder:
    def __init__(self, layers, first, last, debug=(), stage=99):
        self.stage = stage
        self.layers = layers
        self.first = first
        self.last = last
        self.debug = debug
        self.nc = bass.Bass("TRN2", target_bir_lowering=False)
        self.dbg_out = {}

    def sb(self, st, name, shape, dt):
        self._uid = getattr(self, "_uid", 0) + 1
        return st.enter_context(self.nc.sbuf_tensor(f"{name}_{self._uid}", shape, dt))

    def dram_in(self, name, shape, dt=F32):
        return self.nc.dram_tensor(name, list(shape), dt, kind="ExternalInput").ap()

    def mm(self, out, lhsT, rhs, start, stop, reads, writes, inc=None, **kw):
        if inc is None:
            inc = True
        return self.c.op("pe", lambda e: e.matmul(out, lhsT, rhs, start=start, stop=stop, **kw),
                         reads=reads, writes=writes, inc=inc)

    def bank(self):
        i = self.bank_i % self.n_scr_banks
        self.bank_i += 1
        return self.ps[i], self.ps_tok[i]

    def bank_acc(self):
        busy = self.acc_busy
        for i in range(self.n_scr_banks, 8):
            if i not in busy:
                busy.add(i)
                self.last_acc = i
                return self.ps[i], self.ps_tok[i]
        raise RuntimeError("no free accumulator bank")

    def release_acc(self, ob):
        for i in range(8):
            if self.ps[i] is ob:
                self.acc_busy.discard(i)

    def wload(self, src3, kc, ncols, eng="pool"):
        i = self.w_i % 2
        self.w_i += 1
        stg, stok = self.wstg[i], self.wstg_tok[i]
        wb, wtok = self.wbf[i], self.wbf_tok[i]
        n = kc * ncols
        assert n <= self.WMAX
        sv = stg[:, 0:n].rearrange("p (c n) -> p c n", c=kc)
        wv = wb[:, 0:n].rearrange("p (c n) -> p c n", c=kc)
        self.c.dma(sv, src3, reads=(), writes=(stok,))
        self.c.op(eng, lambda e: e.tensor_copy(wb[:, 0:n], stg[:, 0:n]), reads=(stok,), writes=(wtok,))
        return wv, wtok

    def build(self):
        nc = self.nc
        L = len(self.layers)
        A = {}
        A["x"] = self.dram_in("x", [S, D])
        A["w_in"] = self.dram_in("w_in", [L, D, D_IN])
        A["vecs"] = self.dram_in("vecs", [L, 72, 128])
        A["norm_final"] = self.dram_in("norm_final", [8, 128])
        A["positions"] = self.dram_in("positions", [1, S], I32)
        A["cmp_pos_k"] = self.dram_in("cmp_pos_k", [L, 32, 64])
        A["cmp_pos_v"] = self.dram_in("cmp_pos_v", [L, 32, 64])
        A["cmp_wk1"] = self.dram_in("cmp_wk1", [L, 2048, 256])
        A["cmp_wk2"] = self.dram_in("cmp_wk2", [L, 256, 64])
        A["cmp_wv1"] = self.dram_in("cmp_wv1", [L, 2048, 256])
        A["cmp_wv2"] = self.dram_in("cmp_wv2", [L, 256, 64])
        A["fox_b_f"] = self.dram_in("fox_b_f", [L, 8])
        A["gla_w_alpha"] = self.dram_in("gla_w_alpha", [L, 16, 256])
        A["gla_b_alpha"] = self.dram_in("gla_b_alpha", [L, 256])
        A["gla_norm"] = self.dram_in("gla_norm", [L, 128])
        A["w_branch"] = self.dram_in("w_branch", [L, 4, 512, D])
        A["w_out"] = self.dram_in("w_out", [L, D, D])
        A["w_ff1"] = self.dram_in("w_ff1", [L, D, DFF])
        A["w_ff2"] = self.dram_in("w_ff2", [L, DFF, D])
        self.A = A
        out = nc.dram_tensor("out", [S, D], F32, kind="ExternalOutput").ap()
        for name, shape in self.debug:
            self.dbg_out[name] = nc.dram_tensor("dbg_" + name, list(shape), F32, kind="ExternalOutput").ap()

        with ExitStack() as es:
            self.es = es
            c = self.c = Ctx(nc, es)
            self.xT = self.sb(es, "xT", [128, KC, S], F32)
            self.hT = self.sb(es, "hT", [128, KC, S], BF16)
            self.xT_tok = [Tok(f"xT{i}") for i in range(NTC)]
            self.hT_tok = [Tok(f"hT{i}") for i in range(NTC)]
            self.ident_f = self.sb(es, "ident_f", [128, 128], F32)
            self.ident_b = self.sb(es, "ident_b", [128, 128], BF16)
            self.ones_f = self.sb(es, "ones_f", [128, 128], F32)
            self.const_tok = Tok("const")
            self.vecT = self.sb(es, "vecT", [128, L * 72 + 8], F32)
            self.vec_tok = Tok("vec")
            self.WMAX = 1024
            self.wstg = [self.sb(es, f"wstg{i}", [128, self.WMAX], F32) for i in range(2)]
            self.wbf = [self.sb(es, f"wbf{i}", [128, self.WMAX], BF16) for i in range(2)]
            self.wstg_tok = [Tok() for _ in range(2)]
            self.wbf_tok = [Tok() for _ in range(2)]
            self.w_i = 0
            self.scr = [self.sb(es, f"scr{i}", [128, 512], F32) for i in range(4)]
            self.scr_tok = [Tok() for _ in range(4)]
            self.scr_i = 0
            self.ps = [es.enter_context(nc.psum_tensor(f"ps{i}", [128, 512], F32)) for i in range(8)]
            self.ps_tok = [Tok(f"ps{i}") for i in range(8)]
            self.bank_i = 0
            self.bank_j = 0
            self.acc_busy = set()
            self.n_scr_banks = 6

            self.make_consts()
            if self.first:
                self.load_x()
            else:
                self.load_xT()
            for li in range(L):
                if self.stage >= 1:
                    self.layer(li)
            if self.last and self.stage >= 3:
                self.final_norm_store(out)
            else:
                self.store_xT(out)
            c.barrier()
        return nc

    def run_streams(self, gens, k=2):
        gens = iter(gens)
        active = []
        for g in gens:
            active.append(g)
            if len(active) == k:
                break
        while active:
            for g in list(active):
                try:
                    next(g)
                except StopIteration:
                    active.remove(g)
                    nxt = next(gens, None)
                    if nxt is not None:
                        active.append(nxt)

    def nscr(self):
        i = self.scr_i % len(self.scr)
        self.scr_i += 1
        return self.scr[i], self.scr_tok[i]

    def make_consts(self):
        c = self.c
        nc = self.nc
        ct = self.const_tok
        c.op("pool", lambda e: e.memset(self.ones_f[:], 1.0), writes=(ct,))
        c.op("pool", lambda e: e.affine_select(self.ident_f[:], self.ones_f[:], [[-1, 128]], ALU.is_equal, 0.0,
                                               base=0, channel_multiplier=1), reads=(ct,), writes=(ct,))
        c.op("pool", lambda e: e.tensor_copy(self.ident_b[:], self.ident_f[:]), reads=(ct,), writes=(ct,))
        self.tri_f = self.sb(self.es, "tri_f", [128, 128], F32)
        c.op("pool", lambda e: e.affine_select(self.tri_f[:], self.ones_f[:], [[1, 128]], ALU.is_ge, 0.0,
                                               base=0, channel_multiplier=-1), reads=(ct,), writes=(ct,))
        self.zer_f = self.sb(self.es, "zer_f", [128, 128], F32)
        self.cneg_b = self.sb(self.es, "cneg_b", [128, 128], BF16)
        c.op("pool", lambda e: e.memset(self.zer_f[:], 0.0), writes=(ct,))
        self.nones_f = self.sb(self.es, "nones_f", [128, 128], F32)
        self.ones_b = self.sb(self.es, "ones_b", [128, 128], BF16)
        self.nones_b = self.sb(self.es, "nones_b", [128, 128], BF16)
        self.cnegs_b = self.sb(self.es, "cnegs_b", [128, 128], BF16)
        self.ntri_b = self.sb(self.es, "ntri_b", [128, 128], BF16)
        c.op("pool", lambda e: e.memset(self.nones_f[:], -1.0), writes=(ct,))
        c.op("pool", lambda e: e.memset(self.ones_b[:], 1.0), writes=(ct,))
        c.op("pool", lambda e: e.memset(self.nones_b[:], -1.0), writes=(ct,))
        c.op("pool", lambda e: e.affine_select(self.cnegs_b[:], self.zer_f[:], [[1, 128]], ALU.is_gt, NEG,
                                               base=0, channel_multiplier=-1), reads=(ct,), writes=(ct,))
        c.op("pool", lambda e: e.affine_select(self.ntri_b[:], self.nones_f[:], [[-1, 128]], ALU.is_ge, 0.0,
                                               base=0, channel_multiplier=1), reads=(ct,), writes=(ct,))
        c.op("pool", lambda e: e.affine_select(self.cneg_b[:], self.zer_f[:], [[1, 128]], ALU.is_ge, NEG,
                                               base=0, channel_multiplier=-1), reads=(ct,), writes=(ct,))
        L = len(self.layers)
        nrow = L * 72 + 8
        with ExitStack() as st:
            tmp = self.sb(st, "vtmp", [128, 4, 128], F32)
            tt = Tok()
            r0 = 0
            chunks = []
            while r0 < nrow:
                n = min(128, nrow - r0)
                chunks.append((r0, n))
                r0 += n
            for ci, (r0, n) in enumerate(chunks):
                a = r0
                while a < r0 + n:
                    if a < L * 72:
                        b = min(r0 + n, L * 72)
                        src = self.A["vecs"].rearrange("l r p -> (l r) p")[a:b, :]
                    else:
                        b = r0 + n
                        src = self.A["norm_final"][a - L * 72:b - L * 72, :]
                    c.dma(tmp[a - r0:b - r0, ci, :], src, writes=(tt,))
                    a = b
                pb, pt = self.bank()
                c.op("pe", lambda e: e.transpose(pb[:, 0:n], tmp[0:n, ci, :], self.ident_f[0:n, 0:n]),
                     reads=(tt, ct), writes=(pt,))
                c.op("dve", lambda e: e.tensor_copy(self.vecT[:, r0:r0 + n], pb[:, 0:n]), reads=(pt,),
                     writes=(self.vec_tok,))
            c.barrier()

    def vcol(self, li, kind, j):
        base = li * 72 + {"norm_mix": 0, "norm_ff": 8, "b_gate": 16}[kind]
        return self.vecT[:, base + j:base + j + 1]

    def load_x(self):
        c = self.c
        x = self.A["x"]
        with ExitStack() as st:
            xs = [self.sb(st, f"xs{i}", [128, D], F32) for i in range(2)]
            xs_tok = [Tok(), Tok()]
            for t in range(NT):
                b = t % 2
                c.dma(xs[b][:], x[t * 128:(t + 1) * 128, :], writes=(xs_tok[b],))
                for half in range(2):
                    pb, pt = self.bank()
                    for j in range(4):
                        cc = half * 4 + j
                        c.op("pe", lambda e: e.transpose(pb[:, j * 128:(j + 1) * 128], xs[b][:, cc * 128:(cc + 1) * 128],
                                                         self.ident_f[:]),
                             reads=(xs_tok[b], self.const_tok), writes=(pt,), inc=(j == 3))
                    dst = self.xT[:, half * 4:half * 4 + 4, t * 128:(t + 1) * 128]
                    src = pb[:].rearrange("p (j n) -> p j n", j=4)
                    eng = "dve" if half == 0 else "act"
                    if eng == "dve":
                        c.op("dve", lambda e: e.tensor_copy(dst, src), reads=(pt,), writes=(self.xT_tok[t // 4],))
                    else:
                        c.op("act", lambda e: e.copy(dst, src), reads=(pt,), writes=(self.xT_tok[t // 4],))
            c.barrier()

    def load_xT(self):
        raise NotImplementedError

    def store_xT(self, out):
        self.store_tok_major(out, normed=False)

    def rmsnorm_to_hT(self, gcol):
        c = self.c
        for tc in range(NTC):
            ts = slice(tc * 512, (tc + 1) * 512)
            pb, pt = self.bank()
            for cc in range(KC):
                sq, sqt = self.nscr()
                c.op("act", lambda e: e.activation(sq[:], self.xT[:, cc, ts], AF.Square),
                     reads=(self.xT_tok[tc],), writes=(sqt,))
                self.mm(pb[:], self.ones_f[:], sq[:], cc == 0, cc == KC - 1, reads=(sqt, self.const_tok), writes=(pt,))
            rs, rst = self.nscr()
            c.op("dve", lambda e: e.tensor_scalar(rs[:], pb[:], 1.0 / D, EPS, ALU.mult, ALU.add), reads=(pt,),
                 writes=(rst,))
            c.op("act", lambda e: e.activation(rs[:], rs[:], AF.Sqrt), reads=(rst,), writes=(rst,))
            c.op("dve", lambda e: e.reciprocal(rs[:], rs[:]), reads=(rst,), writes=(rst,))
            for cc in range(KC):
                c.op("dve", lambda e: e.scalar_tensor_tensor(self.hT[:, cc, ts], self.xT[:, cc, ts], gcol(cc), rs[:],
                                                             ALU.mult, ALU.mult),
                     reads=(self.xT_tok[tc], rst, self.vec_tok), writes=(self.hT_tok[tc],))

    def layer(self, li):
        self.rmsnorm_to_hT(lambda cc: self.vcol(li, "norm_mix", cc))
        if "hT" in self.dbg_out and li == 0:
            self.dump_featmajor_bf16(self.hT, self.hT_tok, self.dbg_out["hT"])
        if self.stage >= 4:
            self.yT = self.sb(self.es, f"yT{li}", [128, 4, S], BF16) if not hasattr(self, "yT") else self.yT
            self.yT_tok = Tok("yT")
            if self.stage >= 8:
                self.nsa(li)
                if "ynsa" in self.dbg_out and li == 0:
                    self.dump_featmajor_bf16(self.yT, [self.yT_tok], self.dbg_out["ynsa"])
                self.combine(li, 0)
            if self.stage == 8:
                return
            if self.stage >= 7:
                self.gla(li)
                if "ygla" in self.dbg_out and li == 0:
                    self.dump_featmajor_bf16(self.yT, [self.yT_tok], self.dbg_out["ygla"])
                self.combine(li, 2)
            if self.stage >= 6 and self.stage != 7:
                self.sbmix(li)
                if "ysb" in self.dbg_out and li == 0:
                    self.dump_featmajor_bf16(self.yT, [self.yT_tok], self.dbg_out["ysb"])
                self.combine(li, 1)
            if self.stage == 7:
                return
            self.fox(li)
            if "yT" in self.dbg_out and li == 0:
                self.dump_featmajor_bf16(self.yT, [self.yT_tok], self.dbg_out["yT"])
            if self.stage >= 5:
                self.combine(li, 3)
        if self.stage >= 2:
            self.rmsnorm_to_hT(lambda cc: self.vcol(li, "norm_ff", cc))
            self.ffn(li)

    def ffn(self, li):
        c = self.c
        w1 = self.A["w_ff1"][li].rearrange("(c p) n -> p c n", p=128)
        w2 = self.A["w_ff2"][li].rearrange("(f p) n -> p f n", p=128)
        G = 4
        with ExitStack() as st:
            aT = [self.sb(st, f"aT{i}", [128, G, S], BF16) for i in range(2)]
            aT_tok = [Tok(), Tok()]
            import os
            for g in range(int(os.environ.get('FFN_G', DFF // (128 * G)))):
                ab, abt = aT[g % 2], aT_tok[g % 2]
                for half in range(G):
                    f0 = g * G + half
                    wv, wt = self.wload(w1[:, :, f0 * 128:(f0 + 1) * 128], KC, 128)
                    for j in range(1):
                        for tc in range(NTC):
                            ts = slice(tc * 512, (tc + 1) * 512)
                            pb, pt = self.bank()
                            for cc in range(KC):
                                self.mm(pb[:], wv[:, cc, j * 128:(j + 1) * 128], self.hT[:, cc, ts], cc == 0, cc == KC - 1,
                                        reads=(wt, self.hT_tok[tc]), writes=(pt,))
                            r, rt = self.nscr()
                            c.op("act", lambda e: e.activation(r[:], pb[:], AF.Relu), reads=(pt,), writes=(rt,))
                            c.op("dve", lambda e: e.tensor_tensor(ab[:, half, ts], r[:], r[:], ALU.mult),
                                 reads=(rt,), writes=(abt,))
                for dh in range(4):
                    wv, wt = self.wload(w2[:, g * G:(g + 1) * G, dh * 256:(dh + 1) * 256], G, 256)
                    for j in range(2):
                        dt_ = dh * 2 + j
                        for tc in range(NTC):
                            ts = slice(tc * 512, (tc + 1) * 512)
                            pb, pt = self.bank()
                            for f in range(G):
                                self.mm(pb[:], wv[:, f, j * 128:(j + 1) * 128], ab[:, f, ts], f == 0, f == G - 1,
                                        reads=(wt, abt), writes=(pt,))
                            c.op("dve", lambda e: e.tensor_tensor(self.xT[:, dt_, ts], self.xT[:, dt_, ts], pb[:], ALU.add),
                                 reads=(pt,), writes=(self.xT_tok[tc],))
            c.barrier()


    def proj_feat(self, li, col0, ncols, evac):
        w = self.A["w_in"][li].rearrange("(c p) n -> p c n", p=128)
        n0 = 0
        while n0 < ncols:
            nn = min(128, ncols - n0)
            wv, wt = self.wload(w[:, :, col0 + n0:col0 + n0 + nn], KC, nn)
            for j in range((nn + 127) // 128):
                m = min(128, nn - j * 128)
                for tc in range(NTC):
                    ts = slice(tc * 512, (tc + 1) * 512)
                    pb, pt = self.bank()
                    for cc in range(KC):
                        self.mm(pb[0:m, :], wv[:, cc, j * 128:j * 128 + m], self.hT[:, cc, ts], cc == 0, cc == KC - 1,
                                reads=(wt, self.hT_tok[tc]), writes=(pt,))
                    evac((n0 + j * 128) // 128, tc, pb, pt)
            n0 += nn

    def proj_tok(self, li, col0, ncols, evac):
        w = self.A["w_in"][li].rearrange("(c p) n -> p c n", p=128)
        wv, wt = self.wload(w[:, :, col0:col0 + ncols], KC, ncols)
        for t in range(NT):
            pb, pt = self.bank()
            for cc in range(KC):
                self.mm(pb[:, 0:ncols], self.hT[:, cc, t * 128:(t + 1) * 128], wv[:, cc, :], cc == 0, cc == KC - 1,
                        reads=(wt, self.hT_tok[t // 4]), writes=(pt,))
            evac(t, pb, pt)

    def evac_featT(self, dst, dtok, scale=1.0):
        c = self.c
        cnt = [0]

        def f(mt, tc, pb, pt):
            ts = slice(tc * 512, (tc + 1) * 512)
            cnt[0] += 1
            if cnt[0] % 2 == 0:
                c.op("dve", lambda e: e.tensor_scalar(dst[:, mt, ts], pb[:], scale, None, ALU.mult), reads=(pt,),
                     writes=(dtok,))
            else:
                c.op("act", lambda e: e.activation(dst[:, mt, ts], pb[:], AF.Copy, scale=scale), reads=(pt,),
                     writes=(dtok,))
        return f

    def attention(self, st, name, nheads, qT, qtok, kT, ktok, V, vtok, ytok_t, ytok_tok, bias_fn=None, ycol0=0, post_qc=None):
        c = self.c
        pT = [self.sb(st, f"{name}_pT{i}", [128, 512], BF16) for i in range(6)]
        pT_tok = [Tok() for _ in range(6)]
        rc = self.sb(st, f"{name}_rc", [128, 12], F32)
        rc_tok = [Tok(), Tok(), Tok()]
        pi = [0]

        def head_stream(qc, h):
            hp = slice((h % 2) * 64, (h % 2) * 64 + 64)
            hc = h // 2
            ob, ot = self.bank_acc()
            O = ob[:, 0:260].rearrange("p (j d) -> p j d", j=4)
            nkt = 4 * qc + 4
            for kt in range(nkt):
                j0 = max(0, kt - 4 * qc)
                q0 = qc * 512 + j0 * 128
                ncol = 512 - j0 * 128
                sb_, stk = self.bank()
                diag = kt >= 4 * qc
                self.mm(sb_[:, 0:ncol], kT[hp, hc, kt * 128:(kt + 1) * 128], qT[hp, hc, q0:q0 + ncol], True, not diag,
                        reads=(ktok, qtok), writes=(stk,))
                if diag:
                    self.mm(sb_[:, 0:128], self.ident_b[:], self.cneg_b[:], False, True,
                            reads=(self.const_tok,), writes=(stk,), skip_group_check=True)
                p, ptk = pT[pi[0] % 6], pT_tok[pi[0] % 6]
                pi[0] += 1
                for half in range(2):
                    jl, jh = max(j0, 2 * half), 2 * half + 1
                    if jl > jh:
                        continue
                    cs = slice((jl - j0) * 128, (jh - j0 + 1) * 128)
                    b = bias_fn(h, kt, qc * 4 + 2 * half + 1) if bias_fn is not None else 0.0
                    c.op("act", lambda e: e.activation(p[:, cs], sb_[:, cs], AF.Exp, bias=b), reads=(stk, self.aux_tok),
                         writes=(ptk,))
                yield
                for j in range(j0, 4):
                    qt = qc * 4 + j
                    cs = slice((j - j0) * 128, (j - j0 + 1) * 128)
                    self.mm(O[:, j, :], p[:, cs], V[:, kt, h, :], kt == 0 and j == 0, kt == qt, reads=(ptk, vtok), writes=(ot,),
                            skip_group_check=True)
            rct = rc_tok[h % 3]
            for j in range(4):
                rcj = rc[:, (h % 3) * 4 + j:(h % 3) * 4 + j + 1]
                c.op("dve", lambda e: e.reciprocal(rcj, O[:, j, 64:65]), reads=(ot,), writes=(rct,))
                c.op("dve", lambda e: e.tensor_scalar(ytok_t[:, j, ycol0 + h * 64:ycol0 + (h + 1) * 64], O[:, j, 0:64],
                                                      rcj, None, ALU.mult),
                     reads=(ot, rct), writes=(ytok_tok,))
            self.release_acc(ob)

        c.barrier()
        self.n_scr_banks = 5
        for qc in range(NTC):
            self.run_streams([head_stream(qc, h) for h in range(nheads)], 3)
            if post_qc is not None:
                post_qc(qc)
        c.barrier()
        self.n_scr_banks = 6

    def ytok_to_yT(self, ytok_t, ytok_tok, qc):
        c = self.c
        for tl in range(4):
            t = qc * 4 + tl
            pb, pt = self.bank()
            pbb = pb[:].bitcast(BF16)
            for j in range(4):
                c.op("pe", lambda e: e.transpose(pbb[:, j * 128:(j + 1) * 128], ytok_t[:, tl, j * 128:(j + 1) * 128],
                                                 self.ident_b[:]),
                     reads=(ytok_tok, self.const_tok), writes=(pt,))
            c.op("dve", lambda e: e.tensor_copy(self.yT[:, :, t * 128:(t + 1) * 128],
                                                pbb[:, 0:512].rearrange("p (j n) -> p j n", j=4)),
                 reads=(pt,), writes=(self.yT_tok,))


    def combine(self, li, bi):
        c = self.c
        wg = self.A["w_in"][li].rearrange("(c p) n -> p c n", p=128)
        wb = self.A["w_branch"][li, bi].rearrange("(c p) n -> p c n", p=128)
        wo = self.A["w_out"][li].rearrange("(c p) n -> p c n", p=128)
        with ExitStack() as st:
            mT = self.sb(st, "mT", [128, KC, S], BF16)
            mtok = Tok()
            for dt_ in range(KC):
                g0 = O_GATE + bi * D + dt_ * 128
                wgv, wgt = self.wload(wg[:, :, g0:g0 + 128], KC, 128)
                wbv, wbt = self.wload(wb[:, :, dt_ * 128:(dt_ + 1) * 128], 4, 128)
                bcol = self.vcol(li, "b_gate", bi * 8 + dt_)
                for tc in range(NTC):
                    ts = slice(tc * 512, (tc + 1) * 512)
                    pa, pat = self.bank()
                    for cc in range(KC):
                        self.mm(pa[:], wgv[:, cc, :], self.hT[:, cc, ts], cc == 0, cc == KC - 1,
                                reads=(wgt, self.hT_tok[tc]), writes=(pat,))
                    pb, pbt = self.bank()
                    for cc in range(4):
                        self.mm(pb[:], wbv[:, cc, :], self.yT[:, cc, ts], cc == 0, cc == 3,
                                reads=(wbt, self.yT_tok), writes=(pbt,))
                    sg, sgt = self.nscr()
                    c.op("act", lambda e: e.activation(sg[:], pa[:], AF.Sigmoid, bias=bcol), reads=(pat, self.vec_tok),
                         writes=(sgt,))
                    c.op("dve", lambda e: e.tensor_tensor(mT[:, dt_, ts], sg[:], pb[:], ALU.mult), reads=(sgt, pbt),
                         writes=(mtok,))
            for do in range(KC):
                wov, wot = self.wload(wo[:, :, do * 128:(do + 1) * 128], KC, 128)
                for tc in range(NTC):
                    ts = slice(tc * 512, (tc + 1) * 512)
                    pb, pbt = self.bank()
                    for cc in range(KC):
                        self.mm(pb[:], wov[:, cc, :], mT[:, cc, ts], cc == 0, cc == KC - 1, reads=(wot, mtok), writes=(pbt,))
                    c.op("dve", lambda e: e.tensor_tensor(self.xT[:, do, ts], self.xT[:, do, ts], pb[:], ALU.add),
                         reads=(pbt,), writes=(self.xT_tok[tc],))
            c.barrier()


    def sbmix(self, li):
        c = self.c
        with ExitStack() as st:
            qT = self.sb(st, "sb_qT", [128, 4, S], BF16)
            kT = self.sb(st, "sb_kT", [128, 4, S], BF16)
            V = self.sb(st, "sb_V", [128, NT, 8, 64], BF16)
            ytk = self.sb(st, "sb_y", [128, 4, 512], BF16)
            spb = [[self.sb(st, f"sb_sp{k}{i}", [128, 512], BF16) for i in range(2)] for k in range(2)]
            spt = [[Tok(), Tok()] for k in range(2)]
            pT = [[self.sb(st, f"sb_pT{k}{i}", [128, 512], BF16) for i in range(2)] for k in range(2)]
            pTt = [[Tok(), Tok()] for k in range(2)]
            sufs = [(self.sb(st, f"sb_suf{k}", [1, 512], F32), self.sb(st, f"sb_sufh{k}", [1, 512], BF16),
                     self.sb(st, f"sb_sufl{k}", [1, 512], BF16), Tok()) for k in range(2)]
            qtok, ktok, vtok, ytok = Tok(), Tok(), Tok(), Tok()
            self.proj_feat(li, O_SQ, 512, self.evac_featT(qT, qtok, 0.125))
            self.proj_feat(li, O_SK, 512, self.evac_featT(kT, ktok, 1.0))
            for q4 in range(4):
                def evac_vh(t, pb, pt, q4=q4):
                    c.op("act", lambda e: e.copy(V[:, t, q4 * 2:q4 * 2 + 2, :],
                                                 pb[:, 0:128].rearrange("p (h d) -> p h d", h=2)),
                         reads=(pt,), writes=(vtok,))
                self.proj_tok(li, O_SV + q4 * 128, 128, evac_vh)
            def head_stream(qc, h):
                s_ = h % 2
                hp = slice((h % 2) * 64, (h % 2) * 64 + 64)
                hc = h // 2
                ob, ot = self.bank_acc()
                O = ob[:, 0:256].rearrange("p (j d) -> p j d", j=4)
                nkt = 4 * qc + 4
                suf, sufh, sufl, suft = sufs[s_]
                c.op("dve", lambda e: e.memset(suf[:], 0.0), writes=(suft,))
                c.op("dve", lambda e: e.memset(sufh[:], 0.0), writes=(suft,))
                c.op("dve", lambda e: e.memset(sufl[:], 0.0), writes=(suft,))
                first = True
                ti = 0
                for kt in range(nkt - 1, -1, -1):
                    j0 = max(0, kt - 4 * qc)
                    q0 = qc * 512 + j0 * 128
                    ncol = 512 - j0 * 128
                    diag = kt >= 4 * qc
                    ksl = kT[hp, hc, kt * 128:(kt + 1) * 128]
                    qsl = qT[hp, hc, q0:q0 + ncol]
                    pa, pat = self.bank()
                    self.mm(pa[:, 0:ncol], ksl, qsl, True, not diag, reads=(ktok, qtok), writes=(pat,))
                    if diag:
                        self.mm(pa[:, 0:128], self.ident_b[:], self.cnegs_b[:], False, True, reads=(self.const_tok,),
                                writes=(pat,), skip_group_check=True)
                    e_, et = self.nscr()
                    sp, spk = spb[s_][ti % 2], spt[s_][ti % 2]
                    p, ptk = pT[s_][ti % 2], pTt[s_][ti % 2]
                    ti += 1
                    c.op("act", lambda e: e.activation(e_[:, 0:ncol], pa[:, 0:ncol], AF.Exp), reads=(pat,), writes=(et,))
                    c.op("act", lambda e: e.activation(sp[:, 0:ncol], e_[:, 0:ncol], AF.Ln, bias=1.0), reads=(et,),
                         writes=(spk,))
                    yield
                    pb, pbt = self.bank()
                    self.mm(pb[:, 0:ncol], ksl, qsl, True, False, reads=(ktok, qtok), writes=(pbt,))
                    if diag:
                        self.mm(pb[:, 0:128], self.ident_b[:], self.cnegs_b[:], False, False, reads=(self.const_tok,),
                                writes=(pbt,), skip_group_check=True)
                    self.mm(pb[:, 0:ncol], self.nones_b[0:1, :], sufh[0:1, 512 - ncol:512], False, False,
                            reads=(suft, self.const_tok), writes=(pbt,), skip_group_check=True)
                    self.mm(pb[:, 0:ncol], self.nones_b[0:1, :], sufl[0:1, 512 - ncol:512], False, False,
                            reads=(suft, self.const_tok), writes=(pbt,), skip_group_check=True)
                    self.mm(pb[:, 0:ncol], self.ntri_b[:], sp[:, 0:ncol], False, True, reads=(spk, self.const_tok),
                            writes=(pbt,), skip_group_check=True)
                    if kt > 0:
                        pc, pct = self.bank()
                        self.mm(pc[0:1, 0:ncol], self.ones_b[:, 0:1], sp[:, 0:ncol], True, True,
                                reads=(spk, self.const_tok), writes=(pct,))
                        sl = slice(512 - ncol, 512)
                        c.op("dve", lambda e: e.tensor_tensor(suf[0:1, sl], suf[0:1, sl], pc[0:1, 0:ncol], ALU.add),
                             reads=(pct,), writes=(suft,))
                        c.op("dve", lambda e: e.tensor_copy(sufh[0:1, sl], suf[0:1, sl]), reads=(suft,), writes=(suft,))
                        c.op("dve", lambda e: e.tensor_tensor(sufl[0:1, sl], suf[0:1, sl], sufh[0:1, sl], ALU.subtract),
                             reads=(suft,), writes=(suft,))
                    c.op("act", lambda e: e.activation(p[:, 0:ncol], pb[:, 0:ncol], AF.Exp), reads=(pbt,), writes=(ptk,))
                    yield
                    for j in range(j0, 4):
                        cs = slice((j - j0) * 128, (j - j0 + 1) * 128)
                        self.mm(O[:, j, :], p[:, cs], V[:, kt, h, :], first, kt == 0, reads=(ptk, vtok), writes=(ot,),
                                skip_group_check=True)
                        first = False
                for j in range(4):
                    c.op("dve", lambda e: e.tensor_copy(ytk[:, j, h * 64:(h + 1) * 64], O[:, j, :]), reads=(ot,),
                         writes=(ytok,))
                self.release_acc(ob)

            for qc in range(NTC):
                self.run_streams([head_stream(qc, h) for h in range(8)], 2)
                self.ytok_to_yT(ytk, ytok, qc)
            c.barrier()

    def gla(self, li):
        c = self.c
        ct = self.const_tok
        with ExitStack() as st:
            qeT = self.sb(st, "g_qe", [64, 4, S], BF16)
            keT = self.sb(st, "g_ke", [64, 4, S], BF16)
            k2 = self.sb(st, "g_k2", [128, NT, 256], BF16)
            vtk = self.sb(st, "g_v", [128, NT, 512], BF16)
            gnorm = self.sb(st, "g_norm", [128, 1], F32)
            dec = self.sb(st, "g_dec", [64, 4, 32], F32)
            tblk = self.sb(st, "g_tblk", [128, 128], BF16)
            sp_ = ExitStack()
            alrT = self.sb(sp_, "g_alr", [16, S], BF16)
            balb = self.sb(sp_, "g_bal", [128, 256], F32)
            wal_f = self.sb(sp_, "g_walf", [16, 256], F32)
            wal_b = self.sb(sp_, "g_walb", [16, 256], BF16)
            n16 = self.sb(sp_, "g_n16", [128, 128], F32)
            m1 = self.sb(sp_, "g_m1", [128, 128], F32)
            m2 = self.sb(sp_, "g_m2", [128, 128], F32)
            att_tok = [Tok(), Tok()]
            mtok, qtok, ktok, vtok, k2tok, atok, ptok, stok, otok, ontok = [Tok() for _ in range(10)]
            c.op("pool", lambda e: e.memset(n16[:], -1.0 / 16.0), writes=(mtok,))
            c.op("pool", lambda e: e.affine_select(m1[:], n16[:], [[1, 128]], ALU.is_ge, 0.0, base=0, channel_multiplier=-1),
                 reads=(mtok,), writes=(mtok,))
            c.op("pool", lambda e: e.memset(m1[0:64, 64:128], 0.0), reads=(mtok,), writes=(mtok,))
            c.op("pool", lambda e: e.affine_select(m2[:], n16[:], [[-1, 128]], ALU.is_gt, 0.0, base=0, channel_multiplier=1),
                 reads=(mtok,), writes=(mtok,))
            c.op("pool", lambda e: e.memset(m2[64:128, 0:64], 0.0), reads=(mtok,), writes=(mtok,))
            c.op("pool", lambda e: e.tensor_copy(tblk[:], self.tri_f[:]), reads=(ct, mtok), writes=(mtok,))
            c.op("pool", lambda e: e.memset(tblk[0:64, 64:128], 0.0), reads=(mtok,), writes=(mtok,))
            c.dma(balb[:], self.A["gla_b_alpha"][li:li + 1, :].partition_broadcast(128), writes=(ptok,))
            c.dma(wal_f[:], self.A["gla_w_alpha"][li], writes=(ptok,))
            c.dma(gnorm[:], self.A["gla_norm"][li].rearrange("(p o) -> p o", o=1), writes=(ptok,))
            c.op("pool", lambda e: e.tensor_copy(wal_b[:], wal_f[:]), reads=(ptok,), writes=(ptok,))
            for h in range(4):
                def ev_q(mt, tc, pb, pt, h=h):
                    ts = slice(tc * 512, (tc + 1) * 512)
                    c.op("act", lambda e: e.activation(qeT[0:64, h, ts], pb[0:64, :], AF.Copy, scale=0.125), reads=(pt,),
                         writes=(qtok,))

                def ev_k(mt, tc, pb, pt, h=h):
                    ts = slice(tc * 512, (tc + 1) * 512)
                    c.op("dve", lambda e: e.tensor_copy(keT[0:64, h, ts], pb[0:64, :]), reads=(pt,), writes=(ktok,))
                self.proj_feat(li, O_GQ + h * 64, 64, ev_q)
                self.proj_feat(li, O_GK + h * 64, 64, ev_k)

            def ev_a(mt, tc, pb, pt):
                ts = slice(tc * 512, (tc + 1) * 512)
                c.op("act", lambda e: e.copy(alrT[0:16, ts], pb[0:16, :]), reads=(pt,), writes=(atok,))
            self.proj_feat(li, O_GA, 16, ev_a)
            for i in range(4):
                def ev_v(t, pb, pt, i=i):
                    c.op("act", lambda e: e.copy(vtk[:, t, i * 128:(i + 1) * 128], pb[:, 0:128]), reads=(pt,), writes=(vtok,))
                self.proj_tok(li, O_GV + i * 128, 128, ev_v)
            import os
            gstop = int(os.environ.get("GLA_STOP", "99"))
            if gstop == 1:
                c.barrier()
                return
            for t in range(NT):
                tl = slice(t * 128, (t + 1) * 128)
                pa, pat = self.bank()
                self.mm(pa[:, 0:256], alrT[0:16, tl], wal_b[0:16, :], True, True, reads=(atok, ptok), writes=(pat,))
                xs, xst = self.nscr()
                c.op("dve", lambda e: e.tensor_tensor(xs[:, 0:256], pa[:, 0:256], balb[:], ALU.add), reads=(pat, ptok),
                     writes=(xst,))
                c.op("act", lambda e: e.activation(xs[:, 0:256], xs[:, 0:256], AF.Exp, scale=-1.0), reads=(xst,), writes=(xst,))
                c.op("act", lambda e: e.activation(xs[:, 0:256], xs[:, 0:256], AF.Ln, bias=1.0), reads=(xst,), writes=(xst,))
                pw, pwt = self.bank()
                self.mm(pw[:, 0:256], m2[:], xs[:, 0:256], True, True, reads=(mtok, xst), writes=(pwt,))
                c.op("act", lambda e: e.activation(k2[:, t, :], pw[:, 0:256], AF.Exp), reads=(pwt,), writes=(k2tok,))
                pbT, pbTt = self.bank()
                for h in range(4):
                    self.mm(pbT[0:64, h * 128:(h + 1) * 128], xs[:, h * 64:(h + 1) * 64], m1[:], h == 0, h == 3,
                            reads=(mtok, xst), writes=(pbTt,), skip_group_check=True)
                ebp, ebpt = self.nscr()
                ebn, ebnt = self.nscr()
                c.op("act", lambda e: e.activation(ebp[0:64, :], pbT[0:64, :], AF.Exp), reads=(pbTt,), writes=(ebpt,))
                c.op("act", lambda e: e.activation(ebn[0:64, :], pbT[0:64, :], AF.Exp, scale=-1.0), reads=(pbTt,), writes=(ebnt,))
                c.op("dve", lambda e: e.tensor_tensor(qeT[0:64, :, tl], qeT[0:64, :, tl],
                                                      ebp[0:64, :].rearrange("p (h n) -> p h n", h=4), ALU.mult),
                     reads=(ebpt,), writes=(qtok,))
                c.op("dve", lambda e: e.tensor_tensor(keT[0:64, :, tl], keT[0:64, :, tl],
                                                      ebn[0:64, :].rearrange("p (h n) -> p h n", h=4), ALU.mult),
                     reads=(ebnt,), writes=(ktok,))
                c.op("dve", lambda e: e.tensor_copy(dec[0:64, :, 2 * t:2 * t + 2],
                                                    ebp[0:64, :].rearrange("p (h c s) -> p h c s", h=4, c=2)[:, :, :, 63]),
                     reads=(ebpt,), writes=(stok,))
            c.barrier()
            sp_.close()
            if gstop == 2:
                return
            st_f = self.sb(st, "g_stf", [64, 4, 128], F32)
            st_b = self.sb(st, "g_stb", [64, 4, 128], BF16)
            oTs = [self.sb(st, f"g_oT{k}", [128, 512], BF16) for k in range(2)]
            onhs = [self.sb(st, f"g_on{k}", [128, S], BF16) for k in range(2)]
            otoks, ontoks = [Tok(), Tok()], [Tok(), Tok()]
            stoks = [Tok() for _ in range(4)]
            dectok = stok
            attb = [self.sb(st, f"g_att{i}", [128, 128], BF16) for i in range(3)]
            att_tok = [Tok(), Tok(), Tok()]
            att_i = [0]
            for i in range(2):
                def ev_k2(t, pb, pt, i=i):
                    c.op("dve", lambda e: e.tensor_tensor(k2[:, t, i * 128:(i + 1) * 128], k2[:, t, i * 128:(i + 1) * 128],
                                                          pb[:, 0:128], ALU.mult), reads=(pt,), writes=(k2tok,))
                self.proj_tok(li, O_GK + i * 128, 128, ev_k2)
            if gstop == 3:
                c.barrier()
                return
            def head_gen(h):
                s_ = h % 2
                oT, onh = oTs[s_], onhs[s_]
                otok, ontok = otoks[s_], ontoks[s_]
                ai = 0
                c.op("dve", lambda e: e.memset(st_f[0:64, h, :], 0.0), writes=(stoks[h],))
                c.op("dve", lambda e: e.memset(st_b[0:64, h, :], 0.0), writes=(stoks[h],))
                stok = stoks[h]
                for t in range(NT):
                    tl = slice(t * 128, (t + 1) * 128)
                    pa, pat = self.bank()
                    self.mm(pa[:, 0:128], keT[0:64, h, tl], qeT[0:64, h, tl], True, True, reads=(ktok, qtok), writes=(pat,))
                    ab, abt = attb[att_i[0] % 3], att_tok[att_i[0] % 3]
                    att_i[0] += 1
                    c.op("dve", lambda e: e.tensor_tensor(ab[:], pa[:, 0:128], tblk[:], ALU.mult), reads=(pat, mtok),
                         writes=(abt,))
                    for half in range(2):
                        cn = 2 * t + half
                        rs = slice(half * 64, half * 64 + 64)
                        cs = slice(cn * 64, (cn + 1) * 64)
                        vsl = vtk[rs, t, h * 128:(h + 1) * 128]
                        po, pot = self.bank()
                        self.mm(po[:, 0:64], vsl, ab[rs, rs], True, True, reads=(vtok, abt), writes=(pot,))
                        oc = (t % 4) * 128 + half * 64
                        c.op("act", lambda e: e.copy(oT[:, oc:oc + 64], po[:, 0:64]), reads=(pot,), writes=(otok,))
                        if cn > 0:
                            pi_, pit = self.bank()
                            self.mm(pi_[:, 0:64], st_b[0:64, h, :], qeT[0:64, h, cs], True, True, reads=(stok, qtok),
                                    writes=(pit,))
                            c.op("dve", lambda e: e.tensor_tensor(oT[:, oc:oc + 64], oT[:, oc:oc + 64], pi_[:, 0:64], ALU.add),
                                 reads=(pit, otok), writes=(otok,))
                        ps_, pst = self.bank()
                        self.mm(ps_[0:64, 0:128], k2[rs, t, h * 64:(h + 1) * 64], vsl, True, True, reads=(k2tok, vtok),
                                writes=(pst,))
                        c.op("dve", lambda e: e.scalar_tensor_tensor(st_f[0:64, h, :], st_f[0:64, h, :], dec[0:64, h, cn:cn + 1],
                                                                     ps_[0:64, 0:128], ALU.mult, ALU.add),
                             reads=(pst, stok, dectok), writes=(stok,))
                        c.op("dve", lambda e: e.tensor_copy(st_b[0:64, h, :], st_f[0:64, h, :]), reads=(stok,), writes=(stok,))
                        yield
                    if t % 4 == 3:
                        tc = t // 4
                        ts = slice(tc * 512, (tc + 1) * 512)
                        sq, sqt = self.nscr()
                        c.op("act", lambda e: e.activation(sq[:], oT[:], AF.Square), reads=(otok,), writes=(sqt,))
                        pn, pnt = self.bank()
                        self.mm(pn[:], self.ones_f[:], sq[:], True, True, reads=(sqt, ct), writes=(pnt,))
                        rr, rrt = self.nscr()
                        c.op("dve", lambda e: e.tensor_scalar(rr[:], pn[:], 1.0 / 128.0, EPS, ALU.mult, ALU.add), reads=(pnt,),
                             writes=(rrt,))
                        c.op("act", lambda e: e.activation(rr[:], rr[:], AF.Sqrt), reads=(rrt,), writes=(rrt,))
                        c.op("dve", lambda e: e.reciprocal(rr[:], rr[:]), reads=(rrt,), writes=(rrt,))
                        c.op("dve", lambda e: e.scalar_tensor_tensor(onh[:, ts], oT[:], gnorm[:, 0:1], rr[:], ALU.mult, ALU.mult),
                             reads=(otok, rrt, ptok), writes=(ontok,))

                def ev_g(mt, tc, pb, pt):
                    ts = slice(tc * 512, (tc + 1) * 512)
                    sg, sgt = self.nscr()
                    c.op("act", lambda e: e.activation(sg[:], pb[:], AF.Silu), reads=(pt,), writes=(sgt,))
                    c.op("dve", lambda e: e.tensor_tensor(self.yT[:, h, ts], sg[:], onh[:, ts], ALU.mult), reads=(sgt, ontok),
                         writes=(self.yT_tok,))
                self.proj_feat(li, O_GG + h * 128, 128, ev_g)

            self.run_streams([head_gen(h) for h in range(4)], 2)
            c.barrier()

    def proj_feat_dup(self, li, col0, evac):
        c = self.c
        w = self.A["w_in"][li].rearrange("(c p) n -> p c n", p=128)
        wv, wt = self.wload(w[:, :, col0:col0 + 64], KC, 64)
        wd, wdt = self.wdup, self.wdup_tok
        c.op("pool", lambda e: e.tensor_copy(wd[:, :, 0:64], wv), reads=(wt,), writes=(wdt,))
        c.op("pool", lambda e: e.tensor_copy(wd[:, :, 64:128], wv), reads=(wt,), writes=(wdt,))
        for tc in range(NTC):
            ts = slice(tc * 512, (tc + 1) * 512)
            pb, pt = self.bank()
            for cc in range(KC):
                self.mm(pb[:], wd[:, cc, :], self.hT[:, cc, ts], cc == 0, cc == KC - 1, reads=(wdt, self.hT_tok[tc]),
                        writes=(pt,))
            evac(0, tc, pb, pt)

    def rope_apply(self, dst, pb, pt, n, scale, cos_ap, sin_ap, dtok):
        c = self.c
        raw, rawt = self.rraw[self.rr_i % 2], self.rraw_tok[self.rr_i % 2]
        self.rr_i += 1
        c.op("act", lambda e: e.activation(raw[:, 0:n], pb, AF.Copy, scale=scale), reads=(pt,), writes=(rawt,))
        p2, p2t = self.bank()
        self.mm(p2[:, 0:n], self.Pm[:], raw[:, 0:n], True, True, reads=(rawt, self.tbl_tok), writes=(p2t,))
        t1, t1t = self.nscr()
        c.op("pool", lambda e: e.tensor_tensor(t1[:, 0:n], raw[:, 0:n], cos_ap, ALU.mult), reads=(rawt, self.tbl_tok),
             writes=(t1t,))
        t2, t2t = self.nscr()
        c.op("dve", lambda e: e.tensor_tensor(t2[:, 0:n], p2[:, 0:n], sin_ap, ALU.mult), reads=(p2t, self.tbl_tok),
             writes=(t2t,))
        c.op("dve", lambda e: e.tensor_tensor(dst, t1[:, 0:n], t2[:, 0:n], ALU.add), reads=(t1t, t2t), writes=(dtok,))

    def nsa_tables(self, cosT, sinT):
        c = self.c
        tbl = self.tbl_tok
        PI = float(np.pi)
        C1 = 6.28125
        C2 = float(2 * np.pi - 6.28125)
        with ExitStack() as s2:
            pidx = self.sb(s2, "n_pi", [128, 1], I32)
            f = self.sb(s2, "n_f", [128, 8], F32)
            posi = self.sb(s2, "n_posi", [128, 512], I32)
            ki = self.sb(s2, "n_ki", [128, 512], I32)
            ftok, ptok = Tok(), Tok()
            c.op("pool", lambda e: e.iota(pidx[:], [[0, 1]], base=0, channel_multiplier=1), writes=(ftok,))
            PF, GE, DD, G8, II, ACTV, SGN, INV = [f[:, i:i + 1] for i in range(8)]
            V = lambda fn: c.op("dve", fn, reads=(ftok,), writes=(ftok,))
            V(lambda e: e.tensor_copy(PF, pidx[:]))
            V(lambda e: e.tensor_single_scalar(GE, PF, 64.0, ALU.is_ge))
            V(lambda e: e.scalar_tensor_tensor(DD, GE, -64.0, PF, ALU.mult, ALU.add))
            V(lambda e: e.tensor_single_scalar(G8, DD, 8.0, ALU.is_ge))
            V(lambda e: e.scalar_tensor_tensor(II, G8, -8.0, DD, ALU.mult, ALU.add))
            V(lambda e: e.tensor_single_scalar(ACTV, DD, 16.0, ALU.is_lt))
            V(lambda e: e.tensor_scalar(SGN, G8, 2.0, -1.0, ALU.mult, ALU.add))
            V(lambda e: e.memset(INV, 0.0))
            for i in range(8):
                ci = float(np.float32(500000.0) ** np.float32(-i / 8.0))
                V(lambda e: e.tensor_scalar(GE, II, float(i), ci, ALU.is_equal, ALU.mult))
                V(lambda e: e.tensor_tensor(INV, INV, GE, ALU.add))
            V(lambda e: e.tensor_tensor(INV, INV, ACTV, ALU.mult))
            for tc in range(NTC):
                ts = slice(tc * 512, (tc + 1) * 512)
                c.dma(posi[:], self.A["positions"][0:1, ts].partition_broadcast(128), writes=(ptok,))
                ang, angt = self.nscr()
                c.op("dve", lambda e: e.tensor_copy(ang[:], posi[:]), reads=(ptok,), writes=(angt,))
                c.op("dve", lambda e: e.tensor_scalar(ang[:], ang[:], INV, None, ALU.mult), reads=(angt, ftok), writes=(angt,))
                for phase, dstT, use_sign in ((0.0, sinT, True), (PI / 2, cosT, False)):
                    u, ut = self.nscr()
                    r, rt = self.nscr()
                    c.op("dve", lambda e: e.tensor_scalar(u[:], ang[:], phase, 1.0 / (2 * PI), ALU.add, ALU.mult),
                         reads=(angt,), writes=(ut,))
                    c.op("dve", lambda e: e.tensor_copy(ki[:], u[:]), reads=(ut,), writes=(ptok,))
                    c.op("dve", lambda e: e.tensor_copy(u[:], ki[:]), reads=(ptok,), writes=(ut,))
                    c.op("dve", lambda e: e.scalar_tensor_tensor(r[:], u[:], -C1, ang[:], ALU.mult, ALU.add),
                         reads=(ut, angt), writes=(rt,))
                    c.op("dve", lambda e: e.scalar_tensor_tensor(r[:], u[:], -C2, r[:], ALU.mult, ALU.add), reads=(ut, rt),
                         writes=(rt,))
                    if phase != 0.0:
                        c.op("dve", lambda e: e.tensor_scalar(r[:], r[:], phase, None, ALU.add), reads=(rt,), writes=(rt,))
                    c.op("dve", lambda e: e.tensor_single_scalar(u[:], r[:], PI, ALU.is_gt), reads=(rt,), writes=(ut,))
                    c.op("dve", lambda e: e.scalar_tensor_tensor(r[:], u[:], -2 * PI, r[:], ALU.mult, ALU.add), reads=(ut, rt),
                         writes=(rt,))
                    c.op("dve", lambda e: e.tensor_single_scalar(u[:], r[:], -PI, ALU.is_lt), reads=(rt,), writes=(ut,))
                    c.op("dve", lambda e: e.scalar_tensor_tensor(r[:], u[:], 2 * PI, r[:], ALU.mult, ALU.add), reads=(ut, rt),
                         writes=(rt,))
                    c.op("dve", lambda e: e.tensor_scalar(r[:], r[:], PI, -PI, ALU.min, ALU.max), reads=(rt,), writes=(rt,))
                    c.op("act", lambda e: e.activation(r[:], r[:], AF.Sin), reads=(rt,), writes=(rt,))
                    if use_sign:
                        c.op("dve", lambda e: e.tensor_scalar(dstT[:, ts], r[:], SGN, None, ALU.mult), reads=(rt, ftok),
                             writes=(tbl,))
                    else:
                        c.op("dve", lambda e: e.tensor_copy(dstT[:, ts], r[:]), reads=(rt,), writes=(tbl,))
            c.barrier()

    def nsa(self, li):
        c = self.c
        ct = self.const_tok
        import os
        nstop = int(os.environ.get("NSA_STOP", "99"))
        with ExitStack() as st:
            kcT2 = self.sb(st, "n_kcT", [128, 2, 128], BF16)
            VCX = self.sb(st, "n_vcx", [128, 2, 97], BF16)
            cmptok = Tok()

            def open_tables(sx):
                cosT = self.sb(sx, "n_cos", [128, S], BF16)
                sinT = self.sb(sx, "n_sin", [128, S], BF16)
                self.Pm = self.sb(sx, "n_Pm", [128, 128], BF16)
                self.tbl_tok = Tok()
                self.rraw = [self.sb(sx, f"n_raw{i}", [128, 512], BF16) for i in range(2)]
                self.rraw_tok = [Tok(), Tok()]
                self.rr_i = 0
                self.wdup = self.sb(sx, "n_wdup", [128, KC, 128], BF16)
                self.wdup_tok = Tok()
                self.nsa_tables(cosT, sinT)
                c.op("pool", lambda e: e.memset(self.Pm[:], 0.0), writes=(self.tbl_tok,))
                for (d0, s0) in ((0, 8), (8, 0), (64, 72), (72, 64)):
                    c.op("pool", lambda e: e.tensor_copy(self.Pm[:, d0:d0 + 8], self.ident_b[:, s0:s0 + 8]),
                         reads=(ct, self.tbl_tok), writes=(self.tbl_tok,))
                return cosT, sinT
            sA = ExitStack()
            cosT, sinT = open_tables(sA)
            tbl = self.tbl_tok
            if "cosT" in self.dbg_out:
                self.dump_featmajor_bf16(cosT[:].rearrange("p (c s) -> p c s", c=1), [tbl], self.dbg_out["cosT"])
                self.dump_featmajor_bf16(sinT[:].rearrange("p (c s) -> p c s", c=1), [tbl], self.dbg_out["sinT"])
            with ExitStack() as s3:
                xcT = [self.sb(s3, "n_xk", [128, S], BF16), self.sb(s3, "n_xv", [128, S], BF16)]
                xtok = Tok()
                for kv, col0 in ((0, O_NKC), (1, O_NVC)):
                    def ev_x(mt, tc, pb, pt, kv=kv):
                        ts = slice(tc * 512, (tc + 1) * 512)
                        c.op("act", lambda e: e.copy(xcT[kv][:, ts], pb[:]), reads=(pt,), writes=(xtok,))
                    self.proj_feat(li, col0, 128, ev_x)
                W1 = self.sb(s3, "n_w1", [128, 32, 256], BF16)
                stg = self.sb(s3, "n_stg", [128, 2048], F32)
                W2f = self.sb(s3, "n_w2f", [128, 2, 64], F32)
                W2d = self.sb(s3, "n_w2d", [128, 2, 128], BF16)
                pe2 = self.sb(s3, "n_pe2", [32, 128], F32)
                peb = self.sb(s3, "n_peb", [128, 32], BF16)
                gh = self.sb(s3, "n_gh", [128, 2, 128], BF16)
                hb = self.sb(s3, "n_hb", [128, 2], F32)
                ovf = self.sb(s3, "n_ovf", [128, 3, 32], F32)
                stgt, w1t, w2t, pet, ght, hbt, ovt = [Tok() for _ in range(7)]
                c.op("pool", lambda e: e.memset(ovf[:], 0.5), writes=(ovt,))
                for k_, off in ((0, 0), (1, 16)):
                    c.op("pool", lambda e: e.affine_select(ovf[:, k_, :], ovf[:, k_, :], [[-64, 32]], ALU.is_ge, 0.0, base=off,
                                                           channel_multiplier=16), reads=(ovt,), writes=(ovt,))
                    c.op("pool", lambda e: e.affine_select(ovf[:, k_, :], ovf[:, k_, :], [[64, 32]], ALU.is_ge, 0.0,
                                                           base=63 - off, channel_multiplier=-16), reads=(ovt,), writes=(ovt,))
                c.op("pool", lambda e: e.tensor_tensor(ovf[:, 2, :], ovf[:, 0, :], ovf[:, 1, :], ALU.add), reads=(ovt,),
                     writes=(ovt,))
                for g in range(2):
                    c.op("pool", lambda e: e.tensor_copy(VCX[:, g, 65:97], ovf[:, 2, :]), reads=(ovt,), writes=(cmptok,))
                c.op("pool", lambda e: e.memset(VCX[:, :, 64:65], 1.0), writes=(cmptok,))
                for kv in range(2):
                    w1 = self.A["cmp_wk1" if kv == 0 else "cmp_wv1"][li].rearrange("(l d) n -> d l n", d=64)
                    for piece in range(4):
                        for half in range(2):
                            c.dma(stg[half * 64:(half + 1) * 64, :].rearrange("p (l n) -> p l n", l=8),
                                  w1[:, piece * 8:(piece + 1) * 8, :], writes=(stgt,))
                        c.op("pool", lambda e: e.tensor_copy(W1[:, piece * 8:(piece + 1) * 8, :],
                                                             stg[:].rearrange("p (l n) -> p l n", l=8)),
                             reads=(stgt,), writes=(w1t,))
                    w2 = self.A["cmp_wk2" if kv == 0 else "cmp_wv2"][li].rearrange("(c p) n -> p c n", p=128)
                    c.dma(W2f[:], w2, writes=(w2t,))
                    c.op("pool", lambda e: e.tensor_copy(W2d[:, :, 0:64], W2f[:]), reads=(w2t,), writes=(w2t,))
                    c.op("pool", lambda e: e.tensor_copy(W2d[:, :, 64:128], W2f[:]), reads=(w2t,), writes=(w2t,))
                    pe = self.A["cmp_pos_k" if kv == 0 else "cmp_pos_v"][li]
                    c.dma(pe2[:, 0:64], pe, writes=(pet,))
                    c.dma(pe2[:, 64:128], pe, writes=(pet,))
                    pp, ppt = self.bank()
                    c.op("pe", lambda e: e.transpose(pp[:, 0:32], pe2[:], self.ident_f[0:32, 0:32]), reads=(pet, ct),
                         writes=(ppt,))
                    c.op("dve", lambda e: e.tensor_copy(peb[:], pp[:, 0:32]), reads=(ppt,), writes=(pet,))
                    for half in range(2):
                        pk_, pkt = self.bank()
                        for l in range(32):
                            self.mm(pk_[:, 0:1], W1[0:64, l, half * 128:(half + 1) * 128], peb[0:64, l:l + 1], l == 0, l == 31,
                                    reads=(w1t, pet), writes=(pkt,))
                        c.op("dve", lambda e: e.tensor_copy(hb[:, half:half + 1], pk_[:, 0:1]), reads=(pkt,), writes=(hbt,))
                    for g in range(2):
                        gs = slice(g * 64, g * 64 + 64)
                        for half in range(2):
                            ph, pht = self.bank()
                            for l in range(32):
                                self.mm(ph[:, 0:127], W1[gs, l, half * 128:(half + 1) * 128],
                                        xcT[kv][gs, l:l + 16 * 126 + 1:16], l == 0, l == 31, reads=(w1t, xtok), writes=(pht,))
                            x, xt = self.nscr()
                            x2, x2t = self.nscr()
                            N_ = slice(0, 127)
                            c.op("dve", lambda e: e.tensor_scalar(x[:, N_], ph[:, N_], hb[:, half:half + 1], None, ALU.add),
                                 reads=(pht, hbt), writes=(xt,))
                            c.op("dve", lambda e: e.tensor_tensor(x2[:, N_], x[:, N_], x[:, N_], ALU.mult), reads=(xt,),
                                 writes=(x2t,))
                            c.op("dve", lambda e: e.tensor_scalar(x2[:, N_], x2[:, N_], 0.044715, 1.0, ALU.mult, ALU.add),
                                 reads=(x2t,), writes=(x2t,))
                            c.op("dve", lambda e: e.tensor_tensor(x2[:, N_], x2[:, N_], x[:, N_], ALU.mult), reads=(x2t, xt),
                                 writes=(x2t,))
                            c.op("act", lambda e: e.activation(x2[:, N_], x2[:, N_], AF.Tanh, scale=0.7978845608028654),
                                 reads=(x2t,), writes=(x2t,))
                            c.op("dve", lambda e: e.tensor_scalar(x[:, N_], x[:, N_], 0.5, None, ALU.mult), reads=(xt,),
                                 writes=(xt,))
                            c.op("dve", lambda e: e.scalar_tensor_tensor(gh[:, half, 0:127], x2[:, N_], 1.0, x[:, N_], ALU.add,
                                                                         ALU.mult), reads=(x2t, xt), writes=(ght,))
                        if kv == 0:
                            pk, pkt2 = self.bank()
                            for half in range(2):
                                self.mm(pk[:, 0:127], W2d[:, half, :], gh[:, half, 0:127], half == 0, half == 1,
                                        reads=(w2t, ght), writes=(pkt2,))
                            self.rope_apply(kcT2[:, g, 0:127], pk[:, 0:127], pkt2, 127, 1.0,
                                            cosT[:, 31:31 + 16 * 126 + 1:16], sinT[:, 31:31 + 16 * 126 + 1:16], cmptok)
                        else:
                            pv, pvt = self.bank()
                            for half in range(2):
                                self.mm(pv[0:127, 0:64], gh[:, half, 0:127], W2d[:, half, 0:64], half == 0, half == 1,
                                        reads=(w2t, ght), writes=(pvt,))
                            c.op("act", lambda e: e.copy(VCX[0:127, g, 0:64], pv[0:127, 0:64]), reads=(pvt,), writes=(cmptok,))
                if "kcT" in self.dbg_out:
                    self.dump2d("kcT", kcT2[:].rearrange("p g n -> p (g n)"), [cmptok])
                    self.dump2d("vcx", VCX[:].rearrange("p g n -> p (g n)"), [cmptok])
                c.barrier()
            sA.close()
            if nstop == 1:
                return
            qT = self.sb(st, "n_qT", [128, 4, S], BF16)
            ksT2 = self.sb(st, "n_ksT", [128, 2, S], BF16)
            kwT2 = self.sb(st, "n_kwT", [128, 2, S], BF16)
            vs = self.sb(st, "n_vs", [128, NT, 2, 65], BF16)
            vw = self.sb(st, "n_vw", [128, NT, 2, 65], BF16)
            sg = self.sb(st, "n_sg", [128, NT, 24], F32)
            sB = ExitStack()
            cosT, sinT = open_tables(sB)
            qtok, kstok, kwtok, vstok, vwtok, sgtok = [Tok() for _ in range(6)]

            def ev_q(mt, tc, pb, pt):
                ts = slice(tc * 512, (tc + 1) * 512)
                self.rope_apply(qT[:, mt, ts], pb[:], pt, 512, 0.125, cosT[:, ts], sinT[:, ts], qtok)
            self.proj_feat(li, O_NQ, 512, ev_q)
            for g in range(2):
                def ev_ks(mt, tc, pb, pt, g=g):
                    ts = slice(tc * 512, (tc + 1) * 512)
                    self.rope_apply(ksT2[:, g, ts], pb[:], pt, 512, 1.0, cosT[:, ts], sinT[:, ts], kstok)

                def ev_kw(mt, tc, pb, pt, g=g):
                    ts = slice(tc * 512, (tc + 1) * 512)
                    self.rope_apply(kwT2[:, g, ts], pb[:], pt, 512, 1.0, cosT[:, ts], sinT[:, ts], kwtok)
                self.proj_feat_dup(li, O_NKS + g * 64, ev_ks)
                self.proj_feat_dup(li, O_NKW + g * 64, ev_kw)
            c.op("pool", lambda e: e.memset(vs[:, :, :, 64:65], 1.0), writes=(vstok,))
            c.op("pool", lambda e: e.memset(vw[:, :, :, 64:65], 1.0), writes=(vwtok,))

            def ev_vs(t, pb, pt):
                c.op("act", lambda e: e.copy(vs[:, t, :, 0:64], pb[:, 0:128].rearrange("p (g d) -> p g d", g=2)), reads=(pt,),
                     writes=(vstok,))

            def ev_vw(t, pb, pt):
                c.op("act", lambda e: e.copy(vw[:, t, :, 0:64], pb[:, 0:128].rearrange("p (g d) -> p g d", g=2)), reads=(pt,),
                     writes=(vwtok,))

            def ev_sg(t, pb, pt):
                c.op("act", lambda e: e.activation(sg[:, t, :], pb[:, 0:24], AF.Sigmoid), reads=(pt,), writes=(sgtok,))
            self.proj_tok(li, O_NVS, 128, ev_vs)
            self.proj_tok(li, O_NVW, 128, ev_vw)
            self.proj_tok(li, O_NG, 24, ev_sg)
            if "qT" in self.dbg_out:
                self.dump_featmajor_bf16(qT, [qtok], self.dbg_out["qT"])
            c.barrier()
            sB.close()
            if nstop == 2:
                return
            am = self.sb(st, "n_am", [128, NT, 32], F32)
            Esel = self.sb(st, "n_E", [32, NT, 128], BF16)
            wneg = self.sb(st, "n_wneg", [128, 128], BF16)
            cm = self.sb(st, "n_cm", [128, 512], BF16)
            negT = self.sb(st, "n_negT", [32, 2, 512], BF16)
            acc = self.sb(st, "n_acc", [128, 4, 512], F32)
            ybf = self.sb(st, "n_ybf", [128, 512], BF16)
            pT = [self.sb(st, f"n_pT{i}", [128, 512], BF16) for i in range(5)]
            pT_tok = [Tok() for _ in range(5)]
            imp = self.sb(st, "n_imp", [128, 4, 2, 32], F32)
            sm = self.sb(st, "n_sm", [128, 24], F32)
            impm = self.sb(st, "n_impm", [128, 32], F32)
            top8 = self.sb(st, "n_top8", [128, 8], F32)
            nselb = self.sb(st, "n_nsel", [128, 32], BF16)
            mtok, cmtok, negtok, acctok, ytok, imptok, tktok = [Tok() for _ in range(7)]
            smtok = [Tok(), Tok(), Tok()]
            tA, tAt = self.nscr()
            tAv = tA[:].rearrange("p (t j) -> p t j", t=NT)
            c.op("pool", lambda e: e.memset(am[:], 0.0), writes=(mtok,))
            c.op("pool", lambda e: e.affine_select(am[:], am[:], [[128, NT], [-64, 32]], ALU.is_ge, -100.0, base=0,
                                                   channel_multiplier=1), reads=(mtok,), writes=(mtok,))
            c.op("pool", lambda e: e.memset(tA[:], 100.0), writes=(tAt,))
            c.op("pool", lambda e: e.affine_select(tAv, tAv, [[128, NT], [-64, 32]], ALU.is_ge, 0.0, base=0,
                                                   channel_multiplier=1), reads=(tAt,), writes=(tAt,))
            c.op("pool", lambda e: e.affine_select(tAv, tAv, [[-128, NT], [64, 32]], ALU.is_ge, 0.0, base=63,
                                                   channel_multiplier=-1), reads=(tAt,), writes=(tAt,))
            c.op("pool", lambda e: e.memset(tAv[:, :, 0:1], 100.0), reads=(tAt,), writes=(tAt,))
            c.op("pool", lambda e: e.tensor_tensor(am[:], am[:], tAv, ALU.add), reads=(tAt, mtok), writes=(mtok,))
            c.op("pool", lambda e: e.memset(Esel[:], 1.0), writes=(mtok,))
            c.op("pool", lambda e: e.affine_select(Esel[:], Esel[:], [[128, NT], [1, 128]], ALU.is_ge, 0.0, base=0,
                                                   channel_multiplier=-64), reads=(mtok,), writes=(mtok,))
            c.op("pool", lambda e: e.affine_select(Esel[:], Esel[:], [[-128, NT], [-1, 128]], ALU.is_ge, 0.0, base=63,
                                                   channel_multiplier=64), reads=(mtok,), writes=(mtok,))
            c.op("pool", lambda e: e.affine_select(wneg[:], self.zer_f[:], [[-1, 128]], ALU.is_gt, NEG, base=0,
                                                   channel_multiplier=1), reads=(ct,), writes=(mtok,))
            pi = [0]
            c.barrier()
            self.n_scr_banks = 5
            for qc in range(NTC):
                qs = slice(qc * 512, (qc + 1) * 512)
                c.op("pool", lambda e: e.memset(cm[:], 0.0), writes=(cmtok,))
                c.op("pool", lambda e: e.affine_select(cm[:], cm[:], [[1, 512]], ALU.is_ge, NEG, base=qc * 512 - 31,
                                                       channel_multiplier=-16), reads=(cmtok,), writes=(cmtok,))
                c.op("dve", lambda e: e.memset(imp[:], 0.0), writes=(imptok,))
                def cmp_stream(h):
                    g = h // 4
                    hp = slice((h % 2) * 64, (h % 2) * 64 + 64)
                    hc = h // 2
                    hcol = slice(h * 64, (h + 1) * 64)
                    sb_, stk = self.bank()
                    self.mm(sb_[0:127, :], kcT2[hp, g, 0:127], qT[hp, hc, qs], True, False, reads=(cmptok, qtok), writes=(stk,))
                    self.mm(sb_[0:127, :], self.ident_b[0:127, 0:127], cm[0:127, :], False, True, reads=(ct, cmtok),
                            writes=(stk,), skip_group_check=True)
                    p, ptk = pT[pi[0] % 5], pT_tok[pi[0] % 5]
                    pi[0] += 1
                    c.op("act", lambda e: e.activation(p[0:127, :], sb_[0:127, :], AF.Exp), reads=(stk,), writes=(ptk,))
                    yield
                    ob, ot = self.bank_acc()
                    O = ob[:, 0:388].rearrange("p (j d) -> p j d", j=4)
                    for j in range(4):
                        self.mm(O[:, j, :], p[0:127, j * 128:(j + 1) * 128], VCX[0:127, g, :], j == 0, True,
                                reads=(ptk, cmptok), writes=(ot,), skip_group_check=True)
                    yield
                    smt = smtok[h % 3]
                    for j in range(4):
                        qt = qc * 4 + j
                        o_ = (h % 3) * 8 + 2 * j
                        rcv, wv_ = sm[:, o_:o_ + 1], sm[:, o_ + 1:o_ + 2]
                        c.op("dve", lambda e: e.tensor_scalar(rcv, O[:, j, 64:65], 1e-30, None, ALU.max), reads=(ot,),
                             writes=(smt,))
                        c.op("dve", lambda e: e.reciprocal(rcv, rcv), reads=(smt,), writes=(smt,))
                        c.op("dve", lambda e: e.tensor_tensor(wv_, rcv, sg[:, qt, 3 * h:3 * h + 1], ALU.mult),
                             reads=(smt, sgtok), writes=(smt,))
                        c.op("dve", lambda e: e.tensor_scalar(acc[:, j, hcol], O[:, j, 0:64], wv_, None, ALU.mult),
                             reads=(ot, smt), writes=(acctok,))
                        c.op("dve", lambda e: e.scalar_tensor_tensor(imp[:, j, g, :], O[:, j, 65:97], rcv, imp[:, j, g, :],
                                                                     ALU.mult, ALU.add), reads=(ot, smt, imptok),
                             writes=(imptok,))
                    self.release_acc(ob)
                self.run_streams([cmp_stream(h) for h in range(8)], 3)
                for j in range(4):
                    qt = qc * 4 + j
                    for g in range(2):
                        c.op("dve", lambda e: e.tensor_tensor(impm[:], imp[:, j, g, :], am[:, qt, :], ALU.add),
                             reads=(imptok, mtok), writes=(tktok,))
                        c.op("dve", lambda e: e.max(top8[:], impm[:]), reads=(tktok,), writes=(tktok,))
                        c.op("dve", lambda e: e.tensor_scalar(impm[:], impm[:], top8[:, 7:8], None, ALU.is_ge), reads=(tktok,),
                             writes=(tktok,))
                        c.op("dve", lambda e: e.tensor_scalar(nselb[:], impm[:], -1.0, 30000.0, ALU.add, ALU.mult),
                             reads=(tktok,), writes=(tktok,))
                        pb, pt = self.bank()
                        pbb = pb[:].bitcast(BF16)
                        c.op("pe", lambda e: e.transpose(pbb[0:32, 0:128], nselb[:], self.ident_b[:]), reads=(tktok, ct),
                             writes=(pt,))
                        c.op("act", lambda e: e.copy(negT[0:32, g, j * 128:(j + 1) * 128], pbb[0:32, 0:128]), reads=(pt,),
                             writes=(negtok,))
                if "negT" in self.dbg_out and qc == 1:
                    self.dump2d("negT", negT[:].rearrange("p g n -> p (g n)"), [negtok])
                def sw_stream(br, h):
                    g = h // 4
                    hp = slice((h % 2) * 64, (h % 2) * 64 + 64)
                    hc = h // 2
                    hcol = slice(h * 64, (h + 1) * 64)
                    ob, ot = self.bank_acc()
                    O = ob[:, 0:260].rearrange("p (j d) -> p j d", j=4)
                    first = True
                    kt0 = 0 if br == 1 else max(0, 4 * qc - 2)
                    for kt in range(kt0, 4 * qc + 4):
                        rel = kt - 4 * qc
                        jlo = max(0, rel)
                        jhi = 3 if br == 1 else min(3, rel + 2)
                        ncol = (jhi - jlo + 1) * 128
                        q0 = qc * 512 + jlo * 128
                        kl = slice(kt * 128, (kt + 1) * 128)
                        KT = ksT2 if br == 1 else kwT2
                        ktk = kstok if br == 1 else kwtok
                        extra = []
                        if br == 1:
                            extra.append((slice(0, ncol), Esel[0:32, kt, :], negT[0:32, g, jlo * 128:jlo * 128 + ncol],
                                          (mtok, negtok)))
                        if rel >= 0:
                            extra.append((slice(0, 128), self.ident_b[:], self.cneg_b[:], (ct,)))
                        if br == 2 and 0 <= rel + 2 <= 3:
                            o2 = (rel + 2 - jlo) * 128
                            extra.append((slice(o2, o2 + 128), self.ident_b[:], wneg[:], (ct, mtok)))
                        sb_, stk = self.bank()
                        self.mm(sb_[:, 0:ncol], KT[hp, g, kl], qT[hp, hc, q0:q0 + ncol], True, len(extra) == 0,
                                reads=(ktk, qtok), writes=(stk,))
                        for ei, (csl, lh, rh, rd) in enumerate(extra):
                            self.mm(sb_[:, csl], lh, rh, False, ei == len(extra) - 1, reads=rd, writes=(stk,),
                                    skip_group_check=True)
                        p, ptk = pT[pi[0] % 5], pT_tok[pi[0] % 5]
                        pi[0] += 1
                        c.op("act", lambda e: e.activation(p[:, 0:ncol], sb_[:, 0:ncol], AF.Exp), reads=(stk,), writes=(ptk,))
                        yield
                        VV = vs if br == 1 else vw
                        vtk_ = vstok if br == 1 else vwtok
                        for j in range(jlo, jhi + 1):
                            qt = qc * 4 + j
                            cs = slice((j - jlo) * 128, (j - jlo + 1) * 128)
                            self.mm(O[:, j, :], p[:, cs], VV[:, kt, g, :], first, kt == qt, reads=(ptk, vtk_), writes=(ot,),
                                    skip_group_check=True)
                            first = False
                    smt = smtok[h % 3]
                    for j in range(4):
                        qt = qc * 4 + j
                        o_ = (h % 3) * 8 + 2 * j
                        rcv, wv_ = sm[:, o_:o_ + 1], sm[:, o_ + 1:o_ + 2]
                        c.op("dve", lambda e: e.reciprocal(rcv, O[:, j, 64:65]), reads=(ot,), writes=(smt,))
                        c.op("dve", lambda e: e.tensor_tensor(wv_, rcv, sg[:, qt, 3 * h + br:3 * h + br + 1], ALU.mult),
                             reads=(smt, sgtok), writes=(smt,))
                        c.op("dve", lambda e: e.scalar_tensor_tensor(acc[:, j, hcol], O[:, j, 0:64], wv_, acc[:, j, hcol],
                                                                     ALU.mult, ALU.add), reads=(ot, smt, acctok),
                             writes=(acctok,))
                    self.release_acc(ob)
                self.run_streams([sw_stream(br, h) for br in (1, 2) for h in range(8)], 3)
                for j in range(4):
                    t = qc * 4 + j
                    c.op("act", lambda e: e.copy(ybf[:], acc[:, j, :]), reads=(acctok,), writes=(ytok,))
                    pb, pt = self.bank()
                    pbb = pb[:].bitcast(BF16)
                    for jj in range(4):
                        c.op("pe", lambda e: e.transpose(pbb[:, jj * 128:(jj + 1) * 128], ybf[:, jj * 128:(jj + 1) * 128],
                                                         self.ident_b[:]), reads=(ytok, ct), writes=(pt,))
                    c.op("dve", lambda e: e.tensor_copy(self.yT[:, :, t * 128:(t + 1) * 128],
                                                        pbb[:, 0:512].rearrange("p (j n) -> p j n", j=4)),
                         reads=(pt,), writes=(self.yT_tok,))
            c.barrier()
            self.n_scr_banks = 6

    def fox(self, li):
        c = self.c
        with ExitStack() as st:
            qT = self.sb(st, "fx_qT", [128, 4, S], BF16)
            kT = self.sb(st, "fx_kT", [128, 4, S], BF16)
            V = self.sb(st, "fx_V", [128, NT, 8, 65], BF16)
            ytk = self.sb(st, "fx_y", [128, 4, 512], BF16)
            fl = self.sb(st, "fx_f", [128, NT, 8], F32)
            ncum = self.sb(st, "fx_ncum", [128, NT, 8], F32)
            nref = self.sb(st, "fx_nref", [128, NT, 8], F32)
            btab = self.sb(st, "fx_btab", [128, NT, NT, 8], F32)
            bfb = self.sb(st, "fx_bf", [128, 8], F32)
            qtok, ktok, vtok, ytok, ftok = Tok(), Tok(), Tok(), Tok(), Tok()
            self.aux_tok = Tok()
            self.proj_feat(li, O_FQ, 512, self.evac_featT(qT, qtok, 0.125))
            self.proj_feat(li, O_FK, 512, self.evac_featT(kT, ktok, 1.0))
            c.op("pool", lambda e: e.memset(V[:, :, :, 64:65], 1.0), writes=(vtok,))

            def evac_v(t, pb, pt):
                c.op("act", lambda e: e.copy(V[:, t, :, 0:64], pb[:, 0:512].rearrange("p (h d) -> p h d", h=8)),
                     reads=(pt,), writes=(vtok,))
            for half in range(4):
                def evac_vh(t, pb, pt, half=half):
                    c.op("act", lambda e: e.copy(V[:, t, half * 2:half * 2 + 2, 0:64],
                                                 pb[:, 0:128].rearrange("p (h d) -> p h d", h=2)),
                         reads=(pt,), writes=(vtok,))
                self.proj_tok(li, O_FV + half * 128, 128, evac_vh)
            c.dma(bfb[:], self.A["fox_b_f"][li:li + 1, :].partition_broadcast(128), writes=(ftok,))

            def evac_f(t, pb, pt):
                c.op("dve", lambda e: e.tensor_tensor(fl[:, t, :], pb[:, 0:8], bfb[:], ALU.add), reads=(pt, ftok),
                     writes=(ftok,))
            self.proj_tok(li, O_FF, 8, evac_f)
            flat = fl[:].rearrange("p t h -> p (t h)")
            c.op("act", lambda e: e.activation(flat, flat, AF.Exp, scale=-1.0), reads=(ftok,), writes=(ftok,))
            c.op("act", lambda e: e.activation(flat, flat, AF.Ln, bias=1.0), reads=(ftok,), writes=(ftok,))
            for t in range(NT):
                pb, pt = self.bank()
                for j in range(t):
                    self.mm(pb[:, 0:8], self.ones_f[:], fl[:, j, :], j == 0, False, reads=(ftok, self.const_tok),
                            writes=(pt,))
                self.mm(pb[:, 0:8], self.tri_f[:], fl[:, t, :], t == 0, True, reads=(ftok, self.const_tok), writes=(pt,))
                c.op("dve", lambda e: e.tensor_copy(ncum[:, t, :], pb[:, 0:8]), reads=(pt,), writes=(self.aux_tok,))
                if t > 0:
                    pb2, pt2 = self.bank()
                    for j in range(t):
                        self.mm(pb2[:, 0:8], self.ones_f[:], fl[:, j, :], j == 0, j == t - 1,
                                reads=(ftok, self.const_tok), writes=(pt2,))
                    c.op("dve", lambda e: e.tensor_copy(nref[:, t, :], pb2[:, 0:8]), reads=(pt2,), writes=(self.aux_tok,))
                else:
                    c.op("dve", lambda e: e.memset(nref[:, 0, :], 0.0), writes=(self.aux_tok,))
            for kt in range(NT):
                for qt in range(1, NT, 2):
                    if qt >= kt:
                        c.op("pool", lambda e: e.tensor_tensor(btab[:, kt, qt, :], ncum[:, kt, :], nref[:, qt, :],
                                                               ALU.subtract), reads=(self.aux_tok,), writes=(self.aux_tok,))
            self.dump2d("ncum", ncum[:].rearrange("p t h -> p (t h)"), [self.aux_tok])
            self.dump2d("nref", nref[:].rearrange("p t h -> p (t h)"), [self.aux_tok])
            self.dump2d("fl", fl[:].rearrange("p t h -> p (t h)"), [ftok])
            self.attention(st, "fx", 8, qT, qtok, kT, ktok, V, vtok, ytk, ytok,
                           bias_fn=lambda h, kt, qt: btab[:, kt, qt, h:h + 1],
                           post_qc=lambda qc: self.ytok_to_yT(ytk, ytok, qc))
            c.barrier()

    def final_norm_store(self, out):
        self.store_tok_major(out, normed=True)

    def store_tok_major(self, out, normed):
        c = self.c
        L = len(self.layers)
        with ExitStack() as st:
            if normed:
                for tc in range(NTC):
                    ts = slice(tc * 512, (tc + 1) * 512)
                    pb, pt = self.bank()
                    for cc in range(KC):
                        sq, sqt = self.nscr()
                        c.op("act", lambda e: e.activation(sq[:], self.xT[:, cc, ts], AF.Square),
                             reads=(self.xT_tok[tc],), writes=(sqt,))
                        self.mm(pb[:], self.ones_f[:], sq[:], cc == 0, cc == KC - 1, reads=(sqt, self.const_tok),
                                writes=(pt,))
                    rs, rst = self.nscr()
                    c.op("dve", lambda e: e.tensor_scalar(rs[:], pb[:], 1.0 / D, EPS, ALU.mult, ALU.add), reads=(pt,),
                         writes=(rst,))
                    c.op("act", lambda e: e.activation(rs[:], rs[:], AF.Sqrt), reads=(rst,), writes=(rst,))
                    c.op("dve", lambda e: e.reciprocal(rs[:], rs[:]), reads=(rst,), writes=(rst,))
                    for cc in range(KC):
                        g = self.vecT[:, L * 72 + cc:L * 72 + cc + 1]
                        c.op("dve", lambda e: e.scalar_tensor_tensor(self.xT[:, cc, ts], self.xT[:, cc, ts], g, rs[:],
                                                                     ALU.mult, ALU.mult),
                             reads=(rst, self.vec_tok), writes=(self.xT_tok[tc],))
            os_ = [self.sb(st, f"os{i}", [128, D], F32) for i in range(2)]
            os_tok = [Tok(), Tok()]
            for t in range(NT):
                b = t % 2
                for half in range(2):
                    pb, pt = self.bank()
                    for j in range(4):
                        cc = half * 4 + j
                        c.op("pe", lambda e: e.transpose(pb[:, j * 128:(j + 1) * 128],
                                                         self.xT[:, cc, t * 128:(t + 1) * 128], self.ident_f[:]),
                             reads=(self.xT_tok[t // 4], self.const_tok), writes=(pt,), inc=(j == 3))
                    dst = os_[b][:, half * 512:(half + 1) * 512]
                    if half == 0:
                        c.op("dve", lambda e: e.tensor_copy(dst, pb[:]), reads=(pt,), writes=(os_tok[b],))
                    else:
                        c.op("act", lambda e: e.copy(dst, pb[:]), reads=(pt,), writes=(os_tok[b],))
                c.dma(out[t * 128:(t + 1) * 128, :], os_[b][:], reads=(os_tok[b],))
            c.barrier()

    def dump2d(self, name, ap, toks):
        if name in self.dbg_out:
            self.c.barrier()
            self.c.dma(self.dbg_out[name], ap, reads=tuple(toks), q="pool")
            self.c.barrier()

    def dump_featmajor_bf16(self, tT, toks, dst):
        c = self.c
        with ExitStack() as st:
            tmp = self.sb(st, "dmp", [128, S], F32)
            tt = Tok()
            for cc in range(tT.shape[1]):
                c.op("dve", lambda e: e.tensor_copy(tmp[:], tT[:, cc, :]), reads=tuple(toks), writes=(tt,))
                c.dma(dst[cc * 128:(cc + 1) * 128, :], tmp[:], reads=(tt,))
            c.barrier()


def _prep_inputs(inputs, layers, b):
    L = len(layers)
    vecs = np.zeros((L, 72, 128), np.float32)
    for i, l in enumerate(layers):
        vecs[i, 0:8] = inputs["norm_mix"][l].reshape(8, 128)
        vecs[i, 8:16] = inputs["norm_ff"][l].reshape(8, 128)
        vecs[i, 16:48] = inputs["b_gate"][l].reshape(32, 128)
    m = {
        "x": np.ascontiguousarray(inputs["x"][b]),
        "w_in": np.ascontiguousarray(inputs["w_in"][layers]),
        "vecs": vecs,
        "norm_final": np.ascontiguousarray(inputs["norm_final"].reshape(8, 128)),
        "positions": np.ascontiguousarray(inputs["positions"][b:b + 1]).astype(np.int32),
        "cmp_pos_k": np.ascontiguousarray(inputs["nsa_cmp_pos_k"][layers]),
        "cmp_pos_v": np.ascontiguousarray(inputs["nsa_cmp_pos_v"][layers]),
        "cmp_wk1": np.ascontiguousarray(inputs["nsa_cmp_wk1"][layers]),
        "cmp_wk2": np.ascontiguousarray(inputs["nsa_cmp_wk2"][layers]),
        "cmp_wv1": np.ascontiguousarray(inputs["nsa_cmp_wv1"][layers]),
        "cmp_wv2": np.ascontiguousarray(inputs["nsa_cmp_wv2"][layers]),
        "fox_b_f": np.ascontiguousarray(inputs["fox_b_f"][layers]),
        "gla_w_alpha": np.ascontiguousarray(inputs["gla_w_alpha"][layers]),
        "gla_b_alpha": np.ascontiguousarray(inputs["gla_b_alpha"][layers]),
        "gla_norm": np.ascontiguousarray(inputs["gla_norm"][layers]),
        "w_branch": np.ascontiguousarray(inputs["w_branch"][layers]),
        "w_out": np.ascontiguousarray(inputs["w_out"][layers]),
        "w_ff1": np.ascontiguousarray(inputs["w_ff1"][layers]),
        "w_ff2": np.ascontiguousarray(inputs["w_ff2"][layers]),
    }
    return m


def run(inputs, layers=(0, 1, 2, 3), debug=(), ncores=8, trace=False, stage=99):
    layers = list(layers)
    bld = Builder(layers, first=True, last=True, debug=debug, stage=stage)
    nc = bld.build()
    in_maps = [_prep_inputs(inputs, layers, b) for b in range(ncores)]
    res = run_bass_kernel_spmd(nc, in_maps, core_ids=list(range(ncores)), trace=trace)
    return res


def kernel(**inputs):
    inputs = {k: np.asarray(v) for k, v in inputs.items()}
    res = run(inputs)
    out = np.stack([np.asarray(r["out"]) for r in res.results], axis=0)
    return out.astype(np.float32)
```

```python
import numpy as np
from contextlib import ExitStack
import concourse.bass as bass
import concourse.mybir as mybir
from concourse.bass_utils import run_bass_kernel_spmd

F32 = mybir.dt.float32
BF16 = mybir.dt.bfloat16
I32 = mybir.dt.int32
ALU = mybir.AluOpType
AF = mybir.ActivationFunctionType
AX = mybir.AxisListType

S = 2048
D = 1024
NT = S // 128
NTC = S // 512
KC = D // 128
DEPTH = 4
DFF = 4096
D_IN = 10032
EPS = 1e-6
NEG = -30000.0

SPLITS = (512, 128, 128, 128, 128, 128, 128, 24, 512, 512, 512, 256, 256, 512, 16, 512, 512, 512, 512, 8, 4096)
OFFS = np.concatenate([[0], np.cumsum(SPLITS)]).tolist()
(O_NQ, O_NKC, O_NVC, O_NKS, O_NVS, O_NKW, O_NVW, O_NG, O_SQ, O_SK, O_SV, O_GQ, O_GK, O_GV, O_GA, O_GG,
 O_FQ, O_FK, O_FV, O_FF, O_GATE) = OFFS[:21]

EPOCH = 4000


class Tok:
    __slots__ = ("w", "r", "name")

    def __init__(self, name=""):
        self.w = None
        self.r = {}
        self.name = name


class Ctx:
    def __init__(self, nc, es):
        self.nc = nc
        self.es = es
        self.eng = dict(pe=nc.tensor, act=nc.scalar, dve=nc.vector, pool=nc.gpsimd, sp=nc.sync)
        self.cur = {}
        self.nsem = 0
        self.waited = {e: {} for e in self.eng}
        for e in ("pe", "act", "dve", "pool"):
            self.cur[e] = [self._newsem(e), 0]
        self.own = {e: set() for e in self.eng}
        for e in ("pe", "act", "dve", "pool"):
            self.own[e].add(id(self.cur[e][0]))
        self.dq = {}
        for q in ("sp", "act", "pool"):
            sems = [self._newsem("d" + q) for _ in range(8 if q == "sp" else 4)]
            self.dq[q] = dict(sems=sems, tgt=[0] * len(sems), i=0)
        self.all_dma = []

    def _newsem(self, name):
        self.nsem += 1
        return self.es.enter_context(self.nc.semaphore(f"s_{name}_{self.nsem}"))

    def _wait(self, e, deps):
        w = self.waited[e]
        for (sem, val) in deps:
            if val <= 0:
                continue
            k = id(sem)
            if e == "pe" and k in self.own["pe"]:
                continue
            if w.get(k, 0) >= val:
                continue
            self.eng[e].wait_ge(sem, val)
            w[k] = val

    @staticmethod
    def _deps(reads, writes):
        deps = []
        for t in reads:
            if t.w is not None:
                deps.append(t.w)
        for t in writes:
            if t.w is not None:
                deps.append(t.w)
            deps.extend(t.r.values())
        return deps

    @staticmethod
    def _record(stamp, reads, writes):
        sem, val = stamp
        for t in reads:
            t.r[id(sem)] = stamp
        for t in writes:
            t.w = stamp
            t.r = {}

    def op(self, e, fn, reads=(), writes=(), inc=True):
        self._wait(e, self._deps(reads, writes))
        ins = fn(self.eng[e])
        sem, cnt = self.cur[e]
        stamp = (sem, cnt + 1)
        if inc:
            ins.then_inc(sem, 1)
            self.cur[e][1] = cnt + 1
        self._record(stamp, reads, writes)
        if inc and cnt + 1 >= EPOCH:
            ns = self._newsem(e)
            self.own[e].add(id(ns))
            self.cur[e] = [ns, 0]
        return ins

    def dma(self, out, in_, reads=(), writes=(), q="sp", **kw):
        dq = self.dq[q]
        i = dq["i"] % len(dq["sems"])
        dq["i"] += 1
        sem = dq["sems"][i]
        deps = self._deps(reads, writes)
        deps.append((sem, dq["tgt"][i]))
        self._wait(q, deps)
        self.eng[q].dma_start(out=out, in_=in_, **kw).then_inc(sem, 16)
        dq["tgt"][i] += 16
        stamp = (sem, dq["tgt"][i])
        self._record(stamp, reads, writes)
        return stamp

    def barrier(self):
        stamps = []
        for e in ("pe", "act", "dve", "pool"):
            sem, cnt = self.cur[e]
            stamps.append((sem, cnt))
        for q, dq in self.dq.items():
            for s, t in zip(dq["sems"], dq["tgt"]):
                stamps.append((s, t))
        for e in ("pe", "act", "dve", "pool", "sp"):
            self._wait_all(e, stamps)

    def _wait_all(self, e, stamps):
        w = self.waited[e]
        for (sem, val) in stamps:
            if val <= 0:
                continue
            k = id(sem)
            if k in self.own.get(e, ()) and (e == "pe"):
                continue
            if w.get(k, 0) >= val:
                continue
            self.eng[e].wait_ge(sem, val)
            w[k] = val


class Builder:
    def __init__(self, layers, first, last, debug=(), stage=99):
        self.stage = stage
        self.layers = layers
        self.first = first
        self.last = last
        self.debug = debug
        self.nc = bass.Bass("TRN2", target_bir_lowering=False)
        self.dbg_out = {}

    def sb(self, st, name, shape, dt):
        self._uid = getattr(self, "_uid", 0) + 1
        return st.enter_context(self.nc.sbuf_tensor(f"{name}_{self._uid}", shape, dt))

    def dram_in(self, name, shape, dt=F32):
        return self.nc.dram_tensor(name, list(shape), dt, kind="ExternalInput").ap()

    def mm(self, out, lhsT, rhs, start, stop, reads, writes, inc=None, **kw):
        if inc is None:
            inc = True
        return self.c.op("pe", lambda e: e.matmul(out, lhsT, rhs, start=start, stop=stop, **kw),
                         reads=reads, writes=writes, inc=inc)

    def bank(self):
        i = self.bank_i % self.n_scr_banks
        self.bank_i += 1
        return self.ps[i], self.ps_tok[i]

    def bank_acc(self):
        busy = self.acc_busy
        for i in range(self.n_scr_banks, 8):
            if i not in busy:
                busy.add(i)
                self.last_acc = i
                return self.ps[i], self.ps_tok[i]
        raise RuntimeError("no free accumulator bank")

    def release_acc(self, ob):
        for i in range(8):
            if self.ps[i] is ob:
                self.acc_busy.discard(i)

    def wload(self, src3, kc, ncols, eng="pool"):
        i = self.w_i % 2
        self.w_i += 1
        stg, stok = self.wstg[i], self.wstg_tok[i]
        wb, wtok = self.wbf[i], self.wbf_tok[i]
        n = kc * ncols
        assert n <= self.WMAX
        sv = stg[:, 0:n].rearrange("p (c n) -> p c n", c=kc)
        wv = wb[:, 0:n].rearrange("p (c n) -> p c n", c=kc)
        self.c.dma(sv, src3, reads=(), writes=(stok,))
        self.c.op(eng, lambda e: e.tensor_copy(wb[:, 0:n], stg[:, 0:n]), reads=(stok,), writes=(wtok,))
        return wv, wtok

    def build(self):
        nc = self.nc
        L = len(self.layers)
        A = {}
        A["x"] = self.dram_in("x", [S, D])
        A["w_in"] = self.dram_in("w_in", [L, D, D_IN])
        A["vecs"] = self.dram_in("vecs", [L, 72, 128])
        A["norm_final"] = self.dram_in("norm_final", [8, 128])
        A["positions"] = self.dram_in("positions", [1, S], I32)
        A["cmp_pos_k"] = self.dram_in("cmp_pos_k", [L, 32, 64])
        A["cmp_pos_v"] = self.dram_in("cmp_pos_v", [L, 32, 64])
        A["cmp_wk1"] = self.dram_in("cmp_wk1", [L, 2048, 256])
        A["cmp_wk2"] = self.dram_in("cmp_wk2", [L, 256, 64])
        A["cmp_wv1"] = self.dram_in("cmp_wv1", [L, 2048, 256])
        A["cmp_wv2"] = self.dram_in("cmp_wv2", [L, 256, 64])
        A["fox_b_f"] = self.dram_in("fox_b_f", [L, 8])
        A["gla_w_alpha"] = self.dram_in("gla_w_alpha", [L, 16, 256])
        A["gla_b_alpha"] = self.dram_in("gla_b_alpha", [L, 256])
        A["gla_norm"] = self.dram_in("gla_norm", [L, 128])
        A["w_branch"] = self.dram_in("w_branch", [L, 4, 512, D])
        A["w_out"] = self.dram_in("w_out", [L, D, D])
        A["w_ff1"] = self.dram_in("w_ff1", [L, D, DFF])
        A["w_ff2"] = self.dram_in("w_ff2", [L, DFF, D])
        self.A = A
        out = nc.dram_tensor("out", [S, D], F32, kind="ExternalOutput").ap()
        for name, shape in self.debug:
            self.dbg_out[name] = nc.dram_tensor("dbg_" + name, list(shape), F32, kind="ExternalOutput").ap()

        with ExitStack() as es:
            self.es = es
            c = self.c = Ctx(nc, es)
            self.xT = self.sb(es, "xT", [128, KC, S], F32)
            self.hT = self.sb(es, "hT", [128, KC, S], BF16)
            self.xT_tok = [Tok(f"xT{i}") for i in range(NTC)]
            self.hT_tok = [Tok(f"hT{i}") for i in range(NTC)]
            self.ident_f = self.sb(es, "ident_f", [128, 128], F32)
            self.ident_b = self.sb(es, "ident_b", [128, 128], BF16)
            self.ones_f = self.sb(es, "ones_f", [128, 128], F32)
            self.const_tok = Tok("const")
            self.vecT = self.sb(es, "vecT", [128, L * 72 + 8], F32)
            self.vec_tok = Tok("vec")
            self.WMAX = 1024
            self.wstg = [self.sb(es, f"wstg{i}", [128, self.WMAX], F32) for i in range(2)]
            self.wbf = [self.sb(es, f"wbf{i}", [128, self.WMAX], BF16) for i in range(2)]
            self.wstg_tok = [Tok() for _ in range(2)]
            self.wbf_tok = [Tok() for _ in range(2)]
            self.w_i = 0
            self.scr = [self.sb(es, f"scr{i}", [128, 512], F32) for i in range(4)]
            self.scr_tok = [Tok() for _ in range(4)]
            self.scr_i = 0
            self.ps = [es.enter_context(nc.psum_tensor(f"ps{i}", [128, 512], F32)) for i in range(8)]
            self.ps_tok = [Tok(f"ps{i}") for i in range(8)]
            self.bank_i = 0
            self.bank_j = 0
            self.acc_busy = set()
            self.n_scr_banks = 6

            self.make_consts()
            if self.first:
                self.load_x()
            else:
                self.load_xT()
            for li in range(L):
                if self.stage >= 1:
                    self.layer(li)
            if self.last and self.stage >= 3:
                self.final_norm_store(out)
            else:
                self.store_xT(out)
            c.barrier()
        return nc

    def run_streams(self, gens, k=2):
        gens = iter(gens)
        active = []
        for g in gens:
            active.append(g)
            if len(active) == k:
                break
        while active:
            for g in list(active):
                try:
                    next(g)
                except StopIteration:
                    active.remove(g)
                    nxt = next(gens, None)
                    if nxt is not None:
                        active.append(nxt)

    def nscr(self):
        i = self.scr_i % len(self.scr)
        self.scr_i += 1
        return self.scr[i], self.scr_tok[i]

    def make_consts(self):
        c = self.c
        nc = self.nc
        ct = self.const_tok
        c.op("pool", lambda e: e.memset(self.ones_f[:], 1.0), writes=(ct,))
        c.op("pool", lambda e: e.affine_select(self.ident_f[:], self.ones_f[:], [[-1, 128]], ALU.is_equal, 0.0,
                                               base=0, channel_multiplier=1), reads=(ct,), writes=(ct,))
        c.op("pool", lambda e: e.tensor_copy(self.ident_b[:], self.ident_f[:]), reads=(ct,), writes=(ct,))
        self.tri_f = self.sb(self.es, "tri_f", [128, 128], F32)
        c.op("pool", lambda e: e.affine_select(self.tri_f[:], self.ones_f[:], [[1, 128]], ALU.is_ge, 0.0,
                                               base=0, channel_multiplier=-1), reads=(ct,), writes=(ct,))
        self.zer_f = self.sb(self.es, "zer_f", [128, 128], F32)
        self.cneg_b = self.sb(self.es, "cneg_b", [128, 128], BF16)
        c.op("pool", lambda e: e.memset(self.zer_f[:], 0.0), writes=(ct,))
        self.nones_f = self.sb(self.es, "nones_f", [128, 128], F32)
        self.ones_b = self.sb(self.es, "ones_b", [128, 128], BF16)
        self.nones_b = self.sb(self.es, "nones_b", [128, 128], BF16)
        self.cnegs_b = self.sb(self.es, "cnegs_b", [128, 128], BF16)
        self.ntri_b = self.sb(self.es, "ntri_b", [128, 128], BF16)
        c.op("pool", lambda e: e.memset(self.nones_f[:], -1.0), writes=(ct,))
        c.op("pool", lambda e: e.memset(self.ones_b[:], 1.0), writes=(ct,))
        c.op("pool", lambda e: e.memset(self.nones_b[:], -1.0), writes=(ct,))
        c.op("pool", lambda e: e.affine_select(self.cnegs_b[:], self.zer_f[:], [[1, 128]], ALU.is_gt, NEG,
                                               base=0, channel_multiplier=-1), reads=(ct,), writes=(ct,))
        c.op("pool", lambda e: e.affine_select(self.ntri_b[:], self.nones_f[:], [[-1, 128]], ALU.is_ge, 0.0,
                                               base=0, channel_multiplier=1), reads=(ct,), writes=(ct,))
        c.op("pool", lambda e: e.affine_select(self.cneg_b[:], self.zer_f[:], [[1, 128]], ALU.is_ge, NEG,
                                               base=0, channel_multiplier=-1), reads=(ct,), writes=(ct,))
        L = len(self.layers)
        nrow = L * 72 + 8
        with ExitStack() as st:
            tmp = self.sb(st, "vtmp", [128, 4, 128], F32)
            tt = Tok()
            r0 = 0
            chunks = []
            while r0 < nrow:
                n = min(128, nrow - r0)
                chunks.append((r0, n))
                r0 += n
            for ci, (r0, n) in enumerate(chunks):
                a = r0
                while a < r0 + n:
                    if a < L * 72:
                        b = min(r0 + n, L * 72)
                        src = self.A["vecs"].rearrange("l r p -> (l r) p")[a:b, :]
                    else:
                        b = r0 + n
                        src = self.A["norm_final"][a - L * 72:b - L * 72, :]
                    c.dma(tmp[a - r0:b - r0, ci, :], src, writes=(tt,))
                    a = b
                pb, pt = self.bank()
                c.op("pe", lambda e: e.transpose(pb[:, 0:n], tmp[0:n, ci, :], self.ident_f[0:n, 0:n]),
                     reads=(tt, ct), writes=(pt,))
                c.op("dve", lambda e: e.tensor_copy(self.vecT[:, r0:r0 + n], pb[:, 0:n]), reads=(pt,),
                     writes=(self.vec_tok,))
            c.barrier()

    def vcol(self, li, kind, j):
        base = li * 72 + {"norm_mix": 0, "norm_ff": 8, "b_gate": 16}[kind]
        return self.vecT[:, base + j:base + j + 1]

    def load_x(self):
        c = self.c
        x = self.A["x"]
        with ExitStack() as st:
            xs = [self.sb(st, f"xs{i}", [128, D], F32) for i in range(2)]
            xs_tok = [Tok(), Tok()]
            for t in range(NT):
                b = t % 2
                c.dma(xs[b][:], x[t * 128:(t + 1) * 128, :], writes=(xs_tok[b],))
                for half in range(2):
                    pb, pt = self.bank()
                    for j in range(4):
                        cc = half * 4 + j
                        c.op("pe", lambda e: e.transpose(pb[:, j * 128:(j + 1) * 128], xs[b][:, cc * 128:(cc + 1) * 128],
                                                         self.ident_f[:]),
                             reads=(xs_tok[b], self.const_tok), writes=(pt,), inc=(j == 3))
                    dst = self.xT[:, half * 4:half * 4 + 4, t * 128:(t + 1) * 128]
                    src = pb[:].rearrange("p (j n) -> p j n", j=4)
                    eng = "dve" if half == 0 else "act"
                    if eng == "dve":
                        c.op("dve", lambda e: e.tensor_copy(dst, src), reads=(pt,), writes=(self.xT_tok[t // 4],))
                    else:
                        c.op("act", lambda e: e.copy(dst, src), reads=(pt,), writes=(self.xT_tok[t // 4],))
            c.barrier()

    def load_xT(self):
        raise NotImplementedError

    def store_xT(self, out):
        self.store_tok_major(out, normed=False)

    def rmsnorm_to_hT(self, gcol):
        c = self.c
        for tc in range(NTC):
            ts = slice(tc * 512, (tc + 1) * 512)
            pb, pt = self.bank()
            for cc in range(KC):
                sq, sqt = self.nscr()
                c.op("act", lambda e: e.activation(sq[:], self.xT[:, cc, ts], AF.Square),
                     reads=(self.xT_tok[tc],), writes=(sqt,))
                self.mm(pb[:], self.ones_f[:], sq[:], cc == 0, cc == KC - 1, reads=(sqt, self.const_tok), writes=(pt,))
            rs, rst = self.nscr()
            c.op("dve", lambda e: e.tensor_scalar(rs[:], pb[:], 1.0 / D, EPS, ALU.mult, ALU.add), reads=(pt,),
                 writes=(rst,))
            c.op("act", lambda e: e.activation(rs[:], rs[:], AF.Sqrt), reads=(rst,), writes=(rst,))
            c.op("dve", lambda e: e.reciprocal(rs[:], rs[:]), reads=(rst,), writes=(rst,))
            for cc in range(KC):
                c.op("dve", lambda e: e.scalar_tensor_tensor(self.hT[:, cc, ts], self.xT[:, cc, ts], gcol(cc), rs[:],
                                                             ALU.mult, ALU.mult),
                     reads=(self.xT_tok[tc], rst, self.vec_tok), writes=(self.hT_tok[tc],))

    def layer(self, li):
        self.rmsnorm_to_hT(lambda cc: self.vcol(li, "norm_mix", cc))
        if "hT" in self.dbg_out and li == 0:
            self.dump_featmajor_bf16(self.hT, self.hT_tok, self.dbg_out["hT"])
        if self.stage >= 4:
            self.yT = self.sb(self.es, f"yT{li}", [128, 4, S], BF16) if not hasattr(self, "yT") else self.yT
            self.yT_tok = Tok("yT")
            if self.stage >= 8:
                self.nsa(li)
                if "ynsa" in self.dbg_out and li == 0:
                    self.dump_featmajor_bf16(self.yT, [self.yT_tok], self.dbg_out["ynsa"])
                self.combine(li, 0)
            if self.stage == 8:
                return
            if self.stage >= 7:
                self.gla(li)
                if "ygla" in self.dbg_out and li == 0:
                    self.dump_featmajor_bf16(self.yT, [self.yT_tok], self.dbg_out["ygla"])
                self.combine(li, 2)
            if self.stage >= 6 and self.stage != 7:
                self.sbmix(li)
                if "ysb" in self.dbg_out and li == 0:
                    self.dump_featmajor_bf16(self.yT, [self.yT_tok], self.dbg_out["ysb"])
                self.combine(li, 1)
            if self.stage == 7:
                return
            self.fox(li)
            if "yT" in self.dbg_out and li == 0:
                self.dump_featmajor_bf16(self.yT, [self.yT_tok], self.dbg_out["yT"])
            if self.stage >= 5:
                self.combine(li, 3)
        if self.stage >= 2:
            self.rmsnorm_to_hT(lambda cc: self.vcol(li, "norm_ff", cc))
            self.ffn(li)

    def ffn(self, li):
        c = self.c
        w1 = self.A["w_ff1"][li].rearrange("(c p) n -> p c n", p=128)
        w2 = self.A["w_ff2"][li].rearrange("(f p) n -> p f n", p=128)
        G = 4
        with ExitStack() as st:
            aT = [self.sb(st, f"aT{i}", [128, G, S], BF16) for i in range(2)]
            aT_tok = [Tok(), Tok()]
            import os
            for g in range(int(os.environ.get('FFN_G', DFF // (128 * G)))):
                ab, abt = aT[g % 2], aT_tok[g % 2]
                for half in range(G):
                    f0 = g * G + half
                    wv, wt = self.wload(w1[:, :, f0 * 128:(f0 + 1) * 128], KC, 128)
                    for j in range(1):
                        for tc in range(NTC):
                            ts = slice(tc * 512, (tc + 1) * 512)
                            pb, pt = self.bank()
                            for cc in range(KC):
                                self.mm(pb[:], wv[:, cc, j * 128:(j + 1) * 128], self.hT[:, cc, ts], cc == 0, cc == KC - 1,
                                        reads=(wt, self.hT_tok[tc]), writes=(pt,))
                            r, rt = self.nscr()
                            c.op("act", lambda e: e.activation(r[:], pb[:], AF.Relu), reads=(pt,), writes=(rt,))
                            c.op("dve", lambda e: e.tensor_tensor(ab[:, half, ts], r[:], r[:], ALU.mult),
                                 reads=(rt,), writes=(abt,))
                for dh in range(4):
                    wv, wt = self.wload(w2[:, g * G:(g + 1) * G, dh * 256:(dh + 1) * 256], G, 256)
                    for j in range(2):
                        dt_ = dh * 2 + j
                        for tc in range(NTC):
                            ts = slice(tc * 512, (tc + 1) * 512)
                            pb, pt = self.bank()
                            for f in range(G):
                                self.mm(pb[:], wv[:, f, j * 128:(j + 1) * 128], ab[:, f, ts], f == 0, f == G - 1,
                                        reads=(wt, abt), writes=(pt,))
                            c.op("dve", lambda e: e.tensor_tensor(self.xT[:, dt_, ts], self.xT[:, dt_, ts], pb[:], ALU.add),
                                 reads=(pt,), writes=(self.xT_tok[tc],))
            c.barrier()


    def proj_feat(self, li, col0, ncols, evac):
        w = self.A["w_in"][li].rearrange("(c p) n -> p c n", p=128)
        n0 = 0
        while n0 < ncols:
            nn = min(128, ncols - n0)
            wv, wt = self.wload(w[:, :, col0 + n0:col0 + n0 + nn], KC, nn)
            for j in range((nn + 127) // 128):
                m = min(128, nn - j * 128)
                for tc in range(NTC):
                    ts = slice(tc * 512, (tc + 1) * 512)
                    pb, pt = self.bank()
                    for cc in range(KC):
                        self.mm(pb[0:m, :], wv[:, cc, j * 128:j * 128 + m], self.hT[:, cc, ts], cc == 0, cc == KC - 1,
                                reads=(wt, self.hT_tok[tc]), writes=(pt,))
                    evac((n0 + j * 128) // 128, tc, pb, pt)
            n0 += nn

    def proj_tok(self, li, col0, ncols, evac):
        w = self.A["w_in"][li].rearrange("(c p) n -> p c n", p=128)
        wv, wt = self.wload(w[:, :, col0:col0 + ncols], KC, ncols)
        for t in range(NT):
            pb, pt = self.bank()
            for cc in range(KC):
                self.mm(pb[:, 0:ncols], self.hT[:, cc, t * 128:(t + 1) * 128], wv[:, cc, :], cc == 0, cc == KC - 1,
                        reads=(wt, self.hT_tok[t // 4]), writes=(pt,))
            evac(t, pb, pt)

    def evac_featT(self, dst, dtok, scale=1.0):
        c = self.c
        cnt = [0]

        def f(mt, tc, pb, pt):
            ts = slice(tc * 512, (tc + 1) * 512)
            cnt[0] += 1
            if cnt[0] % 2 == 0:
                c.op("dve", lambda e: e.tensor_scalar(dst[:, mt, ts], pb[:], scale, None, ALU.mult), reads=(pt,),
                     writes=(dtok,))
            else:
                c.op("act", lambda e: e.activation(dst[:, mt, ts], pb[:], AF.Copy, scale=scale), reads=(pt,),
                     writes=(dtok,))
        return f

    def attention(self, st, name, nheads, qT, qtok, kT, ktok, V, vtok, ytok_t, ytok_tok, bias_fn=None, ycol0=0, post_qc=None):
        c = self.c
        pT = [self.sb(st, f"{name}_pT{i}", [128, 512], BF16) for i in range(6)]
        pT_tok = [Tok() for _ in range(6)]
        rc = self.sb(st, f"{name}_rc", [128, 12], F32)
        rc_tok = [Tok(), Tok(), Tok()]
        pi = [0]

        def head_stream(qc, h):
            hp = slice((h % 2) * 64, (h % 2) * 64 + 64)
            hc = h // 2
            ob, ot = self.bank_acc()
            O = ob[:, 0:260].rearrange("p (j d) -> p j d", j=4)
            nkt = 4 * qc + 4
            for kt in range(nkt):
                j0 = max(0, kt - 4 * qc)
                q0 = qc * 512 + j0 * 128
                ncol = 512 - j0 * 128
                sb_, stk = self.bank()
                diag = kt >= 4 * qc
                self.mm(sb_[:, 0:ncol], kT[hp, hc, kt * 128:(kt + 1) * 128], qT[hp, hc, q0:q0 + ncol], True, not diag,
                        reads=(ktok, qtok), writes=(stk,))
                if diag:
                    self.mm(sb_[:, 0:128], self.ident_b[:], self.cneg_b[:], False, True,
                            reads=(self.const_tok,), writes=(stk,), skip_group_check=True)
                p, ptk = pT[pi[0] % 6], pT_tok[pi[0] % 6]
                pi[0] += 1
                for half in range(2):
                    jl, jh = max(j0, 2 * half), 2 * half + 1
                    if jl > jh:
                        continue
                    cs = slice((jl - j0) * 128, (jh - j0 + 1) * 128)
                    b = bias_fn(h, kt, qc * 4 + 2 * half + 1) if bias_fn is not None else 0.0
                    c.op("act", lambda e: e.activation(p[:, cs], sb_[:, cs], AF.Exp, bias=b), reads=(stk, self.aux_tok),
                         writes=(ptk,))
                yield
                for j in range(j0, 4):
                    qt = qc * 4 + j
                    cs = slice((j - j0) * 128, (j - j0 + 1) * 128)
                    self.mm(O[:, j, :], p[:, cs], V[:, kt, h, :], kt == 0 and j == 0, kt == qt, reads=(ptk, vtok), writes=(ot,),
                            skip_group_check=True)
            rct = rc_tok[h % 3]
            for j in range(4):
                rcj = rc[:, (h % 3) * 4 + j:(h % 3) * 4 + j + 1]
                c.op("dve", lambda e: e.reciprocal(rcj, O[:, j, 64:65]), reads=(ot,), writes=(rct,))
                c.op("dve", lambda e: e.tensor_scalar(ytok_t[:, j, ycol0 + h * 64:ycol0 + (h + 1) * 64], O[:, j, 0:64],
                                                      rcj, None, ALU.mult),
                     reads=(ot, rct), writes=(ytok_tok,))
            self.release_acc(ob)

        c.barrier()
        self.n_scr_banks = 5
        for qc in range(NTC):
            self.run_streams([head_stream(qc, h) for h in range(nheads)], 3)
            if post_qc is not None:
                post_qc(qc)
        c.barrier()
        self.n_scr_banks = 6

    def ytok_to_yT(self, ytok_t, ytok_tok, qc):
        c = self.c
        for tl in range(4):
            t = qc * 4 + tl
            pb, pt = self.bank()
            pbb = pb[:].bitcast(BF16)
            for j in range(4):
                c.op("pe", lambda e: e.transpose(pbb[:, j * 128:(j + 1) * 128], ytok_t[:, tl, j * 128:(j + 1) * 128],
                                                 self.ident_b[:]),
                     reads=(ytok_tok, self.const_tok), writes=(pt,))
            c.op("dve", lambda e: e.tensor_copy(self.yT[:, :, t * 128:(t + 1) * 128],
                                                pbb[:, 0:512].rearrange("p (j n) -> p j n", j=4)),
                 reads=(pt,), writes=(self.yT_tok,))


    def combine(self, li, bi):
        c = self.c
        wg = self.A["w_in"][li].rearrange("(c p) n -> p c n", p=128)
        wb = self.A["w_branch"][li, bi].rearrange("(c p) n -> p c n", p=128)
        wo = self.A["w_out"][li].rearrange("(c p) n -> p c n", p=128)
        with ExitStack() as st:
            mT = self.sb(st, "mT", [128, KC, S], BF16)
            mtok = Tok()
            for dt_ in range(KC):
                g0 = O_GATE + bi * D + dt_ * 128
                wgv, wgt = self.wload(wg[:, :, g0:g0 + 128], KC, 128)
                wbv, wbt = self.wload(wb[:, :, dt_ * 128:(dt_ + 1) * 128], 4, 128)
                bcol = self.vcol(li, "b_gate", bi * 8 + dt_)
                for tc in range(NTC):
                    ts = slice(tc * 512, (tc + 1) * 512)
                    pa, pat = self.bank()
                    for cc in range(KC):
                        self.mm(pa[:], wgv[:, cc, :], self.hT[:, cc, ts], cc == 0, cc == KC - 1,
                                reads=(wgt, self.hT_tok[tc]), writes=(pat,))
                    pb, pbt = self.bank()
                    for cc in range(4):
                        self.mm(pb[:], wbv[:, cc, :], self.yT[:, cc, ts], cc == 0, cc == 3,
                                reads=(wbt, self.yT_tok), writes=(pbt,))
                    sg, sgt = self.nscr()
                    c.op("act", lambda e: e.activation(sg[:], pa[:], AF.Sigmoid, bias=bcol), reads=(pat, self.vec_tok),
                         writes=(sgt,))
                    c.op("dve", lambda e: e.tensor_tensor(mT[:, dt_, ts], sg[:], pb[:], ALU.mult), reads=(sgt, pbt),
                         writes=(mtok,))
            for do in range(KC):
                wov, wot = self.wload(wo[:, :, do * 128:(do + 1) * 128], KC, 128)
                for tc in range(NTC):
                    ts = slice(tc * 512, (tc + 1) * 512)
                    pb, pbt = self.bank()
                    for cc in range(KC):
                        self.mm(pb[:], wov[:, cc, :], mT[:, cc, ts], cc == 0, cc == KC - 1, reads=(wot, mtok), writes=(pbt,))
                    c.op("dve", lambda e: e.tensor_tensor(self.xT[:, do, ts], self.xT[:, do, ts], pb[:], ALU.add),
                         reads=(pbt,), writes=(self.xT_tok[tc],))
            c.barrier()


    def sbmix(self, li):
        c = self.c
        with ExitStack() as st:
            qT = self.sb(st, "sb_qT", [128, 4, S], BF16)
            kT = self.sb(st, "sb_kT", [128, 4, S], BF16)
            V = self.sb(st, "sb_V", [128, NT, 8, 64], BF16)
            ytk = self.sb(st, "sb_y", [128, 4, 512], BF16)
            spb = [[self.sb(st, f"sb_sp{k}{i}", [128, 512], BF16) for i in range(2)] for k in range(2)]
            spt = [[Tok(), Tok()] for k in range(2)]
            pT = [[self.sb(st, f"sb_pT{k}{i}", [128, 512], BF16) for i in range(2)] for k in range(2)]
            pTt = [[Tok(), Tok()] for k in range(2)]
            sufs = [(self.sb(st, f"sb_suf{k}", [1, 512], F32), self.sb(st, f"sb_sufh{k}", [1, 512], BF16),
                     self.sb(st, f"sb_sufl{k}", [1, 512], BF16), Tok()) for k in range(2)]
            qtok, ktok, vtok, ytok = Tok(), Tok(), Tok(), Tok()
            self.proj_feat(li, O_SQ, 512, self.evac_featT(qT, qtok, 0.125))
            self.proj_feat(li, O_SK, 512, self.evac_featT(kT, ktok, 1.0))
            for q4 in range(4):
                def evac_vh(t, pb, pt, q4=q4):
                    c.op("act", lambda e: e.copy(V[:, t, q4 * 2:q4 * 2 + 2, :],
                                                 pb[:, 0:128].rearrange("p (h d) -> p h d", h=2)),
                         reads=(pt,), writes=(vtok,))
                self.proj_tok(li, O_SV + q4 * 128, 128, evac_vh)
            def head_stream(qc, h):
                s_ = h % 2
                hp = slice((h % 2) * 64, (h % 2) * 64 + 64)
                hc = h // 2
                ob, ot = self.bank_acc()
                O = ob[:, 0:256].rearrange("p (j d) -> p j d", j=4)
                nkt = 4 * qc + 4
                suf, sufh, sufl, suft = sufs[s_]
                c.op("dve", lambda e: e.memset(suf[:], 0.0), writes=(suft,))
                c.op("dve", lambda e: e.memset(sufh[:], 0.0), writes=(suft,))
                c.op("dve", lambda e: e.memset(sufl[:], 0.0), writes=(suft,))
                first = True
                ti = 0
                for kt in range(nkt - 1, -1, -1):
                    j0 = max(0, kt - 4 * qc)
                    q0 = qc * 512 + j0 * 128
                    ncol = 512 - j0 * 128
                    diag = kt >= 4 * qc
                    ksl = kT[hp, hc, kt * 128:(kt + 1) * 128]
                    qsl = qT[hp, hc, q0:q0 + ncol]
                    pa, pat = self.bank()
                    self.mm(pa[:, 0:ncol], ksl, qsl, True, not diag, reads=(ktok, qtok), writes=(pat,))
                    if diag:
                        self.mm(pa[:, 0:128], self.ident_b[:], self.cnegs_b[:], False, True, reads=(self.const_tok,),
                                writes=(pat,), skip_group_check=True)
                    e_, et = self.nscr()
                    sp, spk = spb[s_][ti % 2], spt[s_][ti % 2]
                    p, ptk = pT[s_][ti % 2], pTt[s_][ti % 2]
                    ti += 1
                    c.op("act", lambda e: e.activation(e_[:, 0:ncol], pa[:, 0:ncol], AF.Exp), reads=(pat,), writes=(et,))
                    c.op("act", lambda e: e.activation(sp[:, 0:ncol], e_[:, 0:ncol], AF.Ln, bias=1.0), reads=(et,),
                         writes=(spk,))
                    yield
                    pb, pbt = self.bank()
                    self.mm(pb[:, 0:ncol], ksl, qsl, True, False, reads=(ktok, qtok), writes=(pbt,))
                    if diag:
                        self.mm(pb[:, 0:128], self.ident_b[:], self.cnegs_b[:], False, False, reads=(self.const_tok,),
                                writes=(pbt,), skip_group_check=True)
                    self.mm(pb[:, 0:ncol], self.nones_b[0:1, :], sufh[0:1, 512 - ncol:512], False, False,
                            reads=(suft, self.const_tok), writes=(pbt,), skip_group_check=True)
                    self.mm(pb[:, 0:ncol], self.nones_b[0:1, :], sufl[0:1, 512 - ncol:512], False, False,
                            reads=(suft, self.const_tok), writes=(pbt,), skip_group_check=True)
                    self.mm(pb[:, 0:ncol], self.ntri_b[:], sp[:, 0:ncol], False, True, reads=(spk, self.const_tok),
                            writes=(pbt,), skip_group_check=True)
                    if kt > 0:
                        pc, pct = self.bank()
                        self.mm(pc[0:1, 0:ncol], self.ones_b[:, 0:1], sp[:, 0:ncol], True, True,
                                reads=(spk, self.const_tok), writes=(pct,))
                        sl = slice(512 - ncol, 512)
                        c.op("dve", lambda e: e.tensor_tensor(suf[0:1, sl], suf[0:1, sl], pc[0:1, 0:ncol], ALU.add),
                             reads=(pct,), writes=(suft,))
                        c.op("dve", lambda e: e.tensor_copy(sufh[0:1, sl], suf[0:1, sl]), reads=(suft,), writes=(suft,))
                        c.op("dve", lambda e: e.tensor_tensor(sufl[0:1, sl], suf[0:1, sl], sufh[0:1, sl], ALU.subtract),
                             reads=(suft,), writes=(suft,))
                    c.op("act", lambda e: e.activation(p[:, 0:ncol], pb[:, 0:ncol], AF.Exp), reads=(pbt,), writes=(ptk,))
                    yield
                    for j in range(j0, 4):
                        cs = slice((j - j0) * 128, (j - j0 + 1) * 128)
                        self.mm(O[:, j, :], p[:, cs], V[:, kt, h, :], first, kt == 0, reads=(ptk, vtok), writes=(ot,),
                                skip_group_check=True)
                        first = False
                for j in range(4):
                    c.op("dve", lambda e: e.tensor_copy(ytk[:, j, h * 64:(h + 1) * 64], O[:, j, :]), reads=(ot,),
                         writes=(ytok,))
                self.release_acc(ob)

            for qc in range(NTC):
                self.run_streams([head_stream(qc, h) for h in range(8)], 2)
                self.ytok_to_yT(ytk, ytok, qc)
            c.barrier()

    def gla(self, li):
        c = self.c
        ct = self.const_tok
        with ExitStack() as st:
            qeT = self.sb(st, "g_qe", [64, 4, S], BF16)
            keT = self.sb(st, "g_ke", [64, 4, S], BF16)
            k2 = self.sb(st, "g_k2", [128, NT, 256], BF16)
            vtk = self.sb(st, "g_v", [128, NT, 512], BF16)
            gnorm = self.sb(st, "g_norm", [128, 1], F32)
            dec = self.sb(st, "g_dec", [64, 4, 32], F32)
            tblk = self.sb(st, "g_tblk", [128, 128], BF16)
            sp_ = ExitStack()
            alrT = self.sb(sp_, "g_alr", [16, S], BF16)
            balb = self.sb(sp_, "g_bal", [128, 256], F32)
            wal_f = self.sb(sp_, "g_walf", [16, 256], F32)
            wal_b = self.sb(sp_, "g_walb", [16, 256], BF16)
            n16 = self.sb(sp_, "g_n16", [128, 128], F32)
            m1 = self.sb(sp_, "g_m1", [128, 128], F32)
            m2 = self.sb(sp_, "g_m2", [128, 128], F32)
            att_tok = [Tok(), Tok()]
            mtok, qtok, ktok, vtok, k2tok, atok, ptok, stok, otok, ontok = [Tok() for _ in range(10)]
            c.op("pool", lambda e: e.memset(n16[:], -1.0 / 16.0), writes=(mtok,))
            c.op("pool", lambda e: e.affine_select(m1[:], n16[:], [[1, 128]], ALU.is_ge, 0.0, base=0, channel_multiplier=-1),
                 reads=(mtok,), writes=(mtok,))
            c.op("pool", lambda e: e.memset(m1[0:64, 64:128], 0.0), reads=(mtok,), writes=(mtok,))
            c.op("pool", lambda e: e.affine_select(m2[:], n16[:], [[-1, 128]], ALU.is_gt, 0.0, base=0, channel_multiplier=1),
                 reads=(mtok,), writes=(mtok,))
            c.op("pool", lambda e: e.memset(m2[64:128, 0:64], 0.0), reads=(mtok,), writes=(mtok,))
            c.op("pool", lambda e: e.tensor_copy(tblk[:], self.tri_f[:]), reads=(ct, mtok), writes=(mtok,))
            c.op("pool", lambda e: e.memset(tblk[0:64, 64:128], 0.0), reads=(mtok,), writes=(mtok,))
            c.dma(balb[:], self.A["gla_b_alpha"][li:li + 1, :].partition_broadcast(128), writes=(ptok,))
            c.dma(wal_f[:], self.A["gla_w_alpha"][li], writes=(ptok,))
            c.dma(gnorm[:], self.A["gla_norm"][li].rearrange("(p o) -> p o", o=1), writes=(ptok,))
            c.op("pool", lambda e: e.tensor_copy(wal_b[:], wal_f[:]), reads=(ptok,), writes=(ptok,))
            for h in range(4):
                def ev_q(mt, tc, pb, pt, h=h):
                    ts = slice(tc * 512, (tc + 1) * 512)
                    c.op("act", lambda e: e.activation(qeT[0:64, h, ts], pb[0:64, :], AF.Copy, scale=0.125), reads=(pt,),
                         writes=(qtok,))

                def ev_k(mt, tc, pb, pt, h=h):
                    ts = slice(tc * 512, (tc + 1) * 512)
                    c.op("dve", lambda e: e.tensor_copy(keT[0:64, h, ts], pb[0:64, :]), reads=(pt,), writes=(ktok,))
                self.proj_feat(li, O_GQ + h * 64, 64, ev_q)
                self.proj_feat(li, O_GK + h * 64, 64, ev_k)

            def ev_a(mt, tc, pb, pt):
                ts = slice(tc * 512, (tc + 1) * 512)
                c.op("act", lambda e: e.copy(alrT[0:16, ts], pb[0:16, :]), reads=(pt,), writes=(atok,))
            self.proj_feat(li, O_GA, 16, ev_a)
            for i in range(4):
                def ev_v(t, pb, pt, i=i):
                    c.op("act", lambda e: e.copy(vtk[:, t, i * 128:(i + 1) * 128], pb[:, 0:128]), reads=(pt,), writes=(vtok,))
                self.proj_tok(li, O_GV + i * 128, 128, ev_v)
            import os
            gstop = int(os.environ.get("GLA_STOP", "99"))
            if gstop == 1:
                c.barrier()
                return
            for t in range(NT):
                tl = slice(t * 128, (t + 1) * 128)
                pa, pat = self.bank()
                self.mm(pa[:, 0:256], alrT[0:16, tl], wal_b[0:16, :], True, True, reads=(atok, ptok), writes=(pat,))
                xs, xst = self.nscr()
                c.op("dve", lambda e: e.tensor_tensor(xs[:, 0:256], pa[:, 0:256], balb[:], ALU.add), reads=(pat, ptok),
                     writes=(xst,))
                c.op("act", lambda e: e.activation(xs[:, 0:256], xs[:, 0:256], AF.Exp, scale=-1.0), reads=(xst,), writes=(xst,))
                c.op("act", lambda e: e.activation(xs[:, 0:256], xs[:, 0:256], AF.Ln, bias=1.0), reads=(xst,), writes=(xst,))
                pw, pwt = self.bank()
                self.mm(pw[:, 0:256], m2[:], xs[:, 0:256], True, True, reads=(mtok, xst), writes=(pwt,))
                c.op("act", lambda e: e.activation(k2[:, t, :], pw[:, 0:256], AF.Exp), reads=(pwt,), writes=(k2tok,))
                pbT, pbTt = self.bank()
                for h in range(4):
                    self.mm(pbT[0:64, h * 128:(h + 1) * 128], xs[:, h * 64:(h + 1) * 64], m1[:], h == 0, h == 3,
                            reads=(mtok, xst), writes=(pbTt,), skip_group_check=True)
                ebp, ebpt = self.nscr()
                ebn, ebnt = self.nscr()
                c.op("act", lambda e: e.activation(ebp[0:64, :], pbT[0:64, :], AF.Exp), reads=(pbTt,), writes=(ebpt,))
                c.op("act", lambda e: e.activation(ebn[0:64, :], pbT[0:64, :], AF.Exp, scale=-1.0), reads=(pbTt,), writes=(ebnt,))
                c.op("dve", lambda e: e.tensor_tensor(qeT[0:64, :, tl], qeT[0:64, :, tl],
                                                      ebp[0:64, :].rearrange("p (h n) -> p h n", h=4), ALU.mult),
                     reads=(ebpt,), writes=(qtok,))
                c.op("dve", lambda e: e.tensor_tensor(keT[0:64, :, tl], keT[0:64, :, tl],
                                                      ebn[0:64, :].rearrange("p (h n) -> p h n", h=4), ALU.mult),
                     reads=(ebnt,), writes=(ktok,))
                c.op("dve", lambda e: e.tensor_copy(dec[0:64, :, 2 * t:2 * t + 2],
                                                    ebp[0:64, :].rearrange("p (h c s) -> p h c s", h=4, c=2)[:, :, :, 63]),
                     reads=(ebpt,), writes=(stok,))
            c.barrier()
            sp_.close()
            if gstop == 2:
                return
            st_f = self.sb(st, "g_stf", [64, 4, 128], F32)
            st_b = self.sb(st, "g_stb", [64, 4, 128], BF16)
            oTs = [self.sb(st, f"g_oT{k}", [128, 512], BF16) for k in range(2)]
            onhs = [self.sb(st, f"g_on{k}", [128, S], BF16) for k in range(2)]
            otoks, ontoks = [Tok(), Tok()], [Tok(), Tok()]
            stoks = [Tok() for _ in range(4)]
            dectok = stok
            attb = [self.sb(st, f"g_att{i}", [128, 128], BF16) for i in range(3)]
            att_tok = [Tok(), Tok(), Tok()]
            att_i = [0]
            for i in range(2):
                def ev_k2(t, pb, pt, i=i):
                    c.op("dve", lambda e: e.tensor_tensor(k2[:, t, i * 128:(i + 1) * 128], k2[:, t, i * 128:(i + 1) * 128],
                                                          pb[:, 0:128], ALU.mult), reads=(pt,), writes=(k2tok,))
                self.proj_tok(li, O_GK + i * 128, 128, ev_k2)
            if gstop == 3:
                c.barrier()
                return
            def head_gen(h):
                s_ = h % 2
                oT, onh = oTs[s_], onhs[s_]
                otok, ontok = otoks[s_], ontoks[s_]
                ai = 0
                c.op("dve", lambda e: e.memset(st_f[0:64, h, :], 0.0), writes=(stoks[h],))
                c.op("dve", lambda e: e.memset(st_b[0:64, h, :], 0.0), writes=(stoks[h],))
                stok = stoks[h]
                for t in range(NT):
                    tl = slice(t * 128, (t + 1) * 128)
                    pa, pat = self.bank()
                    self.mm(pa[:, 0:128], keT[0:64, h, tl], qeT[0:64, h, tl], True, True, reads=(ktok, qtok), writes=(pat,))
                    ab, abt = attb[att_i[0] % 3], att_tok[att_i[0] % 3]
                    att_i[0] += 1
                    c.op("dve", lambda e: e.tensor_tensor(ab[:], pa[:, 0:128], tblk[:], ALU.mult), reads=(pat, mtok),
                         writes=(abt,))
                    for half in range(2):
                        cn = 2 * t + half
                        rs = slice(half * 64, half * 64 + 64)
                        cs = slice(cn * 64, (cn + 1) * 64)
                        vsl = vtk[rs, t, h * 128:(h + 1) * 128]
                        po, pot = self.bank()
                        self.mm(po[:, 0:64], vsl, ab[rs, rs], True, True, reads=(vtok, abt), writes=(pot,))
                        oc = (t % 4) * 128 + half * 64
                        c.op("act", lambda e: e.copy(oT[:, oc:oc + 64], po[:, 0:64]), reads=(pot,), writes=(otok,))
                        if cn > 0:
                            pi_, pit = self.bank()
                            self.mm(pi_[:, 0:64], st_b[0:64, h, :], qeT[0:64, h, cs], True, True, reads=(stok, qtok),
                                    writes=(pit,))
                            c.op("dve", lambda e: e.tensor_tensor(oT[:, oc:oc + 64], oT[:, oc:oc + 64], pi_[:, 0:64], ALU.add),
                                 reads=(pit, otok), writes=(otok,))
                        ps_, pst = self.bank()
                        self.mm(ps_[0:64, 0:128], k2[rs, t, h * 64:(h + 1) * 64], vsl, True, True, reads=(k2tok, vtok),
                                writes=(pst,))
                        c.op("dve", lambda e: e.scalar_tensor_tensor(st_f[0:64, h, :], st_f[0:64, h, :], dec[0:64, h, cn:cn + 1],
                                                                     ps_[0:64, 0:128], ALU.mult, ALU.add),
                             reads=(pst, stok, dectok), writes=(stok,))
                        c.op("dve", lambda e: e.tensor_copy(st_b[0:64, h, :], st_f[0:64, h, :]), reads=(stok,), writes=(stok,))
                        yield
                    if t % 4 == 3:
                        tc = t // 4
                        ts = slice(tc * 512, (tc + 1) * 512)
                        sq, sqt = self.nscr()
                        c.op("act", lambda e: e.activation(sq[:], oT[:], AF.Square), reads=(otok,), writes=(sqt,))
                        pn, pnt = self.bank()
                        self.mm(pn[:], self.ones_f[:], sq[:], True, True, reads=(sqt, ct), writes=(pnt,))
                        rr, rrt = self.nscr()
                        c.op("dve", lambda e: e.tensor_scalar(rr[:], pn[:], 1.0 / 128.0, EPS, ALU.mult, ALU.add), reads=(pnt,),
                             writes=(rrt,))
                        c.op("act", lambda e: e.activation(rr[:], rr[:], AF.Sqrt), reads=(rrt,), writes=(rrt,))
                        c.op("dve", lambda e: e.reciprocal(rr[:], rr[:]), reads=(rrt,), writes=(rrt,))
                        c.op("dve", lambda e: e.scalar_tensor_tensor(onh[:, ts], oT[:], gnorm[:, 0:1], rr[:], ALU.mult, ALU.mult),
                             reads=(otok, rrt, ptok), writes=(ontok,))

                def ev_g(mt, tc, pb, pt):
                    ts = slice(tc * 512, (tc + 1) * 512)
                    sg, sgt = self.nscr()
                    c.op("act", lambda e: e.activation(sg[:], pb[:], AF.Silu), reads=(pt,), writes=(sgt,))
                    c.op("dve", lambda e: e.tensor_tensor(self.yT[:, h, ts], sg[:], onh[:, ts], ALU.mult), reads=(sgt, ontok),
                         writes=(self.yT_tok,))
                self.proj_feat(li, O_GG + h * 128, 128, ev_g)

            self.run_streams([head_gen(h) for h in range(4)], 2)
            c.barrier()

    def proj_feat_dup(self, li, col0, evac):
        c = self.c
        w = self.A["w_in"][li].rearrange("(c p) n -> p c n", p=128)
        wv, wt = self.wload(w[:, :, col0:col0 + 64], KC, 64)
        wd, wdt = self.wdup, self.wdup_tok
        c.op("pool", lambda e: e.tensor_copy(wd[:, :, 0:64], wv), reads=(wt,), writes=(wdt,))
        c.op("pool", lambda e: e.tensor_copy(wd[:, :, 64:128], wv), reads=(wt,), writes=(wdt,))
        for tc in range(NTC):
            ts = slice(tc * 512, (tc + 1) * 512)
            pb, pt = self.bank()
            for cc in range(KC):
                self.mm(pb[:], wd[:, cc, :], self.hT[:, cc, ts], cc == 0, cc == KC - 1, reads=(wdt, self.hT_tok[tc]),
                        writes=(pt,))
            evac(0, tc, pb, pt)

    def rope_apply(self, dst, pb, pt, n, scale, cos_ap, sin_ap, dtok):
        c = self.c
        raw, rawt = self.rraw[self.rr_i % 2], self.rraw_tok[self.rr_i % 2]
        self.rr_i += 1
        c.op("act", lambda e: e.activation(raw[:, 0:n], pb, AF.Copy, scale=scale), reads=(pt,), writes=(rawt,))
        p2, p2t = self.bank()
        self.mm(p2[:, 0:n], self.Pm[:], raw[:, 0:n], True, True, reads=(rawt, self.tbl_tok), writes=(p2t,))
        t1, t1t = self.nscr()
        c.op("pool", lambda e: e.tensor_tensor(t1[:, 0:n], raw[:, 0:n], cos_ap, ALU.mult), reads=(rawt, self.tbl_tok),
             writes=(t1t,))
        t2, t2t = self.nscr()
        c.op("dve", lambda e: e.tensor_tensor(t2[:, 0:n], p2[:, 0:n], sin_ap, ALU.mult), reads=(p2t, self.tbl_tok),
             writes=(t2t,))
        c.op("dve", lambda e: e.tensor_tensor(dst, t1[:, 0:n], t2[:, 0:n], ALU.add), reads=(t1t, t2t), writes=(dtok,))

    def nsa_tables(self, cosT, sinT):
        c = self.c
        tbl = self.tbl_tok
        PI = float(np.pi)
        C1 = 6.28125
        C2 = float(2 * np.pi - 6.28125)
        with ExitStack() as s2:
            pidx = self.sb(s2, "n_pi", [128, 1], I32)
            f = self.sb(s2, "n_f", [128, 8], F32)
            posi = self.sb(s2, "n_posi", [128, 512], I32)
            ki = self.sb(s2, "n_ki", [128, 512], I32)
            ftok, ptok = Tok(), Tok()
            c.op("pool", lambda e: e.iota(pidx[:], [[0, 1]], base=0, channel_multiplier=1), writes=(ftok,))
            PF, GE, DD, G8, II, ACTV, SGN, INV = [f[:, i:i + 1] for i in range(8)]
            V = lambda fn: c.op("dve", fn, reads=(ftok,), writes=(ftok,))
            V(lambda e: e.tensor_copy(PF, pidx[:]))
            V(lambda e: e.tensor_single_scalar(GE, PF, 64.0, ALU.is_ge))
            V(lambda e: e.scalar_tensor_tensor(DD, GE, -64.0, PF, ALU.mult, ALU.add))
            V(lambda e: e.tensor_single_scalar(G8, DD, 8.0, ALU.is_ge))
            V(lambda e: e.scalar_tensor_tensor(II, G8, -8.0, DD, ALU.mult, ALU.add))
            V(lambda e: e.tensor_single_scalar(ACTV, DD, 16.0, ALU.is_lt))
            V(lambda e: e.tensor_scalar(SGN, G8, 2.0, -1.0, ALU.mult, ALU.add))
            V(lambda e: e.memset(INV, 0.0))
            for i in range(8):
                ci = float(np.float32(500000.0) ** np.float32(-i / 8.0))
                V(lambda e: e.tensor_scalar(GE, II, float(i), ci, ALU.is_equal, ALU.mult))
                V(lambda e: e.tensor_tensor(INV, INV, GE, ALU.add))
            V(lambda e: e.tensor_tensor(INV, INV, ACTV, ALU.mult))
            for tc in range(NTC):
                ts = slice(tc * 512, (tc + 1) * 512)
                c.dma(posi[:], self.A["positions"][0:1, ts].partition_broadcast(128), writes=(ptok,))
                ang, angt = self.nscr()
                c.op("dve", lambda e: e.tensor_copy(ang[:], posi[:]), reads=(ptok,), writes=(angt,))
                c.op("dve", lambda e: e.tensor_scalar(ang[:], ang[:], INV, None, ALU.mult), reads=(angt, ftok), writes=(angt,))
                for phase, dstT, use_sign in ((0.0, sinT, True), (PI / 2, cosT, False)):
                    u, ut = self.nscr()
                    r, rt = self.nscr()
                    c.op("dve", lambda e: e.tensor_scalar(u[:], ang[:], phase, 1.0 / (2 * PI), ALU.add, ALU.mult),
                         reads=(angt,), writes=(ut,))
                    c.op("dve", lambda e: e.tensor_copy(ki[:], u[:]), reads=(ut,), writes=(ptok,))
                    c.op("dve", lambda e: e.tensor_copy(u[:], ki[:]), reads=(ptok,), writes=(ut,))
                    c.op("dve", lambda e: e.scalar_tensor_tensor(r[:], u[:], -C1, ang[:], ALU.mult, ALU.add),
                         reads=(ut, angt), writes=(rt,))
                    c.op("dve", lambda e: e.scalar_tensor_tensor(r[:], u[:], -C2, r[:], ALU.mult, ALU.add), reads=(ut, rt),
                         writes=(rt,))
                    if phase != 0.0:
                        c.op("dve", lambda e: e.tensor_scalar(r[:], r[:], phase, None, ALU.add), reads=(rt,), writes=(rt,))
                    c.op("dve", lambda e: e.tensor_single_scalar(u[:], r[:], PI, ALU.is_gt), reads=(rt,), writes=(ut,))
                    c.op("dve", lambda e: e.scalar_tensor_tensor(r[:], u[:], -2 * PI, r[:], ALU.mult, ALU.add), reads=(ut, rt),
                         writes=(rt,))
                    c.op("dve", lambda e: e.tensor_single_scalar(u[:], r[:], -PI, ALU.is_lt), reads=(rt,), writes=(ut,))
                    c.op("dve", lambda e: e.scalar_tensor_tensor(r[:], u[:], 2 * PI, r[:], ALU.mult, ALU.add), reads=(ut, rt),
                         writes=(rt,))
                    c.op("dve", lambda e: e.tensor_scalar(r[:], r[:], PI, -PI, ALU.min, ALU.max), reads=(rt,), writes=(rt,))
                    c.op("act", lambda e: e.activation(r[:], r[:], AF.Sin), reads=(rt,), writes=(rt,))
                    if use_sign:
                        c.op("dve", lambda e: e.tensor_scalar(dstT[:, ts], r[:], SGN, None, ALU.mult), reads=(rt, ftok),
                             writes=(tbl,))
                    else:
                        c.op("dve", lambda e: e.tensor_copy(dstT[:, ts], r[:]), reads=(rt,), writes=(tbl,))
            c.barrier()

    def nsa(self, li):
        c = self.c
        ct = self.const_tok
        import os
        nstop = int(os.environ.get("NSA_STOP", "99"))
        with ExitStack() as st:
            kcT2 = self.sb(st, "n_kcT", [128, 2, 128], BF16)
            VCX = self.sb(st, "n_vcx", [128, 2, 97], BF16)
            cmptok = Tok()

            def open_tables(sx):
                cosT = self.sb(sx, "n_cos", [128, S], BF16)
                sinT = self.sb(sx, "n_sin", [128, S], BF16)
                self.Pm = self.sb(sx, "n_Pm", [128, 128], BF16)
                self.tbl_tok = Tok()
                self.rraw = [self.sb(sx, f"n_raw{i}", [128, 512], BF16) for i in range(2)]
                self.rraw_tok = [Tok(), Tok()]
                self.rr_i = 0
                self.wdup = self.sb(sx, "n_wdup", [128, KC, 128], BF16)
                self.wdup_tok = Tok()
                self.nsa_tables(cosT, sinT)
                c.op("pool", lambda e: e.memset(self.Pm[:], 0.0), writes=(self.tbl_tok,))
                for (d0, s0) in ((0, 8), (8, 0), (64, 72), (72, 64)):
                    c.op("pool", lambda e: e.tensor_copy(self.Pm[:, d0:d0 + 8], self.ident_b[:, s0:s0 + 8]),
                         reads=(ct, self.tbl_tok), writes=(self.tbl_tok,))
                return cosT, sinT
            kcraw = self.sb(st, "n_kcraw", [128, 2, 128], BF16)
            rawtok = Tok()
            with ExitStack() as s3:
                xcT = [self.sb(s3, "n_xk", [128, S], BF16), self.sb(s3, "n_xv", [128, S], BF16)]
                xtok = Tok()
                for kv, col0 in ((0, O_NKC), (1, O_NVC)):
                    def ev_x(mt, tc, pb, pt, kv=kv):
                        ts = slice(tc * 512, (tc + 1) * 512)
                        c.op("act", lambda e: e.copy(xcT[kv][:, ts], pb[:]), reads=(pt,), writes=(xtok,))
                    self.proj_feat(li, col0, 128, ev_x)
                W1 = self.sb(s3, "n_w1", [128, 32, 256], BF16)
                stg = self.sb(s3, "n_stg", [128, 2048], F32)
                W2f = self.sb(s3, "n_w2f", [128, 2, 64], F32)
                W2d = self.sb(s3, "n_w2d", [128, 2, 128], BF16)
                pe2 = self.sb(s3, "n_pe2", [32, 128], F32)
                peb = self.sb(s3, "n_peb", [128, 32], BF16)
                gh = self.sb(s3, "n_gh", [128, 2, 128], BF16)
                hb = self.sb(s3, "n_hb", [128, 2], F32)
                ovf = self.sb(s3, "n_ovf", [128, 3, 32], F32)
                stgt, w1t, w2t, pet, ght, hbt, ovt = [Tok() for _ in range(7)]
                c.op("pool", lambda e: e.memset(ovf[:], 0.5), writes=(ovt,))
                for k_, off in ((0, 0), (1, 16)):
                    c.op("pool", lambda e: e.affine_select(ovf[:, k_, :], ovf[:, k_, :], [[-64, 32]], ALU.is_ge, 0.0, base=off,
                                                           channel_multiplier=16), reads=(ovt,), writes=(ovt,))
                    c.op("pool", lambda e: e.affine_select(ovf[:, k_, :], ovf[:, k_, :], [[64, 32]], ALU.is_ge, 0.0,
                                                           base=63 - off, channel_multiplier=-16), reads=(ovt,), writes=(ovt,))
                c.op("pool", lambda e: e.tensor_tensor(ovf[:, 2, :], ovf[:, 0, :], ovf[:, 1, :], ALU.add), reads=(ovt,),
                     writes=(ovt,))
                for g in range(2):
                    c.op("pool", lambda e: e.tensor_copy(VCX[:, g, 65:97], ovf[:, 2, :]), reads=(ovt,), writes=(cmptok,))
                c.op("pool", lambda e: e.memset(VCX[:, :, 64:65], 1.0), writes=(cmptok,))
                for kv in range(2):
                    w1 = self.A["cmp_wk1" if kv == 0 else "cmp_wv1"][li].rearrange("(l d) n -> d l n", d=64)
                    for piece in range(4):
                        for half in range(2):
                            c.dma(stg[half * 64:(half + 1) * 64, :].rearrange("p (l n) -> p l n", l=8),
                                  w1[:, piece * 8:(piece + 1) * 8, :], writes=(stgt,))
                        c.op("pool", lambda e: e.tensor_copy(W1[:, piece * 8:(piece + 1) * 8, :],
                                                             stg[:].rearrange("p (l n) -> p l n", l=8)),
                             reads=(stgt,), writes=(w1t,))
                    w2 = self.A["cmp_wk2" if kv == 0 else "cmp_wv2"][li].rearrange("(c p) n -> p c n", p=128)
                    c.dma(W2f[:], w2, writes=(w2t,))
                    c.op("pool", lambda e: e.tensor_copy(W2d[:, :, 0:64], W2f[:]), reads=(w2t,), writes=(w2t,))
                    c.op("pool", lambda e: e.tensor_copy(W2d[:, :, 64:128], W2f[:]), reads=(w2t,), writes=(w2t,))
                    pe = self.A["cmp_pos_k" if kv == 0 else "cmp_pos_v"][li]
                    c.dma(pe2[:, 0:64], pe, writes=(pet,))
                    c.dma(pe2[:, 64:128], pe, writes=(pet,))
                    pp, ppt = self.bank()
                    c.op("pe", lambda e: e.transpose(pp[:, 0:32], pe2[:], self.ident_f[0:32, 0:32]), reads=(pet, ct),
                         writes=(ppt,))
                    c.op("dve", lambda e: e.tensor_copy(peb[:], pp[:, 0:32]), reads=(ppt,), writes=(pet,))
                    for half in range(2):
                        pk_, pkt = self.bank()
                        for l in range(32):
                            self.mm(pk_[:, 0:1], W1[0:64, l, half * 128:(half + 1) * 128], peb[0:64, l:l + 1], l == 0, l == 31,
                                    reads=(w1t, pet), writes=(pkt,))
                        c.op("dve", lambda e: e.tensor_copy(hb[:, half:half + 1], pk_[:, 0:1]), reads=(pkt,), writes=(hbt,))
                    for g in range(2):
                        gs = slice(g * 64, g * 64 + 64)
                        for half in range(2):
                            ph, pht = self.bank()
                            for l in range(32):
                                self.mm(ph[:, 0:127], W1[gs, l, half * 128:(half + 1) * 128],
                                        xcT[kv][gs, l:l + 16 * 126 + 1:16], l == 0, l == 31, reads=(w1t, xtok), writes=(pht,))
                            x, xt = self.nscr()
                            x2, x2t = self.nscr()
                            N_ = slice(0, 127)
                            c.op("dve", lambda e: e.tensor_scalar(x[:, N_], ph[:, N_], hb[:, half:half + 1], None, ALU.add),
                                 reads=(pht, hbt), writes=(xt,))
                            c.op("dve", lambda e: e.tensor_tensor(x2[:, N_], x[:, N_], x[:, N_], ALU.mult), reads=(xt,),
                                 writes=(x2t,))
                            c.op("dve", lambda e: e.tensor_scalar(x2[:, N_], x2[:, N_], 0.044715, 1.0, ALU.mult, ALU.add),
                                 reads=(x2t,), writes=(x2t,))
                            c.op("dve", lambda e: e.tensor_tensor(x2[:, N_], x2[:, N_], x[:, N_], ALU.mult), reads=(x2t, xt),
                                 writes=(x2t,))
                            c.op("act", lambda e: e.activation(x2[:, N_], x2[:, N_], AF.Tanh, scale=0.7978845608028654),
                                 reads=(x2t,), writes=(x2t,))
                            c.op("dve", lambda e: e.tensor_scalar(x[:, N_], x[:, N_], 0.5, None, ALU.mult), reads=(xt,),
                                 writes=(xt,))
                            c.op("dve", lambda e: e.scalar_tensor_tensor(gh[:, half, 0:127], x2[:, N_], 1.0, x[:, N_], ALU.add,
                                                                         ALU.mult), reads=(x2t, xt), writes=(ght,))
                        if kv == 0:
                            pk, pkt2 = self.bank()
                            for half in range(2):
                                self.mm(pk[:, 0:127], W2d[:, half, :], gh[:, half, 0:127], half == 0, half == 1,
                                        reads=(w2t, ght), writes=(pkt2,))
                            c.op("act", lambda e: e.copy(kcraw[:, g, 0:127], pk[:, 0:127]), reads=(pkt2,), writes=(rawtok,))
                        else:
                            pv, pvt = self.bank()
                            for half in range(2):
                                self.mm(pv[0:127, 0:64], gh[:, half, 0:127], W2d[:, half, 0:64], half == 0, half == 1,
                                        reads=(w2t, ght), writes=(pvt,))
                            c.op("act", lambda e: e.copy(VCX[0:127, g, 0:64], pv[0:127, 0:64]), reads=(pvt,), writes=(cmptok,))
                c.barrier()
            if nstop == 1:
                return
            qT = self.sb(st, "n_qT", [128, 4, S], BF16)
            ksT2 = self.sb(st, "n_ksT", [128, 2, S], BF16)
            kwT2 = self.sb(st, "n_kwT", [128, 2, S], BF16)
            vs = self.sb(st, "n_vs", [128, NT, 2, 65], BF16)
            vw = self.sb(st, "n_vw", [128, NT, 2, 65], BF16)
            sg = self.sb(st, "n_sg", [128, NT, 24], F32)
            sB = ExitStack()
            cosT, sinT = open_tables(sB)
            tbl = self.tbl_tok
            for g in range(2):
                self.rope_apply(kcT2[:, g, 0:127], kcraw[:, g, 0:127], rawtok, 127, 1.0,
                                cosT[:, 31:31 + 16 * 126 + 1:16], sinT[:, 31:31 + 16 * 126 + 1:16], cmptok)
            if "kcT" in self.dbg_out:
                self.dump2d("kcT", kcT2[:].rearrange("p g n -> p (g n)"), [cmptok])
                self.dump2d("vcx", VCX[:].rearrange("p g n -> p (g n)"), [cmptok])
            qtok, kstok, kwtok, vstok, vwtok, sgtok = [Tok() for _ in range(6)]

            def ev_q(mt, tc, pb, pt):
                ts = slice(tc * 512, (tc + 1) * 512)
                self.rope_apply(qT[:, mt, ts], pb[:], pt, 512, 0.125, cosT[:, ts], sinT[:, ts], qtok)
            self.proj_feat(li, O_NQ, 512, ev_q)
            for g in range(2):
                def ev_ks(mt, tc, pb, pt, g=g):
                    ts = slice(tc * 512, (tc + 1) * 512)
                    self.rope_apply(ksT2[:, g, ts], pb[:], pt, 512, 1.0, cosT[:, ts], sinT[:, ts], kstok)

                def ev_kw(mt, tc, pb, pt, g=g):
                    ts = slice(tc * 512, (tc + 1) * 512)
                    self.rope_apply(kwT2[:, g, ts], pb[:], pt, 512, 1.0, cosT[:, ts], sinT[:, ts], kwtok)
                self.proj_feat_dup(li, O_NKS + g * 64, ev_ks)
                self.proj_feat_dup(li, O_NKW + g * 64, ev_kw)
            c.op("pool", lambda e: e.memset(vs[:, :, :, 64:65], 1.0), writes=(vstok,))
            c.op("pool", lambda e: e.memset(vw[:, :, :, 64:65], 1.0), writes=(vwtok,))

            def ev_vs(t, pb, pt):
                c.op("act", lambda e: e.copy(vs[:, t, :, 0:64], pb[:, 0:128].rearrange("p (g d) -> p g d", g=2)), reads=(pt,),
                     writes=(vstok,))

            def ev_vw(t, pb, pt):
                c.op("act", lambda e: e.copy(vw[:, t, :, 0:64], pb[:, 0:128].rearrange("p (g d) -> p g d", g=2)), reads=(pt,),
                     writes=(vwtok,))

            def ev_sg(t, pb, pt):
                c.op("act", lambda e: e.activation(sg[:, t, :], pb[:, 0:24], AF.Sigmoid), reads=(pt,), writes=(sgtok,))
            self.proj_tok(li, O_NVS, 128, ev_vs)
            self.proj_tok(li, O_NVW, 128, ev_vw)
            self.proj_tok(li, O_NG, 24, ev_sg)
            if "qT" in self.dbg_out:
                self.dump_featmajor_bf16(qT, [qtok], self.dbg_out["qT"])
            c.barrier()
            sB.close()
            if nstop == 2:
                return
            am = self.sb(st, "n_am", [128, NT, 32], F32)
            Esel = self.sb(st, "n_E", [32, NT, 128], BF16)
            wneg = self.sb(st, "n_wneg", [128, 128], BF16)
            cm = self.sb(st, "n_cm", [128, 512], BF16)
            negT = self.sb(st, "n_negT", [32, 2, 512], BF16)
            acc = self.sb(st, "n_acc", [128, 4, 512], F32)
            ybf = self.sb(st, "n_ybf", [128, 512], BF16)
            pT = [self.sb(st, f"n_pT{i}", [128, 512], BF16) for i in range(5)]
            pT_tok = [Tok() for _ in range(5)]
            imp = self.sb(st, "n_imp", [128, 4, 2, 32], F32)
            sm = self.sb(st, "n_sm", [128, 24], F32)
            impm = self.sb(st, "n_impm", [128, 32], F32)
            top8 = self.sb(st, "n_top8", [128, 8], F32)
            nselb = self.sb(st, "n_nsel", [128, 32], BF16)
            mtok, cmtok, negtok, acctok, ytok, imptok, tktok = [Tok() for _ in range(7)]
            smtok = [Tok(), Tok(), Tok()]
            tA, tAt = self.nscr()
            tAv = tA[:].rearrange("p (t j) -> p t j", t=NT)
            c.op("pool", lambda e: e.memset(am[:], 0.0), writes=(mtok,))
            c.op("pool", lambda e: e.affine_select(am[:], am[:], [[128, NT], [-64, 32]], ALU.is_ge, -100.0, base=0,
                                                   channel_multiplier=1), reads=(mtok,), writes=(mtok,))
            c.op("pool", lambda e: e.memset(tA[:], 100.0), writes=(tAt,))
            c.op("pool", lambda e: e.affine_select(tAv, tAv, [[128, NT], [-64, 32]], ALU.is_ge, 0.0, base=0,
                                                   channel_multiplier=1), reads=(tAt,), writes=(tAt,))
            c.op("pool", lambda e: e.affine_select(tAv, tAv, [[-128, NT], [64, 32]], ALU.is_ge, 0.0, base=63,
                                                   channel_multiplier=-1), reads=(tAt,), writes=(tAt,))
            c.op("pool", lambda e: e.memset(tAv[:, :, 0:1], 100.0), reads=(tAt,), writes=(tAt,))
            c.op("pool", lambda e: e.tensor_tensor(am[:], am[:], tAv, ALU.add), reads=(tAt, mtok), writes=(mtok,))
            c.op("pool", lambda e: e.memset(Esel[:], 1.0), writes=(mtok,))
            c.op("pool", lambda e: e.affine_select(Esel[:], Esel[:], [[128, NT], [1, 128]], ALU.is_ge, 0.0, base=0,
                                                   channel_multiplier=-64), reads=(mtok,), writes=(mtok,))
            c.op("pool", lambda e: e.affine_select(Esel[:], Esel[:], [[-128, NT], [-1, 128]], ALU.is_ge, 0.0, base=63,
                                                   channel_multiplier=64), reads=(mtok,), writes=(mtok,))
            c.op("pool", lambda e: e.affine_select(wneg[:], self.zer_f[:], [[-1, 128]], ALU.is_gt, NEG, base=0,
                                                   channel_multiplier=1), reads=(ct,), writes=(mtok,))
            pi = [0]
            c.barrier()
            self.n_scr_banks = 5
            for qc in range(NTC):
                qs = slice(qc * 512, (qc + 1) * 512)
                c.op("pool", lambda e: e.memset(cm[:], 0.0), writes=(cmtok,))
                c.op("pool", lambda e: e.affine_select(cm[:], cm[:], [[1, 512]], ALU.is_ge, NEG, base=qc * 512 - 31,
                                                       channel_multiplier=-16), reads=(cmtok,), writes=(cmtok,))
                c.op("dve", lambda e: e.memset(imp[:], 0.0), writes=(imptok,))
                def cmp_stream(h):
                    g = h // 4
                    hp = slice((h % 2) * 64, (h % 2) * 64 + 64)
                    hc = h // 2
                    hcol = slice(h * 64, (h + 1) * 64)
                    sb_, stk = self.bank()
                    self.mm(sb_[0:127, :], kcT2[hp, g, 0:127], qT[hp, hc, qs], True, False, reads=(cmptok, qtok), writes=(stk,))
                    self.mm(sb_[0:127, :], self.ident_b[0:127, 0:127], cm[0:127, :], False, True, reads=(ct, cmtok),
                            writes=(stk,), skip_group_check=True)
                    p, ptk = pT[pi[0] % 5], pT_tok[pi[0] % 5]
                    pi[0] += 1
                    c.op("act", lambda e: e.activation(p[0:127, :], sb_[0:127, :], AF.Exp), reads=(stk,), writes=(ptk,))
                    yield
                    ob, ot = self.bank_acc()
                    O = ob[:, 0:388].rearrange("p (j d) -> p j d", j=4)
                    for j in range(4):
                        self.mm(O[:, j, :], p[0:127, j * 128:(j + 1) * 128], VCX[0:127, g, :], j == 0, True,
                                reads=(ptk, cmptok), writes=(ot,), skip_group_check=True)
                    yield
                    smt = smtok[h % 3]
                    for j in range(4):
                        qt = qc * 4 + j
                        o_ = (h % 3) * 8 + 2 * j
                        rcv, wv_ = sm[:, o_:o_ + 1], sm[:, o_ + 1:o_ + 2]
                        c.op("dve", lambda e: e.tensor_scalar(rcv, O[:, j, 64:65], 1e-30, None, ALU.max), reads=(ot,),
                             writes=(smt,))
                        c.op("dve", lambda e: e.reciprocal(rcv, rcv), reads=(smt,), writes=(smt,))
                        c.op("dve", lambda e: e.tensor_tensor(wv_, rcv, sg[:, qt, 3 * h:3 * h + 1], ALU.mult),
                             reads=(smt, sgtok), writes=(smt,))
                        c.op("dve", lambda e: e.tensor_scalar(acc[:, j, hcol], O[:, j, 0:64], wv_, None, ALU.mult),
                             reads=(ot, smt), writes=(acctok,))
                        c.op("dve", lambda e: e.scalar_tensor_tensor(imp[:, j, g, :], O[:, j, 65:97], rcv, imp[:, j, g, :],
                                                                     ALU.mult, ALU.add), reads=(ot, smt, imptok),
                             writes=(imptok,))
                    self.release_acc(ob)
                self.run_streams([cmp_stream(h) for h in range(8)], 3)
                for j in range(4):
                    qt = qc * 4 + j
                    for g in range(2):
                        c.op("dve", lambda e: e.tensor_tensor(impm[:], imp[:, j, g, :], am[:, qt, :], ALU.add),
                             reads=(imptok, mtok), writes=(tktok,))
                        c.op("dve", lambda e: e.max(top8[:], impm[:]), reads=(tktok,), writes=(tktok,))
                        c.op("dve", lambda e: e.tensor_scalar(impm[:], impm[:], top8[:, 7:8], None, ALU.is_ge), reads=(tktok,),
                             writes=(tktok,))
                        c.op("dve", lambda e: e.tensor_scalar(nselb[:], impm[:], -1.0, 30000.0, ALU.add, ALU.mult),
                             reads=(tktok,), writes=(tktok,))
                        pb, pt = self.bank()
                        pbb = pb[:].bitcast(BF16)
                        c.op("pe", lambda e: e.transpose(pbb[0:32, 0:128], nselb[:], self.ident_b[:]), reads=(tktok, ct),
                             writes=(pt,))
                        c.op("act", lambda e: e.copy(negT[0:32, g, j * 128:(j + 1) * 128], pbb[0:32, 0:128]), reads=(pt,),
                             writes=(negtok,))
                if "negT" in self.dbg_out and qc == 1:
                    self.dump2d("negT", negT[:].rearrange("p g n -> p (g n)"), [negtok])
                def sw_stream(br, h):
                    g = h // 4
                    hp = slice((h % 2) * 64, (h % 2) * 64 + 64)
                    hc = h // 2
                    hcol = slice(h * 64, (h + 1) * 64)
                    ob, ot = self.bank_acc()
                    O = ob[:, 0:260].rearrange("p (j d) -> p j d", j=4)
                    first = True
                    kt0 = 0 if br == 1 else max(0, 4 * qc - 2)
                    for kt in range(kt0, 4 * qc + 4):
                        rel = kt - 4 * qc
                        jlo = max(0, rel)
                        jhi = 3 if br == 1 else min(3, rel + 2)
                        ncol = (jhi - jlo + 1) * 128
                        q0 = qc * 512 + jlo * 128
                        kl = slice(kt * 128, (kt + 1) * 128)
                        KT = ksT2 if br == 1 else kwT2
                        ktk = kstok if br == 1 else kwtok
                        extra = []
                        if br == 1:
                            extra.append((slice(0, ncol), Esel[0:32, kt, :], negT[0:32, g, jlo * 128:jlo * 128 + ncol],
                                          (mtok, negtok)))
                        if rel >= 0:
                            extra.append((slice(0, 128), self.ident_b[:], self.cneg_b[:], (ct,)))
                        if br == 2 and 0 <= rel + 2 <= 3:
                            o2 = (rel + 2 - jlo) * 128
                            extra.append((slice(o2, o2 + 128), self.ident_b[:], wneg[:], (ct, mtok)))
                        sb_, stk = self.bank()
                        self.mm(sb_[:, 0:ncol], KT[hp, g, kl], qT[hp, hc, q0:q0 + ncol], True, len(extra) == 0,
                                reads=(ktk, qtok), writes=(stk,))
                        for ei, (csl, lh, rh, rd) in enumerate(extra):
                            self.mm(sb_[:, csl], lh, rh, False, ei == len(extra) - 1, reads=rd, writes=(stk,),
                                    skip_group_check=True)
                        p, ptk = pT[pi[0] % 5], pT_tok[pi[0] % 5]
                        pi[0] += 1
                        c.op("act", lambda e: e.activation(p[:, 0:ncol], sb_[:, 0:ncol], AF.Exp), reads=(stk,), writes=(ptk,))
                        yield
                        VV = vs if br == 1 else vw
                        vtk_ = vstok if br == 1 else vwtok
                        for j in range(jlo, jhi + 1):
                            qt = qc * 4 + j
                            cs = slice((j - jlo) * 128, (j - jlo + 1) * 128)
                            self.mm(O[:, j, :], p[:, cs], VV[:, kt, g, :], first, kt == qt, reads=(ptk, vtk_), writes=(ot,),
                                    skip_group_check=True)
                            first = False
                    smt = smtok[h % 3]
                    for j in range(4):
                        qt = qc * 4 + j
                        o_ = (h % 3) * 8 + 2 * j
                        rcv, wv_ = sm[:, o_:o_ + 1], sm[:, o_ + 1:o_ + 2]
                        c.op("dve", lambda e: e.reciprocal(rcv, O[:, j, 64:65]), reads=(ot,), writes=(smt,))
                        c.op("dve", lambda e: e.tensor_tensor(wv_, rcv, sg[:, qt, 3 * h + br:3 * h + br + 1], ALU.mult),
                             reads=(smt, sgtok), writes=(smt,))
                        c.op("dve", lambda e: e.scalar_tensor_tensor(acc[:, j, hcol], O[:, j, 0:64], wv_, acc[:, j, hcol],
                                                                     ALU.mult, ALU.add), reads=(ot, smt, acctok),
                             writes=(acctok,))
                    self.release_acc(ob)
                self.run_streams([sw_stream(br, h) for br in (1, 2) for h in range(8)], 3)
                for j in range(4):
                    t = qc * 4 + j
                    c.op("act", lambda e: e.copy(ybf[:], acc[:, j, :]), reads=(acctok,), writes=(ytok,))
                    pb, pt = self.bank()
                    pbb = pb[:].bitcast(BF16)
                    for jj in range(4):
                        c.op("pe", lambda e: e.transpose(pbb[:, jj * 128:(jj + 1) * 128], ybf[:, jj * 128:(jj + 1) * 128],
                                                         self.ident_b[:]), reads=(ytok, ct), writes=(pt,))
                    c.op("dve", lambda e: e.tensor_copy(self.yT[:, :, t * 128:(t + 1) * 128],
                                                        pbb[:, 0:512].rearrange("p (j n) -> p j n", j=4)),
                         reads=(pt,), writes=(self.yT_tok,))
            c.barrier()
            self.n_scr_banks = 6

    def fox(self, li):
        c = self.c
        with ExitStack() as st:
            qT = self.sb(st, "fx_qT", [128, 4, S], BF16)
            kT = self.sb(st, "fx_kT", [128, 4, S], BF16)
            V = self.sb(st, "fx_V", [128, NT, 8, 65], BF16)
            ytk = self.sb(st, "fx_y", [128, 4, 512], BF16)
            fl = self.sb(st, "fx_f", [128, NT, 8], F32)
            ncum = self.sb(st, "fx_ncum", [128, NT, 8], F32)
            nref = self.sb(st, "fx_nref", [128, NT, 8], F32)
            btab = self.sb(st, "fx_btab", [128, NT, NT, 8], F32)
            bfb = self.sb(st, "fx_bf", [128, 8], F32)
            qtok, ktok, vtok, ytok, ftok = Tok(), Tok(), Tok(), Tok(), Tok()
            self.aux_tok = Tok()
            self.proj_feat(li, O_FQ, 512, self.evac_featT(qT, qtok, 0.125))
            self.proj_feat(li, O_FK, 512, self.evac_featT(kT, ktok, 1.0))
            c.op("pool", lambda e: e.memset(V[:, :, :, 64:65], 1.0), writes=(vtok,))

            def evac_v(t, pb, pt):
                c.op("act", lambda e: e.copy(V[:, t, :, 0:64], pb[:, 0:512].rearrange("p (h d) -> p h d", h=8)),
                     reads=(pt,), writes=(vtok,))
            for half in range(4):
                def evac_vh(t, pb, pt, half=half):
                    c.op("act", lambda e: e.copy(V[:, t, half * 2:half * 2 + 2, 0:64],
                                                 pb[:, 0:128].rearrange("p (h d) -> p h d", h=2)),
                         reads=(pt,), writes=(vtok,))
                self.proj_tok(li, O_FV + half * 128, 128, evac_vh)
            c.dma(bfb[:], self.A["fox_b_f"][li:li + 1, :].partition_broadcast(128), writes=(ftok,))

            def evac_f(t, pb, pt):
                c.op("dve", lambda e: e.tensor_tensor(fl[:, t, :], pb[:, 0:8], bfb[:], ALU.add), reads=(pt, ftok),
                     writes=(ftok,))
            self.proj_tok(li, O_FF, 8, evac_f)
            flat = fl[:].rearrange("p t h -> p (t h)")
            c.op("act", lambda e: e.activation(flat, flat, AF.Exp, scale=-1.0), reads=(ftok,), writes=(ftok,))
            c.op("act", lambda e: e.activation(flat, flat, AF.Ln, bias=1.0), reads=(ftok,), writes=(ftok,))
            for t in range(NT):
                pb, pt = self.bank()
                for j in range(t):
                    self.mm(pb[:, 0:8], self.ones_f[:], fl[:, j, :], j == 0, False, reads=(ftok, self.const_tok),
                            writes=(pt,))
                self.mm(pb[:, 0:8], self.tri_f[:], fl[:, t, :], t == 0, True, reads=(ftok, self.const_tok), writes=(pt,))
                c.op("dve", lambda e: e.tensor_copy(ncum[:, t, :], pb[:, 0:8]), reads=(pt,), writes=(self.aux_tok,))
                if t > 0:
                    pb2, pt2 = self.bank()
                    for j in range(t):
                        self.mm(pb2[:, 0:8], self.ones_f[:], fl[:, j, :], j == 0, j == t - 1,
                                reads=(ftok, self.const_tok), writes=(pt2,))
                    c.op("dve", lambda e: e.tensor_copy(nref[:, t, :], pb2[:, 0:8]), reads=(pt2,), writes=(self.aux_tok,))
                else:
                    c.op("dve", lambda e: e.memset(nref[:, 0, :], 0.0), writes=(self.aux_tok,))
            for kt in range(NT):
                for qt in range(1, NT, 2):
                    if qt >= kt:
                        c.op("pool", lambda e: e.tensor_tensor(btab[:, kt, qt, :], ncum[:, kt, :], nref[:, qt, :],
                                                               ALU.subtract), reads=(self.aux_tok,), writes=(self.aux_tok,))
            self.dump2d("ncum", ncum[:].rearrange("p t h -> p (t h)"), [self.aux_tok])
            self.dump2d("nref", nref[:].rearrange("p t h -> p (t h)"), [self.aux_tok])
            self.dump2d("fl", fl[:].rearrange("p t h -> p (t h)"), [ftok])
            self.attention(st, "fx", 8, qT, qtok, kT, ktok, V, vtok, ytk, ytok,
                           bias_fn=lambda h, kt, qt: btab[:, kt, qt, h:h + 1],
                           post_qc=lambda qc: self.ytok_to_yT(ytk, ytok, qc))
            c.barrier()

    def final_norm_store(self, out):
        self.store_tok_major(out, normed=True)

    def store_tok_major(self, out, normed):
        c = self.c
        L = len(self.layers)
        with ExitStack() as st:
            if normed:
                for tc in range(NTC):
                    ts = slice(tc * 512, (tc + 1) * 512)
                    pb, pt = self.bank()
                    for cc in range(KC):
                        sq, sqt = self.nscr()
                        c.op("act", lambda e: e.activation(sq[:], self.xT[:, cc, ts], AF.Square),
                             reads=(self.xT_tok[tc],), writes=(sqt,))
                        self.mm(pb[:], self.ones_f[:], sq[:], cc == 0, cc == KC - 1, reads=(sqt, self.const_tok),
                                writes=(pt,))
                    rs, rst = self.nscr()
                    c.op("dve", lambda e: e.tensor_scalar(rs[:], pb[:], 1.0 / D, EPS, ALU.mult, ALU.add), reads=(pt,),
                         writes=(rst,))
                    c.op("act", lambda e: e.activation(rs[:], rs[:], AF.Sqrt), reads=(rst,), writes=(rst,))
                    c.op("dve", lambda e: e.reciprocal(rs[:], rs[:]), reads=(rst,), writes=(rst,))
                    for cc in range(KC):
                        g = self.vecT[:, L * 72 + cc:L * 72 + cc + 1]
                        c.op("dve", lambda e: e.scalar_tensor_tensor(self.xT[:, cc, ts], self.xT[:, cc, ts], g, rs[:],
                                                                     ALU.mult, ALU.mult),
                             reads=(rst, self.vec_tok), writes=(self.xT_tok[tc],))
            os_ = [self.sb(st, f"os{i}", [128, D], F32) for i in range(2)]
            os_tok = [Tok(), Tok()]
            for t in range(NT):
                b = t % 2
                for half in range(2):
                    pb, pt = self.bank()
                    for j in range(4):
                        cc = half * 4 + j
                        c.op("pe", lambda e: e.transpose(pb[:, j * 128:(j + 1) * 128],
                                                         self.xT[:, cc, t * 128:(t + 1) * 128], self.ident_f[:]),
                             reads=(self.xT_tok[t // 4], self.const_tok), writes=(pt,), inc=(j == 3))
                    dst = os_[b][:, half * 512:(half + 1) * 512]
                    if half == 0:
                        c.op("dve", lambda e: e.tensor_copy(dst, pb[:]), reads=(pt,), writes=(os_tok[b],))
                    else:
                        c.op("act", lambda e: e.copy(dst, pb[:]), reads=(pt,), writes=(os_tok[b],))
                c.dma(out[t * 128:(t + 1) * 128, :], os_[b][:], reads=(os_tok[b],))
            c.barrier()

    def dump2d(self, name, ap, toks):
        if name in self.dbg_out:
            self.c.barrier()
            self.c.dma(self.dbg_out[name], ap, reads=tuple(toks), q="pool")
            self.c.barrier()

    def dump_featmajor_bf16(self, tT, toks, dst):
        c = self.c
        with ExitStack() as st:
            tmp = self.sb(st, "dmp", [128, S], F32)
            tt = Tok()
            for cc in range(tT.shape[1]):
                c.op("dve", lambda e: e.tensor_copy(tmp[:], tT[:, cc, :]), reads=tuple(toks), writes=(tt,))
                c.dma(dst[cc * 128:(cc + 1) * 128, :], tmp[:], reads=(tt,))
            c.barrier()


def _prep_inputs(inputs, layers, b):
    L = len(layers)
    vecs = np.zeros((L, 72, 128), np.float32)
    for i, l in enumerate(layers):
        vecs[i, 0:8] = inputs["norm_mix"][l].reshape(8, 128)
        vecs[i, 8:16] = inputs["norm_ff"][l].reshape(8, 128)
        vecs[i, 16:48] = inputs["b_gate"][l].reshape(32, 128)
    m = {
        "x": np.ascontiguousarray(inputs["x"][b]),
        "w_in": np.ascontiguousarray(inputs["w_in"][layers]),
        "vecs": vecs,
        "norm_final": np.ascontiguousarray(inputs["norm_final"].reshape(8, 128)),
        "positions": np.ascontiguousarray(inputs["positions"][b:b + 1]).astype(np.int32),
        "cmp_pos_k": np.ascontiguousarray(inputs["nsa_cmp_pos_k"][layers]),
        "cmp_pos_v": np.ascontiguousarray(inputs["nsa_cmp_pos_v"][layers]),
        "cmp_wk1": np.ascontiguousarray(inputs["nsa_cmp_wk1"][layers]),
        "cmp_wk2": np.ascontiguousarray(inputs["nsa_cmp_wk2"][layers]),
        "cmp_wv1": np.ascontiguousarray(inputs["nsa_cmp_wv1"][layers]),
        "cmp_wv2": np.ascontiguousarray(inputs["nsa_cmp_wv2"][layers]),
        "fox_b_f": np.ascontiguousarray(inputs["fox_b_f"][layers]),
        "gla_w_alpha": np.ascontiguousarray(inputs["gla_w_alpha"][layers]),
        "gla_b_alpha": np.ascontiguousarray(inputs["gla_b_alpha"][layers]),
        "gla_norm": np.ascontiguousarray(inputs["gla_norm"][layers]),
        "w_branch": np.ascontiguousarray(inputs["w_branch"][layers]),
        "w_out": np.ascontiguousarray(inputs["w_out"][layers]),
        "w_ff1": np.ascontiguousarray(inputs["w_ff1"][layers]),
        "w_ff2": np.ascontiguousarray(inputs["w_ff2"][layers]),
    }
    return m


def run(inputs, layers=(0, 1, 2, 3), debug=(), ncores=8, trace=False, stage=99):
    layers = list(layers)
    bld = Builder(layers, first=True, last=True, debug=debug, stage=stage)
    nc = bld.build()
    in_maps = [_prep_inputs(inputs, layers, b) for b in range(ncores)]
    res = run_bass_kernel_spmd(nc, in_maps, core_ids=list(range(ncores)), trace=trace)
    return res


def kernel(**inputs):
    inputs = {k: np.asarray(v) for k, v in inputs.items()}
    res = run(inputs)
    out = np.stack([np.asarray(r["out"]) for r in res.results], axis=0)
    return out.astype(np.float32)
```

```python
import numpy as np
from contextlib import ExitStack
import concourse.bass as bass
import concourse.mybir as mybir
from concourse.bass_utils import run_bass_kernel_spmd

F32 = mybir.dt.float32
BF16 = mybir.dt.bfloat16
I32 = mybir.dt.int32
ALU = mybir.AluOpType
AF = mybir.ActivationFunctionType
AX = mybir.AxisListType

S = 2048
D = 1024
NT = S // 128
NTC = S // 512
KC = D // 128
DEPTH = 4
DFF = 4096
D_IN = 10032
EPS = 1e-6
NEG = -30000.0

SPLITS = (512, 128, 128, 128, 128, 128, 128, 24, 512, 512, 512, 256, 256, 512, 16, 512, 512, 512, 512, 8, 4096)
OFFS = np.concatenate([[0], np.cumsum(SPLITS)]).tolist()
(O_NQ, O_NKC, O_NVC, O_NKS, O_NVS, O_NKW, O_NVW, O_NG, O_SQ, O_SK, O_SV, O_GQ, O_GK, O_GV, O_GA, O_GG,
 O_FQ, O_FK, O_FV, O_FF, O_GATE) = OFFS[:21]

EPOCH = 4000


class Tok:
    __slots__ = ("w", "r", "name")

    def __init__(self, name=""):
        self.w = None
        self.r = {}
        self.name = name


class Ctx:
    def __init__(self, nc, es):
        self.nc = nc
        self.es = es
        self.eng = dict(pe=nc.tensor, act=nc.scalar, dve=nc.vector, pool=nc.gpsimd, sp=nc.sync)
        self.cur = {}
        self.nsem = 0
        self.waited = {e: {} for e in self.eng}
        for e in ("pe", "act", "dve", "pool"):
            self.cur[e] = [self._newsem(e), 0]
        self.own = {e: set() for e in self.eng}
        for e in ("pe", "act", "dve", "pool"):
            self.own[e].add(id(self.cur[e][0]))
        self.dq = {}
        for q in ("sp", "act", "pool"):
            sems = [self._newsem("d" + q) for _ in range(8 if q == "sp" else 4)]
            self.dq[q] = dict(sems=sems, tgt=[0] * len(sems), i=0)
        self.all_dma = []

    def _newsem(self, name):
        self.nsem += 1
        return self.es.enter_context(self.nc.semaphore(f"s_{name}_{self.nsem}"))

    def _wait(self, e, deps):
        w = self.waited[e]
        for (sem, val) in deps:
            if val <= 0:
                continue
            k = id(sem)
            if e == "pe" and k in self.own["pe"]:
                continue
            if w.get(k, 0) >= val:
                continue
            self.eng[e].wait_ge(sem, val)
            w[k] = val

    @staticmethod
    def _deps(reads, writes):
        deps = []
        for t in reads:
            if t.w is not None:
                deps.append(t.w)
        for t in writes:
            if t.w is not None:
                deps.append(t.w)
            deps.extend(t.r.values())
        return deps

    @staticmethod
    def _record(stamp, reads, writes):
        sem, val = stamp
        for t in reads:
            t.r[id(sem)] = stamp
        for t in writes:
            t.w = stamp
            t.r = {}

    def op(self, e, fn, reads=(), writes=(), inc=True):
        self._wait(e, self._deps(reads, writes))
        ins = fn(self.eng[e])
        sem, cnt = self.cur[e]
        stamp = (sem, cnt + 1)
        if inc:
            ins.then_inc(sem, 1)
            self.cur[e][1] = cnt + 1
        self._record(stamp, reads, writes)
        if inc and cnt + 1 >= EPOCH:
            ns = self._newsem(e)
            self.own[e].add(id(ns))
            self.cur[e] = [ns, 0]
        return ins

    def dma(self, out, in_, reads=(), writes=(), q="sp", **kw):
        dq = self.dq[q]
        i = dq["i"] % len(dq["sems"])
        dq["i"] += 1
        sem = dq["sems"][i]
        deps = self._deps(reads, writes)
        deps.append((sem, dq["tgt"][i]))
        self._wait(q, deps)
        self.eng[q].dma_start(out=out, in_=in_, **kw).then_inc(sem, 16)
        dq["tgt"][i] += 16
        stamp = (sem, dq["tgt"][i])
        self._record(stamp, reads, writes)
        return stamp

    def barrier(self):
        stamps = []
        for e in ("pe", "act", "dve", "pool"):
            sem, cnt = self.cur[e]
            stamps.append((sem, cnt))
        for q, dq in self.dq.items():
            for s, t in zip(dq["sems"], dq["tgt"]):
                stamps.append((s, t))
        for e in ("pe", "act", "dve", "pool", "sp"):
            self._wait_all(e, stamps)

    def _wait_all(self, e, stamps):
        w = self.waited[e]
        for (sem, val) in stamps:
            if val <= 0:
                continue
            k = id(sem)
            if k in self.own.get(e, ()) and (e == "pe"):
                continue
            if w.get(k, 0) >= val:
                continue
            self.eng[e].wait_ge(sem, val)
            w[k] = val


class Builder:
    def __init__(self, layers, first, last, debug=(), stage=99):
        self.stage = stage
        self.layers = layers
        self.first = first
        self.last = last
        self.debug = debug
        self.nc = bass.Bass("TRN2", target_bir_lowering=False)
        self.dbg_out = {}

    def sb(self, st, name, shape, dt):
        self._uid = getattr(self, "_uid", 0) + 1
        return st.enter_context(self.nc.sbuf_tensor(f"{name}_{self._uid}", shape, dt))

    def dram_in(self, name, shape, dt=F32):
        return self.nc.dram_tensor(name, list(shape), dt, kind="ExternalInput").ap()

    def mm(self, out, lhsT, rhs, start, stop, reads, writes, inc=None, **kw):
        if inc is None:
            inc = True
        return self.c.op("pe", lambda e: e.matmul(out, lhsT, rhs, start=start, stop=stop, **kw),
                         reads=reads, writes=writes, inc=inc)

    def bank(self):
        i = self.bank_i % self.n_scr_banks
        self.bank_i += 1
        return self.ps[i], self.ps_tok[i]

    def bank_acc(self):
        busy = self.acc_busy
        for i in range(self.n_scr_banks, 8):
            if i not in busy:
                busy.add(i)
                self.last_acc = i
                return self.ps[i], self.ps_tok[i]
        raise RuntimeError("no free accumulator bank")

    def release_acc(self, ob):
        for i in range(8):
            if self.ps[i] is ob:
                self.acc_busy.discard(i)

    def wload(self, src3, kc, ncols, eng="pool"):
        i = self.w_i % 2
        self.w_i += 1
        stg, stok = self.wstg[i], self.wstg_tok[i]
        wb, wtok = self.wbf[i], self.wbf_tok[i]
        n = kc * ncols
        assert n <= self.WMAX
        sv = stg[:, 0:n].rearrange("p (c n) -> p c n", c=kc)
        wv = wb[:, 0:n].rearrange("p (c n) -> p c n", c=kc)
        self.c.dma(sv, src3, reads=(), writes=(stok,))
        self.c.op(eng, lambda e: e.tensor_copy(wb[:, 0:n], stg[:, 0:n]), reads=(stok,), writes=(wtok,))
        return wv, wtok

    def build(self):
        nc = self.nc
        L = len(self.layers)
        A = {}
        A["x"] = self.dram_in("x", [S, D])
        A["w_in"] = self.dram_in("w_in", [L, D, D_IN])
        A["vecs"] = self.dram_in("vecs", [L, 72, 128])
        A["norm_final"] = self.dram_in("norm_final", [8, 128])
        A["positions"] = self.dram_in("positions", [1, S], I32)
        A["cmp_pos_k"] = self.dram_in("cmp_pos_k", [L, 32, 64])
        A["cmp_pos_v"] = self.dram_in("cmp_pos_v", [L, 32, 64])
        A["cmp_wk1"] = self.dram_in("cmp_wk1", [L, 2048, 256])
        A["cmp_wk2"] = self.dram_in("cmp_wk2", [L, 256, 64])
        A["cmp_wv1"] = self.dram_in("cmp_wv1", [L, 2048, 256])
        A["cmp_wv2"] = self.dram_in("cmp_wv2", [L, 256, 64])
        A["fox_b_f"] = self.dram_in("fox_b_f", [L, 8])
        A["gla_w_alpha"] = self.dram_in("gla_w_alpha", [L, 16, 256])
        A["gla_b_alpha"] = self.dram_in("gla_b_alpha", [L, 256])
        A["gla_norm"] = self.dram_in("gla_norm", [L, 128])
        A["w_branch"] = self.dram_in("w_branch", [L, 4, 512, D])
        A["w_out"] = self.dram_in("w_out", [L, D, D])
        A["w_ff1"] = self.dram_in("w_ff1", [L, D, DFF])
        A["w_ff2"] = self.dram_in("w_ff2", [L, DFF, D])
        self.A = A
        out = nc.dram_tensor("out", [S, D], F32, kind="ExternalOutput").ap()
        for name, shape in self.debug:
            self.dbg_out[name] = nc.dram_tensor("dbg_" + name, list(shape), F32, kind="ExternalOutput").ap()

        with ExitStack() as es:
            self.es = es
            c = self.c = Ctx(nc, es)
            self.xT = self.sb(es, "xT", [128, KC, S], F32)
            self.hT = self.sb(es, "hT", [128, KC, S], BF16)
            self.xT_tok = [Tok(f"xT{i}") for i in range(NTC)]
            self.hT_tok = [Tok(f"hT{i}") for i in range(NTC)]
            self.ident_f = self.sb(es, "ident_f", [128, 128], F32)
            self.ident_b = self.sb(es, "ident_b", [128, 128], BF16)
            self.ones_f = self.sb(es, "ones_f", [128, 128], F32)
            self.const_tok = Tok("const")
            self.vecT = self.sb(es, "vecT", [128, L * 72 + 8], F32)
            self.vec_tok = Tok("vec")
            self.WMAX = 1024
            self.wstg = [self.sb(es, f"wstg{i}", [128, self.WMAX], F32) for i in range(2)]
            self.wbf = [self.sb(es, f"wbf{i}", [128, self.WMAX], BF16) for i in range(2)]
            self.wstg_tok = [Tok() for _ in range(2)]
            self.wbf_tok = [Tok() for _ in range(2)]
            self.w_i = 0
            self.scr = [self.sb(es, f"scr{i}", [128, 512], F32) for i in range(4)]
            self.scr_tok = [Tok() for _ in range(4)]
            self.scr_i = 0
            self.ps = [es.enter_context(nc.psum_tensor(f"ps{i}", [128, 512], F32)) for i in range(8)]
            self.ps_tok = [Tok(f"ps{i}") for i in range(8)]
            self.bank_i = 0
            self.bank_j = 0
            self.acc_busy = set()
            self.n_scr_banks = 6

            self.make_consts()
            if self.first:
                self.load_x()
            else:
                self.load_xT()
            for li in range(L):
                if self.stage >= 1:
                    self.layer(li)
            if self.last and self.stage >= 3:
                self.final_norm_store(out)
            else:
                self.store_xT(out)
            c.barrier()
        return nc

    def run_streams(self, gens, k=2):
        gens = iter(gens)
        active = []
        for g in gens:
            active.append(g)
            if len(active) == k:
                break
        while active:
            for g in list(active):
                try:
                    next(g)
                except StopIteration:
                    active.remove(g)
                    nxt = next(gens, None)
                    if nxt is not None:
                        active.append(nxt)

    def nscr(self):
        i = self.scr_i % len(self.scr)
        self.scr_i += 1
        return self.scr[i], self.scr_tok[i]

    def make_consts(self):
        c = self.c
        nc = self.nc
        ct = self.const_tok
        c.op("pool", lambda e: e.memset(self.ones_f[:], 1.0), writes=(ct,))
        c.op("pool", lambda e: e.affine_select(self.ident_f[:], self.ones_f[:], [[-1, 128]], ALU.is_equal, 0.0,
                                               base=0, channel_multiplier=1), reads=(ct,), writes=(ct,))
        c.op("pool", lambda e: e.tensor_copy(self.ident_b[:], self.ident_f[:]), reads=(ct,), writes=(ct,))
        self.tri_f = self.sb(self.es, "tri_f", [128, 128], F32)
        c.op("pool", lambda e: e.affine_select(self.tri_f[:], self.ones_f[:], [[1, 128]], ALU.is_ge, 0.0,
                                               base=0, channel_multiplier=-1), reads=(ct,), writes=(ct,))
        self.zer_f = self.sb(self.es, "zer_f", [128, 128], F32)
        self.cneg_b = self.sb(self.es, "cneg_b", [128, 128], BF16)
        c.op("pool", lambda e: e.memset(self.zer_f[:], 0.0), writes=(ct,))
        self.nones_f = self.sb(self.es, "nones_f", [128, 128], F32)
        self.ones_b = self.sb(self.es, "ones_b", [128, 128], BF16)
        self.nones_b = self.sb(self.es, "nones_b", [128, 128], BF16)
        self.cnegs_b = self.sb(self.es, "cnegs_b", [128, 128], BF16)
        self.ntri_b = self.sb(self.es, "ntri_b", [128, 128], BF16)
        c.op("pool", lambda e: e.memset(self.nones_f[:], -1.0), writes=(ct,))
        c.op("pool", lambda e: e.memset(self.ones_b[:], 1.0), writes=(ct,))
        c.op("pool", lambda e: e.memset(self.nones_b[:], -1.0), writes=(ct,))
        c.op("pool", lambda e: e.affine_select(self.cnegs_b[:], self.zer_f[:], [[1, 128]], ALU.is_gt, NEG,
                                               base=0, channel_multiplier=-1), reads=(ct,), writes=(ct,))
        c.op("pool", lambda e: e.affine_select(self.ntri_b[:], self.nones_f[:], [[-1, 128]], ALU.is_ge, 0.0,
                                               base=0, channel_multiplier=1), reads=(ct,), writes=(ct,))
        c.op("pool", lambda e: e.affine_select(self.cneg_b[:], self.zer_f[:], [[1, 128]], ALU.is_ge, NEG,
                                               base=0, channel_multiplier=-1), reads=(ct,), writes=(ct,))
        L = len(self.layers)
        nrow = L * 72 + 8
        with ExitStack() as st:
            tmp = self.sb(st, "vtmp", [128, 4, 128], F32)
            tt = Tok()
            r0 = 0
            chunks = []
            while r0 < nrow:
                n = min(128, nrow - r0)
                chunks.append((r0, n))
                r0 += n
            for ci, (r0, n) in enumerate(chunks):
                a = r0
                while a < r0 + n:
                    if a < L * 72:
                        b = min(r0 + n, L * 72)
                        src = self.A["vecs"].rearrange("l r p -> (l r) p")[a:b, :]
                    else:
                        b = r0 + n
                        src = self.A["norm_final"][a - L * 72:b - L * 72, :]
                    c.dma(tmp[a - r0:b - r0, ci, :], src, writes=(tt,))
                    a = b
                pb, pt = self.bank()
                c.op("pe", lambda e: e.transpose(pb[:, 0:n], tmp[0:n, ci, :], self.ident_f[0:n, 0:n]),
                     reads=(tt, ct), writes=(pt,))
                c.op("dve", lambda e: e.tensor_copy(self.vecT[:, r0:r0 + n], pb[:, 0:n]), reads=(pt,),
                     writes=(self.vec_tok,))
            c.barrier()

    def vcol(self, li, kind, j):
        base = li * 72 + {"norm_mix": 0, "norm_ff": 8, "b_gate": 16}[kind]
        return self.vecT[:, base + j:base + j + 1]

    def load_x(self):
        c = self.c
        x = self.A["x"]
        with ExitStack() as st:
            xs = [self.sb(st, f"xs{i}", [128, D], F32) for i in range(2)]
            xs_tok = [Tok(), Tok()]
            for t in range(NT):
                b = t % 2
                c.dma(xs[b][:], x[t * 128:(t + 1) * 128, :], writes=(xs_tok[b],))
                for half in range(2):
                    pb, pt = self.bank()
                    for j in range(4):
                        cc = half * 4 + j
                        c.op("pe", lambda e: e.transpose(pb[:, j * 128:(j + 1) * 128], xs[b][:, cc * 128:(cc + 1) * 128],
                                                         self.ident_f[:]),
                             reads=(xs_tok[b], self.const_tok), writes=(pt,), inc=(j == 3))
                    dst = self.xT[:, half * 4:half * 4 + 4, t * 128:(t + 1) * 128]
                    src = pb[:].rearrange("p (j n) -> p j n", j=4)
                    eng = "dve" if half == 0 else "act"
                    if eng == "dve":
                        c.op("dve", lambda e: e.tensor_copy(dst, src), reads=(pt,), writes=(self.xT_tok[t // 4],))
                    else:
                        c.op("act", lambda e: e.copy(dst, src), reads=(pt,), writes=(self.xT_tok[t // 4],))
            c.barrier()

    def load_xT(self):
        raise NotImplementedError

    def store_xT(self, out):
        self.store_tok_major(out, normed=False)

    def rmsnorm_to_hT(self, gcol):
        c = self.c
        for tc in range(NTC):
            ts = slice(tc * 512, (tc + 1) * 512)
            pb, pt = self.bank()
            for cc in range(KC):
                sq, sqt = self.nscr()
                c.op("act", lambda e: e.activation(sq[:], self.xT[:, cc, ts], AF.Square),
                     reads=(self.xT_tok[tc],), writes=(sqt,))
                self.mm(pb[:], self.ones_f[:], sq[:], cc == 0, cc == KC - 1, reads=(sqt, self.const_tok), writes=(pt,))
            rs, rst = self.nscr()
            c.op("dve", lambda e: e.tensor_scalar(rs[:], pb[:], 1.0 / D, EPS, ALU.mult, ALU.add), reads=(pt,),
                 writes=(rst,))
            c.op("act", lambda e: e.activation(rs[:], rs[:], AF.Sqrt), reads=(rst,), writes=(rst,))
            c.op("dve", lambda e: e.reciprocal(rs[:], rs[:]), reads=(rst,), writes=(rst,))
            for cc in range(KC):
                c.op("dve", lambda e: e.scalar_tensor_tensor(self.hT[:, cc, ts], self.xT[:, cc, ts], gcol(cc), rs[:],
                                                             ALU.mult, ALU.mult),
                     reads=(self.xT_tok[tc], rst, self.vec_tok), writes=(self.hT_tok[tc],))

    def layer(self, li):
        self.rmsnorm_to_hT(lambda cc: self.vcol(li, "norm_mix", cc))
        if "hT" in self.dbg_out and li == 0:
            self.dump_featmajor_bf16(self.hT, self.hT_tok, self.dbg_out["hT"])
        if self.stage >= 4:
            self.yT = self.sb(self.es, f"yT{li}", [128, 4, S], BF16) if not hasattr(self, "yT") else self.yT
            self.yT_tok = Tok("yT")
            if self.stage >= 8:
                self.nsa(li)
                if "ynsa" in self.dbg_out and li == 0:
                    self.dump_featmajor_bf16(self.yT, [self.yT_tok], self.dbg_out["ynsa"])
                self.combine(li, 0)
            if self.stage == 8:
                return
            if self.stage >= 7:
                self.gla(li)
                if "ygla" in self.dbg_out and li == 0:
                    self.dump_featmajor_bf16(self.yT, [self.yT_tok], self.dbg_out["ygla"])
                self.combine(li, 2)
            if self.stage >= 6 and self.stage != 7:
                self.sbmix(li)
                if "ysb" in self.dbg_out and li == 0:
                    self.dump_featmajor_bf16(self.yT, [self.yT_tok], self.dbg_out["ysb"])
                self.combine(li, 1)
            if self.stage == 7:
                return
            self.fox(li)
            if "yT" in self.dbg_out and li == 0:
                self.dump_featmajor_bf16(self.yT, [self.yT_tok], self.dbg_out["yT"])
            if self.stage >= 5:
                self.combine(li, 3)
        if self.stage >= 2:
            self.rmsnorm_to_hT(lambda cc: self.vcol(li, "norm_ff", cc))
            self.ffn(li)

    def ffn(self, li):
        c = self.c
        w1 = self.A["w_ff1"][li].rearrange("(c p) n -> p c n", p=128)
        w2 = self.A["w_ff2"][li].rearrange("(f p) n -> p f n", p=128)
        G = 4
        with ExitStack() as st:
            aT = [self.sb(st, f"aT{i}", [128, G, S], BF16) for i in range(2)]
            aT_tok = [Tok(), Tok()]
            import os
            for g in range(int(os.environ.get('FFN_G', DFF // (128 * G)))):
                ab, abt = aT[g % 2], aT_tok[g % 2]
                for half in range(G):
                    f0 = g * G + half
                    wv, wt = self.wload(w1[:, :, f0 * 128:(f0 + 1) * 128], KC, 128)
                    for j in range(1):
                        for tc in range(NTC):
                            ts = slice(tc * 512, (tc + 1) * 512)
                            pb, pt = self.bank()
                            for cc in range(KC):
                                self.mm(pb[:], wv[:, cc, j * 128:(j + 1) * 128], self.hT[:, cc, ts], cc == 0, cc == KC - 1,
                                        reads=(wt, self.hT_tok[tc]), writes=(pt,))
                            r, rt = self.nscr()
                            c.op("act", lambda e: e.activation(r[:], pb[:], AF.Relu), reads=(pt,), writes=(rt,))
                            c.op("dve", lambda e: e.tensor_tensor(ab[:, half, ts], r[:], r[:], ALU.mult),
                                 reads=(rt,), writes=(abt,))
                for dh in range(4):
                    wv, wt = self.wload(w2[:, g * G:(g + 1) * G, dh * 256:(dh + 1) * 256], G, 256)
                    for j in range(2):
                        dt_ = dh * 2 + j
                        for tc in range(NTC):
                            ts = slice(tc * 512, (tc + 1) * 512)
                            pb, pt = self.bank()
                            for f in range(G):
                                self.mm(pb[:], wv[:, f, j * 128:(j + 1) * 128], ab[:, f, ts], f == 0, f == G - 1,
                                        reads=(wt, abt), writes=(pt,))
                            c.op("dve", lambda e: e.tensor_tensor(self.xT[:, dt_, ts], self.xT[:, dt_, ts], pb[:], ALU.add),
                                 reads=(pt,), writes=(self.xT_tok[tc],))
            c.barrier()


    def proj_feat(self, li, col0, ncols, evac):
        w = self.A["w_in"][li].rearrange("(c p) n -> p c n", p=128)
        n0 = 0
        while n0 < ncols:
            nn = min(128, ncols - n0)
            wv, wt = self.wload(w[:, :, col0 + n0:col0 + n0 + nn], KC, nn)
            for j in range((nn + 127) // 128):
                m = min(128, nn - j * 128)
                for tc in range(NTC):
                    ts = slice(tc * 512, (tc + 1) * 512)
                    pb, pt = self.bank()
                    for cc in range(KC):
                        self.mm(pb[0:m, :], wv[:, cc, j * 128:j * 128 + m], self.hT[:, cc, ts], cc == 0, cc == KC - 1,
                                reads=(wt, self.hT_tok[tc]), writes=(pt,))
                    evac((n0 + j * 128) // 128, tc, pb, pt)
            n0 += nn

    def proj_tok(self, li, col0, ncols, evac):
        w = self.A["w_in"][li].rearrange("(c p) n -> p c n", p=128)
        wv, wt = self.wload(w[:, :, col0:col0 + ncols], KC, ncols)
        for t in range(NT):
            pb, pt = self.bank()
            for cc in range(KC):
                self.mm(pb[:, 0:ncols], self.hT[:, cc, t * 128:(t + 1) * 128], wv[:, cc, :], cc == 0, cc == KC - 1,
                        reads=(wt, self.hT_tok[t // 4]), writes=(pt,))
            evac(t, pb, pt)

    def evac_featT(self, dst, dtok, scale=1.0):
        c = self.c
        cnt = [0]

        def f(mt, tc, pb, pt):
            ts = slice(tc * 512, (tc + 1) * 512)
            cnt[0] += 1
            if cnt[0] % 2 == 0:
                c.op("dve", lambda e: e.tensor_scalar(dst[:, mt, ts], pb[:], scale, None, ALU.mult), reads=(pt,),
                     writes=(dtok,))
            else:
                c.op("act", lambda e: e.activation(dst[:, mt, ts], pb[:], AF.Copy, scale=scale), reads=(pt,),
                     writes=(dtok,))
        return f

    def attention(self, st, name, nheads, qT, qtok, kT, ktok, V, vtok, ytok_t, ytok_tok, bias_fn=None, ycol0=0, post_qc=None):
        c = self.c
        pT = [self.sb(st, f"{name}_pT{i}", [128, 512], BF16) for i in range(6)]
        pT_tok = [Tok() for _ in range(6)]
        rc = self.sb(st, f"{name}_rc", [128, 12], F32)
        rc_tok = [Tok(), Tok(), Tok()]
        pi = [0]

        def head_stream(qc, h):
            hp = slice((h % 2) * 64, (h % 2) * 64 + 64)
            hc = h // 2
            ob, ot = self.bank_acc()
            O = ob[:, 0:260].rearrange("p (j d) -> p j d", j=4)
            nkt = 4 * qc + 4
            for kt in range(nkt):
                j0 = max(0, kt - 4 * qc)
                q0 = qc * 512 + j0 * 128
                ncol = 512 - j0 * 128
                sb_, stk = self.bank()
                diag = kt >= 4 * qc
                self.mm(sb_[:, 0:ncol], kT[hp, hc, kt * 128:(kt + 1) * 128], qT[hp, hc, q0:q0 + ncol], True, not diag,
                        reads=(ktok, qtok), writes=(stk,))
                if diag:
                    self.mm(sb_[:, 0:128], self.ident_b[:], self.cneg_b[:], False, True,
                            reads=(self.const_tok,), writes=(stk,), skip_group_check=True)
                p, ptk = pT[pi[0] % 6], pT_tok[pi[0] % 6]
                pi[0] += 1
                for half in range(2):
                    jl, jh = max(j0, 2 * half), 2 * half + 1
                    if jl > jh:
                        continue
                    cs = slice((jl - j0) * 128, (jh - j0 + 1) * 128)
                    b = bias_fn(h, kt, qc * 4 + 2 * half + 1) if bias_fn is not None else 0.0
                    c.op("act", lambda e: e.activation(p[:, cs], sb_[:, cs], AF.Exp, bias=b), reads=(stk, self.aux_tok),
                         writes=(ptk,))
                yield
                for j in range(j0, 4):
                    qt = qc * 4 + j
                    cs = slice((j - j0) * 128, (j - j0 + 1) * 128)
                    self.mm(O[:, j, :], p[:, cs], V[:, kt, h, :], kt == 0 and j == 0, kt == qt, reads=(ptk, vtok), writes=(ot,),
                            skip_group_check=True)
            rct = rc_tok[h % 3]
            for j in range(4):
                rcj = rc[:, (h % 3) * 4 + j:(h % 3) * 4 + j + 1]
                c.op("dve", lambda e: e.reciprocal(rcj, O[:, j, 64:65]), reads=(ot,), writes=(rct,))
                c.op("dve", lambda e: e.tensor_scalar(ytok_t[:, j, ycol0 + h * 64:ycol0 + (h + 1) * 64], O[:, j, 0:64],
                                                      rcj, None, ALU.mult),
                     reads=(ot, rct), writes=(ytok_tok,))
            self.release_acc(ob)

        c.barrier()
        self.n_scr_banks = 5
        for qc in range(NTC):
            self.run_streams([head_stream(qc, h) for h in range(nheads)], 3)
            if post_qc is not None:
                post_qc(qc)
        c.barrier()
        self.n_scr_banks = 6

    def ytok_to_yT(self, ytok_t, ytok_tok, qc):
        c = self.c
        for tl in range(4):
            t = qc * 4 + tl
            pb, pt = self.bank()
            pbb = pb[:].bitcast(BF16)
            for j in range(4):
                c.op("pe", lambda e: e.transpose(pbb[:, j * 128:(j + 1) * 128], ytok_t[:, tl, j * 128:(j + 1) * 128],
                                                 self.ident_b[:]),
                     reads=(ytok_tok, self.const_tok), writes=(pt,))
            c.op("dve", lambda e: e.tensor_copy(self.yT[:, :, t * 128:(t + 1) * 128],
                                                pbb[:, 0:512].rearrange("p (j n) -> p j n", j=4)),
                 reads=(pt,), writes=(self.yT_tok,))


    def combine(self, li, bi):
        c = self.c
        wg = self.A["w_in"][li].rearrange("(c p) n -> p c n", p=128)
        wb = self.A["w_branch"][li, bi].rearrange("(c p) n -> p c n", p=128)
        wo = self.A["w_out"][li].rearrange("(c p) n -> p c n", p=128)
        with ExitStack() as st:
            mT = self.sb(st, "mT", [128, KC, S], BF16)
            mtok = Tok()
            for dt_ in range(KC):
                g0 = O_GATE + bi * D + dt_ * 128
                wgv, wgt = self.wload(wg[:, :, g0:g0 + 128], KC, 128)
                wbv, wbt = self.wload(wb[:, :, dt_ * 128:(dt_ + 1) * 128], 4, 128)
                bcol = self.vcol(li, "b_gate", bi * 8 + dt_)
                for tc in range(NTC):
                    ts = slice(tc * 512, (tc + 1) * 512)
                    pa, pat = self.bank()
                    for cc in range(KC):
                        self.mm(pa[:], wgv[:, cc, :], self.hT[:, cc, ts], cc == 0, cc == KC - 1,
                                reads=(wgt, self.hT_tok[tc]), writes=(pat,))
                    pb, pbt = self.bank()
                    for cc in range(4):
                        self.mm(pb[:], wbv[:, cc, :], self.yT[:, cc, ts], cc == 0, cc == 3,
                                reads=(wbt, self.yT_tok), writes=(pbt,))
                    sg, sgt = self.nscr()
                    c.op("act", lambda e: e.activation(sg[:], pa[:], AF.Sigmoid, bias=bcol), reads=(pat, self.vec_tok),
                         writes=(sgt,))
                    c.op("dve", lambda e: e.tensor_tensor(mT[:, dt_, ts], sg[:], pb[:], ALU.mult), reads=(sgt, pbt),
                         writes=(mtok,))
            for do in range(KC):
                wov, wot = self.wload(wo[:, :, do * 128:(do + 1) * 128], KC, 128)
                for tc in range(NTC):
                    ts = slice(tc * 512, (tc + 1) * 512)
                    pb, pbt = self.bank()
                    for cc in range(KC):
                        self.mm(pb[:], wov[:, cc, :], mT[:, cc, ts], cc == 0, cc == KC - 1, reads=(wot, mtok), writes=(pbt,))
                    c.op("dve", lambda e: e.tensor_tensor(self.xT[:, do, ts], self.xT[:, do, ts], pb[:], ALU.add),
                         reads=(pbt,), writes=(self.xT_tok[tc],))
            c.barrier()


    def sbmix(self, li):
        c = self.c
        with ExitStack() as st:
            qT = self.sb(st, "sb_qT", [128, 4, S], BF16)
            kT = self.sb(st, "sb_kT", [128, 4, S], BF16)
            V = self.sb(st, "sb_V", [128, NT, 8, 64], BF16)
            ytk = self.sb(st, "sb_y", [128, 4, 512], BF16)
            spb = [[self.sb(st, f"sb_sp{k}{i}", [128, 512], BF16) for i in range(2)] for k in range(2)]
            spt = [[Tok(), Tok()] for k in range(2)]
            pT = [[self.sb(st, f"sb_pT{k}{i}", [128, 512], BF16) for i in range(2)] for k in range(2)]
            pTt = [[Tok(), Tok()] for k in range(2)]
            sufs = [(self.sb(st, f"sb_suf{k}", [1, 512], F32), self.sb(st, f"sb_sufh{k}", [1, 512], BF16),
                     self.sb(st, f"sb_sufl{k}", [1, 512], BF16), Tok()) for k in range(2)]
            qtok, ktok, vtok, ytok = Tok(), Tok(), Tok(), Tok()
            self.proj_feat(li, O_SQ, 512, self.evac_featT(qT, qtok, 0.125))
            self.proj_feat(li, O_SK, 512, self.evac_featT(kT, ktok, 1.0))
            for q4 in range(4):
                def evac_vh(t, pb, pt, q4=q4):
                    c.op("act", lambda e: e.copy(V[:, t, q4 * 2:q4 * 2 + 2, :],
                                                 pb[:, 0:128].rearrange("p (h d) -> p h d", h=2)),
                         reads=(pt,), writes=(vtok,))
                self.proj_tok(li, O_SV + q4 * 128, 128, evac_vh)
            def head_stream(qc, h):
                s_ = h % 2
                hp = slice((h % 2) * 64, (h % 2) * 64 + 64)
                hc = h // 2
                ob, ot = self.bank_acc()
                O = ob[:, 0:256].rearrange("p (j d) -> p j d", j=4)
                nkt = 4 * qc + 4
                suf, sufh, sufl, suft = sufs[s_]
                c.op("dve", lambda e: e.memset(suf[:], 0.0), writes=(suft,))
                c.op("dve", lambda e: e.memset(sufh[:], 0.0), writes=(suft,))
                c.op("dve", lambda e: e.memset(sufl[:], 0.0), writes=(suft,))
                first = True
                ti = 0
                for kt in range(nkt - 1, -1, -1):
                    j0 = max(0, kt - 4 * qc)
                    q0 = qc * 512 + j0 * 128
                    ncol = 512 - j0 * 128
                    diag = kt >= 4 * qc
                    ksl = kT[hp, hc, kt * 128:(kt + 1) * 128]
                    qsl = qT[hp, hc, q0:q0 + ncol]
                    pa, pat = self.bank()
                    self.mm(pa[:, 0:ncol], ksl, qsl, True, not diag, reads=(ktok, qtok), writes=(pat,))
                    if diag:
                        self.mm(pa[:, 0:128], self.ident_b[:], self.cnegs_b[:], False, True, reads=(self.const_tok,),
                                writes=(pat,), skip_group_check=True)
                    e_, et = self.nscr()
                    sp, spk = spb[s_][ti % 2], spt[s_][ti % 2]
                    p, ptk = pT[s_][ti % 2], pTt[s_][ti % 2]
                    ti += 1
                    c.op("act", lambda e: e.activation(e_[:, 0:ncol], pa[:, 0:ncol], AF.Exp), reads=(pat,), writes=(et,))
                    c.op("act", lambda e: e.activation(sp[:, 0:ncol], e_[:, 0:ncol], AF.Ln, bias=1.0), reads=(et,),
                         writes=(spk,))
                    yield
                    pb, pbt = self.bank()
                    self.mm(pb[:, 0:ncol], ksl, qsl, True, False, reads=(ktok, qtok), writes=(pbt,))
                    if diag:
                        self.mm(pb[:, 0:128], self.ident_b[:], self.cnegs_b[:], False, False, reads=(self.const_tok,),
                                writes=(pbt,), skip_group_check=True)
                    self.mm(pb[:, 0:ncol], self.nones_b[0:1, :], sufh[0:1, 512 - ncol:512], False, False,
                            reads=(suft, self.const_tok), writes=(pbt,), skip_group_check=True)
                    self.mm(pb[:, 0:ncol], self.nones_b[0:1, :], sufl[0:1, 512 - ncol:512], False, False,
                            reads=(suft, self.const_tok), writes=(pbt,), skip_group_check=True)
                    self.mm(pb[:, 0:ncol], self.ntri_b[:], sp[:, 0:ncol], False, True, reads=(spk, self.const_tok),
                            writes=(pbt,), skip_group_check=True)
                    if kt > 0:
                        pc, pct = self.bank()
                        self.mm(pc[0:1, 0:ncol], self.ones_b[:, 0:1], sp[:, 0:ncol], True, True,
                                reads=(spk, self.const_tok), writes=(pct,))
                        sl = slice(512 - ncol, 512)
                        c.op("dve", lambda e: e.tensor_tensor(suf[0:1, sl], suf[0:1, sl], pc[0:1, 0:ncol], ALU.add),
                             reads=(pct,), writes=(suft,))
                        c.op("dve", lambda e: e.tensor_copy(sufh[0:1, sl], suf[0:1, sl]), reads=(suft,), writes=(suft,))
                        c.op("dve", lambda e: e.tensor_tensor(sufl[0:1, sl], suf[0:1, sl], sufh[0:1, sl], ALU.subtract),
                             reads=(suft,), writes=(suft,))
                    c.op("act", lambda e: e.activation(p[:, 0:ncol], pb[:, 0:ncol], AF.Exp), reads=(pbt,), writes=(ptk,))
                    yield
                    for j in range(j0, 4):
                        cs = slice((j - j0) * 128, (j - j0 + 1) * 128)
                        self.mm(O[:, j, :], p[:, cs], V[:, kt, h, :], first, kt == 0, reads=(ptk, vtok), writes=(ot,),
                                skip_group_check=True)
                        first = False
                for j in range(4):
                    c.op("dve", lambda e: e.tensor_copy(ytk[:, j, h * 64:(h + 1) * 64], O[:, j, :]), reads=(ot,),
                         writes=(ytok,))
                self.release_acc(ob)

            for qc in range(NTC):
                self.run_streams([head_stream(qc, h) for h in range(8)], 2)
                self.ytok_to_yT(ytk, ytok, qc)
            c.barrier()

    def gla(self, li):
        c = self.c
        ct = self.const_tok
        with ExitStack() as st:
            qeT = self.sb(st, "g_qe", [64, 4, S], BF16)
            keT = self.sb(st, "g_ke", [64, 4, S], BF16)
            k2 = self.sb(st, "g_k2", [128, NT, 256], BF16)
            vtk = self.sb(st, "g_v", [128, NT, 512], BF16)
            gnorm = self.sb(st, "g_norm", [128, 1], F32)
            dec = self.sb(st, "g_dec", [64, 4, 32], F32)
            tblk = self.sb(st, "g_tblk", [128, 128], BF16)
            sp_ = ExitStack()
            alrT = self.sb(sp_, "g_alr", [16, S], BF16)
            balb = self.sb(sp_, "g_bal", [128, 256], F32)
            wal_f = self.sb(sp_, "g_walf", [16, 256], F32)
            wal_b = self.sb(sp_, "g_walb", [16, 256], BF16)
            n16 = self.sb(sp_, "g_n16", [128, 128], F32)
            m1 = self.sb(sp_, "g_m1", [128, 128], F32)
            m2 = self.sb(sp_, "g_m2", [128, 128], F32)
            att_tok = [Tok(), Tok()]
            mtok, qtok, ktok, vtok, k2tok, atok, ptok, stok, otok, ontok = [Tok() for _ in range(10)]
            c.op("pool", lambda e: e.memset(n16[:], -1.0 / 16.0), writes=(mtok,))
            c.op("pool", lambda e: e.affine_select(m1[:], n16[:], [[1, 128]], ALU.is_ge, 0.0, base=0, channel_multiplier=-1),
                 reads=(mtok,), writes=(mtok,))
            c.op("pool", lambda e: e.memset(m1[0:64, 64:128], 0.0), reads=(mtok,), writes=(mtok,))
            c.op("pool", lambda e: e.affine_select(m2[:], n16[:], [[-1, 128]], ALU.is_gt, 0.0, base=0, channel_multiplier=1),
                 reads=(mtok,), writes=(mtok,))
            c.op("pool", lambda e: e.memset(m2[64:128, 0:64], 0.0), reads=(mtok,), writes=(mtok,))
            c.op("pool", lambda e: e.tensor_copy(tblk[:], self.tri_f[:]), reads=(ct, mtok), writes=(mtok,))
            c.op("pool", lambda e: e.memset(tblk[0:64, 64:128], 0.0), reads=(mtok,), writes=(mtok,))
            c.dma(balb[:], self.A["gla_b_alpha"][li:li + 1, :].partition_broadcast(128), writes=(ptok,))
            c.dma(wal_f[:], self.A["gla_w_alpha"][li], writes=(ptok,))
            c.dma(gnorm[:], self.A["gla_norm"][li].rearrange("(p o) -> p o", o=1), writes=(ptok,))
            c.op("pool", lambda e: e.tensor_copy(wal_b[:], wal_f[:]), reads=(ptok,), writes=(ptok,))
            for h in range(4):
                def ev_q(mt, tc, pb, pt, h=h):
                    ts = slice(tc * 512, (tc + 1) * 512)
                    c.op("act", lambda e: e.activation(qeT[0:64, h, ts], pb[0:64, :], AF.Copy, scale=0.125), reads=(pt,),
                         writes=(qtok,))

                def ev_k(mt, tc, pb, pt, h=h):
                    ts = slice(tc * 512, (tc + 1) * 512)
                    c.op("dve", lambda e: e.tensor_copy(keT[0:64, h, ts], pb[0:64, :]), reads=(pt,), writes=(ktok,))
                self.proj_feat(li, O_GQ + h * 64, 64, ev_q)
                self.proj_feat(li, O_GK + h * 64, 64, ev_k)

            def ev_a(mt, tc, pb, pt):
                ts = slice(tc * 512, (tc + 1) * 512)
                c.op("act", lambda e: e.copy(alrT[0:16, ts], pb[0:16, :]), reads=(pt,), writes=(atok,))
            self.proj_feat(li, O_GA, 16, ev_a)
            for i in range(4):
                def ev_v(t, pb, pt, i=i):
                    c.op("act", lambda e: e.copy(vtk[:, t, i * 128:(i + 1) * 128], pb[:, 0:128]), reads=(pt,), writes=(vtok,))
                self.proj_tok(li, O_GV + i * 128, 128, ev_v)
            import os
            gstop = int(os.environ.get("GLA_STOP", "99"))
            if gstop == 1:
                c.barrier()
                return
            for t in range(NT):
                tl = slice(t * 128, (t + 1) * 128)
                pa, pat = self.bank()
                self.mm(pa[:, 0:256], alrT[0:16, tl], wal_b[0:16, :], True, True, reads=(atok, ptok), writes=(pat,))
                xs, xst = self.nscr()
                c.op("dve", lambda e: e.tensor_tensor(xs[:, 0:256], pa[:, 0:256], balb[:], ALU.add), reads=(pat, ptok),
                     writes=(xst,))
                c.op("act", lambda e: e.activation(xs[:, 0:256], xs[:, 0:256], AF.Exp, scale=-1.0), reads=(xst,), writes=(xst,))
                c.op("act", lambda e: e.activation(xs[:, 0:256], xs[:, 0:256], AF.Ln, bias=1.0), reads=(xst,), writes=(xst,))
                pw, pwt = self.bank()
                self.mm(pw[:, 0:256], m2[:], xs[:, 0:256], True, True, reads=(mtok, xst), writes=(pwt,))
                c.op("act", lambda e: e.activation(k2[:, t, :], pw[:, 0:256], AF.Exp), reads=(pwt,), writes=(k2tok,))
                pbT, pbTt = self.bank()
                for h in range(4):
                    self.mm(pbT[0:64, h * 128:(h + 1) * 128], xs[:, h * 64:(h + 1) * 64], m1[:], h == 0, h == 3,
                            reads=(mtok, xst), writes=(pbTt,), skip_group_check=True)
                ebp, ebpt = self.nscr()
                ebn, ebnt = self.nscr()
                c.op("act", lambda e: e.activation(ebp[0:64, :], pbT[0:64, :], AF.Exp), reads=(pbTt,), writes=(ebpt,))
                c.op("act", lambda e: e.activation(ebn[0:64, :], pbT[0:64, :], AF.Exp, scale=-1.0), reads=(pbTt,), writes=(ebnt,))
                c.op("dve", lambda e: e.tensor_tensor(qeT[0:64, :, tl], qeT[0:64, :, tl],
                                                      ebp[0:64, :].rearrange("p (h n) -> p h n", h=4), ALU.mult),
                     reads=(ebpt,), writes=(qtok,))
                c.op("dve", lambda e: e.tensor_tensor(keT[0:64, :, tl], keT[0:64, :, tl],
                                                      ebn[0:64, :].rearrange("p (h n) -> p h n", h=4), ALU.mult),
                     reads=(ebnt,), writes=(ktok,))
                c.op("dve", lambda e: e.tensor_copy(dec[0:64, :, 2 * t:2 * t + 2],
                                                    ebp[0:64, :].rearrange("p (h c s) -> p h c s", h=4, c=2)[:, :, :, 63]),
                     reads=(ebpt,), writes=(stok,))
            c.barrier()
            sp_.close()
            if gstop == 2:
                return
            st_f = self.sb(st, "g_stf", [64, 4, 128], F32)
            st_b = self.sb(st, "g_stb", [64, 4, 128], BF16)
            oTs = [self.sb(st, f"g_oT{k}", [128, 512], BF16) for k in range(2)]
            onhs = [self.sb(st, f"g_on{k}", [128, S], BF16) for k in range(2)]
            otoks, ontoks = [Tok(), Tok()], [Tok(), Tok()]
            stoks = [Tok() for _ in range(4)]
            dectok = stok
            attb = [self.sb(st, f"g_att{i}", [128, 128], BF16) for i in range(3)]
            att_tok = [Tok(), Tok(), Tok()]
            att_i = [0]
            for i in range(2):
                def ev_k2(t, pb, pt, i=i):
                    c.op("dve", lambda e: e.tensor_tensor(k2[:, t, i * 128:(i + 1) * 128], k2[:, t, i * 128:(i + 1) * 128],
                                                          pb[:, 0:128], ALU.mult), reads=(pt,), writes=(k2tok,))
                self.proj_tok(li, O_GK + i * 128, 128, ev_k2)
            if gstop == 3:
                c.barrier()
                return
            def head_gen(h):
                s_ = h % 2
                oT, onh = oTs[s_], onhs[s_]
                otok, ontok = otoks[s_], ontoks[s_]
                ai = 0
                c.op("dve", lambda e: e.memset(st_f[0:64, h, :], 0.0), writes=(stoks[h],))
                c.op("dve", lambda e: e.memset(st_b[0:64, h, :], 0.0), writes=(stoks[h],))
                stok = stoks[h]
                for t in range(NT):
                    tl = slice(t * 128, (t + 1) * 128)
                    pa, pat = self.bank()
                    self.mm(pa[:, 0:128], keT[0:64, h, tl], qeT[0:64, h, tl], True, True, reads=(ktok, qtok), writes=(pat,))
                    ab, abt = attb[att_i[0] % 3], att_tok[att_i[0] % 3]
                    att_i[0] += 1
                    c.op("dve", lambda e: e.tensor_tensor(ab[:], pa[:, 0:128], tblk[:], ALU.mult), reads=(pat, mtok),
                         writes=(abt,))
                    for half in range(2):
                        cn = 2 * t + half
                        rs = slice(half * 64, half * 64 + 64)
                        cs = slice(cn * 64, (cn + 1) * 64)
                        vsl = vtk[rs, t, h * 128:(h + 1) * 128]
                        po, pot = self.bank()
                        self.mm(po[:, 0:64], vsl, ab[rs, rs], True, True, reads=(vtok, abt), writes=(pot,))
                        oc = (t % 4) * 128 + half * 64
                        c.op("act", lambda e: e.copy(oT[:, oc:oc + 64], po[:, 0:64]), reads=(pot,), writes=(otok,))
                        if cn > 0:
                            pi_, pit = self.bank()
                            self.mm(pi_[:, 0:64], st_b[0:64, h, :], qeT[0:64, h, cs], True, True, reads=(stok, qtok),
                                    writes=(pit,))
                            c.op("dve", lambda e: e.tensor_tensor(oT[:, oc:oc + 64], oT[:, oc:oc + 64], pi_[:, 0:64], ALU.add),
                                 reads=(pit, otok), writes=(otok,))
                        ps_, pst = self.bank()
                        self.mm(ps_[0:64, 0:128], k2[rs, t, h * 64:(h + 1) * 64], vsl, True, True, reads=(k2tok, vtok),
                                writes=(pst,))
                        c.op("dve", lambda e: e.scalar_tensor_tensor(st_f[0:64, h, :], st_f[0:64, h, :], dec[0:64, h, cn:cn + 1],
                                                                     ps_[0:64, 0:128], ALU.mult, ALU.add),
                             reads=(pst, stok, dectok), writes=(stok,))
                        c.op("dve", lambda e: e.tensor_copy(st_b[0:64, h, :], st_f[0:64, h, :]), reads=(stok,), writes=(stok,))
                        yield
                    if t % 4 == 3:
                        tc = t // 4
                        ts = slice(tc * 512, (tc + 1) * 512)
                        sq, sqt = self.nscr()
                        c.op("act", lambda e: e.activation(sq[:], oT[:], AF.Square), reads=(otok,), writes=(sqt,))
                        pn, pnt = self.bank()
                        self.mm(pn[:], self.ones_f[:], sq[:], True, True, reads=(sqt, ct), writes=(pnt,))
                        rr, rrt = self.nscr()
                        c.op("dve", lambda e: e.tensor_scalar(rr[:], pn[:], 1.0 / 128.0, EPS, ALU.mult, ALU.add), reads=(pnt,),
                             writes=(rrt,))
                        c.op("act", lambda e: e.activation(rr[:], rr[:], AF.Sqrt), reads=(rrt,), writes=(rrt,))
                        c.op("dve", lambda e: e.reciprocal(rr[:], rr[:]), reads=(rrt,), writes=(rrt,))
                        c.op("dve", lambda e: e.scalar_tensor_tensor(onh[:, ts], oT[:], gnorm[:, 0:1], rr[:], ALU.mult, ALU.mult),
                             reads=(otok, rrt, ptok), writes=(ontok,))

                def ev_g(mt, tc, pb, pt):
                    ts = slice(tc * 512, (tc + 1) * 512)
                    sg, sgt = self.nscr()
                    c.op("act", lambda e: e.activation(sg[:], pb[:], AF.Silu), reads=(pt,), writes=(sgt,))
                    c.op("dve", lambda e: e.tensor_tensor(self.yT[:, h, ts], sg[:], onh[:, ts], ALU.mult), reads=(sgt, ontok),
                         writes=(self.yT_tok,))
                self.proj_feat(li, O_GG + h * 128, 128, ev_g)

            self.run_streams([head_gen(h) for h in range(4)], 2)
            c.barrier()

    def proj_feat_dup(self, li, col0, evac):
        c = self.c
        w = self.A["w_in"][li].rearrange("(c p) n -> p c n", p=128)
        wv, wt = self.wload(w[:, :, col0:col0 + 64], KC, 64)
        wd, wdt = self.wdup, self.wdup_tok
        c.op("pool", lambda e: e.tensor_copy(wd[:, :, 0:64], wv), reads=(wt,), writes=(wdt,))
        c.op("pool", lambda e: e.tensor_copy(wd[:, :, 64:128], wv), reads=(wt,), writes=(wdt,))
        for tc in range(NTC):
            ts = slice(tc * 512, (tc + 1) * 512)
            pb, pt = self.bank()
            for cc in range(KC):
                self.mm(pb[:], wd[:, cc, :], self.hT[:, cc, ts], cc == 0, cc == KC - 1, reads=(wdt, self.hT_tok[tc]),
                        writes=(pt,))
            evac(0, tc, pb, pt)

    def rope_apply(self, dst, pb, pt, n, scale, cos_ap, sin_ap, dtok):
        c = self.c
        raw, rawt = self.rraw[self.rr_i % 2], self.rraw_tok[self.rr_i % 2]
        self.rr_i += 1
        c.op("act", lambda e: e.activation(raw[:, 0:n], pb, AF.Copy, scale=scale), reads=(pt,), writes=(rawt,))
        p2, p2t = self.bank()
        self.mm(p2[:, 0:n], self.Pm[:], raw[:, 0:n], True, True, reads=(rawt, self.tbl_tok), writes=(p2t,))
        t1, t1t = self.nscr()
        c.op("pool", lambda e: e.tensor_tensor(t1[:, 0:n], raw[:, 0:n], cos_ap, ALU.mult), reads=(rawt, self.tbl_tok),
             writes=(t1t,))
        t2, t2t = self.nscr()
        c.op("dve", lambda e: e.tensor_tensor(t2[:, 0:n], p2[:, 0:n], sin_ap, ALU.mult), reads=(p2t, self.tbl_tok),
             writes=(t2t,))
        c.op("dve", lambda e: e.tensor_tensor(dst, t1[:, 0:n], t2[:, 0:n], ALU.add), reads=(t1t, t2t), writes=(dtok,))

    def nsa_tables(self, cosT, sinT):
        c = self.c
        tbl = self.tbl_tok
        PI = float(np.pi)
        C1 = 6.28125
        C2 = float(2 * np.pi - 6.28125)
        with ExitStack() as s2:
            pidx = self.sb(s2, "n_pi", [128, 1], I32)
            f = self.sb(s2, "n_f", [128, 8], F32)
            posi = self.sb(s2, "n_posi", [128, 512], I32)
            ki = self.sb(s2, "n_ki", [128, 512], I32)
            ftok, ptok = Tok(), Tok()
            c.op("pool", lambda e: e.iota(pidx[:], [[0, 1]], base=0, channel_multiplier=1), writes=(ftok,))
            PF, GE, DD, G8, II, ACTV, SGN, INV = [f[:, i:i + 1] for i in range(8)]
            V = lambda fn: c.op("dve", fn, reads=(ftok,), writes=(ftok,))
            V(lambda e: e.tensor_copy(PF, pidx[:]))
            V(lambda e: e.tensor_single_scalar(GE, PF, 64.0, ALU.is_ge))
            V(lambda e: e.scalar_tensor_tensor(DD, GE, -64.0, PF, ALU.mult, ALU.add))
            V(lambda e: e.tensor_single_scalar(G8, DD, 8.0, ALU.is_ge))
            V(lambda e: e.scalar_tensor_tensor(II, G8, -8.0, DD, ALU.mult, ALU.add))
            V(lambda e: e.tensor_single_scalar(ACTV, DD, 16.0, ALU.is_lt))
            V(lambda e: e.tensor_scalar(SGN, G8, 2.0, -1.0, ALU.mult, ALU.add))
            V(lambda e: e.memset(INV, 0.0))
            for i in range(8):
                ci = float(np.float32(500000.0) ** np.float32(-i / 8.0))
                V(lambda e: e.tensor_scalar(GE, II, float(i), ci, ALU.is_equal, ALU.mult))
                V(lambda e: e.tensor_tensor(INV, INV, GE, ALU.add))
            V(lambda e: e.tensor_tensor(INV, INV, ACTV, ALU.mult))
            for tc in range(NTC):
                ts = slice(tc * 512, (tc + 1) * 512)
                c.dma(posi[:], self.A["positions"][0:1, ts].partition_broadcast(128), writes=(ptok,))
                ang, angt = self.nscr()
                c.op("dve", lambda e: e.tensor_copy(ang[:], posi[:]), reads=(ptok,), writes=(angt,))
                c.op("dve", lambda e: e.tensor_scalar(ang[:], ang[:], INV, None, ALU.mult), reads=(angt, ftok), writes=(angt,))
                for phase, dstT, use_sign in ((0.0, sinT, True), (PI / 2, cosT, False)):
                    u, ut = self.nscr()
                    r, rt = self.nscr()
                    c.op("dve", lambda e: e.tensor_scalar(u[:], ang[:], phase, 1.0 / (2 * PI), ALU.add, ALU.mult),
                         reads=(angt,), writes=(ut,))
                    c.op("dve", lambda e: e.tensor_copy(ki[:], u[:]), reads=(ut,), writes=(ptok,))
                    c.op("dve", lambda e: e.tensor_copy(u[:], ki[:]), reads=(ptok,), writes=(ut,))
                    c.op("dve", lambda e: e.scalar_tensor_tensor(r[:], u[:], -C1, ang[:], ALU.mult, ALU.add),
                         reads=(ut, angt), writes=(rt,))
                    c.op("dve", lambda e: e.scalar_tensor_tensor(r[:], u[:], -C2, r[:], ALU.mult, ALU.add), reads=(ut, rt),
                         writes=(rt,))
                    if phase != 0.0:
                        c.op("dve", lambda e: e.tensor_scalar(r[:], r[:], phase, None, ALU.add), reads=(rt,), writes=(rt,))
                    c.op("dve", lambda e: e.tensor_single_scalar(u[:], r[:], PI, ALU.is_gt), reads=(rt,), writes=(ut,))
                    c.op("dve", lambda e: e.scalar_tensor_tensor(r[:], u[:], -2 * PI, r[:], ALU.mult, ALU.add), reads=(ut, rt),
                         writes=(rt,))
                    c.op("dve", lambda e: e.tensor_single_scalar(u[:], r[:], -PI, ALU.is_lt), reads=(rt,), writes=(ut,))
                    c.op("dve", lambda e: e.scalar_tensor_tensor(r[:], u[:], 2 * PI, r[:], ALU.mult, ALU.add), reads=(ut, rt),
                         writes=(rt,))
                    c.op("dve", lambda e: e.tensor_scalar(r[:], r[:], PI, -PI, ALU.min, ALU.max), reads=(rt,), writes=(rt,))
                    c.op("act", lambda e: e.activation(r[:], r[:], AF.Sin), reads=(rt,), writes=(rt,))
                    if use_sign:
                        c.op("dve", lambda e: e.tensor_scalar(dstT[:, ts], r[:], SGN, None, ALU.mult), reads=(rt, ftok),
                             writes=(tbl,))
                    else:
                        c.op("dve", lambda e: e.tensor_copy(dstT[:, ts], r[:]), reads=(rt,), writes=(tbl,))
            c.barrier()

    def nsa(self, li):
        c = self.c
        ct = self.const_tok
        import os
        nstop = int(os.environ.get("NSA_STOP", "99"))
        with ExitStack() as st:
            kcT2 = self.sb(st, "n_kcT", [128, 2, 128], BF16)
            VCX = self.sb(st, "n_vcx", [128, 2, 97], BF16)
            cmptok = Tok()

            def open_tables(sx):
                cosT = self.sb(sx, "n_cos", [128, S], BF16)
                sinT = self.sb(sx, "n_sin", [128, S], BF16)
                self.Pm = self.sb(sx, "n_Pm", [128, 128], BF16)
                self.tbl_tok = Tok()
                self.rraw = [self.sb(sx, f"n_raw{i}", [128, 512], BF16) for i in range(2)]
                self.rraw_tok = [Tok(), Tok()]
                self.rr_i = 0
                self.wdup = self.sb(sx, "n_wdup", [128, KC, 128], BF16)
                self.wdup_tok = Tok()
                if not getattr(self, "rope_saved", False):
                    self.nsa_tables(cosT, sinT)
                    self.rope_dram = self.nc.dram_tensor("rope_tbl", [2, 128, S], BF16, kind="Internal").ap()
                    self.rope_dram_tok = Tok()
                    c.dma(self.rope_dram[0], cosT[:], reads=(self.tbl_tok,), writes=(self.rope_dram_tok,))
                    c.dma(self.rope_dram[1], sinT[:], reads=(self.tbl_tok,), writes=(self.rope_dram_tok,))
                    self.rope_saved = True
                else:
                    c.dma(cosT[:], self.rope_dram[0], reads=(self.rope_dram_tok,), writes=(self.tbl_tok,))
                    c.dma(sinT[:], self.rope_dram[1], reads=(self.rope_dram_tok,), writes=(self.tbl_tok,))
                c.op("pool", lambda e: e.memset(self.Pm[:], 0.0), writes=(self.tbl_tok,))
                for (d0, s0) in ((0, 8), (8, 0), (64, 72), (72, 64)):
                    c.op("pool", lambda e: e.tensor_copy(self.Pm[:, d0:d0 + 8], self.ident_b[:, s0:s0 + 8]),
                         reads=(ct, self.tbl_tok), writes=(self.tbl_tok,))
                return cosT, sinT
            kcraw = self.sb(st, "n_kcraw", [128, 2, 128], BF16)
            rawtok = Tok()
            with ExitStack() as s3:
                xcT = [self.sb(s3, "n_xk", [128, S], BF16), self.sb(s3, "n_xv", [128, S], BF16)]
                xtok = Tok()
                for kv, col0 in ((0, O_NKC), (1, O_NVC)):
                    def ev_x(mt, tc, pb, pt, kv=kv):
                        ts = slice(tc * 512, (tc + 1) * 512)
                        c.op("act", lambda e: e.copy(xcT[kv][:, ts], pb[:]), reads=(pt,), writes=(xtok,))
                    self.proj_feat(li, col0, 128, ev_x)
                W1 = self.sb(s3, "n_w1", [128, 32, 256], BF16)
                stg = self.sb(s3, "n_stg", [128, 2048], F32)
                W2f = self.sb(s3, "n_w2f", [128, 2, 64], F32)
                W2d = self.sb(s3, "n_w2d", [128, 2, 128], BF16)
                pe2 = self.sb(s3, "n_pe2", [32, 128], F32)
                peb = self.sb(s3, "n_peb", [128, 32], BF16)
                gh = self.sb(s3, "n_gh", [128, 2, 128], BF16)
                hb = self.sb(s3, "n_hb", [128, 2], F32)
                ovf = self.sb(s3, "n_ovf", [128, 3, 32], F32)
                stgt, w1t, w2t, pet, ght, hbt, ovt = [Tok() for _ in range(7)]
                c.op("pool", lambda e: e.memset(ovf[:], 0.5), writes=(ovt,))
                for k_, off in ((0, 0), (1, 16)):
                    c.op("pool", lambda e: e.affine_select(ovf[:, k_, :], ovf[:, k_, :], [[-64, 32]], ALU.is_ge, 0.0, base=off,
                                                           channel_multiplier=16), reads=(ovt,), writes=(ovt,))
                    c.op("pool", lambda e: e.affine_select(ovf[:, k_, :], ovf[:, k_, :], [[64, 32]], ALU.is_ge, 0.0,
                                                           base=63 - off, channel_multiplier=-16), reads=(ovt,), writes=(ovt,))
                c.op("pool", lambda e: e.tensor_tensor(ovf[:, 2, :], ovf[:, 0, :], ovf[:, 1, :], ALU.add), reads=(ovt,),
                     writes=(ovt,))
                for g in range(2):
                    c.op("pool", lambda e: e.tensor_copy(VCX[:, g, 65:97], ovf[:, 2, :]), reads=(ovt,), writes=(cmptok,))
                c.op("pool", lambda e: e.memset(VCX[:, :, 64:65], 1.0), writes=(cmptok,))
                for kv in range(2):
                    w1 = self.A["cmp_wk1" if kv == 0 else "cmp_wv1"][li].rearrange("(l d) n -> d l n", d=64)
                    for piece in range(4):
                        for half in range(2):
                            c.dma(stg[half * 64:(half + 1) * 64, :].rearrange("p (l n) -> p l n", l=8),
                                  w1[:, piece * 8:(piece + 1) * 8, :], writes=(stgt,))
                        c.op("pool", lambda e: e.tensor_copy(W1[:, piece * 8:(piece + 1) * 8, :],
                                                             stg[:].rearrange("p (l n) -> p l n", l=8)),
                             reads=(stgt,), writes=(w1t,))
                    w2 = self.A["cmp_wk2" if kv == 0 else "cmp_wv2"][li].rearrange("(c p) n -> p c n", p=128)
                    c.dma(W2f[:], w2, writes=(w2t,))
                    c.op("pool", lambda e: e.tensor_copy(W2d[:, :, 0:64], W2f[:]), reads=(w2t,), writes=(w2t,))
                    c.op("pool", lambda e: e.tensor_copy(W2d[:, :, 64:128], W2f[:]), reads=(w2t,), writes=(w2t,))
                    pe = self.A["cmp_pos_k" if kv == 0 else "cmp_pos_v"][li]
                    c.dma(pe2[:, 0:64], pe, writes=(pet,))
                    c.dma(pe2[:, 64:128], pe, writes=(pet,))
                    pp, ppt = self.bank()
                    c.op("pe", lambda e: e.transpose(pp[:, 0:32], pe2[:], self.ident_f[0:32, 0:32]), reads=(pet, ct),
                         writes=(ppt,))
                    c.op("dve", lambda e: e.tensor_copy(peb[:], pp[:, 0:32]), reads=(ppt,), writes=(pet,))
                    for half in range(2):
                        pk_, pkt = self.bank()
                        for l in range(32):
                            self.mm(pk_[:, 0:1], W1[0:64, l, half * 128:(half + 1) * 128], peb[0:64, l:l + 1], l == 0, l == 31,
                                    reads=(w1t, pet), writes=(pkt,))
                        c.op("dve", lambda e: e.tensor_copy(hb[:, half:half + 1], pk_[:, 0:1]), reads=(pkt,), writes=(hbt,))
                    for g in range(2):
                        gs = slice(g * 64, g * 64 + 64)
                        for half in range(2):
                            ph, pht = self.bank()
                            for l in range(32):
                                self.mm(ph[:, 0:127], W1[gs, l, half * 128:(half + 1) * 128],
                                        xcT[kv][gs, l:l + 16 * 126 + 1:16], l == 0, l == 31, reads=(w1t, xtok), writes=(pht,))
                            x, xt = self.nscr()
                            x2, x2t = self.nscr()
                            N_ = slice(0, 127)
                            c.op("dve", lambda e: e.tensor_scalar(x[:, N_], ph[:, N_], hb[:, half:half + 1], None, ALU.add),
                                 reads=(pht, hbt), writes=(xt,))
                            c.op("dve", lambda e: e.tensor_tensor(x2[:, N_], x[:, N_], x[:, N_], ALU.mult), reads=(xt,),
                                 writes=(x2t,))
                            c.op("dve", lambda e: e.tensor_scalar(x2[:, N_], x2[:, N_], 0.044715, 1.0, ALU.mult, ALU.add),
                                 reads=(x2t,), writes=(x2t,))
                            c.op("dve", lambda e: e.tensor_tensor(x2[:, N_], x2[:, N_], x[:, N_], ALU.mult), reads=(x2t, xt),
                                 writes=(x2t,))
                            c.op("act", lambda e: e.activation(x2[:, N_], x2[:, N_], AF.Tanh, scale=0.7978845608028654),
                                 reads=(x2t,), writes=(x2t,))
                            c.op("dve", lambda e: e.tensor_scalar(x[:, N_], x[:, N_], 0.5, None, ALU.mult), reads=(xt,),
                                 writes=(xt,))
                            c.op("dve", lambda e: e.scalar_tensor_tensor(gh[:, half, 0:127], x2[:, N_], 1.0, x[:, N_], ALU.add,
                                                                         ALU.mult), reads=(x2t, xt), writes=(ght,))
                        if kv == 0:
                            pk, pkt2 = self.bank()
                            for half in range(2):
                                self.mm(pk[:, 0:127], W2d[:, half, :], gh[:, half, 0:127], half == 0, half == 1,
                                        reads=(w2t, ght), writes=(pkt2,))
                            c.op("act", lambda e: e.copy(kcraw[:, g, 0:127], pk[:, 0:127]), reads=(pkt2,), writes=(rawtok,))
                        else:
                            pv, pvt = self.bank()
                            for half in range(2):
                                self.mm(pv[0:127, 0:64], gh[:, half, 0:127], W2d[:, half, 0:64], half == 0, half == 1,
                                        reads=(w2t, ght), writes=(pvt,))
                            c.op("act", lambda e: e.copy(VCX[0:127, g, 0:64], pv[0:127, 0:64]), reads=(pvt,), writes=(cmptok,))
                c.barrier()
            if nstop == 1:
                return
            qT = self.sb(st, "n_qT", [128, 4, S], BF16)
            ksT2 = self.sb(st, "n_ksT", [128, 2, S], BF16)
            kwT2 = self.sb(st, "n_kwT", [128, 2, S], BF16)
            vs = self.sb(st, "n_vs", [128, NT, 2, 65], BF16)
            vw = self.sb(st, "n_vw", [128, NT, 2, 65], BF16)
            sg = self.sb(st, "n_sg", [128, NT, 24], F32)
            sB = ExitStack()
            cosT, sinT = open_tables(sB)
            tbl = self.tbl_tok
            for g in range(2):
                self.rope_apply(kcT2[:, g, 0:127], kcraw[:, g, 0:127], rawtok, 127, 1.0,
                                cosT[:, 31:31 + 16 * 126 + 1:16], sinT[:, 31:31 + 16 * 126 + 1:16], cmptok)
            if "kcT" in self.dbg_out:
                self.dump2d("kcT", kcT2[:].rearrange("p g n -> p (g n)"), [cmptok])
                self.dump2d("vcx", VCX[:].rearrange("p g n -> p (g n)"), [cmptok])
            qtok, kstok, kwtok, vstok, vwtok, sgtok = [Tok() for _ in range(6)]

            def ev_q(mt, tc, pb, pt):
                ts = slice(tc * 512, (tc + 1) * 512)
                self.rope_apply(qT[:, mt, ts], pb[:], pt, 512, 0.125, cosT[:, ts], sinT[:, ts], qtok)
            self.proj_feat(li, O_NQ, 512, ev_q)
            for g in range(2):
                def ev_ks(mt, tc, pb, pt, g=g):
                    ts = slice(tc * 512, (tc + 1) * 512)
                    self.rope_apply(ksT2[:, g, ts], pb[:], pt, 512, 1.0, cosT[:, ts], sinT[:, ts], kstok)

                def ev_kw(mt, tc, pb, pt, g=g):
                    ts = slice(tc * 512, (tc + 1) * 512)
                    self.rope_apply(kwT2[:, g, ts], pb[:], pt, 512, 1.0, cosT[:, ts], sinT[:, ts], kwtok)
                self.proj_feat_dup(li, O_NKS + g * 64, ev_ks)
                self.proj_feat_dup(li, O_NKW + g * 64, ev_kw)
            c.op("pool", lambda e: e.memset(vs[:, :, :, 64:65], 1.0), writes=(vstok,))
            c.op("pool", lambda e: e.memset(vw[:, :, :, 64:65], 1.0), writes=(vwtok,))

            def ev_vs(t, pb, pt):
                c.op("act", lambda e: e.copy(vs[:, t, :, 0:64], pb[:, 0:128].rearrange("p (g d) -> p g d", g=2)), reads=(pt,),
                     writes=(vstok,))

            def ev_vw(t, pb, pt):
                c.op("act", lambda e: e.copy(vw[:, t, :, 0:64], pb[:, 0:128].rearrange("p (g d) -> p g d", g=2)), reads=(pt,),
                     writes=(vwtok,))

            def ev_sg(t, pb, pt):
                c.op("act", lambda e: e.activation(sg[:, t, :], pb[:, 0:24], AF.Sigmoid), reads=(pt,), writes=(sgtok,))
            self.proj_tok(li, O_NVS, 128, ev_vs)
            self.proj_tok(li, O_NVW, 128, ev_vw)
            self.proj_tok(li, O_NG, 24, ev_sg)
            if "qT" in self.dbg_out:
                self.dump_featmajor_bf16(qT, [qtok], self.dbg_out["qT"])
            c.barrier()
            sB.close()
            if nstop == 2:
                return
            am = self.sb(st, "n_am", [128, NT, 32], F32)
            Esel = self.sb(st, "n_E", [32, NT, 128], BF16)
            wneg = self.sb(st, "n_wneg", [128, 128], BF16)
            cm = self.sb(st, "n_cm", [128, 512], BF16)
            negT = self.sb(st, "n_negT", [32, 2, 512], BF16)
            acc = self.sb(st, "n_acc", [128, 4, 512], F32)
            ybf = self.sb(st, "n_ybf", [128, 512], BF16)
            pT = [self.sb(st, f"n_pT{i}", [128, 512], BF16) for i in range(5)]
            pT_tok = [Tok() for _ in range(5)]
            imp = self.sb(st, "n_imp", [128, 4, 2, 32], F32)
            sm = self.sb(st, "n_sm", [128, 24], F32)
            impm = self.sb(st, "n_impm", [128, 32], F32)
            top8 = self.sb(st, "n_top8", [128, 8], F32)
            nselb = self.sb(st, "n_nsel", [128, 32], BF16)
            mtok, cmtok, negtok, acctok, ytok, imptok, tktok = [Tok() for _ in range(7)]
            smtok = [Tok(), Tok(), Tok()]
            tA, tAt = self.nscr()
            tAv = tA[:].rearrange("p (t j) -> p t j", t=NT)
            c.op("pool", lambda e: e.memset(am[:], 0.0), writes=(mtok,))
            c.op("pool", lambda e: e.affine_select(am[:], am[:], [[128, NT], [-64, 32]], ALU.is_ge, -100.0, base=0,
                                                   channel_multiplier=1), reads=(mtok,), writes=(mtok,))
            c.op("pool", lambda e: e.memset(tA[:], 100.0), writes=(tAt,))
            c.op("pool", lambda e: e.affine_select(tAv, tAv, [[128, NT], [-64, 32]], ALU.is_ge, 0.0, base=0,
                                                   channel_multiplier=1), reads=(tAt,), writes=(tAt,))
            c.op("pool", lambda e: e.affine_select(tAv, tAv, [[-128, NT], [64, 32]], ALU.is_ge, 0.0, base=63,
                                                   channel_multiplier=-1), reads=(tAt,), writes=(tAt,))
            c.op("pool", lambda e: e.memset(tAv[:, :, 0:1], 100.0), reads=(tAt,), writes=(tAt,))
            c.op("pool", lambda e: e.tensor_tensor(am[:], am[:], tAv, ALU.add), reads=(tAt, mtok), writes=(mtok,))
            c.op("pool", lambda e: e.memset(Esel[:], 1.0), writes=(mtok,))
            c.op("pool", lambda e: e.affine_select(Esel[:], Esel[:], [[128, NT], [1, 128]], ALU.is_ge, 0.0, base=0,
                                                   channel_multiplier=-64), reads=(mtok,), writes=(mtok,))
            c.op("pool", lambda e: e.affine_select(Esel[:], Esel[:], [[-128, NT], [-1, 128]], ALU.is_ge, 0.0, base=63,
                                                   channel_multiplier=64), reads=(mtok,), writes=(mtok,))
            c.op("pool", lambda e: e.affine_select(wneg[:], self.zer_f[:], [[-1, 128]], ALU.is_gt, NEG, base=0,
                                                   channel_multiplier=1), reads=(ct,), writes=(mtok,))
            pi = [0]
            c.barrier()
            self.n_scr_banks = 5
            for qc in range(NTC):
                qs = slice(qc * 512, (qc + 1) * 512)
                c.op("pool", lambda e: e.memset(cm[:], 0.0), writes=(cmtok,))
                c.op("pool", lambda e: e.affine_select(cm[:], cm[:], [[1, 512]], ALU.is_ge, NEG, base=qc * 512 - 31,
                                                       channel_multiplier=-16), reads=(cmtok,), writes=(cmtok,))
                c.op("dve", lambda e: e.memset(imp[:], 0.0), writes=(imptok,))
                def cmp_stream(h):
                    g = h // 4
                    hp = slice((h % 2) * 64, (h % 2) * 64 + 64)
                    hc = h // 2
                    hcol = slice(h * 64, (h + 1) * 64)
                    sb_, stk = self.bank()
                    self.mm(sb_[0:127, :], kcT2[hp, g, 0:127], qT[hp, hc, qs], True, False, reads=(cmptok, qtok), writes=(stk,))
                    self.mm(sb_[0:127, :], self.ident_b[0:127, 0:127], cm[0:127, :], False, True, reads=(ct, cmtok),
                            writes=(stk,), skip_group_check=True)
                    p, ptk = pT[pi[0] % 5], pT_tok[pi[0] % 5]
                    pi[0] += 1
                    c.op("act", lambda e: e.activation(p[0:127, :], sb_[0:127, :], AF.Exp), reads=(stk,), writes=(ptk,))
                    yield
                    ob, ot = self.bank_acc()
                    O = ob[:, 0:388].rearrange("p (j d) -> p j d", j=4)
                    for j in range(4):
                        self.mm(O[:, j, :], p[0:127, j * 128:(j + 1) * 128], VCX[0:127, g, :], j == 0, True,
                                reads=(ptk, cmptok), writes=(ot,), skip_group_check=True)
                    yield
                    smt = smtok[h % 3]
                    for j in range(4):
                        qt = qc * 4 + j
                        o_ = (h % 3) * 8 + 2 * j
                        rcv, wv_ = sm[:, o_:o_ + 1], sm[:, o_ + 1:o_ + 2]
                        c.op("dve", lambda e: e.tensor_scalar(rcv, O[:, j, 64:65], 1e-30, None, ALU.max), reads=(ot,),
                             writes=(smt,))
                        c.op("dve", lambda e: e.reciprocal(rcv, rcv), reads=(smt,), writes=(smt,))
                        c.op("dve", lambda e: e.tensor_tensor(wv_, rcv, sg[:, qt, 3 * h:3 * h + 1], ALU.mult),
                             reads=(smt, sgtok), writes=(smt,))
                        c.op("dve", lambda e: e.tensor_scalar(acc[:, j, hcol], O[:, j, 0:64], wv_, None, ALU.mult),
                             reads=(ot, smt), writes=(acctok,))
                        c.op("dve", lambda e: e.scalar_tensor_tensor(imp[:, j, g, :], O[:, j, 65:97], rcv, imp[:, j, g, :],
                                                                     ALU.mult, ALU.add), reads=(ot, smt, imptok),
                             writes=(imptok,))
                    self.release_acc(ob)
                self.run_streams([cmp_stream(h) for h in range(8)], 3)
                for j in range(4):
                    qt = qc * 4 + j
                    for g in range(2):
                        c.op("dve", lambda e: e.tensor_tensor(impm[:], imp[:, j, g, :], am[:, qt, :], ALU.add),
                             reads=(imptok, mtok), writes=(tktok,))
                        c.op("dve", lambda e: e.max(top8[:], impm[:]), reads=(tktok,), writes=(tktok,))
                        c.op("dve", lambda e: e.tensor_scalar(impm[:], impm[:], top8[:, 7:8], None, ALU.is_ge), reads=(tktok,),
                             writes=(tktok,))
                        c.op("dve", lambda e: e.tensor_scalar(nselb[:], impm[:], -1.0, 30000.0, ALU.add, ALU.mult),
                             reads=(tktok,), writes=(tktok,))
                        pb, pt = self.bank()
                        pbb = pb[:].bitcast(BF16)
                        c.op("pe", lambda e: e.transpose(pbb[0:32, 0:128], nselb[:], self.ident_b[:]), reads=(tktok, ct),
                             writes=(pt,))
                        c.op("act", lambda e: e.copy(negT[0:32, g, j * 128:(j + 1) * 128], pbb[0:32, 0:128]), reads=(pt,),
                             writes=(negtok,))
                if "negT" in self.dbg_out and qc == 1:
                    self.dump2d("negT", negT[:].rearrange("p g n -> p (g n)"), [negtok])
                def sw_stream(br, h):
                    g = h // 4
                    hp = slice((h % 2) * 64, (h % 2) * 64 + 64)
                    hc = h // 2
                    hcol = slice(h * 64, (h + 1) * 64)
                    ob, ot = self.bank_acc()
                    O = ob[:, 0:260].rearrange("p (j d) -> p j d", j=4)
                    first = True
                    kt0 = 0 if br == 1 else max(0, 4 * qc - 2)
                    for kt in range(kt0, 4 * qc + 4):
                        rel = kt - 4 * qc
                        jlo = max(0, rel)
                        jhi = 3 if br == 1 else min(3, rel + 2)
                        ncol = (jhi - jlo + 1) * 128
                        q0 = qc * 512 + jlo * 128
                        kl = slice(kt * 128, (kt + 1) * 128)
                        KT = ksT2 if br == 1 else kwT2
                        ktk = kstok if br == 1 else kwtok
                        extra = []
                        if br == 1:
                            extra.append((slice(0, ncol), Esel[0:32, kt, :], negT[0:32, g, jlo * 128:jlo * 128 + ncol],
                                          (mtok, negtok)))
                        if rel >= 0:
                            extra.append((slice(0, 128), self.ident_b[:], self.cneg_b[:], (ct,)))
                        if br == 2 and 0 <= rel + 2 <= 3:
                            o2 = (rel + 2 - jlo) * 128
                            extra.append((slice(o2, o2 + 128), self.ident_b[:], wneg[:], (ct, mtok)))
                        sb_, stk = self.bank()
                        self.mm(sb_[:, 0:ncol], KT[hp, g, kl], qT[hp, hc, q0:q0 + ncol], True, len(extra) == 0,
                                reads=(ktk, qtok), writes=(stk,))
                        for ei, (csl, lh, rh, rd) in enumerate(extra):
                            self.mm(sb_[:, csl], lh, rh, False, ei == len(extra) - 1, reads=rd, writes=(stk,),
                                    skip_group_check=True)
                        p, ptk = pT[pi[0] % 5], pT_tok[pi[0] % 5]
                        pi[0] += 1
                        c.op("act", lambda e: e.activation(p[:, 0:ncol], sb_[:, 0:ncol], AF.Exp), reads=(stk,), writes=(ptk,))
                        yield
                        VV = vs if br == 1 else vw
                        vtk_ = vstok if br == 1 else vwtok
                        for j in range(jlo, jhi + 1):
                            qt = qc * 4 + j
                            cs = slice((j - jlo) * 128, (j - jlo + 1) * 128)
                            self.mm(O[:, j, :], p[:, cs], VV[:, kt, g, :], first, kt == qt, reads=(ptk, vtk_), writes=(ot,),
                                    skip_group_check=True)
                            first = False
                    smt = smtok[h % 3]
                    for j in range(4):
                        qt = qc * 4 + j
                        o_ = (h % 3) * 8 + 2 * j
                        rcv, wv_ = sm[:, o_:o_ + 1], sm[:, o_ + 1:o_ + 2]
                        c.op("dve", lambda e: e.reciprocal(rcv, O[:, j, 64:65]), reads=(ot,), writes=(smt,))
                        c.op("dve", lambda e: e.tensor_tensor(wv_, rcv, sg[:, qt, 3 * h + br:3 * h + br + 1], ALU.mult),
                             reads=(smt, sgtok), writes=(smt,))
                        c.op("dve", lambda e: e.scalar_tensor_tensor(acc[:, j, hcol], O[:, j, 0:64], wv_, acc[:, j, hcol],
                                                                     ALU.mult, ALU.add), reads=(ot, smt, acctok),
                             writes=(acctok,))
                    self.release_acc(ob)
                self.run_streams([sw_stream(br, h) for br in (1, 2) for h in range(8)], 3)
                for j in range(4):
                    t = qc * 4 + j
                    c.op("act", lambda e: e.copy(ybf[:], acc[:, j, :]), reads=(acctok,), writes=(ytok,))
                    pb, pt = self.bank()
                    pbb = pb[:].bitcast(BF16)
                    for jj in range(4):
                        c.op("pe", lambda e: e.transpose(pbb[:, jj * 128:(jj + 1) * 128], ybf[:, jj * 128:(jj + 1) * 128],
                                                         self.ident_b[:]), reads=(ytok, ct), writes=(pt,))
                    c.op("dve", lambda e: e.tensor_copy(self.yT[:, :, t * 128:(t + 1) * 128],
                                                        pbb[:, 0:512].rearrange("p (j n) -> p j n", j=4)),
                         reads=(pt,), writes=(self.yT_tok,))
            c.barrier()
            self.n_scr_banks = 6

    def fox(self, li):
        c = self.c
        with ExitStack() as st:
            qT = self.sb(st, "fx_qT", [128, 4, S], BF16)
            kT = self.sb(st, "fx_kT", [128, 4, S], BF16)
            V = self.sb(st, "fx_V", [128, NT, 8, 65], BF16)
            ytk = self.sb(st, "fx_y", [128, 4, 512], BF16)
            fl = self.sb(st, "fx_f", [128, NT, 8], F32)
            ncum = self.sb(st, "fx_ncum", [128, NT, 8], F32)
            nref = self.sb(st, "fx_nref", [128, NT, 8], F32)
            btab = self.sb(st, "fx_btab", [128, NT, NT, 8], F32)
            bfb = self.sb(st, "fx_bf", [128, 8], F32)
            qtok, ktok, vtok, ytok, ftok = Tok(), Tok(), Tok(), Tok(), Tok()
            self.aux_tok = Tok()
            self.proj_feat(li, O_FQ, 512, self.evac_featT(qT, qtok, 0.125))
            self.proj_feat(li, O_FK, 512, self.evac_featT(kT, ktok, 1.0))
            c.op("pool", lambda e: e.memset(V[:, :, :, 64:65], 1.0), writes=(vtok,))

            def evac_v(t, pb, pt):
                c.op("act", lambda e: e.copy(V[:, t, :, 0:64], pb[:, 0:512].rearrange("p (h d) -> p h d", h=8)),
                     reads=(pt,), writes=(vtok,))
            for half in range(4):
                def evac_vh(t, pb, pt, half=half):
                    c.op("act", lambda e: e.copy(V[:, t, half * 2:half * 2 + 2, 0:64],
                                                 pb[:, 0:128].rearrange("p (h d) -> p h d", h=2)),
                         reads=(pt,), writes=(vtok,))
                self.proj_tok(li, O_FV + half * 128, 128, evac_vh)
            c.dma(bfb[:], self.A["fox_b_f"][li:li + 1, :].partition_broadcast(128), writes=(ftok,))

            def evac_f(t, pb, pt):
                c.op("dve", lambda e: e.tensor_tensor(fl[:, t, :], pb[:, 0:8], bfb[:], ALU.add), reads=(pt, ftok),
                     writes=(ftok,))
            self.proj_tok(li, O_FF, 8, evac_f)
            flat = fl[:].rearrange("p t h -> p (t h)")
            c.op("act", lambda e: e.activation(flat, flat, AF.Exp, scale=-1.0), reads=(ftok,), writes=(ftok,))
            c.op("act", lambda e: e.activation(flat, flat, AF.Ln, bias=1.0), reads=(ftok,), writes=(ftok,))
            for t in range(NT):
                pb, pt = self.bank()
                for j in range(t):
                    self.mm(pb[:, 0:8], self.ones_f[:], fl[:, j, :], j == 0, False, reads=(ftok, self.const_tok),
                            writes=(pt,))
                self.mm(pb[:, 0:8], self.tri_f[:], fl[:, t, :], t == 0, True, reads=(ftok, self.const_tok), writes=(pt,))
                c.op("dve", lambda e: e.tensor_copy(ncum[:, t, :], pb[:, 0:8]), reads=(pt,), writes=(self.aux_tok,))
                if t > 0:
                    pb2, pt2 = self.bank()
                    for j in range(t):
                        self.mm(pb2[:, 0:8], self.ones_f[:], fl[:, j, :], j == 0, j == t - 1,
                                reads=(ftok, self.const_tok), writes=(pt2,))
                    c.op("dve", lambda e: e.tensor_copy(nref[:, t, :], pb2[:, 0:8]), reads=(pt2,), writes=(self.aux_tok,))
                else:
                    c.op("dve", lambda e: e.memset(nref[:, 0, :], 0.0), writes=(self.aux_tok,))
            for kt in range(NT):
                for qt in range(1, NT, 2):
                    if qt >= kt:
                        c.op("pool", lambda e: e.tensor_tensor(btab[:, kt, qt, :], ncum[:, kt, :], nref[:, qt, :],
                                                               ALU.subtract), reads=(self.aux_tok,), writes=(self.aux_tok,))
            self.dump2d("ncum", ncum[:].rearrange("p t h -> p (t h)"), [self.aux_tok])
            self.dump2d("nref", nref[:].rearrange("p t h -> p (t h)"), [self.aux_tok])
            self.dump2d("fl", fl[:].rearrange("p t h -> p (t h)"), [ftok])
            self.attention(st, "fx", 8, qT, qtok, kT, ktok, V, vtok, ytk, ytok,
                           bias_fn=lambda h, kt, qt: btab[:, kt, qt, h:h + 1],
                           post_qc=lambda qc: self.ytok_to_yT(ytk, ytok, qc))
            c.barrier()

    def final_norm_store(self, out):
        self.store_tok_major(out, normed=True)

    def store_tok_major(self, out, normed):
        c = self.c
        L = len(self.layers)
        with ExitStack() as st:
            if normed:
                for tc in range(NTC):
                    ts = slice(tc * 512, (tc + 1) * 512)
                    pb, pt = self.bank()
                    for cc in range(KC):
                        sq, sqt = self.nscr()
                        c.op("act", lambda e: e.activation(sq[:], self.xT[:, cc, ts], AF.Square),
                             reads=(self.xT_tok[tc],), writes=(sqt,))
                        self.mm(pb[:], self.ones_f[:], sq[:], cc == 0, cc == KC - 1, reads=(sqt, self.const_tok),
                                writes=(pt,))
                    rs, rst = self.nscr()
                    c.op("dve", lambda e: e.tensor_scalar(rs[:], pb[:], 1.0 / D, EPS, ALU.mult, ALU.add), reads=(pt,),
                         writes=(rst,))
                    c.op("act", lambda e: e.activation(rs[:], rs[:], AF.Sqrt), reads=(rst,), writes=(rst,))
                    c.op("dve", lambda e: e.reciprocal(rs[:], rs[:]), reads=(rst,), writes=(rst,))
                    for cc in range(KC):
                        g = self.vecT[:, L * 72 + cc:L * 72 + cc + 1]
                        c.op("dve", lambda e: e.scalar_tensor_tensor(self.xT[:, cc, ts], self.xT[:, cc, ts], g, rs[:],
                                                                     ALU.mult, ALU.mult),
                             reads=(rst, self.vec_tok), writes=(self.xT_tok[tc],))
            os_ = [self.sb(st, f"os{i}", [128, D], F32) for i in range(2)]
            os_tok = [Tok(), Tok()]
            for t in range(NT):
                b = t % 2
                for half in range(2):
                    pb, pt = self.bank()
                    for j in range(4):
                        cc = half * 4 + j
                        c.op("pe", lambda e: e.transpose(pb[:, j * 128:(j + 1) * 128],
                                                         self.xT[:, cc, t * 128:(t + 1) * 128], self.ident_f[:]),
                             reads=(self.xT_tok[t // 4], self.const_tok), writes=(pt,), inc=(j == 3))
                    dst = os_[b][:, half * 512:(half + 1) * 512]
                    if half == 0:
                        c.op("dve", lambda e: e.tensor_copy(dst, pb[:]), reads=(pt,), writes=(os_tok[b],))
                    else:
                        c.op("act", lambda e: e.copy(dst, pb[:]), reads=(pt,), writes=(os_tok[b],))
                c.dma(out[t * 128:(t + 1) * 128, :], os_[b][:], reads=(os_tok[b],))
            c.barrier()

    def dump2d(self, name, ap, toks):
        if name in self.dbg_out:
            self.c.barrier()
            self.c.dma(self.dbg_out[name], ap, reads=tuple(toks), q="pool")
            self.c.barrier()

    def dump_featmajor_bf16(self, tT, toks, dst):
        c = self.c
        with ExitStack() as st:
            tmp = self.sb(st, "dmp", [128, S], F32)
            tt = Tok()
            for cc in range(tT.shape[1]):
                c.op("dve", lambda e: e.tensor_copy(tmp[:], tT[:, cc, :]), reads=tuple(toks), writes=(tt,))
                c.dma(dst[cc * 128:(cc + 1) * 128, :], tmp[:], reads=(tt,))
            c.barrier()


def _prep_inputs(inputs, layers, b):
    L = len(layers)
    vecs = np.zeros((L, 72, 128), np.float32)
    for i, l in enumerate(layers):
        vecs[i, 0:8] = inputs["norm_mix"][l].reshape(8, 128)
        vecs[i, 8:16] = inputs["norm_ff"][l].reshape(8, 128)
        vecs[i, 16:48] = inputs["b_gate"][l].reshape(32, 128)
    m = {
        "x": np.ascontiguousarray(inputs["x"][b]),
        "w_in": np.ascontiguousarray(inputs["w_in"][layers]),
        "vecs": vecs,
        "norm_final": np.ascontiguousarray(inputs["norm_final"].reshape(8, 128)),
        "positions": np.ascontiguousarray(inputs["positions"][b:b + 1]).astype(np.int32),
        "cmp_pos_k": np.ascontiguousarray(inputs["nsa_cmp_pos_k"][layers]),
        "cmp_pos_v": np.ascontiguousarray(inputs["nsa_cmp_pos_v"][layers]),
        "cmp_wk1": np.ascontiguousarray(inputs["nsa_cmp_wk1"][layers]),
        "cmp_wk2": np.ascontiguousarray(inputs["nsa_cmp_wk2"][layers]),
        "cmp_wv1": np.ascontiguousarray(inputs["nsa_cmp_wv1"][layers]),
        "cmp_wv2": np.ascontiguousarray(inputs["nsa_cmp_wv2"][layers]),
        "fox_b_f": np.ascontiguousarray(inputs["fox_b_f"][layers]),
        "gla_w_alpha": np.ascontiguousarray(inputs["gla_w_alpha"][layers]),
        "gla_b_alpha": np.ascontiguousarray(inputs["gla_b_alpha"][layers]),
        "gla_norm": np.ascontiguousarray(inputs["gla_norm"][layers]),
        "w_branch": np.ascontiguousarray(inputs["w_branch"][layers]),
        "w_out": np.ascontiguousarray(inputs["w_out"][layers]),
        "w_ff1": np.ascontiguousarray(inputs["w_ff1"][layers]),
        "w_ff2": np.ascontiguousarray(inputs["w_ff2"][layers]),
    }
    return m


def run(inputs, layers=(0, 1, 2, 3), debug=(), ncores=8, trace=False, stage=99):
    layers = list(layers)
    bld = Builder(layers, first=True, last=True, debug=debug, stage=stage)
    nc = bld.build()
    in_maps = [_prep_inputs(inputs, layers, b) for b in range(ncores)]
    res = run_bass_kernel_spmd(nc, in_maps, core_ids=list(range(ncores)), trace=trace)
    return res


def kernel(**inputs):
    inputs = {k: np.asarray(v) for k, v in inputs.items()}
    res = run(inputs)
    out = np.stack([np.asarray(r["out"]) for r in res.results], axis=0)
    return out.astype(np.float32)
```
